# Optimizing a Trainium2 kernel written in Bass

```python
import math
import jax
import jax.numpy as jnp
from jax import lax
import numpy as np

D_MODEL = 2048
BATCH = 1
SEQ = 8192
DEPTH = 4

GRID_W = 64
CTX_LEN = 256
EPS = 1e-6

DA_HEADS = 8
DA_QK_DIM = 64
DA_V_DIM = 2 * DA_QK_DIM
DA_WIDTH = DA_HEADS * DA_V_DIM
Q_BLOCK = 128
ROPE_THETA = 10000.0
NA_HEADS = 8
NA_HEAD_DIM = 128
NA_WIDTH = NA_HEADS * NA_HEAD_DIM
NA_WIN_ROWS = 8
NA_WIN_COLS = 16
ATT_IN = 3 * DA_WIDTH + 3 * NA_WIDTH
HG_HEADS = 16
HG_KEY_DIM = D_MODEL // HG_HEADS
HG_VAL_DIM = D_MODEL // HG_HEADS
HG_CHUNK = 64
REC_IN = 5 * D_MODEL
N_GROUPS = 4
EXPERTS_PER_GROUP = 8
N_EXPERTS = N_GROUPS * EXPERTS_PER_GROUP
TOP_K = 2
D_EXPERT = 768
MOE_BLOCK = 128

N_ATT_LAYERS = (DEPTH + 1) // 2
N_REC_LAYERS = DEPTH // 2

kernel_name = 'hybrid_diffattn_natten_hgrn2_hmoe_dit'


def rms_norm(x, g):
    xf = x.astype(jnp.float32)
    y = xf * lax.rsqrt(jnp.mean(xf * xf, axis=-1, keepdims=True) + EPS)
    return (y * g.astype(jnp.float32)).astype(x.dtype)


def modulate(h, shift, scale):
    return h * (1 + scale) + shift


def _axial_rope_tables(n_tok):
    t = jnp.arange(n_tok, dtype=jnp.int32)
    rows = (t // GRID_W).astype(jnp.float32)
    cols = (t % GRID_W).astype(jnp.float32)
    n_freq = DA_QK_DIM // 4
    inv_freq = ROPE_THETA ** (-jnp.arange(n_freq, dtype=jnp.float32) / n_freq)
    ang = jnp.stack([rows[:, None] * inv_freq, cols[:, None] * inv_freq], axis=1)
    return jnp.cos(ang), jnp.sin(ang)


def _apply_axial_rope(x, cos, sin):
    shp = x.shape
    xr = x.reshape(shp[:-1] + (2, 2, DA_QK_DIM // 4))
    x1, x2 = xr[..., 0, :], xr[..., 1, :]
    cos = cos.astype(x.dtype)
    sin = sin.astype(x.dtype)
    out = jnp.stack([x1 * cos - x2 * sin, x2 * cos + x1 * sin], axis=-2)
    return out.reshape(shp)


def _diff_lambda(lam_p, layer_idx):
    lam_init = 0.8 - 0.6 * math.exp(-0.3 * layer_idx)
    lf = lam_p.astype(jnp.float32)
    lam = jnp.exp(jnp.sum(lf[0] * lf[1])) - jnp.exp(jnp.sum(lf[2] * lf[3])) + lam_init
    return lam, lam_init


def _diff_attend(q, k, v, lam):
    s = jnp.einsum('bhmqd,bhmkd->bhmqk', q, k).astype(jnp.float32)
    p = jax.nn.softmax(s, axis=-1)
    a = p[:, :, 0] - lam * p[:, :, 1]
    return jnp.einsum('bhqk,bhkv->bhqv', a.astype(v.dtype), v)


def _plain_attend(q, k, v):
    s = jnp.einsum('bhqd,bhkd->bhqk', q, k).astype(jnp.float32)
    p = jax.nn.softmax(s, axis=-1).astype(v.dtype)
    return jnp.einsum('bhqk,bhkd->bhqd', p, v)


def _neighbourhood_attend(q_lat, k_lat, v_lat, k_ctx, v_ctx, rpb):
    b_, h_, n_, d_ = q_lat.shape
    rows = n_ // GRID_W
    wr = min(NA_WIN_ROWS, rows)
    wc = NA_WIN_COLS
    c_ = k_ctx.shape[2]
    qg = q_lat.reshape(b_, h_, rows, GRID_W, d_)
    kg = k_lat.reshape(b_, h_, rows, GRID_W, d_)
    vg = v_lat.reshape(b_, h_, rows, GRID_W, d_)
    cols = np.arange(GRID_W)
    col_idx = np.clip(cols - wc // 2, 0, GRID_W - wc)[:, None] + np.arange(wc)[None, :]
    col_off = col_idx - cols[:, None]

    def row_block(r):
        rs = jnp.clip(r - wr // 2, 0, rows - wr)
        row_off = rs + jnp.arange(wr) - r
        bias = rpb[:, (row_off + NA_WIN_ROWS - 1)[None, :, None], (col_off + wc - 1)[:, None, :]]
        kr = lax.dynamic_slice_in_dim(kg, rs, wr, axis=2)[:, :, :, col_idx]
        vr = lax.dynamic_slice_in_dim(vg, rs, wr, axis=2)[:, :, :, col_idx]
        qr = lax.dynamic_index_in_dim(qg, r, axis=2, keepdims=False)
        s_loc = jnp.einsum('bhwd,bhiwjd->bhwij', qr, kr).astype(jnp.float32) + bias.astype(jnp.float32)
        s_ctx = jnp.einsum('bhwd,bhcd->bhwc', qr, k_ctx).astype(jnp.float32)
        s = jnp.concatenate([s_ctx, s_loc.reshape(b_, h_, GRID_W, wr * wc)], axis=-1)
        p = jax.nn.softmax(s, axis=-1).astype(v_ctx.dtype)
        p_ctx = p[..., :c_]
        p_loc = p[..., c_:].reshape(b_, h_, GRID_W, wr, wc)
        return jnp.einsum('bhwc,bhcd->bhwd', p_ctx, v_ctx) + jnp.einsum('bhwij,bhiwjd->bhwd', p_loc, vr)

    out = lax.map(row_block, jnp.arange(rows))
    return out.transpose(1, 2, 0, 3, 4).reshape(b_, h_, n_, d_)


def _split_att(p):
    b_, n_, _ = p.shape
    o0, o1, o2, o3 = 0, DA_WIDTH, 2 * DA_WIDTH, 3 * DA_WIDTH
    o4, o5, o6 = o3 + NA_WIDTH, o3 + 2 * NA_WIDTH, ATT_IN
    qa = p[..., o0:o1].reshape(b_, n_, DA_HEADS, 2, DA_QK_DIM).transpose(0, 2, 3, 1, 4)
    ka = p[..., o1:o2].reshape(b_, n_, DA_HEADS, 2, DA_QK_DIM).transpose(0, 2, 3, 1, 4)
    va = p[..., o2:o3].reshape(b_, n_, DA_HEADS, DA_V_DIM).transpose(0, 2, 1, 3)
    qn = p[..., o3:o4].reshape(b_, n_, NA_HEADS, NA_HEAD_DIM).transpose(0, 2, 1, 3)
    kn = p[..., o4:o5].reshape(b_, n_, NA_HEADS, NA_HEAD_DIM).transpose(0, 2, 1, 3)
    vn = p[..., o5:o6].reshape(b_, n_, NA_HEADS, NA_HEAD_DIM).transpose(0, 2, 1, 3)
    return qa, ka, va, qn, kn, vn


def attn_mixer(h_lat, h_ctx, w_in, w_out, lam_p, subln_g, rpb, layer_idx, need_ctx_out):
    b_, n_, _ = h_lat.shape
    qa_l, ka_l, va_l, qn_l, kn_l, vn_l = _split_att(h_lat @ w_in)
    qa_c, ka_c, va_c, qn_c, kn_c, vn_c = _split_att(h_ctx @ w_in)
    lam, lam_init = _diff_lambda(lam_p, layer_idx)
    sa = DA_QK_DIM ** -0.5
    sn = NA_HEAD_DIM ** -0.5
    cos, sin = _axial_rope_tables(n_)
    qa_l = _apply_axial_rope(qa_l, cos, sin) * sa
    ka_l = _apply_axial_rope(ka_l, cos, sin)
    ka_all = jnp.concatenate([ka_c, ka_l], axis=3)
    va_all = jnp.concatenate([va_c, va_l], axis=2)
    nqb = n_ // Q_BLOCK
    qb = qa_l.reshape(b_, DA_HEADS, 2, nqb, Q_BLOCK, DA_QK_DIM).transpose(3, 0, 1, 2, 4, 5)
    da_l = lax.map(lambda qq: _diff_attend(qq, ka_all, va_all, lam), qb)
    da_l = da_l.transpose(1, 2, 0, 3, 4).reshape(b_, DA_HEADS, n_, DA_V_DIM)
    na_l = _neighbourhood_attend(qn_l * sn, kn_l, vn_l, kn_c, vn_c, rpb)

    def merge(da, na):
        da = rms_norm(da, subln_g) * (1.0 - lam_init)
        n = da.shape[2]
        cat = jnp.concatenate([da.transpose(0, 2, 1, 3).reshape(b_, n, DA_WIDTH),
                               na.transpose(0, 2, 1, 3).reshape(b_, n, NA_WIDTH)], axis=-1)
        return cat @ w_out

    y_lat = merge(da_l, na_l)
    y_ctx = None
    if need_ctx_out:
        da_c = _diff_attend(qa_c * sa, ka_c, va_c, lam)
        na_c = _plain_attend(qn_c * sn, kn_c, vn_c)
        y_ctx = merge(da_c, na_c)
    return y_lat, y_ctx


def _gla_chunk_scan(q, k, v, logf, s0):
    b_, h_, l_, _ = q.shape
    nc = l_ // HG_CHUNK

    def chunks(a):
        return a.reshape(b_, h_, nc, HG_CHUNK, a.shape[-1]).transpose(2, 0, 1, 3, 4)

    lower = jnp.tril(jnp.ones((HG_CHUNK, HG_CHUNK), dtype=bool))[:, :, None]

    def step(state, xs):
        qc, kc, vc, gc = xs
        cum = jnp.cumsum(gc, axis=2)
        o_inter = jnp.einsum('bhtk,bhkv->bhtv', qc * jnp.exp(cum), state)
        rel = cum[:, :, :, None, :] - cum[:, :, None, :, :]
        decay = jnp.exp(jnp.where(lower, rel, -jnp.inf))
        att = jnp.einsum('bhtk,bhsk,bhtsk->bhts', qc, kc, decay)
        o = o_inter + jnp.einsum('bhts,bhsv->bhtv', att, vc)
        last = cum[:, :, -1:, :]
        state = jnp.exp(last[:, :, 0, :])[..., None] * state + jnp.einsum('bhsk,bhsv->bhkv', kc * jnp.exp(last - cum), vc)
        return state, o

    s_fin, o = lax.scan(step, s0, (chunks(q), chunks(k), chunks(v), chunks(logf)))
    o = o.transpose(1, 2, 0, 3, 4).reshape(b_, h_, l_, v.shape[-1])
    return o, s_fin


def _split_rec(p):
    b_, n_, _ = p.shape
    q, i, ff, fb, g = jnp.split(p, 5, axis=-1)
    heads = lambda a: a.reshape(b_, n_, HG_HEADS, -1).transpose(0, 2, 1, 3)
    return heads(q), heads(i), heads(ff), heads(fb), heads(g)


def rec_mixer(h_lat, h_ctx, w_in, w_out, lb, gnorm_g, need_ctx_out):
    f32 = jnp.float32
    dt = h_lat.dtype
    b_ = h_lat.shape[0]
    q_l, i_l, ff_l, fb_l, g_l = _split_rec(h_lat @ w_in)
    q_c, i_c, ff_c, fb_c, g_c = _split_rec(h_ctx @ w_in)

    def gates(z, lb_d):
        lbh = lb_d.reshape(HG_HEADS, 1, HG_KEY_DIM)
        f = lbh + (1.0 - lbh) * jax.nn.sigmoid(z.astype(f32))
        return 1.0 - f, jnp.log(f)

    qs = lambda a: jax.nn.silu(a.astype(f32)) * HG_KEY_DIM ** -0.5
    Qc, Ql = qs(q_c), qs(q_l)
    Vc, Vl = i_c.astype(f32), i_l.astype(f32)
    s0 = jnp.zeros((b_, HG_HEADS, HG_KEY_DIM, HG_VAL_DIM), f32)
    flip = lambda a: jnp.flip(a, axis=2)
    kc, gc = gates(ff_c, lb[0])
    kl, gl = gates(ff_l, lb[0])
    oc_f, sc_f = _gla_chunk_scan(Qc, kc, Vc, gc, s0)
    ol_f, _ = _gla_chunk_scan(Ql, kl, Vl, gl, sc_f)
    kc, gc = gates(fb_c, lb[1])
    kl, gl = gates(fb_l, lb[1])
    oc_b, sc_b = _gla_chunk_scan(flip(Qc), flip(kc), flip(Vc), flip(gc), s0)
    ol_b, _ = _gla_chunk_scan(flip(Ql), flip(kl), flip(Vl), flip(gl), sc_b)

    def readout(o, g):
        o = rms_norm(o, gnorm_g) * jax.nn.silu(g.astype(f32))
        n = o.shape[2]
        return o.transpose(0, 2, 1, 3).reshape(b_, n, D_MODEL).astype(dt) @ w_out

    y_lat = readout(ol_f + flip(ol_b), g_l)
    y_ctx = None
    if need_ctx_out:
        y_ctx = readout(oc_f + flip(oc_b), g_c)
    return y_lat, y_ctx


def hier_moe(h, w_grp, b_grp, w_rt, b_rt, w_gate, w_up, w_down):
    t_ = h.shape[0]
    g_logits = (h @ w_grp).astype(jnp.float32) + b_grp.astype(jnp.float32)
    g_prob = jax.nn.softmax(g_logits, axis=-1)
    g_sel = jnp.argmax(g_logits, axis=-1).astype(jnp.int32)
    g_w = jnp.take_along_axis(g_prob, g_sel[:, None], axis=1)[:, 0]
    e_logits = (h @ w_rt).astype(jnp.float32).reshape(t_, N_GROUPS, EXPERTS_PER_GROUP)
    e_logits = e_logits + b_rt.astype(jnp.float32).reshape(N_GROUPS, EXPERTS_PER_GROUP)
    e_logits = jnp.take_along_axis(e_logits, g_sel[:, None, None], axis=1)[:, 0]
    top_v, top_i = lax.top_k(e_logits, TOP_K)
    weights = g_w[:, None] * jax.nn.softmax(top_v, axis=-1)
    expert = g_sel[:, None] * EXPERTS_PER_GROUP + top_i.astype(jnp.int32)
    n_as = t_ * TOP_K
    eid = expert.reshape(-1)
    gate = weights.reshape(-1)
    tok = jnp.repeat(jnp.arange(t_, dtype=jnp.int32), TOP_K)
    order = jnp.argsort(eid).astype(jnp.int32)
    eid_s = eid[order]
    counts = jnp.zeros((N_EXPERTS,), jnp.int32).at[eid].add(1)
    padded = (counts + MOE_BLOCK - 1) // MOE_BLOCK * MOE_BLOCK
    start = jnp.cumsum(counts) - counts
    pstart = jnp.cumsum(padded) - padded
    dest = pstart[eid_s] + jnp.arange(n_as, dtype=jnp.int32) - start[eid_s]
    n_blk = -(-n_as // MOE_BLOCK) + N_EXPERTS
    n_slot = n_blk * MOE_BLOCK
    slot_src = jnp.full((n_slot,), n_as, jnp.int32).at[dest].set(order)
    slot_tok = jnp.concatenate([tok, jnp.zeros((1,), jnp.int32)])[slot_src]
    slot_gate = jnp.concatenate([gate, jnp.zeros((1,), gate.dtype)])[slot_src]
    blk_expert = jnp.searchsorted(jnp.cumsum(padded), jnp.arange(n_blk, dtype=jnp.int32) * MOE_BLOCK, side='right')
    blk_expert = jnp.minimum(blk_expert, N_EXPERTS - 1).astype(jnp.int32)
    xs = h[slot_tok].reshape(n_blk, MOE_BLOCK, h.shape[-1])

    def expert_block(args):
        xb, e = args
        hid = jax.nn.silu(xb @ w_gate[e]) * (xb @ w_up[e])
        return hid @ w_down[e]

    ys = lax.map(expert_block, (xs, blk_expert)).reshape(n_slot, h.shape[-1])
    return jnp.zeros_like(h).at[slot_tok].add(ys * slot_gate[:, None].astype(ys.dtype))


def setup_inputs(seed: int = 0) -> dict:
    key = jax.random.key(seed)
    ks = jax.random.split(key, 25)
    f32 = jnp.float32
    D = D_MODEL
    nrm = lambda k, shape, s: jax.random.normal(k, shape, f32) * s
    return {
        'x': nrm(ks[0], (BATCH, SEQ, D), 1.0),
        'c': nrm(ks[1], (BATCH, D), 1.0),
        'ctx': nrm(ks[2], (BATCH, CTX_LEN, D), 1.0),
        'c_ctx': nrm(ks[3], (D,), 1.0),
        'w_mod': nrm(ks[4], (DEPTH, D, 6 * D), 0.5 * D ** -0.5),
        'b_mod': nrm(ks[5], (DEPTH, 6 * D), 0.02),
        'norm1_g': 1.0 + nrm(ks[6], (DEPTH, D), 0.02),
        'norm2_g': 1.0 + nrm(ks[7], (DEPTH, D), 0.02),
        'att_w_in': nrm(ks[8], (N_ATT_LAYERS, D, ATT_IN), D ** -0.5),
        'att_w_out': nrm(ks[9], (N_ATT_LAYERS, DA_WIDTH + NA_WIDTH, D), (DA_WIDTH + NA_WIDTH) ** -0.5),
        'att_lambda': nrm(ks[10], (N_ATT_LAYERS, 4, DA_QK_DIM), 0.1),
        'att_subln_g': 1.0 + nrm(ks[11], (N_ATT_LAYERS, DA_V_DIM), 0.02),
        'att_rpb': nrm(ks[12], (N_ATT_LAYERS, NA_HEADS, 2 * NA_WIN_ROWS - 1, 2 * NA_WIN_COLS - 1), 0.1),
        'rec_w_in': nrm(ks[13], (N_REC_LAYERS, D, REC_IN), D ** -0.5),
        'rec_w_out': nrm(ks[14], (N_REC_LAYERS, D, D), D ** -0.5),
        'rec_lb_logits': nrm(ks[15], (DEPTH, 2, D), 0.5),
        'rec_gnorm_g': 1.0 + nrm(ks[16], (N_REC_LAYERS, HG_VAL_DIM), 0.02),
        'moe_w_group': nrm(ks[17], (DEPTH, D, N_GROUPS), D ** -0.5),
        'moe_b_group': nrm(ks[18], (DEPTH, N_GROUPS), 0.01),
        'moe_w_router': nrm(ks[19], (DEPTH, D, N_EXPERTS), D ** -0.5),
        'moe_b_router': nrm(ks[20], (DEPTH, N_EXPERTS), 0.01),
        'moe_w_gate': nrm(ks[21], (DEPTH, N_EXPERTS, D, D_EXPERT), D ** -0.5),
        'moe_w_up': nrm(ks[22], (DEPTH, N_EXPERTS, D, D_EXPERT), D ** -0.5),
        'moe_w_down': nrm(ks[23], (DEPTH, N_EXPERTS, D_EXPERT, D), D_EXPERT ** -0.5),
        'final_norm_g': 1.0 + nrm(ks[24], (D,), 0.02),
    }


def reference(x, c, ctx, c_ctx, w_mod, b_mod, norm1_g, norm2_g, att_w_in, att_w_out, att_lambda,
              att_subln_g, att_rpb, rec_w_in, rec_w_out, rec_lb_logits, rec_gnorm_g, moe_w_group,
              moe_b_group, moe_w_router, moe_b_router, moe_w_gate, moe_w_up, moe_w_down, final_norm_g):
    b_, n_, d_ = x.shape
    c_len = ctx.shape[1]
    lbp = jax.nn.softmax(rec_lb_logits.astype(jnp.float32), axis=0)
    lbs = jnp.cumsum(lbp, axis=0) - lbp[0:1]
    h_lat, h_ctx = x, ctx
    for l in range(DEPTH):
        need_ctx = l < DEPTH - 1
        j = l // 2
        mod_l = jnp.split((jax.nn.silu(c) @ w_mod[l] + b_mod[l])[:, None, :], 6, axis=-1)
        mod_c = jnp.split(jax.nn.silu(c_ctx) @ w_mod[l] + b_mod[l], 6, axis=-1)
        a_lat = modulate(rms_norm(h_lat, norm1_g[l]), mod_l[0], mod_l[1])
        a_ctx = modulate(rms_norm(h_ctx, norm1_g[l]), mod_c[0], mod_c[1])
        if l % 2 == 0:
            y_lat, y_ctx = attn_mixer(a_lat, a_ctx, att_w_in[j], att_w_out[j], att_lambda[j],
                                      att_subln_g[j], att_rpb[j], l, need_ctx)
        else:
            y_lat, y_ctx = rec_mixer(a_lat, a_ctx, rec_w_in[j], rec_w_out[j], lbs[l], rec_gnorm_g[j], need_ctx)
        h_lat = h_lat + mod_l[2] * y_lat
        m_lat = modulate(rms_norm(h_lat, norm2_g[l]), mod_l[3], mod_l[4])
        moe_p = (moe_w_group[l], moe_b_group[l], moe_w_router[l], moe_b_router[l],
                 moe_w_gate[l], moe_w_up[l], moe_w_down[l])
        if need_ctx:
            h_ctx = h_ctx + mod_c[2] * y_ctx
            m_ctx = modulate(rms_norm(h_ctx, norm2_g[l]), mod_c[3], mod_c[4])
            tokens = jnp.concatenate([m_ctx.reshape(b_ * c_len, d_), m_lat.reshape(b_ * n_, d_)], axis=0)
            out = hier_moe(tokens, *moe_p)
            h_ctx = h_ctx + mod_c[5] * out[:b_ * c_len].reshape(b_, c_len, d_)
            h_lat = h_lat + mod_l[5] * out[b_ * c_len:].reshape(b_, n_, d_)
        else:
            h_lat = h_lat + mod_l[5] * hier_moe(m_lat.reshape(b_ * n_, d_), *moe_p).reshape(b_, n_, d_)
    return rms_norm(h_lat, final_norm_g)
```

```python
import contextlib
import math
import numpy as np
import ml_dtypes
import concourse.bass as bass
import concourse.mybir as mybir
from concourse.bass_utils import run_bass_kernel_spmd

F32 = mybir.dt.float32
BF16 = mybir.dt.bfloat16
AF = mybir.ActivationFunctionType
ALU = mybir.AluOpType
AX = mybir.AxisListType

D = 2048
KT = 16
CTX = 256
NLAT = 8192
T = CTX + NLAT
DEPTH = 4
EPS = 1e-6
NEG = -30000.0
NEXP = 32
DEXP = 768


class Tok:
    __slots__ = ("eng", "isdma", "sig", "need")

    def __init__(self, eng, isdma):
        self.eng = eng
        self.isdma = isdma
        self.sig = None
        self.need = False


class Prog:
    NSLOT = 8

    def __init__(self, nc, es):
        self.nc = nc
        self.es = es
        self.E = {"pe": nc.tensor, "act": nc.scalar, "dve": nc.vector, "pool": nc.gpsimd, "sync": nc.sync}
        self.csem = {e: es.enter_context(nc.semaphore("cs_" + e)) for e in ("pe", "act", "dve", "pool")}
        self.ccnt = {e: 0 for e in self.csem}
        self.nslot = {"sync": 8, "pool": 3}
        self.dsem = {q: [es.enter_context(nc.semaphore(f"ds_{q}{i}")) for i in range(self.nslot[q])] for q in ("sync", "pool")}
        self.dcnt = {q: 0 for q in self.dsem}
        self.waited = {e: {} for e in self.E}
        self.last_w = {}
        self.readers = {}
        self.ops = []
        self.semobj = {}
        self.pending_barrier = []
        self.n_inst = 0

    def op(self, eng, fn, r=(), w=()):
        self.ops.append((eng, fn, tuple(r), tuple(w), False))

    def dma(self, q, fn, r=(), w=()):
        self.ops.append((q, fn, tuple(r), tuple(w), True))

    def _wait(self, eng, sem, val):
        k = id(sem)
        if self.waited[eng].get(k, 0) < val:
            self.E[eng].wait_ge(sem, val)
            self.waited[eng][k] = val

    def flush(self, barrier=False):
        ops = self.ops
        self.ops = []
        n = len(ops)
        toks = [Tok(o[0], o[4]) for o in ops]
        deps = [None] * n
        last_idx = {}
        for i, (eng, fn, r, w, isdma) in enumerate(ops):
            d = set()
            for k in r:
                t = self.last_w.get(k)
                if t is not None:
                    d.add(t)
            for k in w:
                t = self.last_w.get(k)
                if t is not None:
                    d.add(t)
                for t2 in self.readers.get(k, ()):
                    d.add(t2)
            me = toks[i]
            for k in w:
                self.last_w[k] = me
                self.readers[k] = []
            for k in r:
                self.readers.setdefault(k, []).append(me)
            dd = []
            for t in d:
                if t is me:
                    continue
                if t.eng == eng and eng == "pe" and not t.isdma and not isdma:
                    continue
                dd.append(t)
                t.need = True
            deps[i] = dd
            if not isdma:
                last_idx[eng] = i
        for e, i in last_idx.items():
            toks[i].need = True
        unsig = {e: [] for e in self.csem}
        first_on = set()
        for i, (eng, fn, r, w, isdma) in enumerate(ops):
            me = toks[i]
            if self.pending_barrier and eng not in first_on:
                first_on.add(eng)
                for (sem, val) in self.pending_barrier:
                    self._wait(eng, sem, val)
            if isdma:
                j = self.dcnt[eng]
                ns = self.nslot[eng]
                slot = j % ns
                sem = self.dsem[eng][slot]
                if j >= ns:
                    self._wait(eng, sem, 16 * (j // ns))
                self.dcnt[eng] = j + 1
                me.sig = (sem, 16 * (j // ns + 1))
            need = {}
            for t in deps[i]:
                sem, val = t.sig
                k = id(sem)
                if k not in need or need[k][1] < val:
                    need[k] = (sem, val)
            for sem, val in need.values():
                self._wait(eng, sem, val)
            inst = fn(self.E[eng])
            self.n_inst += 1
            if isdma:
                inst.then_inc(me.sig[0], 16)
            elif me.need:
                self.ccnt[eng] += 1
                inst.then_inc(self.csem[eng], 1)
                me.sig = (self.csem[eng], self.ccnt[eng])
                for t in unsig[eng]:
                    t.sig = me.sig
                unsig[eng] = []
            else:
                unsig[eng].append(me)
        for e in unsig:
            assert not unsig[e]
        if barrier:
            self.pending_barrier = self.all_sigs()

    def all_sigs(self):
        sigs = [(self.csem[e], self.ccnt[e]) for e in self.csem if self.ccnt[e] > 0]
        for q in self.dsem:
            j = self.dcnt[q]
            ns = self.nslot[q]
            for s in range(ns):
                if j > s:
                    cnt = (j - 1 - s) // ns + 1
                    sigs.append((self.dsem[q][s], 16 * cnt))
        return sigs

    def finish(self):
        self.flush()
        for sem, val in self.all_sigs():
            self._wait("sync", sem, val)


def _consts():
    ident = np.eye(128, dtype=np.float32)
    ones = np.ones((128, 128), np.float32)
    tpos = np.arange(NLAT)
    rows = (tpos // 64).astype(np.float32)
    cols = (tpos % 64).astype(np.float32)
    inv = (10000.0 ** (-np.arange(16, dtype=np.float32) / 16)).astype(np.float32)
    cosT = np.zeros((128, NLAT), np.float32)
    sinT = np.zeros((128, NLAT), np.float32)
    for d in range(128):
        dd = d % 64
        axis = dd // 32
        fr = dd % 16
        ang = (rows if axis == 0 else cols) * inv[fr]
        cosT[d] = np.cos(ang)
        sinT[d] = np.sin(ang)
    RT = np.zeros((128, 128), np.float32)
    for m in range(128):
        if (m % 32) < 16:
            RT[m + 16, m] = -1.0
        else:
            RT[m - 16, m] = 1.0
    s_ = np.arange(64)[:, None]
    t_ = np.arange(64)[None, :]
    mfwd = (s_ <= t_).astype(np.float32)
    mbwd = (s_ >= t_).astype(np.float32)
    m128f = np.zeros((128, 128), np.float32)
    m128b = np.zeros((128, 128), np.float32)
    for c in range(2):
        m128f[c * 64:(c + 1) * 64, c * 64:(c + 1) * 64] = mfwd
        m128b[c * 64:(c + 1) * 64, c * 64:(c + 1) * 64] = mbwd
    return dict(ident=ident, ones=ones, cosT=cosT, sinT=sinT, RT=RT, m128f=m128f, m128b=m128b)


NA_CLASS_J = [0, 1, 2, 62, 63]


def _na_class(j):
    if j <= 1:
        return j
    if j >= 62:
        return j - 59
    return 2


def _na_geom():
    idx = np.zeros((5, 640, 128, 2), np.int64)
    valid = np.zeros((5, 640, 128), bool)
    for c, j in enumerate(NA_CLASS_J):
        kbase = int(np.clip(2 * j - 4, 0, 118))
        ql = np.arange(128)
        r = 2 * j + ql // 64
        cq = ql % 64
        rs = np.clip(r - 4, 0, 120)
        cs = np.clip(cq - 8, 0, 48)
        kl = np.arange(640)
        kr = kbase + kl // 64
        kc = kl % 64
        v = (kr[:, None] >= rs[None, :]) & (kr[:, None] < rs[None, :] + 8) & (kc[:, None] >= cs[None, :]) & (kc[:, None] < cs[None, :] + 16)
        ro = kr[:, None] - r[None, :] + 7
        co = kc[:, None] - cq[None, :] + 15
        valid[c] = v
        idx[c, :, :, 0] = np.clip(ro, 0, 14)
        idx[c, :, :, 1] = np.clip(co, 0, 30)
    return idx, valid


def _na_bias(rpb):
    idx, valid = _na_geom()
    out = np.empty((8, 5, 640, 128), np.float32)
    for h in range(8):
        g = rpb[h][idx[..., 0], idx[..., 1]]
        out[h] = np.where(valid, g, np.float32(NEG))
    out = out.reshape(8, 5, 5, 128, 128).transpose(0, 3, 1, 2, 4)
    return np.ascontiguousarray(out)


class Builder:
    def __init__(self, n_layers=DEPTH, debug_out=None):
        self.n_layers = n_layers
        self.debug_out = debug_out
        self.nc = bass.Bass("TRN2", target_bir_lowering=False)
        self.es = contextlib.ExitStack()
        self.pr = Prog(self.nc, self.es)
        self.uid = 0
        self._ins = {}

    def din(self, name, shape, dt=F32):
        return self.nc.dram_tensor(name, list(shape), dt, kind="ExternalInput").ap()

    def dint(self, name, shape, dt):
        return self.nc.dram_tensor(name, list(shape), dt, kind="Internal").ap()

    def sb(self, stack, name, shape, dt):
        self.uid += 1
        return stack.enter_context(self.nc.sbuf_tensor(f"{name}_{self.uid}", list(shape), dt))

    def ps(self, stack, name, shape, dt=F32):
        self.uid += 1
        return stack.enter_context(self.nc.psum_tensor(f"{name}_{self.uid}", list(shape), dt))

    def build(self):
        nc, pr = self.nc, self.pr
        L = self.n_layers
        self.outT = self.nc.dram_tensor("outT", [D, NLAT], F32, kind="ExternalOutput").ap()
        self.hA = self.dint("hA", [D, T], F32)
        self.hB = self.dint("hB", [D, T], F32)
        self.aT = self.dint("aT", [D, T], BF16)
        self.QK = self.dint("QK", [5 * D, T], BF16)
        self.V = self.dint("V", [T, D], BF16)
        self.catT = self.dint("catT", [D, T], BF16)
        self.wb_in = self.dint("wb_in", [D, 5 * D], BF16)
        self.wb_out = self.dint("wb_out", [D, D], BF16)
        self.alloc_moe()

        es = self.es
        self.ident = self.sb(es, "ident", [128, 128], F32)
        self.ones = self.sb(es, "ones", [128, 128], F32)
        self.onesb = self.sb(es, "onesb", [128, 128], BF16)
        self.identb = self.sb(es, "identb", [128, 128], BF16)
        self.MOD = self.sb(es, "MOD", [128, DEPTH * 6 * 16 * 2], F32)
        self.AB = self.sb(es, "AB", [128, DEPTH * 2 * 2 * 16 * 2], F32)
        self.GF = self.sb(es, "GF", [128, 16], F32)
        self.G12 = self.sb(es, "G12", [128, 128], F32)
        pr.dma("sync", lambda e: e.dma_start(out=self.ident[:], in_=self.k_ident[:, :]), w=["ident"])
        pr.op("dve", lambda e: e.memset(self.ones[:], 1.0), w=["ones"])
        pr.op("dve", lambda e: e.memset(self.onesb[:], 1.0), w=["onesb"])
        pr.op("dve", lambda e: e.tensor_copy(out=self.identb[:], in_=self.ident[:]), r=["ident"], w=["identb"])
        self.psb = [self.ps(es, f"bank{i}", [128, 512], F32) for i in range(8)]

        self.phase_mod()
        h_in = self.hT0
        for l in range(L):
            h_mid = self.hA
            h_out = self.hB
            self.phase_norm(l, 1, h_in, router=False)
            if l % 2 == 0:
                self.phase_att(l)
            else:
                self.phase_rec(l)
            self.phase_outproj(l, h_in, h_mid)
            self.phase_norm(l, 2, h_mid, router=True)
            self.phase_moe(l, h_mid, h_out)
            h_in = h_out
        self.phase_final(h_in)
        pr.finish()
        return nc

    def alloc_moe(self):
        self.wb_g2 = self.dint("wb_g2", [NEXP * 128, 16 * DEXP], BF16)
        self.wb_u2 = self.dint("wb_u2", [NEXP * 128, 16 * DEXP], BF16)
        self.wb_d2 = self.dint("wb_d2", [NEXP * 128, 6 * D], BF16)
        self.XS = self.dint("XS", [164 * 128, D], BF16)
        self.YS = self.dint("YS", [164 * 128, D], F32)
        es = self.es
        self.Lsb = self.sb(es, "Lsb", [128, 66, 36], F32)
        self.W1 = self.sb(es, "W1", [128, 66], F32)
        self.W2 = self.sb(es, "W2", [128, 66], F32)
        self.D1 = self.sb(es, "D1", [128, 66], mybir.dt.uint32)
        self.D2 = self.sb(es, "D2", [128, 66], mybir.dt.uint32)
        self.WIDX = self.sb(es, "WIDX", [128, 164], mybir.dt.uint32)

    def modv(self, l, j, f, col):
        o = ((l * 6 + j) * 16 + f) * 2 + col
        return self.MOD[:, o:o + 1]

    def abv(self, l, which, ab, f, col):
        o = ((((l * 2 + (which - 1)) * 2 + ab) * 16) + f) * 2 + col
        return self.AB[:, o:o + 1]

    def cast_w(self, src2d, dst2d, rows, cols, tag):
        pr = self.pr
        for r0 in range(0, rows, 1024):
            r1 = min(rows, r0 + 1024)
            for c0 in range(0, cols, 2048):
                c1 = min(cols, c0 + 2048)
                pr.dma("pool", lambda e, r0=r0, r1=r1, c0=c0, c1=c1: e.dma_start(out=dst2d[r0:r1, c0:c1], in_=src2d[r0:r1, c0:c1]),
                       r=[], w=[(tag, r0, c0)])

    def transpose_rows(self, stack, src_rows_ap, R, dst_ap, tag, bank=0):
        pr = self.pr
        tmp = self.sb(stack, "trtmp", [128, 128], F32)
        kt = ("trtmp", self.uid)
        pr.dma("sync", lambda e: e.dma_start(out=tmp[0:R, :], in_=src_rows_ap), w=[kt])
        pb = self.psb[bank]
        pr.op("pe", lambda e: e.matmul(pb[:, 0:R], lhsT=tmp[0:R, :], rhs=self.ident[0:R, 0:R], start=True, stop=True),
              r=[kt, "ident"], w=[("ps", bank)])
        pr.op("dve", lambda e: e.tensor_copy(out=dst_ap, in_=pb[:, 0:R]), r=[("ps", bank)], w=[tag])

    def phase_mod(self):
        pr = self.pr
        with contextlib.ExitStack() as st:
            cc = self.sb(st, "cc", [128, 32], F32)
            BM = self.sb(st, "BM", [128, 384], F32)
            self.transpose_rows(st, self.c2[:, :], 32, cc[:, :], "cc", bank=0)
            pr.op("act", lambda e: e.activation(out=cc[:, :], in_=cc[:, :], func=AF.Silu), r=["cc"], w=["cc"])
            for a in range(3):
                self.transpose_rows(st, self.b_mod[a * 128:(a + 1) * 128, :], 128, BM[:, a * 128:(a + 1) * 128], ("BM", a), bank=1 + a)
            self.transpose_rows(st, self.g12[:, :], 128, self.G12[:, :], "G12", bank=4)
            self.transpose_rows(st, self.gfin[:, :], 16, self.GF[:, :], "GF", bank=5)
            cc2 = self.sb(st, "cc2", [128, 16, 2], F32)
            pr.op("dve", lambda e: e.tensor_copy(out=cc2[:, :, 0], in_=cc[:, 0:16]), r=["cc"], w=["cc2a"])
            pr.op("dve", lambda e: e.tensor_copy(out=cc2[:, :, 1], in_=cc[:, 16:32]), r=["cc"], w=["cc2b"])
            wm = [self.sb(st, f"wm{i}", [128, 16, 1024], F32) for i in range(2)]
            it = 0
            for l in range(self.n_layers):
                for j in range(6):
                    for half in range(2):
                        b = it % 2
                        bank = 6 + (it % 2)
                        c0 = j * D + half * 1024
                        src = self.w_mod[l, :, c0:c0 + 1024].rearrange("(k p) c -> p k c", p=128)
                        pr.dma("sync", lambda e, b=b, src=src: e.dma_start(out=wm[b][:, :, :], in_=src), w=[("wm", b)])
                        pb = self.psb[bank]
                        for f8 in range(8):
                            for k in range(16):
                                pr.op("pe", lambda e, b=b, f8=f8, k=k, pb=pb: e.matmul(
                                    pb[:, f8 * 2:f8 * 2 + 2], lhsT=wm[b][:, k, f8 * 128:(f8 + 1) * 128], rhs=cc2[:, k, :],
                                    start=(k == 0), stop=(k == 15)), r=[("wm", b), "cc2a", "cc2b"], w=[("ps", bank)])
                        o = ((l * 6 + j) * 16 + half * 8) * 2
                        bo = l * 96 + j * 16 + half * 8
                        for col in range(2):
                            pr.op("dve", lambda e, o=o, bo=bo, col=col, pb=pb: e.tensor_tensor(
                                out=self.MOD[:, o + col:o + 16:2], in0=pb[:, col:16:2], in1=BM[:, bo:bo + 8], op=ALU.add),
                                r=[("ps", bank), ("BM", 0), ("BM", 1), ("BM", 2)], w=["MOD"])
                        it += 1
            for l in range(self.n_layers):
                for which in (1, 2):
                    jsh, jsc = (0, 1) if which == 1 else (3, 4)
                    g = self.G12[:, (which - 1) * 64 + l * 16:(which - 1) * 64 + l * 16 + 16]
                    for col in range(2):
                        osc = ((l * 6 + jsc) * 16) * 2 + col
                        osh = ((l * 6 + jsh) * 16) * 2 + col
                        oa = ((((l * 2 + (which - 1)) * 2 + 0) * 16)) * 2 + col
                        ob = ((((l * 2 + (which - 1)) * 2 + 1) * 16)) * 2 + col
                        pr.op("dve", lambda e, osc=osc, oa=oa, g=g: e.scalar_tensor_tensor(
                            out=self.AB[:, oa:oa + 31:2], in0=self.MOD[:, osc:osc + 31:2], scalar=1.0, in1=g, op0=ALU.add, op1=ALU.mult),
                            r=["MOD", "G12"], w=["AB"])
                        pr.op("dve", lambda e, osh=osh, ob=ob: e.tensor_copy(out=self.AB[:, ob:ob + 31:2], in_=self.MOD[:, osh:osh + 31:2]),
                              r=["MOD"], w=["AB"])
            pr.flush(barrier=True)

    def tok_tiles(self, n):
        out = []
        t0 = 0
        while t0 < T:
            lim = CTX if t0 < CTX else T
            m = min(n, lim - t0)
            out.append((t0, m))
            t0 += m
        return out

    def phase_norm(self, l, which, h_src, router, final=False):
        pr = self.pr
        NT = 256
        with contextlib.ExitStack() as st:
            hb = [self.sb(st, f"nh{i}", [128, 16, NT], F32) for i in range(2)]
            sq = [self.sb(st, f"nsq{i}", [128, 16, NT], F32) for i in range(2)]
            ab = [self.sb(st, f"nab{i}", [128, 16, NT], BF16) for i in range(2)]
            rs = [self.sb(st, f"nrs{i}", [128, NT], F32) for i in range(2)]
            if router:
                WR = self.sb(st, "WR", [128, 16, 36], F32)
                Lsb = self.Lsb
                pr.dma("sync", lambda e: e.dma_start(out=WR[:, :, :], in_=self.moe_wr[l].rearrange("(k p) c -> p k c", p=128)), w=["WR"])
            for i, (t0, n) in enumerate(self.tok_tiles(NT)):
                b = i % 2
                col = 1 if t0 < CTX else 0
                bank = i % 2
                pr.dma("sync", lambda e, b=b, t0=t0, n=n: e.dma_start(
                    out=hb[b][:, :, 0:n], in_=h_src[:, t0:t0 + n].rearrange("(k p) t -> p k t", p=128)), w=[("nh", b)])
                pr.op("act", lambda e, b=b, n=n: e.activation(out=sq[b][:, :, 0:n], in_=hb[b][:, :, 0:n], func=AF.Square),
                      r=[("nh", b)], w=[("nsq", b)])
                pb = self.psb[bank]
                for k in range(16):
                    pr.op("pe", lambda e, b=b, k=k, n=n, pb=pb: e.matmul(pb[:, 0:n], lhsT=self.ones[:, :], rhs=sq[b][:, k, 0:n],
                                                                       start=(k == 0), stop=(k == 15)),
                          r=[("nsq", b), "ones"], w=[("ps", bank)])
                pr.op("dve", lambda e, b=b, n=n, pb=pb: e.tensor_scalar(out=rs[b][:, 0:n], in0=pb[:, 0:n], scalar1=1.0 / D, scalar2=EPS,
                                                                      op0=ALU.mult, op1=ALU.add), r=[("ps", bank)], w=[("nrs", b)])
                pr.op("act", lambda e, b=b, n=n: e.activation(out=rs[b][:, 0:n], in_=rs[b][:, 0:n], func=AF.Sqrt), r=[("nrs", b)], w=[("nrs", b)])
                pr.op("dve", lambda e, b=b, n=n: e.reciprocal(out=rs[b][:, 0:n], in_=rs[b][:, 0:n]), r=[("nrs", b)], w=[("nrs", b)])
                for k in range(16):
                    pr.op("dve", lambda e, b=b, k=k, n=n: e.tensor_tensor(out=sq[b][:, k, 0:n], in0=hb[b][:, k, 0:n], in1=rs[b][:, 0:n],
                                                                        op=ALU.mult), r=[("nh", b), ("nrs", b)], w=[("nsq", b)])
                for k in range(16):
                    if final:
                        sc1, sc2, o1 = self.GF[:, k:k + 1], None, ALU.bypass
                        pr.op("dve", lambda e, b=b, k=k, n=n, sc1=sc1: e.tensor_scalar(
                            out=hb[b][:, k, 0:n], in0=sq[b][:, k, 0:n], scalar1=sc1, scalar2=None, op0=ALU.mult),
                            r=[("nsq", b), "GF"], w=[("nh", b)])
                    else:
                        A = self.abv(l, which, 0, k, col)
                        Bv = self.abv(l, which, 1, k, col)
                        dst = sq[b] if router else ab[b]
                        pr.op("dve", lambda e, b=b, k=k, n=n, A=A, Bv=Bv, dst=dst: e.tensor_scalar(
                            out=dst[:, k, 0:n], in0=sq[b][:, k, 0:n], scalar1=A, scalar2=Bv, op0=ALU.mult, op1=ALU.add),
                            r=[("nsq", b), "AB"], w=[("nsq", b) if router else ("nab", b)])
                if final:
                    if t0 >= CTX:
                        pr.dma("pool", lambda e, b=b, t0=t0, n=n: e.dma_start(
                            out=self.outT[:, t0 - CTX:t0 - CTX + n].rearrange("(k p) t -> p k t", p=128), in_=hb[b][:, :, 0:n]),
                            r=[("nh", b)], w=[("outT", t0)])
                    continue
                if router:
                    pr.op("act", lambda e, b=b, n=n: e.activation(out=ab[b][:, :, 0:n], in_=sq[b][:, :, 0:n], func=AF.Copy),
                          r=[("nsq", b)], w=[("nab", b)])
                    for s in range(n // 128):
                        rb = 2 + (s % 2)
                        pbl = self.psb[rb]
                        for k in range(16):
                            pr.op("pe", lambda e, b=b, k=k, s=s, pbl=pbl: e.matmul(
                                pbl[:, 0:36], lhsT=sq[b][:, k, s * 128:(s + 1) * 128], rhs=WR[:, k, :], start=(k == 0), stop=(k == 15)),
                                r=[("nsq", b), "WR"], w=[("ps", rb)])
                        blk = t0 // 128 + s
                        pr.op("act", lambda e, blk=blk, pbl=pbl: e.activation(out=Lsb[:, blk, :], in_=pbl[:, 0:36], func=AF.Copy),
                              r=[("ps", rb)], w=[("Lsb", blk)])
                pr.dma("pool", lambda e, b=b, t0=t0, n=n: e.dma_start(
                    out=self.aT[:, t0:t0 + n].rearrange("(k p) t -> p k t", p=128), in_=ab[b][:, :, 0:n]),
                    r=[("nab", b)], w=[("aT", t0)])
            pr.flush(barrier=True)
        if router:
            with contextlib.ExitStack() as st2:
                self.route(st2, l, self.Lsb)
                pr.flush(barrier=True)

    def phase_final(self, h_src):
        self.phase_norm(0, 1, h_src, router=False, final=True)

    def route(self, st, l, Lsb):
        pr = self.pr
        NB = 66
        br = self.sb(st, "br", [128, 36], F32)
        pr.dma("sync", lambda e: e.dma_start(out=br[:, :], in_=self.moe_br[l:l + 1, :].partition_broadcast(128)), w=["br"])
        allL = [("Lsb", b) for b in range(NB)]
        Lb = self.sb(st, "Lb", [128, NB, 36], F32)
        pr.op("dve", lambda e: e.tensor_tensor(out=Lb[:, :, :], in0=Lsb[:, :, :], in1=br[:, :].unsqueeze(1).to_broadcast([128, NB, 36]),
                                               op=ALU.add), r=allL + ["br"], w=["Lb"])
        gmax = self.sb(st, "gmax", [128, NB], F32)
        pr.op("dve", lambda e: e.tensor_reduce(out=gmax[:, :], in_=Lb[:, :, 0:4], axis=AX.X, op=ALU.max), r=["Lb"], w=["gmax"])
        gd = self.sb(st, "gd", [128, NB, 4], F32)
        pr.op("dve", lambda e: e.tensor_tensor(out=gd[:, :, :], in0=Lb[:, :, 0:4], in1=gmax[:, :].unsqueeze(2).to_broadcast([128, NB, 4]),
                                               op=ALU.subtract), r=["Lb", "gmax"], w=["gd"])
        ohg = self.sb(st, "ohg", [128, NB, 4], F32)
        pr.op("dve", lambda e: e.tensor_scalar(out=ohg[:, :, :], in0=gd[:, :, :], scalar1=0.0, scalar2=None, op0=ALU.is_ge), r=["gd"], w=["ohg"])
        ge = self.sb(st, "ge", [128, NB, 4], F32)
        pr.op("act", lambda e: e.activation(out=ge[:, :, :], in_=gd[:, :, :], func=AF.Exp), r=["gd"], w=["ge"])
        gs = self.sb(st, "gs", [128, NB], F32)
        pr.op("dve", lambda e: e.tensor_reduce(out=gs[:, :], in_=ge[:, :, :], axis=AX.X, op=ALU.add), r=["ge"], w=["gs"])
        gw = self.sb(st, "gw", [128, NB], F32)
        pr.op("dve", lambda e: e.reciprocal(out=gw[:, :], in_=gs[:, :]), r=["gs"], w=["gw"])
        pen = self.sb(st, "pen", [128, NB, 4], F32)
        pr.op("dve", lambda e: e.tensor_scalar(out=pen[:, :, :], in0=ohg[:, :, :], scalar1=-1.0, scalar2=1.0e4, op0=ALU.add, op1=ALU.mult),
              r=["ohg"], w=["pen"])
        el = self.sb(st, "el", [128, NB, 32], F32)
        for g in range(4):
            pr.op("dve", lambda e, g=g: e.tensor_tensor(out=el[:, :, g * 8:(g + 1) * 8], in0=Lb[:, :, 4 + g * 8:4 + (g + 1) * 8],
                                                        in1=pen[:, :, g:g + 1].to_broadcast([128, NB, 8]), op=ALU.add),
                  r=["Lb", "pen"], w=["el"])
        v1 = self.sb(st, "v1", [128, NB], F32)
        pr.op("dve", lambda e: e.tensor_reduce(out=v1[:, :], in_=el[:, :, :], axis=AX.X, op=ALU.max), r=["el"], w=["v1"])
        oh1 = self.sb(st, "oh1", [128, NB, 32], F32)
        pr.op("dve", lambda e: e.tensor_tensor(out=oh1[:, :, :], in0=el[:, :, :], in1=v1[:, :].unsqueeze(2).to_broadcast([128, NB, 32]),
                                               op=ALU.is_ge), r=["el", "v1"], w=["oh1"])
        el2 = self.sb(st, "el2", [128, NB, 32], F32)
        pr.op("dve", lambda e: e.scalar_tensor_tensor(out=el2[:, :, :], in0=oh1[:, :, :], scalar=-1.0e4, in1=el[:, :, :],
                                                      op0=ALU.mult, op1=ALU.add), r=["oh1", "el"], w=["el2"])
        v2 = self.sb(st, "v2", [128, NB], F32)
        pr.op("dve", lambda e: e.tensor_reduce(out=v2[:, :], in_=el2[:, :, :], axis=AX.X, op=ALU.max), r=["el2"], w=["v2"])
        oh2 = self.sb(st, "oh2", [128, NB, 32], F32)
        pr.op("dve", lambda e: e.tensor_tensor(out=oh2[:, :, :], in0=el2[:, :, :], in1=v2[:, :].unsqueeze(2).to_broadcast([128, NB, 32]),
                                               op=ALU.is_ge), r=["el2", "v2"], w=["oh2"])
        rr = self.sb(st, "rr", [128, NB], F32)
        pr.op("dve", lambda e: e.tensor_tensor(out=rr[:, :], in0=v2[:, :], in1=v1[:, :], op=ALU.subtract), r=["v1", "v2"], w=["rr"])
        pr.op("act", lambda e: e.activation(out=rr[:, :], in_=rr[:, :], func=AF.Exp), r=["rr"], w=["rr"])
        W1, W2 = self.W1, self.W2
        pr.op("dve", lambda e: e.tensor_scalar(out=W1[:, :], in0=rr[:, :], scalar1=1.0, scalar2=None, op0=ALU.add), r=["rr"], w=["W1"])
        pr.op("dve", lambda e: e.reciprocal(out=W1[:, :], in_=W1[:, :]), r=["W1"], w=["W1"])
        pr.op("dve", lambda e: e.tensor_tensor(out=W1[:, :], in0=W1[:, :], in1=gw[:, :], op=ALU.mult), r=["W1", "gw"], w=["W1"])
        pr.op("dve", lambda e: e.tensor_tensor(out=W2[:, :], in0=W1[:, :], in1=rr[:, :], op=ALU.mult), r=["W1", "rr"], w=["W2"])
        cnt = el
        pr.op("dve", lambda e: e.tensor_tensor(out=cnt[:, :, :], in0=oh1[:, :, :], in1=oh2[:, :, :], op=ALU.add), r=["oh1", "oh2", "el2"], w=["el"])
        cntT = self.sb(st, "cntT", [32, T], F32)
        PSi = self.sb(st, "PSi", [32, T], F32)
        one32 = self.sb(st, "one32", [32, T], F32)
        pr.op("dve", lambda e: e.memset(one32[:, :], 1.0), w=["one32"])
        for blk in range(NB):
            bank = 4 + (blk % 2)
            pb = self.psb[bank]
            pr.op("pe", lambda e, blk=blk, pb=pb: e.matmul(pb[0:32, 0:128], lhsT=cnt[:, blk, :], rhs=self.ident[:, :], start=True, stop=True),
                  r=["el", "ident"], w=[("ps", bank)])
            pr.op("act", lambda e, blk=blk, pb=pb: e.activation(out=cntT[:, blk * 128:(blk + 1) * 128], in_=pb[0:32, 0:128], func=AF.Copy),
                  r=[("ps", bank)], w=[("cntT", blk)])
        allc = [("cntT", b) for b in range(NB)]
        pr.op("dve", lambda e: e.tensor_tensor_scan(out=PSi[:, :], data0=one32[:, :], data1=cntT[:, :], initial=0.0, op0=ALU.mult, op1=ALU.add),
              r=allc + ["one32"], w=["PSi"])
        kio = self.sb(st, "kio", [32, 164], F32)
        pr.dma("sync", lambda e: e.dma_start(out=kio[:, :], in_=self.k_iota[:, :]), w=["kio"])
        UT = self.sb(st, "UT", [32, 32], F32)
        pr.dma("sync", lambda e: e.dma_start(out=UT[:, :], in_=self.k_uincl[:, :]), w=["UT"])
        pidx = self.sb(st, "pidx", [128, 1], F32)
        pr.dma("sync", lambda e: e.dma_start(out=pidx[:, :], in_=self.k_pidx[:, :]), w=["pidx"])
        cmpb = self.sb(st, "cmpb", [32, 164], F32)
        pr.op("dve", lambda e: e.tensor_scalar(out=cmpb[:, 0:132], in0=kio[:, 0:132], scalar1=PSi[:, T - 1:T], scalar2=None, op0=ALU.is_lt),
              r=["kio", "PSi"], w=["cmpb"])
        padded = self.sb(st, "padded", [32, 1], F32)
        pr.op("dve", lambda e: e.tensor_reduce(out=padded[:, :], in_=cmpb[:, 0:132], axis=AX.X, op=ALU.add), r=["cmpb"], w=["padded"])
        pr.op("dve", lambda e: e.tensor_scalar(out=padded[:, :], in0=padded[:, :], scalar1=128.0, scalar2=None, op0=ALU.mult), r=["padded"], w=["padded"])
        pend = self.sb(st, "pend", [32, 1], F32)
        pstart = self.sb(st, "pstart", [32, 1], F32)
        pr.op("pe", lambda e: e.matmul(self.psb[6][0:32, 0:1], lhsT=UT[:, :], rhs=padded[:, :], start=True, stop=True), r=["UT", "padded"], w=[("ps", 6)])
        pr.op("dve", lambda e: e.tensor_copy(out=pend[:, :], in_=self.psb[6][0:32, 0:1]), r=[("ps", 6)], w=["pend"])
        pr.op("dve", lambda e: e.tensor_tensor(out=pstart[:, :], in0=pend[:, :], in1=padded[:, :], op=ALU.subtract), r=["pend", "padded"], w=["pstart"])
        pr.op("dve", lambda e: e.tensor_scalar(out=cmpb[:, :], in0=kio[:, :], scalar1=pend[:, 0:1], scalar2=None, op0=ALU.is_ge),
              r=["kio", "pend", "padded"], w=["cmpb"])
        pr.op("pe", lambda e: e.matmul(self.psb[7][:, 0:164], lhsT=self.ones[0:32, :], rhs=cmpb[:, :], start=True, stop=True), r=["cmpb", "ones"], w=[("ps", 7)])
        EBf = self.sb(st, "EBf", [128, 164], F32)
        pr.op("dve", lambda e: e.tensor_scalar(out=EBf[:, :], in0=self.psb[7][:, 0:164], scalar1=31.0, scalar2=128.0, op0=ALU.min, op1=ALU.mult),
              r=[("ps", 7)], w=["EBf"])
        pr.op("dve", lambda e: e.tensor_scalar(out=EBf[:, :], in0=EBf[:, :], scalar1=pidx[:, 0:1], scalar2=None, op0=ALU.add), r=["EBf", "pidx"], w=["EBf"])
        pr.op("dve", lambda e: e.tensor_copy(out=self.WIDX[:, :], in_=EBf[:, :]), r=["EBf"], w=["WIDX"])
        pr.op("dve", lambda e: e.tensor_tensor(out=PSi[:, :], in0=PSi[:, :], in1=cntT[:, :], op=ALU.subtract), r=["PSi", "cmpb"] + allc, w=["PSi"])
        pr.op("dve", lambda e: e.tensor_scalar(out=PSi[:, :], in0=PSi[:, :], scalar1=pstart[:, 0:1], scalar2=None, op0=ALU.add), r=["PSi", "pstart"], w=["PSi"])
        DT = el2
        for blk in range(NB):
            bank = 4 + (blk % 2)
            pb = self.psb[bank]
            pr.op("pe", lambda e, blk=blk, pb=pb: e.matmul(pb[:, 0:32], lhsT=PSi[:, blk * 128:(blk + 1) * 128], rhs=self.ident[0:32, 0:32], start=True, stop=True),
                  r=["PSi", "ident"], w=[("ps", bank)])
            pr.op("act", lambda e, blk=blk, pb=pb: e.activation(out=DT[:, blk, :], in_=pb[:, 0:32], func=AF.Copy), r=[("ps", bank), "oh2"], w=[("DT", blk)])
        allD = [("DT", b) for b in range(NB)]
        d1 = self.sb(st, "d1f", [128, NB], F32)
        pr.op("dve", lambda e: e.tensor_tensor(out=oh1[:, :, :], in0=oh1[:, :, :], in1=DT[:, :, :], op=ALU.mult), r=allD + ["oh1", "el"], w=["oh1"])
        pr.op("dve", lambda e: e.tensor_reduce(out=d1[:, :], in_=oh1[:, :, :], axis=AX.X, op=ALU.add), r=["oh1"], w=["d1f"])
        pr.op("dve", lambda e: e.tensor_copy(out=self.D1[:, :], in_=d1[:, :]), r=["d1f"], w=["D1"])
        pr.op("dve", lambda e: e.tensor_tensor(out=oh2[:, :, :], in0=oh2[:, :, :], in1=DT[:, :, :], op=ALU.mult), r=allD + ["oh2", "el"], w=["oh2"])
        pr.op("dve", lambda e: e.tensor_reduce(out=d1[:, :], in_=oh2[:, :, :], axis=AX.X, op=ALU.add), r=["oh2", "D1"], w=["d1f"])
        pr.op("dve", lambda e: e.tensor_copy(out=self.D2[:, :], in_=d1[:, :]), r=["d1f"], w=["D2"])

    def linear(self, st, xT, Wb, K, M, evac, tm_cols=None, tm_evac=None, NB=1024, tag="lin"):
        pr = self.pr
        kt = K // 128
        xb = [self.sb(st, f"{tag}x{i}", [128, kt, NB], BF16) for i in range(2)]
        wp = [self.sb(st, f"{tag}w{i}", [128, kt, 512], BF16) for i in range(2)]
        blocks = self.tok_tiles(NB)
        wi = 0
        bi = 0
        for bidx, (t0, n) in enumerate(blocks):
            xbuf = bidx % 2
            pr.dma("sync", lambda e, xbuf=xbuf, t0=t0, n=n: e.dma_start(
                out=xb[xbuf][:, :, 0:n], in_=xT[:, t0:t0 + n].rearrange("(k p) t -> p k t", p=128)), r=[(tag + "src",)], w=[(tag + "x", xbuf)])
            for c0 in range(0, M, 512):
                cw = min(512, M - c0)
                wbuf = wi % 2
                wi += 1
                pr.dma("sync", lambda e, wbuf=wbuf, c0=c0, cw=cw: e.dma_start(
                    out=wp[wbuf][:, :, 0:cw], in_=Wb[:, c0:c0 + cw].rearrange("(k p) c -> p k c", p=128)), r=[(tag + "wsrc",)], w=[(tag + "w", wbuf)])
                if tm_cols is not None and c0 in tm_cols:
                    for s in range(n // 128):
                        bank = bi % 4
                        bi += 1
                        pb = self.psb[bank]
                        for k in range(kt):
                            pr.op("pe", lambda e, xbuf=xbuf, wbuf=wbuf, k=k, s=s, cw=cw, pb=pb: e.matmul(
                                pb[:, 0:cw], lhsT=xb[xbuf][:, k, s * 128:(s + 1) * 128], rhs=wp[wbuf][:, k, 0:cw],
                                start=(k == 0), stop=(k == kt - 1)), r=[(tag + "x", xbuf), (tag + "w", wbuf)], w=[("ps", bank)])
                        tm_evac(c0, t0 + s * 128, pb[:, 0:cw], bank)
                    continue
                for mi in range(cw // 128):
                    for s0 in range(0, n, 512):
                        sn = min(512, n - s0)
                        bank = bi % 4
                        bi += 1
                        pb = self.psb[bank]
                        for k in range(kt):
                            pr.op("pe", lambda e, xbuf=xbuf, wbuf=wbuf, k=k, mi=mi, s0=s0, sn=sn, pb=pb: e.matmul(
                                pb[:, 0:sn], lhsT=wp[wbuf][:, k, mi * 128:(mi + 1) * 128], rhs=xb[xbuf][:, k, s0:s0 + sn],
                                start=(k == 0), stop=(k == kt - 1)), r=[(tag + "x", xbuf), (tag + "w", wbuf)], w=[("ps", bank)])
                        evac(c0 // 128 + mi, t0 + s0, sn, pb[:, 0:sn], bank)

    def phase_outproj(self, l, h_in, h_out):
        pr = self.pr
        j = l // 2
        wsrc = self.att_w_out if l % 2 == 0 else self.rec_w_out
        self.cast_w(wsrc[j], self.wb_out, D, D, "wbo")
        pr.flush(barrier=True)
        with contextlib.ExitStack() as st:
            ho = [self.sb(st, f"ho{i}", [128, 512], F32) for i in range(4)]
            cnt = [0]

            def evac(mt, t0, n, pap, bank):
                b = cnt[0] % 4
                cnt[0] += 1
                col = 1 if t0 < CTX else 0
                pr.dma("sync", lambda e: e.dma_start(out=ho[b][:, 0:n], in_=h_in[mt * 128:(mt + 1) * 128, t0:t0 + n]), w=[("ho", b)])
                gate = self.modv(l, 2, mt, col)
                pr.op("dve", lambda e: e.scalar_tensor_tensor(out=ho[b][:, 0:n], in0=pap, scalar=gate, in1=ho[b][:, 0:n],
                                                              op0=ALU.mult, op1=ALU.add), r=[("ps", bank), ("ho", b), "MOD"], w=[("ho", b)])
                pr.dma("pool", lambda e: e.dma_start(out=h_out[mt * 128:(mt + 1) * 128, t0:t0 + n], in_=ho[b][:, 0:n]), r=[("ho", b)], w=[("hout", mt, t0)])

            self.linear(st, self.catT, self.wb_out, D, D, evac, tag="op")
            pr.flush(barrier=True)

    def phase_moe(self, l, h_in, h_out):
        pr = self.pr
        IOA = bass.IndirectOffsetOnAxis
        NBLK = 164
        for e_ in range(NEXP):
            pr.dma("pool", lambda e, e_=e_: e.dma_start(out=self.wb_g2[e_ * 128:(e_ + 1) * 128, :].rearrange("p (k c) -> p k c", k=16),
                                                       in_=self.moe_wg[l, e_].rearrange("(k p) c -> p k c", p=128)), w=[("wbg", e_)])
            pr.dma("pool", lambda e, e_=e_: e.dma_start(out=self.wb_u2[e_ * 128:(e_ + 1) * 128, :].rearrange("p (k c) -> p k c", k=16),
                                                       in_=self.moe_wu[l, e_].rearrange("(k p) c -> p k c", p=128)), w=[("wbu", e_)])
            pr.dma("pool", lambda e, e_=e_: e.dma_start(out=self.wb_d2[e_ * 128:(e_ + 1) * 128, :].rearrange("p (k c) -> p k c", k=6),
                                                       in_=self.moe_wd[l, e_].rearrange("(k p) c -> p k c", p=128)), w=[("wbd", e_)])
        pr.flush(barrier=True)
        with contextlib.ExitStack() as st:
            xa = [self.sb(st, f"dxa{i}", [128, 16, 128], BF16) for i in range(2)]
            xt = [self.sb(st, f"dxt{i}", [128, D], BF16) for i in range(2)]
            zt = self.sb(st, "dzt", [128, D], BF16)
            pr.op("dve", lambda e: e.memset(zt[:, :], 0.0), w=["dzt"])
            for blk in range(NBLK):
                pr.dma("sync", lambda e, blk=blk: e.dma_start(out=self.XS[blk * 128:(blk + 1) * 128, :], in_=zt[:, :]), r=["dzt"], w=[("XSz", blk)])
            pr.flush(barrier=True)
            for tt in range(66):
                b = tt % 2
                pr.dma("sync", lambda e, b=b, tt=tt: e.dma_start(out=xa[b][:, :, :], in_=self.aT[:, tt * 128:(tt + 1) * 128].rearrange("(k p) t -> p k t", p=128)),
                       w=[("dxa", b)])
                for q in range(4):
                    bank = (tt * 4 + q) % 4
                    pb = self.psb[bank]
                    for kk in range(4):
                        k = q * 4 + kk
                        pr.op("pe", lambda e, b=b, k=k, kk=kk, pb=pb: e.matmul(pb[:, kk * 128:(kk + 1) * 128], lhsT=xa[b][:, k, :], rhs=self.identb[:, :], start=True, stop=True),
                              r=[("dxa", b), "identb"], w=[("ps", bank)])
                    pr.op("act" if q % 2 == 0 else "dve",
                          (lambda e, b=b, q=q, pb=pb: e.activation(out=xt[b][:, q * 512:(q + 1) * 512], in_=pb[:, :], func=AF.Copy)) if q % 2 == 0 else
                          (lambda e, b=b, q=q, pb=pb: e.tensor_copy(out=xt[b][:, q * 512:(q + 1) * 512], in_=pb[:, :])),
                          r=[("ps", bank)], w=[("dxt", b, q)])
                xk = [("dxt", b, q) for q in range(4)]
                pr.dma("pool", lambda e, b=b, tt=tt: e.indirect_dma_start(out=self.XS[:, :], out_offset=IOA(ap=self.D1[:, tt:tt + 1], axis=0),
                                                                        in_=xt[b][:, :], in_offset=None), r=xk + ["D1"], w=[("XS", tt, 0)])
                pr.dma("pool", lambda e, b=b, tt=tt: e.indirect_dma_start(out=self.XS[:, :], out_offset=IOA(ap=self.D2[:, tt:tt + 1], axis=0),
                                                                        in_=xt[b][:, :], in_offset=None), r=xk + ["D2"], w=[("XS", tt, 1)])
            pr.flush(barrier=True)
        with contextlib.ExitStack() as st:
            wg = [self.sb(st, f"swg{i}", [128, 16 * DEXP], BF16) for i in range(2)]
            wu = [self.sb(st, f"swu{i}", [128, 16 * DEXP], BF16) for i in range(2)]
            wd = [self.sb(st, f"swd{i}", [128, 6 * D], BF16) for i in range(2)]
            xs = [self.sb(st, f"sxs{i}", [128, D], BF16) for i in range(2)]
            xT = [self.sb(st, f"sxT{i}", [128, 16, 128], BF16) for i in range(2)]
            sg = [self.sb(st, f"ssg{i}", [128, DEXP], F32) for i in range(2)]
            hT = [self.sb(st, f"shT{i}", [128, 6, 128], BF16) for i in range(2)]
            ysb = [self.sb(st, "sys0", [128, D], F32)] * 2
            bi = 0
            for blk in range(NBLK):
                b = blk % 2
                pr.dma("pool", lambda e, b=b, blk=blk: e.indirect_dma_start(out=wg[b][:, :], out_offset=None, in_=self.wb_g2[:, :],
                                                                          in_offset=IOA(ap=self.WIDX[:, blk:blk + 1], axis=0)), r=["WIDX"], w=[("swg", b)])
                pr.dma("pool", lambda e, b=b, blk=blk: e.indirect_dma_start(out=wu[b][:, :], out_offset=None, in_=self.wb_u2[:, :],
                                                                          in_offset=IOA(ap=self.WIDX[:, blk:blk + 1], axis=0)), r=["WIDX"], w=[("swu", b)])
                pr.dma("pool", lambda e, b=b, blk=blk: e.indirect_dma_start(out=wd[b][:, :], out_offset=None, in_=self.wb_d2[:, :],
                                                                          in_offset=IOA(ap=self.WIDX[:, blk:blk + 1], axis=0)), r=["WIDX"], w=[("swd", b)])
                pr.dma("sync", lambda e, b=b, blk=blk: e.dma_start(out=xs[b][:, :], in_=self.XS[blk * 128:(blk + 1) * 128, :]), w=[("sxs", b)])
                for q in range(4):
                    bank = q
                    pb = self.psb[bank]
                    for kk in range(4):
                        k = q * 4 + kk
                        pr.op("pe", lambda e, b=b, k=k, kk=kk, pb=pb: e.matmul(pb[:, kk * 128:(kk + 1) * 128], lhsT=xs[b][:, k * 128:(k + 1) * 128], rhs=self.identb[:, :], start=True, stop=True),
                              r=[("sxs", b), "identb"], w=[("ps", bank)])
                    if q % 2 == 0:
                        pr.op("act", lambda e, b=b, q=q, pb=pb: e.activation(out=xT[b][:, q * 4:(q + 1) * 4, :], in_=pb[:, :].rearrange("p (a t) -> p a t", a=4), func=AF.Copy),
                              r=[("ps", bank)], w=[("sxT", b, q)])
                    else:
                        pr.op("dve", lambda e, b=b, q=q, pb=pb: e.tensor_copy(out=xT[b][:, q * 4:(q + 1) * 4, :], in_=pb[:, :].rearrange("p (a t) -> p a t", a=4)),
                              r=[("ps", bank)], w=[("sxT", b, q)])
                xk = [("sxT", b, q) for q in range(4)]
                for (wsb, wkey, b0) in ((wg, "swg", 4), (wu, "swu", 6)):
                    for jt in range(6):
                        pbk = b0 + (0 if jt < 4 else 1)
                        dst = self.psb[pbk][:, (jt % 4) * 128:(jt % 4 + 1) * 128]
                        for k in range(16):
                            pr.op("pe", lambda e, wsb=wsb, b=b, k=k, jt=jt, dst=dst: e.matmul(
                                dst, lhsT=wsb[b][:, k * DEXP + jt * 128:k * DEXP + (jt + 1) * 128], rhs=xT[b][:, k, :], start=(k == 0), stop=(k == 15)),
                                r=[(wkey, b)] + xk, w=[("ps", pbk)])
                pr.op("act", lambda e, b=b: e.activation(out=sg[b][:, 0:512], in_=self.psb[4][:, :], func=AF.Silu), r=[("ps", 4)], w=[("ssg", b, 0)])
                pr.op("act", lambda e, b=b: e.activation(out=sg[b][:, 512:768], in_=self.psb[5][:, 0:256], func=AF.Silu), r=[("ps", 5)], w=[("ssg", b, 1)])
                pr.op("dve", lambda e, b=b: e.tensor_tensor(out=hT[b][:, 0:4, :], in0=sg[b][:, 0:512].rearrange("p (a t) -> p a t", a=4),
                                                            in1=self.psb[6][:, :].rearrange("p (a t) -> p a t", a=4), op=ALU.mult),
                      r=[("ssg", b, 0), ("ps", 6)], w=[("shT", b, 0)])
                pr.op("dve", lambda e, b=b: e.tensor_tensor(out=hT[b][:, 4:6, :], in0=sg[b][:, 512:768].rearrange("p (a t) -> p a t", a=2),
                                                            in1=self.psb[7][:, 0:256].rearrange("p (a t) -> p a t", a=2), op=ALU.mult),
                      r=[("ssg", b, 1), ("ps", 7)], w=[("shT", b, 1)])
                for c4 in range(4):
                    bank = c4
                    pb = self.psb[bank]
                    for k in range(6):
                        pr.op("pe", lambda e, b=b, k=k, c4=c4, pb=pb: e.matmul(pb[:, :], lhsT=hT[b][:, k, :], rhs=wd[b][:, k * D + c4 * 512:k * D + (c4 + 1) * 512],
                                                                             start=(k == 0), stop=(k == 5)), r=[("shT", b, 0), ("shT", b, 1), ("swd", b)], w=[("ps", bank)])
                    if c4 % 2 == 0:
                        pr.op("act", lambda e, b=b, c4=c4, pb=pb: e.activation(out=ysb[b][:, c4 * 512:(c4 + 1) * 512], in_=pb[:, :], func=AF.Copy),
                              r=[("ps", bank)], w=[("sys", b, c4)])
                    else:
                        pr.op("dve", lambda e, b=b, c4=c4, pb=pb: e.tensor_copy(out=ysb[b][:, c4 * 512:(c4 + 1) * 512], in_=pb[:, :]),
                              r=[("ps", bank)], w=[("sys", b, c4)])
                pr.dma("sync", lambda e, b=b, blk=blk: e.dma_start(out=self.YS[blk * 128:(blk + 1) * 128, :], in_=ysb[b][:, :]),
                       r=[("sys", b, c) for c in range(4)], w=[("YS", blk)])
            pr.flush(barrier=True)
        with contextlib.ExitStack() as st:
            y1 = [self.sb(st, f"cy1{i}", [128, D], F32) for i in range(2)]
            y2 = [self.sb(st, f"cy2{i}", [128, D], F32) for i in range(2)]
            ho = [self.sb(st, f"cho{i}", [128, 16, 128], F32) for i in range(2)]
            for tt in range(66):
                b = tt % 2
                col = 1 if tt < 2 else 0
                pr.dma("pool", lambda e, b=b, tt=tt: e.indirect_dma_start(out=y1[b][:, :], out_offset=None, in_=self.YS[:, :],
                                                                        in_offset=IOA(ap=self.D1[:, tt:tt + 1], axis=0)), r=["D1"], w=[("cy1", b)])
                pr.dma("pool", lambda e, b=b, tt=tt: e.indirect_dma_start(out=y2[b][:, :], out_offset=None, in_=self.YS[:, :],
                                                                        in_offset=IOA(ap=self.D2[:, tt:tt + 1], axis=0)), r=["D2"], w=[("cy2", b)])
                pr.dma("sync", lambda e, b=b, tt=tt: e.dma_start(out=ho[b][:, :, :], in_=h_in[:, tt * 128:(tt + 1) * 128].rearrange("(k p) t -> p k t", p=128)),
                       w=[("cho", b)])
                pr.op("dve", lambda e, b=b, tt=tt: e.tensor_scalar(out=y1[b][:, :], in0=y1[b][:, :], scalar1=self.W1[:, tt:tt + 1], scalar2=None, op0=ALU.mult),
                      r=[("cy1", b), "W1"], w=[("cy1", b)])
                pr.op("dve", lambda e, b=b, tt=tt: e.scalar_tensor_tensor(out=y1[b][:, :], in0=y2[b][:, :], scalar=self.W2[:, tt:tt + 1], in1=y1[b][:, :],
                                                                         op0=ALU.mult, op1=ALU.add), r=[("cy1", b), ("cy2", b), "W2"], w=[("cy1", b)])
                for k in range(16):
                    bank = k % 8
                    pb = self.psb[bank]
                    pr.op("pe", lambda e, b=b, k=k, pb=pb: e.matmul(pb[:, 0:128], lhsT=y1[b][:, k * 128:(k + 1) * 128], rhs=self.ident[:, :], start=True, stop=True),
                          r=[("cy1", b), "ident"], w=[("ps", bank)])
                    gate = self.modv(l, 5, k, col)
                    pr.op("dve", lambda e, b=b, k=k, pb=pb, gate=gate: e.scalar_tensor_tensor(out=ho[b][:, k, :], in0=pb[:, 0:128], scalar=gate, in1=ho[b][:, k, :],
                                                                                            op0=ALU.mult, op1=ALU.add), r=[("ps", bank), ("cho", b), "MOD"], w=[("cho", b)])
                pr.dma("sync", lambda e, b=b, tt=tt: e.dma_start(out=h_out[:, tt * 128:(tt + 1) * 128].rearrange("(k p) t -> p k t", p=128), in_=ho[b][:, :, :]),
                       r=[("cho", b)], w=[("hout2", tt)])
            pr.flush(barrier=True)

    def phase_att(self, l):
        pr = self.pr
        j = l // 2
        sa = 64 ** -0.5
        sn = 128 ** -0.5
        lam_init = 0.8 - 0.6 * math.exp(-0.3 * l)
        self.cast_w(self.att_w_in[j], self.wb_in[:, 0:6144], D, 6144, "wbi")
        pr.flush(barrier=True)
        with contextlib.ExitStack() as st:
            RT = self.sb(st, "RT", [128, 128], F32)
            pr.dma("sync", lambda e: e.dma_start(out=RT[:, :], in_=self.k_RT[:, :]), w=["RT"])
            xs = [self.sb(st, f"xs{i}", [128, 512], F32) for i in range(2)]
            tmp = [self.sb(st, f"tp{i}", [128, 512], F32) for i in range(2)]
            stg = [self.sb(st, f"stg{i}", [128, 512], BF16) for i in range(2)]
            cs = [self.sb(st, f"cs{i}", [128, 2, 512], F32) for i in range(2)]
            cnt = [0]

            def evac(mt, t0, n, pap, bank):
                grp, h = mt // 8, mt % 8
                row0 = {0: 0, 1: 1024, 3: 2048, 4: 3072}[grp] + h * 128
                scale = {0: sa, 1: 1.0, 3: sn, 4: 1.0}[grp]
                b = cnt[0] % 2
                cnt[0] += 1
                if grp >= 3 or t0 < CTX:
                    pr.op("act", lambda e: e.activation(out=stg[b][:, 0:n], in_=pap, func=AF.Copy, scale=scale), r=[("ps", bank)], w=[("stg", b)])
                else:
                    rb = 4 + b
                    pr.dma("sync", lambda e: e.dma_start(out=cs[b][:, 0, 0:n], in_=self.k_cos[:, t0 - CTX:t0 - CTX + n]), w=[("cs", b, 0)])
                    pr.dma("sync", lambda e: e.dma_start(out=cs[b][:, 1, 0:n], in_=self.k_sin[:, t0 - CTX:t0 - CTX + n]), w=[("cs", b, 1)])
                    pr.op("act", lambda e: e.activation(out=xs[b][:, 0:n], in_=pap, func=AF.Copy, scale=scale), r=[("ps", bank)], w=[("xs", b)])
                    pr.op("pe", lambda e: e.matmul(self.psb[rb][:, 0:n], lhsT=RT[:, :], rhs=xs[b][:, 0:n], start=True, stop=True),
                          r=[("xs", b), "RT"], w=[("ps", rb)])
                    pr.op("dve", lambda e: e.tensor_tensor(out=tmp[b][:, 0:n], in0=self.psb[rb][:, 0:n], in1=cs[b][:, 1, 0:n], op=ALU.mult),
                          r=[("ps", rb), ("cs", b, 1)], w=[("tp", b)])
                    pr.op("dve", lambda e: e.tensor_tensor(out=xs[b][:, 0:n], in0=xs[b][:, 0:n], in1=cs[b][:, 0, 0:n], op=ALU.mult),
                          r=[("xs", b), ("cs", b, 0)], w=[("xs", b)])
                    pr.op("dve", lambda e: e.tensor_tensor(out=stg[b][:, 0:n], in0=xs[b][:, 0:n], in1=tmp[b][:, 0:n], op=ALU.add),
                          r=[("xs", b), ("tp", b)], w=[("stg", b)])
                pr.dma("pool", lambda e: e.dma_start(out=self.QK[row0:row0 + 128, t0:t0 + n], in_=stg[b][:, 0:n]), r=[("stg", b)], w=[("QK", row0, t0)])

            def tm_evac(c0, t0, pap, bank):
                colo = c0 - 2048 if c0 < 3072 else c0 - 5120 + 1024
                b = cnt[0] % 2
                cnt[0] += 1
                pr.op("act", lambda e: e.activation(out=stg[b][:, :], in_=pap, func=AF.Copy), r=[("ps", bank)], w=[("stg", b)])
                pr.dma("pool", lambda e: e.dma_start(out=self.V[t0:t0 + 128, colo:colo + 512], in_=stg[b][:, :]), r=[("stg", b)], w=[("V", t0, colo)])

            self.linear(st, self.aT, self.wb_in[:, 0:6144], D, 6144, evac, tm_cols={2048, 2560, 5120, 5632}, tm_evac=tm_evac, tag="ip")
            pr.flush(barrier=True)
        with contextlib.ExitStack() as st:
            lrow = self.sb(st, "lrow", [1, 2, 2, 64], F32)
            pr.dma("sync", lambda e: e.dma_start(out=lrow[:, :, :, :], in_=self.att_lambda[j:j + 1, :].rearrange("o (a b c) -> o a b c", a=2, b=2)), w=["lrow"])
            lp = self.sb(st, "lp", [1, 2, 64], F32)
            pr.op("dve", lambda e: e.tensor_tensor(out=lp[:, :, :], in0=lrow[:, :, 0, :], in1=lrow[:, :, 1, :], op=ALU.mult), r=["lrow"], w=["lp"])
            l2 = self.sb(st, "l2", [1, 2], F32)
            pr.op("dve", lambda e: e.tensor_reduce(out=l2[:, :], in_=lp[:, :, :], axis=AX.X, op=ALU.add), r=["lp"], w=["l2"])
            pr.op("act", lambda e: e.activation(out=l2[:, :], in_=l2[:, :], func=AF.Exp), r=["l2"], w=["l2"])
            nl = self.sb(st, "nl", [1, 2], F32)
            pr.op("dve", lambda e: e.tensor_tensor(out=nl[:, 0:1], in0=l2[:, 1:2], in1=l2[:, 0:1], op=ALU.subtract), r=["l2"], w=["nl"])
            pr.op("dve", lambda e: e.tensor_scalar(out=nl[:, 0:1], in0=nl[:, 0:1], scalar1=-lam_init, scalar2=None, op0=ALU.add), r=["nl"], w=["nl"])
            nlam = self.sb(st, "nlam", [128, 1], F32)
            pr.op("pe", lambda e: e.matmul(self.psb[0][:, 0:1], lhsT=self.ones[0:1, :], rhs=nl[0:1, 0:1], start=True, stop=True),
                  r=["nl", "ones"], w=[("ps", 0)])
            pr.op("dve", lambda e: e.tensor_copy(out=nlam[:, :], in_=self.psb[0][:, 0:1]), r=[("ps", 0)], w=["nlam"])
            scl = self.sb(st, "scl", [128, 1], F32)
            self.transpose_rows(st, self.att_subln[j:j + 1, :], 1, scl[:, 0:1], "scl", bank=1)
            pr.op("dve", lambda e: e.tensor_scalar(out=scl[:, :], in0=scl[:, :], scalar1=1.0 - lam_init, scalar2=None, op0=ALU.mult), r=["scl"], w=["scl"])
            qT = self.sb(st, "qT", [128, T], BF16)
            kT = self.sb(st, "kT", [128, T], BF16)
            Vh = self.sb(st, "Vh", [128, 66, 128], BF16)
            P = [[self.sb(st, f"P{m}{i}", [128, 512], BF16) for i in range(2)] for m in range(2)]
            rD = [self.sb(st, f"rD{i}", [128, 512], F32) for i in range(2)]
            tA = self.sb(st, "tA", [128, 512], F32)
            tB = self.sb(st, "tB", [128, 512], F32)
            ost = [self.sb(st, f"ost{i}", [128, 512], BF16) for i in range(2)]
            oi = 0
            for h in range(8):
                pr.dma("sync", lambda e, h=h: e.dma_start(out=qT[:, :], in_=self.QK[h * 128:(h + 1) * 128, :]), w=["qT"])
                pr.dma("sync", lambda e, h=h: e.dma_start(out=kT[:, :], in_=self.QK[1024 + h * 128:1024 + (h + 1) * 128, :]), w=["kT"])
                pr.dma("sync", lambda e, h=h: e.dma_start(out=Vh[:, :, :], in_=self.V[:, h * 128:(h + 1) * 128].rearrange("(j p) c -> p j c", p=128)), w=["Vh"])
                qtiles = [(0, 256, 2)] + [(CTX + 512 * i, 512, 66) for i in range(16)]
                for (q0, qn_, nk) in qtiles:
                    for kt in range(nk):
                        for m in range(2):
                            sbk = m * 2 + (kt % 2)
                            pb = self.psb[sbk]
                            pr.op("pe", lambda e, m=m, kt=kt, q0=q0, qn_=qn_, pb=pb: e.matmul(
                                pb[:, 0:qn_], lhsT=kT[64 * m:64 * m + 64, kt * 128:(kt + 1) * 128], rhs=qT[64 * m:64 * m + 64, q0:q0 + qn_],
                                start=True, stop=True), r=["kT", "qT"], w=[("ps", sbk)])
                            Pm = P[m][kt % 2]
                            pr.op("act", lambda e, qn_=qn_, pb=pb, Pm=Pm: e.activation(out=Pm[:, 0:qn_], in_=pb[:, 0:qn_], func=AF.Exp),
                                  r=[("ps", sbk)], w=[("P", m, kt % 2)])
                            pr.op("pe", lambda e, m=m, kt=kt, qn_=qn_, Pm=Pm, nk=nk: e.matmul(
                                self.psb[4 + m][:, 0:qn_], lhsT=Vh[:, kt, :], rhs=Pm[:, 0:qn_], start=(kt == 0), stop=(kt == nk - 1)),
                                r=[("P", m, kt % 2), "Vh"], w=[("ps", 4 + m)])
                            pr.op("pe", lambda e, m=m, kt=kt, qn_=qn_, Pm=Pm, nk=nk: e.matmul(
                                self.psb[6 + m][:, 0:qn_], lhsT=self.onesb[:, :], rhs=Pm[:, 0:qn_], start=(kt == 0), stop=(kt == nk - 1)),
                                r=[("P", m, kt % 2), "onesb"], w=[("ps", 6 + m)])
                    n = qn_
                    pr.op("dve", lambda e, n=n: e.reciprocal(out=rD[0][:, 0:n], in_=self.psb[6][:, 0:n]), r=[("ps", 6)], w=[("rD", 0)])
                    pr.op("dve", lambda e, n=n: e.reciprocal(out=rD[1][:, 0:n], in_=self.psb[7][:, 0:n]), r=[("ps", 7)], w=[("rD", 1)])
                    pr.op("dve", lambda e, n=n: e.tensor_tensor(out=tA[:, 0:n], in0=self.psb[4][:, 0:n], in1=rD[0][:, 0:n], op=ALU.mult),
                          r=[("ps", 4), ("rD", 0)], w=["tA"])
                    pr.op("dve", lambda e, n=n: e.tensor_tensor(out=tB[:, 0:n], in0=self.psb[5][:, 0:n], in1=rD[1][:, 0:n], op=ALU.mult),
                          r=[("ps", 5), ("rD", 1)], w=["tB"])
                    pr.op("dve", lambda e, n=n: e.scalar_tensor_tensor(out=tA[:, 0:n], in0=tB[:, 0:n], scalar=nlam[:, 0:1], in1=tA[:, 0:n],
                                                                     op0=ALU.mult, op1=ALU.add), r=["tA", "tB", "nlam"], w=["tA"])
                    pr.op("act", lambda e, n=n: e.activation(out=tB[:, 0:n], in_=tA[:, 0:n], func=AF.Square), r=["tA"], w=["tB"])
                    pr.op("pe", lambda e, n=n: e.matmul(self.psb[0][:, 0:n], lhsT=self.ones[:, :], rhs=tB[:, 0:n], start=True, stop=True),
                          r=["tB", "ones"], w=[("ps", 0)])
                    pr.op("dve", lambda e, n=n: e.tensor_scalar(out=rD[0][:, 0:n], in0=self.psb[0][:, 0:n], scalar1=1.0 / 128, scalar2=EPS,
                                                              op0=ALU.mult, op1=ALU.add), r=[("ps", 0)], w=[("rD", 0)])
                    pr.op("act", lambda e, n=n: e.activation(out=rD[0][:, 0:n], in_=rD[0][:, 0:n], func=AF.Sqrt), r=[("rD", 0)], w=[("rD", 0)])
                    pr.op("dve", lambda e, n=n: e.reciprocal(out=rD[0][:, 0:n], in_=rD[0][:, 0:n]), r=[("rD", 0)], w=[("rD", 0)])
                    pr.op("dve", lambda e, n=n: e.tensor_tensor(out=tA[:, 0:n], in0=tA[:, 0:n], in1=rD[0][:, 0:n], op=ALU.mult),
                          r=["tA", ("rD", 0)], w=["tA"])
                    ob = oi % 2
                    oi += 1
                    pr.op("dve", lambda e, n=n, ob=ob: e.tensor_scalar(out=ost[ob][:, 0:n], in0=tA[:, 0:n], scalar1=scl[:, 0:1], scalar2=None, op0=ALU.mult),
                          r=["tA", "scl"], w=[("ost", ob)])
                    pr.dma("pool", lambda e, n=n, ob=ob, h=h, q0=q0: e.dma_start(out=self.catT[h * 128:(h + 1) * 128, q0:q0 + n], in_=ost[ob][:, 0:n]),
                           r=[("ost", ob)], w=[("catT", h, q0)])
                pr.flush()
            pr.flush(barrier=True)
        with contextlib.ExitStack() as st:
            qT = self.sb(st, "nqT", [128, T], BF16)
            kT = self.sb(st, "nkT", [128, T], BF16)
            Vh = self.sb(st, "nVh", [128, 66, 128], BF16)
            naT = self.sb(st, "naT", [128, T], BF16)
            Bt = self.sb(st, "Bt", [128, 5, 5, 128], F32)
            Ssb = [self.sb(st, f"Ssb{i}", [128, 640], F32) for i in range(2)]
            P = [self.sb(st, f"nP{i}", [128, 896], BF16) for i in range(2)]
            rD = [self.sb(st, f"nrD{i}", [128, 128], F32) for i in range(2)]
            for h in range(8):
                pr.dma("sync", lambda e, h=h: e.dma_start(out=qT[:, :], in_=self.QK[2048 + h * 128:2048 + (h + 1) * 128, :]), w=["nqT"])
                pr.dma("sync", lambda e, h=h: e.dma_start(out=kT[:, :], in_=self.QK[3072 + h * 128:3072 + (h + 1) * 128, :]), w=["nkT"])
                pr.dma("sync", lambda e, h=h: e.dma_start(out=Vh[:, :, :], in_=self.V[:, 1024 + h * 128:1024 + (h + 1) * 128].rearrange("(j p) c -> p j c", p=128)), w=["nVh"])
                pr.dma("sync", lambda e, h=h: e.dma_start(out=Bt[:, :, :, :], in_=self.na_bias[j, h].rearrange("p (a b q) -> p a b q", a=5, b=5)), w=["Bt"])
                for qi in range(66):
                    b = qi % 2
                    bA, bB = (0, 1) if b == 0 else (2, 3)
                    if qi < 2:
                        q0 = qi * 128
                        keys = [0, 128]
                        cls = None
                    else:
                        jq = qi - 2
                        q0 = CTX + jq * 128
                        kbase = int(np.clip(2 * jq - 4, 0, 118))
                        k0 = CTX + kbase * 64
                        keys = [0, 128] + [k0 + 128 * i for i in range(5)]
                        cls = _na_class(jq)
                    nkk = len(keys)
                    for i, ks in enumerate(keys):
                        dst = self.psb[bA][:, i * 128:(i + 1) * 128] if i < 4 else self.psb[bB][:, (i - 4) * 128:(i - 3) * 128]
                        pr.op("pe", lambda e, ks=ks, q0=q0, dst=dst: e.matmul(dst, lhsT=kT[:, ks:ks + 128], rhs=qT[:, q0:q0 + 128], start=True, stop=True),
                              r=["nkT", "nqT"], w=[("ps", bA if i < 4 else bB)])
                    Pb = P[b]
                    pr.op("act", lambda e, Pb=Pb, bA=bA: e.activation(out=Pb[:, 0:256], in_=self.psb[bA][:, 0:256], func=AF.Exp),
                          r=[("ps", bA)], w=[("nP", b, 0)])
                    if cls is not None:
                        pr.op("dve", lambda e, b=b, bA=bA, cls=cls: e.tensor_tensor(
                            out=Ssb[b][:, 0:256], in0=self.psb[bA][:, 256:512], in1=Bt[:, cls, 0:2, :].rearrange("p a q -> p (a q)"), op=ALU.add),
                            r=[("ps", bA), "Bt"], w=[("Ssb", b, 0)])
                        pr.op("dve", lambda e, b=b, bB=bB, cls=cls: e.tensor_tensor(
                            out=Ssb[b][:, 256:640], in0=self.psb[bB][:, 0:384], in1=Bt[:, cls, 2:5, :].rearrange("p a q -> p (a q)"), op=ALU.add),
                            r=[("ps", bB), "Bt"], w=[("Ssb", b, 1)])
                        pr.op("act", lambda e, Pb=Pb, b=b: e.activation(out=Pb[:, 256:896], in_=Ssb[b][:, 0:640], func=AF.Exp),
                              r=[("Ssb", b, 0), ("Ssb", b, 1)], w=[("nP", b, 1)])
                    for i, ks in enumerate(keys):
                        pr.op("pe", lambda e, i=i, ks=ks, Pb=Pb, b=b, nkk=nkk: e.matmul(
                            self.psb[4 + b][:, 0:128], lhsT=Vh[:, ks // 128, :], rhs=Pb[:, i * 128:(i + 1) * 128], start=(i == 0), stop=(i == nkk - 1)),
                            r=[("nP", b, 0), ("nP", b, 1), "nVh"], w=[("ps", 4 + b)])
                    for i, ks in enumerate(keys):
                        pr.op("pe", lambda e, i=i, Pb=Pb, b=b, nkk=nkk: e.matmul(
                            self.psb[6 + b][:, 0:128], lhsT=self.onesb[:, :], rhs=Pb[:, i * 128:(i + 1) * 128], start=(i == 0), stop=(i == nkk - 1)),
                            r=[("nP", b, 0), ("nP", b, 1), "onesb"], w=[("ps", 6 + b)])
                    pr.op("dve", lambda e, b=b: e.reciprocal(out=rD[b][:, :], in_=self.psb[6 + b][:, 0:128]), r=[("ps", 6 + b)], w=[("nrD", b)])
                    pr.op("dve", lambda e, b=b, q0=q0: e.tensor_tensor(out=naT[:, q0:q0 + 128], in0=self.psb[4 + b][:, 0:128], in1=rD[b][:, :], op=ALU.mult),
                          r=[("ps", 4 + b), ("nrD", b)], w=[("naT", q0)])
                pr.dma("pool", lambda e, h=h: e.dma_start(out=self.catT[1024 + h * 128:1024 + (h + 1) * 128, :], in_=naT[:, :]),
                       r=[("naT", qq) for qq in range(0, T, 128)], w=[("catTn", h)])
                pr.flush()
            pr.flush(barrier=True)

    def phase_rec(self, l):
        pr = self.pr
        j = l // 2
        if not hasattr(self, "GL"):
            self.GL = self.dint("GL", [2 * D, T], F32)
        self.cast_w(self.rec_w_in[j], self.wb_in, D, 5 * D, "wbi")
        pr.flush(barrier=True)
        SEG = 2112
        NCH = 33
        with contextlib.ExitStack() as st0:
            LBL = self.sb(st0, "LBL", [128, 128], F32)
            self.transpose_rows(st0, self.rec_lb[:, :], 128, LBL[:, :], "LBL", bank=0)
            pr.op("act", lambda e: e.activation(out=LBL[:, :], in_=LBL[:, :], func=AF.Exp), r=["LBL"], w=["LBL"])
            den = self.sb(st0, "lbden", [128, 32], F32)
            num = self.sb(st0, "lbnum", [128, 32], F32)
            pr.op("dve", lambda e: e.tensor_tensor(out=den[:, :], in0=LBL[:, 0:32], in1=LBL[:, 32:64], op=ALU.add), r=["LBL"], w=["lbden"])
            pr.op("dve", lambda e: e.tensor_tensor(out=den[:, :], in0=den[:, :], in1=LBL[:, 64:96], op=ALU.add), r=["LBL", "lbden"], w=["lbden"])
            pr.op("dve", lambda e: e.tensor_tensor(out=den[:, :], in0=den[:, :], in1=LBL[:, 96:128], op=ALU.add), r=["LBL", "lbden"], w=["lbden"])
            pr.op("dve", lambda e: e.tensor_copy(out=num[:, :], in_=LBL[:, 32:64]), r=["LBL"], w=["lbnum"])
            for lp in range(2, l + 1):
                pr.op("dve", lambda e, lp=lp: e.tensor_tensor(out=num[:, :], in0=num[:, :], in1=LBL[:, lp * 32:(lp + 1) * 32], op=ALU.add),
                      r=["LBL", "lbnum"], w=["lbnum"])
            LB = self.sb(st0, "LB", [128, 32], F32)
            OML = self.sb(st0, "OML", [128, 32], F32)
            pr.op("dve", lambda e: e.reciprocal(out=den[:, :], in_=den[:, :]), r=["lbden"], w=["lbden"])
            pr.op("dve", lambda e: e.tensor_tensor(out=LB[:, :], in0=num[:, :], in1=den[:, :], op=ALU.mult), r=["lbnum", "lbden"], w=["LB"])
            pr.op("dve", lambda e: e.tensor_scalar(out=OML[:, :], in0=LB[:, :], scalar1=-1.0, scalar2=1.0, op0=ALU.mult, op1=ALU.add), r=["LB"], w=["OML"])
            gnv = self.sb(st0, "gnv", [128, 1], F32)
            self.transpose_rows(st0, self.rec_gn[j:j + 1, :], 1, gnv[:, 0:1], "gnv", bank=1)
            with contextlib.ExitStack() as st:
                xs = [self.sb(st, f"rxs{i}", [128, 512], F32) for i in range(2)]
                tmp = [self.sb(st, f"rtp{i}", [128, 512], F32) for i in range(2)]
                stg = [self.sb(st, f"rstg{i}", [128, 512], BF16) for i in range(2)]
                cnt = [0]

                def evac(mt, t0, n, pap, bank):
                    grp, hh = mt // 16, mt % 16
                    b = cnt[0] % 2
                    cnt[0] += 1
                    if grp == 0:
                        pr.op("act", lambda e: e.activation(out=xs[b][:, 0:n], in_=pap, func=AF.Silu), r=[("ps", bank)], w=[("rxs", b)])
                        pr.op("dve", lambda e: e.tensor_scalar(out=stg[b][:, 0:n], in0=xs[b][:, 0:n], scalar1=128 ** -0.5, scalar2=None, op0=ALU.mult),
                              r=[("rxs", b)], w=[("rstg", b)])
                        row0 = hh * 128
                    elif grp == 4:
                        pr.op("act", lambda e: e.activation(out=stg[b][:, 0:n], in_=pap, func=AF.Silu), r=[("ps", bank)], w=[("rstg", b)])
                        row0 = 3 * D + hh * 128
                    else:
                        d = grp - 2
                        ci = d * 16 + hh
                        pr.op("act", lambda e: e.activation(out=xs[b][:, 0:n], in_=pap, func=AF.Sigmoid), r=[("ps", bank)], w=[("rxs", b)])
                        pr.op("dve", lambda e: e.tensor_scalar(out=xs[b][:, 0:n], in0=xs[b][:, 0:n], scalar1=OML[:, ci:ci + 1], scalar2=LB[:, ci:ci + 1],
                                                             op0=ALU.mult, op1=ALU.add), r=[("rxs", b), "OML", "LB"], w=[("rxs", b)])
                        pr.op("act", lambda e: e.activation(out=tmp[b][:, 0:n], in_=xs[b][:, 0:n], func=AF.Ln), r=[("rxs", b)], w=[("rtp", b)])
                        g0 = d * D + hh * 128
                        pr.dma("pool", lambda e: e.dma_start(out=self.GL[g0:g0 + 128, t0:t0 + n], in_=tmp[b][:, 0:n]), r=[("rtp", b)], w=[("GL", g0, t0)])
                        pr.op("dve", lambda e: e.tensor_scalar(out=stg[b][:, 0:n], in0=xs[b][:, 0:n], scalar1=-1.0, scalar2=1.0, op0=ALU.mult, op1=ALU.add),
                              r=[("rxs", b)], w=[("rstg", b)])
                        row0 = D + d * D + hh * 128
                    pr.dma("pool", lambda e: e.dma_start(out=self.QK[row0:row0 + 128, t0:t0 + n], in_=stg[b][:, 0:n]), r=[("rstg", b)], w=[("QK", row0, t0)])

                def tm_evac(c0, t0, pap, bank):
                    colo = c0 - D
                    b = cnt[0] % 2
                    cnt[0] += 1
                    pr.op("act", lambda e: e.activation(out=stg[b][:, :], in_=pap, func=AF.Copy), r=[("ps", bank)], w=[("rstg", b)])
                    pr.dma("pool", lambda e: e.dma_start(out=self.V[t0:t0 + 128, colo:colo + 512], in_=stg[b][:, :]), r=[("rstg", b)], w=[("V", t0, colo)])

                self.linear(st, self.aT, self.wb_in, D, 5 * D, evac, tm_cols={2048, 2560, 3072, 3584}, tm_evac=tm_evac, tag="rp")
                pr.flush(barrier=True)
            with contextlib.ExitStack() as st:
                mask = self.sb(st, "smask", [128, SEG], F32)
                pr.op("dve", lambda e: e.memset(mask[:, :], 1.0), w=["smask"])
                pr.op("dve", lambda e: e.memset(mask[:, :].rearrange("p (n c) -> p n c", c=64)[:, :, 0:1], 0.0), r=["smask"], w=["smask"])
                mk = [self.sb(st, "m128f", [128, 128], F32), self.sb(st, "m128b", [128, 128], F32)]
                pr.dma("sync", lambda e: e.dma_start(out=mk[0][:, :], in_=self.k_m128f[:, :]), w=["m128f"])
                pr.dma("sync", lambda e: e.dma_start(out=mk[1][:, :], in_=self.k_m128b[:, :]), w=["m128b"])
                gs = self.sb(st, "gs", [128, SEG], F32)
                Cs = self.sb(st, "Cs", [128, SEG], F32)
                As = self.sb(st, "As", [128, SEG], F32)
                Es = self.sb(st, "Es", [128, SEG], F32)
                qss = self.sb(st, "qss", [128, SEG], BF16)
                kks = self.sb(st, "kks", [128, SEG], BF16)
                qt_ = self.sb(st, "qt_", [128, T], BF16)
                kt_ = self.sb(st, "kt_", [128, T], BF16)
                kh_ = self.sb(st, "kh_", [128, T], BF16)
                qst = self.sb(st, "qst", [128, T], BF16)
                dsb = self.sb(st, "dsb", [128, 132], F32)
                Vh = self.sb(st, "rVh", [128, 66, 128], BF16)
                oT = self.sb(st, "oT", [128, T], F32)
                S = [self.sb(st, f"S{i}", [128, 128], F32) for i in range(2)]
                Sb = [self.sb(st, f"Sb{i}", [128, 128], BF16) for i in range(2)]
                attm = [self.sb(st, f"attm{i}", [128, 128], BF16) for i in range(2)]
                khT = [self.sb(st, f"khT{i}", [128, 128], BF16) for i in range(2)]
                rr = self.sb(st, "rrs", [128, 512], F32)
                gt = self.sb(st, "rgt", [128, 512], BF16)
                ost = [self.sb(st, f"rost{i}", [128, 512], BF16) for i in range(2)]
                v3 = lambda a: a[:, :].rearrange("p (n c) -> p n c", c=64)
                oi = 0
                for hh in range(16):
                    pr.dma("sync", lambda e, hh=hh: e.dma_start(out=Vh[:, :, :], in_=self.V[:, hh * 128:(hh + 1) * 128].rearrange("(j p) c -> p j c", p=128)), w=["rVh"])
                    for d in range(2):
                        mi, ei = (31, 63) if d == 0 else (32, 0)
                        for sg_ in range(4):
                            t0 = sg_ * SEG
                            g0 = d * D + hh * 128
                            k0 = D + d * D + hh * 128
                            pr.dma("sync", lambda e, g0=g0, t0=t0: e.dma_start(out=gs[:, :], in_=self.GL[g0:g0 + 128, t0:t0 + SEG]), w=["gs"])
                            pr.dma("sync", lambda e, hh=hh, t0=t0: e.dma_start(out=qss[:, :], in_=self.QK[hh * 128:(hh + 1) * 128, t0:t0 + SEG]), w=["qss"])
                            pr.dma("sync", lambda e, k0=k0, t0=t0: e.dma_start(out=kks[:, :], in_=self.QK[k0:k0 + 128, t0:t0 + SEG]), w=["kks"])
                            pr.op("dve", lambda e: e.tensor_tensor_scan(out=Cs[:, :], data0=mask[:, :], data1=gs[:, :], initial=0.0, op0=ALU.mult, op1=ALU.add),
                                  r=["gs", "smask"], w=["Cs"])
                            if d == 1:
                                pr.op("dve", lambda e: e.tensor_tensor(out=v3(As), in0=v3(Cs)[:, :, 63:64].to_broadcast([128, NCH, 64]), in1=v3(Cs), op=ALU.subtract),
                                      r=["Cs"], w=["As"])
                                pr.op("dve", lambda e: e.tensor_tensor(out=Cs[:, :], in0=As[:, :], in1=gs[:, :], op=ALU.add), r=["As", "gs"], w=["Cs"])
                            pr.op("dve", lambda e, mi=mi: e.tensor_tensor(out=v3(As), in0=v3(Cs), in1=v3(Cs)[:, :, mi:mi + 1].to_broadcast([128, NCH, 64]), op=ALU.subtract),
                                  r=["Cs"], w=["As"])
                            pr.op("act", lambda e: e.activation(out=Es[:, :], in_=As[:, :], func=AF.Exp), r=["As"], w=["Es"])
                            pr.op("dve", lambda e, t0=t0: e.tensor_tensor(out=qt_[:, t0:t0 + SEG], in0=qss[:, :], in1=Es[:, :], op=ALU.mult), r=["qss", "Es"], w=[("qt_", sg_)])
                            pr.op("act", lambda e: e.activation(out=Es[:, :], in_=As[:, :], func=AF.Exp, scale=-1.0), r=["As"], w=["Es"])
                            pr.op("dve", lambda e, t0=t0: e.tensor_tensor(out=kt_[:, t0:t0 + SEG], in0=kks[:, :], in1=Es[:, :], op=ALU.mult), r=["kks", "Es"], w=[("kt_", sg_)])
                            pr.op("dve", lambda e, ei=ei: e.tensor_tensor(out=v3(As), in0=v3(Cs)[:, :, ei:ei + 1].to_broadcast([128, NCH, 64]), in1=v3(Cs), op=ALU.subtract),
                                  r=["Cs"], w=["As"])
                            pr.op("act", lambda e: e.activation(out=Es[:, :], in_=As[:, :], func=AF.Exp), r=["As"], w=["Es"])
                            pr.op("dve", lambda e, t0=t0: e.tensor_tensor(out=kh_[:, t0:t0 + SEG], in0=kks[:, :], in1=Es[:, :], op=ALU.mult), r=["kks", "Es"], w=[("kh_", sg_)])
                            pr.op("act", lambda e: e.activation(out=Es[:, :], in_=Cs[:, :], func=AF.Exp), r=["Cs"], w=["Es"])
                            pr.op("dve", lambda e, t0=t0: e.tensor_tensor(out=qst[:, t0:t0 + SEG], in0=qss[:, :], in1=Es[:, :], op=ALU.mult), r=["qss", "Es"], w=[("qst", sg_)])
                            pr.op("dve", lambda e, sg_=sg_, ei=ei: e.tensor_copy(out=dsb[:, sg_ * NCH:(sg_ + 1) * NCH], in_=v3(Es)[:, :, ei]), r=["Es"], w=[("dsb", sg_)])
                        allk = [(nm, q) for nm in ("qt_", "kt_", "kh_", "qst", "dsb") for q in range(4)]
                        pr.op("dve", lambda e: e.memset(S[0][:, :], 0.0), w=[("S", 0)])
                        pr.op("dve", lambda e: e.memset(Sb[0][:, :], 0.0), w=[("Sb", 0)])
                        cur = 0
                        if d == 0:
                            tiles = [(tt, (0, 1)) for tt in range(66)]
                        else:
                            tiles = [(1, (1, 0)), (0, (1, 0))] + [(tt, (1, 0)) for tt in range(65, 1, -1)]
                        for ti, (tt, corder) in enumerate(tiles):
                            ab = ti % 2
                            tl = tt * 128
                            pr.op("pe", lambda e, ab=ab, tl=tl: e.matmul(self.psb[ab][:, 0:128], lhsT=kt_[:, tl:tl + 128], rhs=qt_[:, tl:tl + 128], start=True, stop=True),
                                  r=allk, w=[("ps", ab)])
                            pr.op("dve", lambda e, ab=ab, d=d: e.tensor_tensor(out=attm[ab][:, :], in0=self.psb[ab][:, 0:128], in1=mk[d][:, :], op=ALU.mult),
                                  r=[("ps", ab), "m128f", "m128b"], w=[("attm", ab)])
                            pr.op("pe", lambda e, ab=ab, tl=tl: e.matmul(self.psb[2 + ab][:, 0:128], lhsT=kh_[:, tl:tl + 128], rhs=self.identb[:, :], start=True, stop=True),
                                  r=allk + ["identb"], w=[("ps", 2 + ab)])
                            pr.op("act", lambda e, ab=ab: e.activation(out=khT[ab][:, :], in_=self.psb[2 + ab][:, 0:128], func=AF.Copy), r=[("ps", 2 + ab)], w=[("khT", ab)])
                            po = self.psb[6 + ab]
                            for c in corder:
                                n_ = tt * 2 + c
                                cl = slice(64 * c, 64 * c + 64)
                                pdb = 4 + (n_ % 2)
                                pr.op("pe", lambda e, ab=ab, tt=tt, cl=cl, po=po: e.matmul(po[:, cl], lhsT=Vh[cl, tt, :], rhs=attm[ab][cl, cl], start=True, stop=False),
                                      r=["rVh", ("attm", ab)], w=[("ps", 6 + ab)])
                                pr.op("pe", lambda e, cur=cur, tl=tl, c=c, cl=cl, po=po: e.matmul(po[:, cl], lhsT=Sb[cur][:, :], rhs=qst[:, tl + 64 * c:tl + 64 * c + 64], start=False, stop=True),
                                      r=[("Sb", cur)] + allk, w=[("ps", 6 + ab)])
                                pr.op("pe", lambda e, ab=ab, tt=tt, cl=cl, pdb=pdb: e.matmul(self.psb[pdb][:, 0:128], lhsT=khT[ab][cl, :], rhs=Vh[cl, tt, :], start=True, stop=True),
                                      r=[("khT", ab), "rVh"], w=[("ps", pdb)])
                                nx = 1 - cur
                                pr.op("dve", lambda e, cur=cur, nx=nx, n_=n_, pdb=pdb: e.scalar_tensor_tensor(
                                    out=S[nx][:, :], in0=S[cur][:, :], scalar=dsb[:, n_:n_ + 1], in1=self.psb[pdb][:, 0:128], op0=ALU.mult, op1=ALU.add),
                                    r=[("S", cur), ("ps", pdb)] + [("dsb", q) for q in range(4)], w=[("S", nx)])
                                pr.op("act", lambda e, nx=nx: e.activation(out=Sb[nx][:, :], in_=S[nx][:, :], func=AF.Copy), r=[("S", nx)], w=[("Sb", nx)])
                                cur = nx
                            if d == 0:
                                pr.op("act", lambda e, tl=tl, po=po: e.activation(out=oT[:, tl:tl + 128], in_=po[:, 0:128], func=AF.Copy), r=[("ps", 6 + ab)], w=[("oT", tt)])
                            else:
                                pr.op("dve", lambda e, tl=tl, po=po: e.tensor_tensor(out=oT[:, tl:tl + 128], in0=oT[:, tl:tl + 128], in1=po[:, 0:128], op=ALU.add),
                                      r=[("ps", 6 + ab), ("oT", tt)], w=[("oT", tt)])
                        pr.flush()
                    for (t0, n) in self.tok_tiles(512):
                        okeys = [("oT", tt) for tt in range(t0 // 128, (t0 + n) // 128)]
                        g0 = 3 * D + hh * 128
                        pr.dma("sync", lambda e, g0=g0, t0=t0, n=n: e.dma_start(out=gt[:, 0:n], in_=self.QK[g0:g0 + 128, t0:t0 + n]), w=["rgt"])
                        pr.op("act", lambda e, t0=t0, n=n: e.activation(out=rr[:, 0:n], in_=oT[:, t0:t0 + n], func=AF.Square), r=okeys, w=["rrs"])
                        pr.op("pe", lambda e, n=n: e.matmul(self.psb[0][:, 0:n], lhsT=self.ones[:, :], rhs=rr[:, 0:n], start=True, stop=True), r=["rrs", "ones"], w=[("ps", 0)])
                        pr.op("dve", lambda e, n=n: e.tensor_scalar(out=rr[:, 0:n], in0=self.psb[0][:, 0:n], scalar1=1.0 / 128, scalar2=EPS, op0=ALU.mult, op1=ALU.add),
                              r=[("ps", 0)], w=["rrs"])
                        pr.op("act", lambda e, n=n: e.activation(out=rr[:, 0:n], in_=rr[:, 0:n], func=AF.Sqrt), r=["rrs"], w=["rrs"])
                        pr.op("dve", lambda e, n=n: e.reciprocal(out=rr[:, 0:n], in_=rr[:, 0:n]), r=["rrs"], w=["rrs"])
                        pr.op("dve", lambda e, t0=t0, n=n: e.tensor_tensor(out=rr[:, 0:n], in0=oT[:, t0:t0 + n], in1=rr[:, 0:n], op=ALU.mult), r=okeys + ["rrs"], w=["rrs"])
                        ob = oi % 2
                        oi += 1
                        pr.op("dve", lambda e, n=n, ob=ob: e.scalar_tensor_tensor(out=ost[ob][:, 0:n], in0=rr[:, 0:n], scalar=gnv[:, 0:1], in1=gt[:, 0:n],
                                                                                 op0=ALU.mult, op1=ALU.mult), r=["rrs", "rgt", "gnv"], w=[("rost", ob)])
                        pr.dma("pool", lambda e, hh=hh, t0=t0, n=n, ob=ob: e.dma_start(out=self.catT[hh * 128:(hh + 1) * 128, t0:t0 + n], in_=ost[ob][:, 0:n]),
                               r=[("rost", ob)], w=[("catT", hh, t0)])
                    pr.flush()
                pr.flush(barrier=True)


IN_SHAPES = {
    "hT0": [D, T], "c2": [32, 128], "w_mod": [DEPTH, D, 6 * D], "b_mod": [384, 128], "g12": [128, 128], "gfin": [16, 128],
    "att_w_in": [2, D, 6144], "att_w_out": [2, D, D], "att_lambda": [2, 256], "att_subln": [2, 128],
    "na_bias": [2, 8, 128, 5 * 5 * 128], "rec_w_in": [2, D, 5 * D], "rec_w_out": [2, D, D], "rec_lb": [DEPTH * 2 * 16, 128],
    "rec_gn": [2, 128], "moe_wr": [DEPTH, D, 36], "moe_br": [DEPTH, 36], "moe_wg": [DEPTH, NEXP, D, DEXP],
    "moe_wu": [DEPTH, NEXP, D, DEXP], "moe_wd": [DEPTH, NEXP, DEXP, D], "k_ident": [128, 128], "k_cos": [128, NLAT],
    "k_sin": [128, NLAT], "k_RT": [128, 128], "k_m128f": [128, 128], "k_m128b": [128, 128],
    "k_iota": [32, 164], "k_uincl": [32, 32], "k_pidx": [128, 1],
}


def _mkprop(name):
    def get(self):
        if name not in self._ins:
            self._ins[name] = self.din(name, IN_SHAPES[name])
        return self._ins[name]
    return property(get)


for _n in IN_SHAPES:
    setattr(Builder, _n, _mkprop(_n))


def prep_inputs(inp):
    f = lambda a: np.ascontiguousarray(np.asarray(a, dtype=np.float32))
    k = _consts()
    out = {}
    out["hT0"] = f(np.concatenate([inp["ctx"][0], inp["x"][0]], axis=0).T)
    out["c2"] = f(np.concatenate([inp["c"].reshape(16, 128), inp["c_ctx"].reshape(16, 128)], axis=0))
    out["w_mod"] = f(inp["w_mod"])
    out["b_mod"] = f(inp["b_mod"].reshape(384, 128))
    out["g12"] = f(np.concatenate([inp["norm1_g"].reshape(64, 128), inp["norm2_g"].reshape(64, 128)], axis=0))
    out["gfin"] = f(inp["final_norm_g"].reshape(16, 128))
    out["att_w_in"] = f(inp["att_w_in"])
    out["att_w_out"] = f(inp["att_w_out"])
    out["att_lambda"] = f(inp["att_lambda"].reshape(2, 256))
    out["att_subln"] = f(inp["att_subln_g"])
    out["na_bias"] = f(np.stack([_na_bias(np.asarray(inp["att_rpb"][j])) for j in range(2)]).reshape(2, 8, 128, 5 * 5 * 128))
    out["rec_w_in"] = f(inp["rec_w_in"])
    out["rec_w_out"] = f(inp["rec_w_out"])
    out["rec_lb"] = f(inp["rec_lb_logits"].reshape(DEPTH * 2 * 16, 128))
    out["rec_gn"] = f(inp["rec_gnorm_g"])
    out["moe_wr"] = f(np.concatenate([inp["moe_w_group"], inp["moe_w_router"]], axis=2))
    out["moe_br"] = f(np.concatenate([inp["moe_b_group"], inp["moe_b_router"]], axis=1))
    out["moe_wg"] = f(inp["moe_w_gate"])
    out["moe_wu"] = f(inp["moe_w_up"])
    out["moe_wd"] = f(inp["moe_w_down"])
    out["k_ident"] = k["ident"]
    out["k_cos"] = k["cosT"]
    out["k_sin"] = k["sinT"]
    out["k_RT"] = k["RT"]
    out["k_m128f"] = k["m128f"]
    out["k_m128b"] = k["m128b"]
    out["k_iota"] = np.ascontiguousarray(np.broadcast_to(128.0 * np.arange(164, dtype=np.float32), (32, 164)))
    out["k_uincl"] = np.triu(np.ones((32, 32), np.float32))
    out["k_pidx"] = np.arange(128, dtype=np.float32).reshape(128, 1)
    return out


def kernel(**inputs):
    b = Builder()
    nc = b.build()
    allin = prep_inputs(inputs)
    in_map = {n: allin[n] for n in b._ins}
    res = run_bass_kernel_spmd(nc, [in_map], core_ids=[0])
    outT = res.results[0]["outT"]
    return np.ascontiguousarray(outT.T)[None].astype(np.float32)
```

```python
import contextlib
import math
import numpy as np
import ml_dtypes
import concourse.bass as bass
import concourse.mybir as mybir
from concourse.bass_utils import run_bass_kernel_spmd

F32 = mybir.dt.float32
BF16 = mybir.dt.bfloat16
AF = mybir.ActivationFunctionType
ALU = mybir.AluOpType
AX = mybir.AxisListType

D = 2048
KT = 16
CTX = 256
NLAT = 8192
T = CTX + NLAT
DEPTH = 4
EPS = 1e-6
NEG = -30000.0
NEXP = 32
DEXP = 768


class Tok:
    __slots__ = ("eng", "isdma", "sig", "need")

    def __init__(self, eng, isdma):
        self.eng = eng
        self.isdma = isdma
        self.sig = None
        self.need = False


class Prog:
    NSLOT = 8

    def __init__(self, nc, es):
        self.nc = nc
        self.es = es
        self.E = {"pe": nc.tensor, "act": nc.scalar, "dve": nc.vector, "pool": nc.gpsimd, "sync": nc.sync}
        self.csem = {e: es.enter_context(nc.semaphore("cs_" + e)) for e in ("pe", "act", "dve", "pool")}
        self.ccnt = {e: 0 for e in self.csem}
        self.nslot = {"sync": 8, "pool": 3}
        self.dsem = {q: [es.enter_context(nc.semaphore(f"ds_{q}{i}")) for i in range(self.nslot[q])] for q in ("sync", "pool")}
        self.dcnt = {q: 0 for q in self.dsem}
        self.waited = {e: {} for e in self.E}
        self.last_w = {}
        self.readers = {}
        self.ops = []
        self.semobj = {}
        self.pending_barrier = []
        self.n_inst = 0

    def op(self, eng, fn, r=(), w=()):
        self.ops.append((eng, fn, tuple(r), tuple(w), False))

    def dma(self, q, fn, r=(), w=()):
        self.ops.append((q, fn, tuple(r), tuple(w), True))

    def _wait(self, eng, sem, val):
        k = id(sem)
        if self.waited[eng].get(k, 0) < val:
            self.E[eng].wait_ge(sem, val)
            self.waited[eng][k] = val

    def flush(self, barrier=False):
        ops = self.ops
        self.ops = []
        n = len(ops)
        toks = [Tok(o[0], o[4]) for o in ops]
        deps = [None] * n
        last_idx = {}
        for i, (eng, fn, r, w, isdma) in enumerate(ops):
            d = set()
            for k in r:
                t = self.last_w.get(k)
                if t is not None:
                    d.add(t)
            for k in w:
                t = self.last_w.get(k)
                if t is not None:
                    d.add(t)
                for t2 in self.readers.get(k, ()):
                    d.add(t2)
            me = toks[i]
            for k in w:
                self.last_w[k] = me
                self.readers[k] = []
            for k in r:
                self.readers.setdefault(k, []).append(me)
            dd = []
            for t in d:
                if t is me:
                    continue
                if t.eng == eng and eng == "pe" and not t.isdma and not isdma:
                    continue
                dd.append(t)
                t.need = True
            deps[i] = dd
            if not isdma:
                last_idx[eng] = i
        for e, i in last_idx.items():
            toks[i].need = True
        unsig = {e: [] for e in self.csem}
        first_on = set()
        for i, (eng, fn, r, w, isdma) in enumerate(ops):
            me = toks[i]
            if self.pending_barrier and eng not in first_on:
                first_on.add(eng)
                for (sem, val) in self.pending_barrier:
                    self._wait(eng, sem, val)
            if isdma:
                j = self.dcnt[eng]
                ns = self.nslot[eng]
                slot = j % ns
                sem = self.dsem[eng][slot]
                if j >= ns:
                    self._wait(eng, sem, 16 * (j // ns))
                self.dcnt[eng] = j + 1
                me.sig = (sem, 16 * (j // ns + 1))
            need = {}
            for t in deps[i]:
                sem, val = t.sig
                k = id(sem)
                if k not in need or need[k][1] < val:
                    need[k] = (sem, val)
            for sem, val in need.values():
                self._wait(eng, sem, val)
            inst = fn(self.E[eng])
            self.n_inst += 1
            if isdma:
                inst.then_inc(me.sig[0], 16)
            elif me.need:
                self.ccnt[eng] += 1
                inst.then_inc(self.csem[eng], 1)
                me.sig = (self.csem[eng], self.ccnt[eng])
                for t in unsig[eng]:
                    t.sig = me.sig
                unsig[eng] = []
            else:
                unsig[eng].append(me)
        for e in unsig:
            assert not unsig[e]
        if barrier:
            self.pending_barrier = self.all_sigs()

    def all_sigs(self):
        sigs = [(self.csem[e], self.ccnt[e]) for e in self.csem if self.ccnt[e] > 0]
        for q in self.dsem:
            j = self.dcnt[q]
            ns = self.nslot[q]
            for s in range(ns):
                if j > s:
                    cnt = (j - 1 - s) // ns + 1
                    sigs.append((self.dsem[q][s], 16 * cnt))
        return sigs

    def finish(self):
        self.flush()
        for sem, val in self.all_sigs():
            self._wait("sync", sem, val)


def _consts():
    ident = np.eye(128, dtype=np.float32)
    ones = np.ones((128, 128), np.float32)
    tpos = np.arange(NLAT)
    rows = (tpos // 64).astype(np.float32)
    cols = (tpos % 64).astype(np.float32)
    inv = (10000.0 ** (-np.arange(16, dtype=np.float32) / 16)).astype(np.float32)
    cosT = np.zeros((128, NLAT), np.float32)
    sinT = np.zeros((128, NLAT), np.float32)
    for d in range(128):
        dd = d % 64
        axis = dd // 32
        fr = dd % 16
        ang = (rows if axis == 0 else cols) * inv[fr]
        cosT[d] = np.cos(ang)
        sinT[d] = np.sin(ang)
    RT = np.zeros((128, 128), np.float32)
    for m in range(128):
        if (m % 32) < 16:
            RT[m + 16, m] = -1.0
        else:
            RT[m - 16, m] = 1.0
    s_ = np.arange(64)[:, None]
    t_ = np.arange(64)[None, :]
    mfwd = (s_ <= t_).astype(np.float32)
    mbwd = (s_ >= t_).astype(np.float32)
    m128f = np.zeros((128, 128), np.float32)
    m128b = np.zeros((128, 128), np.float32)
    for c in range(2):
        m128f[c * 64:(c + 1) * 64, c * 64:(c + 1) * 64] = mfwd
        m128b[c * 64:(c + 1) * 64, c * 64:(c + 1) * 64] = mbwd
    return dict(ident=ident, ones=ones, cosT=cosT, sinT=sinT, RT=RT, m128f=m128f, m128b=m128b)


NA_CLASS_J = [0, 1, 2, 62, 63]


def _na_class(j):
    if j <= 1:
        return j
    if j >= 62:
        return j - 59
    return 2


def _na_geom():
    idx = np.zeros((5, 640, 128, 2), np.int64)
    valid = np.zeros((5, 640, 128), bool)
    for c, j in enumerate(NA_CLASS_J):
        kbase = int(np.clip(2 * j - 4, 0, 118))
        ql = np.arange(128)
        r = 2 * j + ql // 64
        cq = ql % 64
        rs = np.clip(r - 4, 0, 120)
        cs = np.clip(cq - 8, 0, 48)
        kl = np.arange(640)
        kr = kbase + kl // 64
        kc = kl % 64
        v = (kr[:, None] >= rs[None, :]) & (kr[:, None] < rs[None, :] + 8) & (kc[:, None] >= cs[None, :]) & (kc[:, None] < cs[None, :] + 16)
        ro = kr[:, None] - r[None, :] + 7
        co = kc[:, None] - cq[None, :] + 15
        valid[c] = v
        idx[c, :, :, 0] = np.clip(ro, 0, 14)
        idx[c, :, :, 1] = np.clip(co, 0, 30)
    return idx, valid


def _na_bias(rpb):
    idx, valid = _na_geom()
    out = np.empty((8, 5, 640, 128), np.float32)
    for h in range(8):
        g = rpb[h][idx[..., 0], idx[..., 1]]
        out[h] = np.where(valid, g, np.float32(NEG))
    out = out.reshape(8, 5, 5, 128, 128).transpose(0, 3, 1, 2, 4)
    return np.ascontiguousarray(out)


class Builder:
    def __init__(self, n_layers=DEPTH, debug_out=None):
        self.n_layers = n_layers
        self.debug_out = debug_out
        self.nc = bass.Bass("TRN2", target_bir_lowering=False)
        self.es = contextlib.ExitStack()
        self.pr = Prog(self.nc, self.es)
        self.uid = 0
        self._ins = {}

    def din(self, name, shape, dt=F32):
        return self.nc.dram_tensor(name, list(shape), dt, kind="ExternalInput").ap()

    def dint(self, name, shape, dt):
        return self.nc.dram_tensor(name, list(shape), dt, kind="Internal").ap()

    def sb(self, stack, name, shape, dt):
        self.uid += 1
        return stack.enter_context(self.nc.sbuf_tensor(f"{name}_{self.uid}", list(shape), dt))

    def ps(self, stack, name, shape, dt=F32):
        self.uid += 1
        return stack.enter_context(self.nc.psum_tensor(f"{name}_{self.uid}", list(shape), dt))

    def build(self):
        nc, pr = self.nc, self.pr
        L = self.n_layers
        self.outT = self.nc.dram_tensor("outT", [D, NLAT], F32, kind="ExternalOutput").ap()
        self.hA = self.dint("hA", [D, T], F32)
        self.hB = self.dint("hB", [D, T], F32)
        self.aT = self.dint("aT", [D, T], BF16)
        self.QK = self.dint("QK", [5 * D, T], BF16)
        self.V = self.dint("V", [T, D], BF16)
        self.catT = self.dint("catT", [D, T], BF16)
        self.wb_in = self.dint("wb_in", [D, 5 * D], BF16)
        self.wb_out = self.dint("wb_out", [D, D], BF16)
        self.alloc_moe()

        es = self.es
        self.ident = self.sb(es, "ident", [128, 128], F32)
        self.ones = self.sb(es, "ones", [128, 128], F32)
        self.onesb = self.sb(es, "onesb", [128, 128], BF16)
        self.identb = self.sb(es, "identb", [128, 128], BF16)
        self.MOD = self.sb(es, "MOD", [128, DEPTH * 6 * 16 * 2], F32)
        self.AB = self.sb(es, "AB", [128, DEPTH * 2 * 2 * 16 * 2], F32)
        self.GF = self.sb(es, "GF", [128, 16], F32)
        self.G12 = self.sb(es, "G12", [128, 128], F32)
        pr.dma("sync", lambda e: e.dma_start(out=self.ident[:], in_=self.k_ident[:, :]), w=["ident"])
        pr.op("dve", lambda e: e.memset(self.ones[:], 1.0), w=["ones"])
        pr.op("dve", lambda e: e.memset(self.onesb[:], 1.0), w=["onesb"])
        pr.op("dve", lambda e: e.tensor_copy(out=self.identb[:], in_=self.ident[:]), r=["ident"], w=["identb"])
        self.psb = [self.ps(es, f"bank{i}", [128, 512], F32) for i in range(8)]

        self.phase_mod()
        h_in = self.hT0
        for l in range(L):
            h_mid = self.hA
            h_out = self.hB
            self.phase_norm(l, 1, h_in, router=False)
            if l % 2 == 0:
                self.phase_att(l)
            else:
                self.phase_rec(l)
            self.phase_outproj(l, h_in, h_mid)
            self.phase_norm(l, 2, h_mid, router=True)
            self.phase_moe(l, h_mid, h_out)
            h_in = h_out
        self.phase_final(h_in)
        pr.finish()
        return nc

    def alloc_moe(self):
        self.wb_g2 = self.dint("wb_g2", [NEXP * 128, 16 * DEXP], BF16)
        self.wb_u2 = self.dint("wb_u2", [NEXP * 128, 16 * DEXP], BF16)
        self.wb_d2 = self.dint("wb_d2", [NEXP * 128, 6 * D], BF16)
        self.XS = self.dint("XS", [164 * 128, D], BF16)
        self.YS = self.dint("YS", [164 * 128, D], F32)
        es = self.es
        self.Lsb = self.sb(es, "Lsb", [128, 66, 36], F32)
        self.W1 = self.sb(es, "W1", [128, 66], F32)
        self.W2 = self.sb(es, "W2", [128, 66], F32)
        self.D1 = self.sb(es, "D1", [128, 66], mybir.dt.uint32)
        self.D2 = self.sb(es, "D2", [128, 66], mybir.dt.uint32)
        self.WIDX = self.sb(es, "WIDX", [128, 164], mybir.dt.uint32)

    def modv(self, l, j, f, col):
        o = ((l * 6 + j) * 16 + f) * 2 + col
        return self.MOD[:, o:o + 1]

    def abv(self, l, which, ab, f, col):
        o = ((((l * 2 + (which - 1)) * 2 + ab) * 16) + f) * 2 + col
        return self.AB[:, o:o + 1]

    def cast_w(self, src2d, dst2d, rows, cols, tag):
        pr = self.pr
        for r0 in range(0, rows, 1024):
            r1 = min(rows, r0 + 1024)
            for c0 in range(0, cols, 2048):
                c1 = min(cols, c0 + 2048)
                pr.dma("pool", lambda e, r0=r0, r1=r1, c0=c0, c1=c1: e.dma_start(out=dst2d[r0:r1, c0:c1], in_=src2d[r0:r1, c0:c1]),
                       r=[], w=[(tag, r0, c0)])

    def transpose_rows(self, stack, src_rows_ap, R, dst_ap, tag, bank=0):
        pr = self.pr
        tmp = self.sb(stack, "trtmp", [128, 128], F32)
        kt = ("trtmp", self.uid)
        pr.dma("sync", lambda e: e.dma_start(out=tmp[0:R, :], in_=src_rows_ap), w=[kt])
        pb = self.psb[bank]
        pr.op("pe", lambda e: e.matmul(pb[:, 0:R], lhsT=tmp[0:R, :], rhs=self.ident[0:R, 0:R], start=True, stop=True),
              r=[kt, "ident"], w=[("ps", bank)])
        pr.op("dve", lambda e: e.tensor_copy(out=dst_ap, in_=pb[:, 0:R]), r=[("ps", bank)], w=[tag])

    def phase_mod(self):
        pr = self.pr
        with contextlib.ExitStack() as st:
            cc = self.sb(st, "cc", [128, 32], F32)
            BM = self.sb(st, "BM", [128, 384], F32)
            self.transpose_rows(st, self.c2[:, :], 32, cc[:, :], "cc", bank=0)
            pr.op("act", lambda e: e.activation(out=cc[:, :], in_=cc[:, :], func=AF.Silu), r=["cc"], w=["cc"])
            for a in range(3):
                self.transpose_rows(st, self.b_mod[a * 128:(a + 1) * 128, :], 128, BM[:, a * 128:(a + 1) * 128], ("BM", a), bank=1 + a)
            self.transpose_rows(st, self.g12[:, :], 128, self.G12[:, :], "G12", bank=4)
            self.transpose_rows(st, self.gfin[:, :], 16, self.GF[:, :], "GF", bank=5)
            cc2 = self.sb(st, "cc2", [128, 16, 2], F32)
            pr.op("dve", lambda e: e.tensor_copy(out=cc2[:, :, 0], in_=cc[:, 0:16]), r=["cc"], w=["cc2a"])
            pr.op("dve", lambda e: e.tensor_copy(out=cc2[:, :, 1], in_=cc[:, 16:32]), r=["cc"], w=["cc2b"])
            wm = [self.sb(st, f"wm{i}", [128, 16, 1024], F32) for i in range(2)]
            it = 0
            for l in range(self.n_layers):
                for j in range(6):
                    for half in range(2):
                        b = it % 2
                        bank = 6 + (it % 2)
                        c0 = j * D + half * 1024
                        src = self.w_mod[l, :, c0:c0 + 1024].rearrange("(k p) c -> p k c", p=128)
                        pr.dma("sync", lambda e, b=b, src=src: e.dma_start(out=wm[b][:, :, :], in_=src), w=[("wm", b)])
                        pb = self.psb[bank]
                        for f8 in range(8):
                            for k in range(16):
                                pr.op("pe", lambda e, b=b, f8=f8, k=k, pb=pb: e.matmul(
                                    pb[:, f8 * 2:f8 * 2 + 2], lhsT=wm[b][:, k, f8 * 128:(f8 + 1) * 128], rhs=cc2[:, k, :],
                                    start=(k == 0), stop=(k == 15)), r=[("wm", b), "cc2a", "cc2b"], w=[("ps", bank)])
                        o = ((l * 6 + j) * 16 + half * 8) * 2
                        bo = l * 96 + j * 16 + half * 8
                        for col in range(2):
                            pr.op("dve", lambda e, o=o, bo=bo, col=col, pb=pb: e.tensor_tensor(
                                out=self.MOD[:, o + col:o + 16:2], in0=pb[:, col:16:2], in1=BM[:, bo:bo + 8], op=ALU.add),
                                r=[("ps", bank), ("BM", 0), ("BM", 1), ("BM", 2)], w=["MOD"])
                        it += 1
            for l in range(self.n_layers):
                for which in (1, 2):
                    jsh, jsc = (0, 1) if which == 1 else (3, 4)
                    g = self.G12[:, (which - 1) * 64 + l * 16:(which - 1) * 64 + l * 16 + 16]
                    for col in range(2):
                        osc = ((l * 6 + jsc) * 16) * 2 + col
                        osh = ((l * 6 + jsh) * 16) * 2 + col
                        oa = ((((l * 2 + (which - 1)) * 2 + 0) * 16)) * 2 + col
                        ob = ((((l * 2 + (which - 1)) * 2 + 1) * 16)) * 2 + col
                        pr.op("dve", lambda e, osc=osc, oa=oa, g=g: e.scalar_tensor_tensor(
                            out=self.AB[:, oa:oa + 31:2], in0=self.MOD[:, osc:osc + 31:2], scalar=1.0, in1=g, op0=ALU.add, op1=ALU.mult),
                            r=["MOD", "G12"], w=["AB"])
                        pr.op("dve", lambda e, osh=osh, ob=ob: e.tensor_copy(out=self.AB[:, ob:ob + 31:2], in_=self.MOD[:, osh:osh + 31:2]),
                              r=["MOD"], w=["AB"])
            pr.flush(barrier=True)

    def tok_tiles(self, n):
        out = []
        t0 = 0
        while t0 < T:
            lim = CTX if t0 < CTX else T
            m = min(n, lim - t0)
            out.append((t0, m))
            t0 += m
        return out

    def phase_norm(self, l, which, h_src, router, final=False):
        pr = self.pr
        NT = 256
        with contextlib.ExitStack() as st:
            hb = [self.sb(st, f"nh{i}", [128, 16, NT], F32) for i in range(2)]
            sq = [self.sb(st, f"nsq{i}", [128, 16, NT], F32) for i in range(2)]
            ab = [self.sb(st, f"nab{i}", [128, 16, NT], BF16) for i in range(2)]
            rs = [self.sb(st, f"nrs{i}", [128, NT], F32) for i in range(2)]
            if router:
                WR = self.sb(st, "WR", [128, 16, 36], F32)
                Lsb = self.Lsb
                pr.dma("sync", lambda e: e.dma_start(out=WR[:, :, :], in_=self.moe_wr[l].rearrange("(k p) c -> p k c", p=128)), w=["WR"])
            for i, (t0, n) in enumerate(self.tok_tiles(NT)):
                b = i % 2
                col = 1 if t0 < CTX else 0
                bank = i % 2
                pr.dma("sync", lambda e, b=b, t0=t0, n=n: e.dma_start(
                    out=hb[b][:, :, 0:n], in_=h_src[:, t0:t0 + n].rearrange("(k p) t -> p k t", p=128)), w=[("nh", b)])
                pr.op("act", lambda e, b=b, n=n: e.activation(out=sq[b][:, :, 0:n], in_=hb[b][:, :, 0:n], func=AF.Square),
                      r=[("nh", b)], w=[("nsq", b)])
                pb = self.psb[bank]
                for k in range(16):
                    pr.op("pe", lambda e, b=b, k=k, n=n, pb=pb: e.matmul(pb[:, 0:n], lhsT=self.ones[:, :], rhs=sq[b][:, k, 0:n],
                                                                       start=(k == 0), stop=(k == 15)),
                          r=[("nsq", b), "ones"], w=[("ps", bank)])
                pr.op("dve", lambda e, b=b, n=n, pb=pb: e.tensor_scalar(out=rs[b][:, 0:n], in0=pb[:, 0:n], scalar1=1.0 / D, scalar2=EPS,
                                                                      op0=ALU.mult, op1=ALU.add), r=[("ps", bank)], w=[("nrs", b)])
                pr.op("act", lambda e, b=b, n=n: e.activation(out=rs[b][:, 0:n], in_=rs[b][:, 0:n], func=AF.Sqrt), r=[("nrs", b)], w=[("nrs", b)])
                pr.op("dve", lambda e, b=b, n=n: e.reciprocal(out=rs[b][:, 0:n], in_=rs[b][:, 0:n]), r=[("nrs", b)], w=[("nrs", b)])
                for k in range(16):
                    pr.op("dve", lambda e, b=b, k=k, n=n: e.tensor_tensor(out=sq[b][:, k, 0:n], in0=hb[b][:, k, 0:n], in1=rs[b][:, 0:n],
                                                                        op=ALU.mult), r=[("nh", b), ("nrs", b)], w=[("nsq", b)])
                for k in range(16):
                    if final:
                        sc1, sc2, o1 = self.GF[:, k:k + 1], None, ALU.bypass
                        pr.op("dve", lambda e, b=b, k=k, n=n, sc1=sc1: e.tensor_scalar(
                            out=hb[b][:, k, 0:n], in0=sq[b][:, k, 0:n], scalar1=sc1, scalar2=None, op0=ALU.mult),
                            r=[("nsq", b), "GF"], w=[("nh", b)])
                    else:
                        A = self.abv(l, which, 0, k, col)
                        Bv = self.abv(l, which, 1, k, col)
                        dst = sq[b] if router else ab[b]
                        pr.op("dve", lambda e, b=b, k=k, n=n, A=A, Bv=Bv, dst=dst: e.tensor_scalar(
                            out=dst[:, k, 0:n], in0=sq[b][:, k, 0:n], scalar1=A, scalar2=Bv, op0=ALU.mult, op1=ALU.add),
                            r=[("nsq", b), "AB"], w=[("nsq", b) if router else ("nab", b)])
                if final:
                    if t0 >= CTX:
                        pr.dma("pool", lambda e, b=b, t0=t0, n=n: e.dma_start(
                            out=self.outT[:, t0 - CTX:t0 - CTX + n].rearrange("(k p) t -> p k t", p=128), in_=hb[b][:, :, 0:n]),
                            r=[("nh", b)], w=[("outT", t0)])
                    continue
                if router:
                    pr.op("act", lambda e, b=b, n=n: e.activation(out=ab[b][:, :, 0:n], in_=sq[b][:, :, 0:n], func=AF.Copy),
                          r=[("nsq", b)], w=[("nab", b)])
                    for s in range(n // 128):
                        rb = 2 + (s % 2)
                        pbl = self.psb[rb]
                        for k in range(16):
                            pr.op("pe", lambda e, b=b, k=k, s=s, pbl=pbl: e.matmul(
                                pbl[:, 0:36], lhsT=sq[b][:, k, s * 128:(s + 1) * 128], rhs=WR[:, k, :], start=(k == 0), stop=(k == 15)),
                                r=[("nsq", b), "WR"], w=[("ps", rb)])
                        blk = t0 // 128 + s
                        pr.op("act", lambda e, blk=blk, pbl=pbl: e.activation(out=Lsb[:, blk, :], in_=pbl[:, 0:36], func=AF.Copy),
                              r=[("ps", rb)], w=[("Lsb", blk)])
                pr.dma("pool", lambda e, b=b, t0=t0, n=n: e.dma_start(
                    out=self.aT[:, t0:t0 + n].rearrange("(k p) t -> p k t", p=128), in_=ab[b][:, :, 0:n]),
                    r=[("nab", b)], w=[("aT", t0)])
            pr.flush(barrier=True)
        if router:
            with contextlib.ExitStack() as st2:
                self.route(st2, l, self.Lsb)
                pr.flush(barrier=True)

    def phase_final(self, h_src):
        self.phase_norm(0, 1, h_src, router=False, final=True)

    def route(self, st, l, Lsb):
        pr = self.pr
        NB = 66
        br = self.sb(st, "br", [128, 36], F32)
        pr.dma("sync", lambda e: e.dma_start(out=br[:, :], in_=self.moe_br[l:l + 1, :].partition_broadcast(128)), w=["br"])
        allL = [("Lsb", b) for b in range(NB)]
        Lb = self.sb(st, "Lb", [128, NB, 36], F32)
        pr.op("dve", lambda e: e.tensor_tensor(out=Lb[:, :, :], in0=Lsb[:, :, :], in1=br[:, :].unsqueeze(1).to_broadcast([128, NB, 36]),
                                               op=ALU.add), r=allL + ["br"], w=["Lb"])
        gmax = self.sb(st, "gmax", [128, NB], F32)
        pr.op("dve", lambda e: e.tensor_reduce(out=gmax[:, :], in_=Lb[:, :, 0:4], axis=AX.X, op=ALU.max), r=["Lb"], w=["gmax"])
        gd = self.sb(st, "gd", [128, NB, 4], F32)
        pr.op("dve", lambda e: e.tensor_tensor(out=gd[:, :, :], in0=Lb[:, :, 0:4], in1=gmax[:, :].unsqueeze(2).to_broadcast([128, NB, 4]),
                                               op=ALU.subtract), r=["Lb", "gmax"], w=["gd"])
        ohg = self.sb(st, "ohg", [128, NB, 4], F32)
        pr.op("dve", lambda e: e.tensor_scalar(out=ohg[:, :, :], in0=gd[:, :, :], scalar1=0.0, scalar2=None, op0=ALU.is_ge), r=["gd"], w=["ohg"])
        ge = self.sb(st, "ge", [128, NB, 4], F32)
        pr.op("act", lambda e: e.activation(out=ge[:, :, :], in_=gd[:, :, :], func=AF.Exp), r=["gd"], w=["ge"])
        gs = self.sb(st, "gs", [128, NB], F32)
        pr.op("dve", lambda e: e.tensor_reduce(out=gs[:, :], in_=ge[:, :, :], axis=AX.X, op=ALU.add), r=["ge"], w=["gs"])
        gw = self.sb(st, "gw", [128, NB], F32)
        pr.op("dve", lambda e: e.reciprocal(out=gw[:, :], in_=gs[:, :]), r=["gs"], w=["gw"])
        pen = self.sb(st, "pen", [128, NB, 4], F32)
        pr.op("dve", lambda e: e.tensor_scalar(out=pen[:, :, :], in0=ohg[:, :, :], scalar1=-1.0, scalar2=1.0e4, op0=ALU.add, op1=ALU.mult),
              r=["ohg"], w=["pen"])
        el = self.sb(st, "el", [128, NB, 32], F32)
        for g in range(4):
            pr.op("dve", lambda e, g=g: e.tensor_tensor(out=el[:, :, g * 8:(g + 1) * 8], in0=Lb[:, :, 4 + g * 8:4 + (g + 1) * 8],
                                                        in1=pen[:, :, g:g + 1].to_broadcast([128, NB, 8]), op=ALU.add),
                  r=["Lb", "pen"], w=["el"])
        v1 = self.sb(st, "v1", [128, NB], F32)
        pr.op("dve", lambda e: e.tensor_reduce(out=v1[:, :], in_=el[:, :, :], axis=AX.X, op=ALU.max), r=["el"], w=["v1"])
        oh1 = self.sb(st, "oh1", [128, NB, 32], F32)
        pr.op("dve", lambda e: e.tensor_tensor(out=oh1[:, :, :], in0=el[:, :, :], in1=v1[:, :].unsqueeze(2).to_broadcast([128, NB, 32]),
                                               op=ALU.is_ge), r=["el", "v1"], w=["oh1"])
        el2 = self.sb(st, "el2", [128, NB, 32], F32)
        pr.op("dve", lambda e: e.scalar_tensor_tensor(out=el2[:, :, :], in0=oh1[:, :, :], scalar=-1.0e4, in1=el[:, :, :],
                                                      op0=ALU.mult, op1=ALU.add), r=["oh1", "el"], w=["el2"])
        v2 = self.sb(st, "v2", [128, NB], F32)
        pr.op("dve", lambda e: e.tensor_reduce(out=v2[:, :], in_=el2[:, :, :], axis=AX.X, op=ALU.max), r=["el2"], w=["v2"])
        oh2 = self.sb(st, "oh2", [128, NB, 32], F32)
        pr.op("dve", lambda e: e.tensor_tensor(out=oh2[:, :, :], in0=el2[:, :, :], in1=v2[:, :].unsqueeze(2).to_broadcast([128, NB, 32]),
                                               op=ALU.is_ge), r=["el2", "v2"], w=["oh2"])
        rr = self.sb(st, "rr", [128, NB], F32)
        pr.op("dve", lambda e: e.tensor_tensor(out=rr[:, :], in0=v2[:, :], in1=v1[:, :], op=ALU.subtract), r=["v1", "v2"], w=["rr"])
        pr.op("act", lambda e: e.activation(out=rr[:, :], in_=rr[:, :], func=AF.Exp), r=["rr"], w=["rr"])
        W1, W2 = self.W1, self.W2
        pr.op("dve", lambda e: e.tensor_scalar(out=W1[:, :], in0=rr[:, :], scalar1=1.0, scalar2=None, op0=ALU.add), r=["rr"], w=["W1"])
        pr.op("dve", lambda e: e.reciprocal(out=W1[:, :], in_=W1[:, :]), r=["W1"], w=["W1"])
        pr.op("dve", lambda e: e.tensor_tensor(out=W1[:, :], in0=W1[:, :], in1=gw[:, :], op=ALU.mult), r=["W1", "gw"], w=["W1"])
        pr.op("dve", lambda e: e.tensor_tensor(out=W2[:, :], in0=W1[:, :], in1=rr[:, :], op=ALU.mult), r=["W1", "rr"], w=["W2"])
        cnt = el
        pr.op("dve", lambda e: e.tensor_tensor(out=cnt[:, :, :], in0=oh1[:, :, :], in1=oh2[:, :, :], op=ALU.add), r=["oh1", "oh2", "el2"], w=["el"])
        cntT = self.sb(st, "cntT", [32, T], F32)
        PSi = self.sb(st, "PSi", [32, T], F32)
        one32 = self.sb(st, "one32", [32, T], F32)
        pr.op("dve", lambda e: e.memset(one32[:, :], 1.0), w=["one32"])
        for blk in range(NB):
            bank = 4 + (blk % 2)
            pb = self.psb[bank]
            pr.op("pe", lambda e, blk=blk, pb=pb: e.matmul(pb[0:32, 0:128], lhsT=cnt[:, blk, :], rhs=self.ident[:, :], start=True, stop=True),
                  r=["el", "ident"], w=[("ps", bank)])
            pr.op("act", lambda e, blk=blk, pb=pb: e.activation(out=cntT[:, blk * 128:(blk + 1) * 128], in_=pb[0:32, 0:128], func=AF.Copy),
                  r=[("ps", bank)], w=[("cntT", blk)])
        allc = [("cntT", b) for b in range(NB)]
        pr.op("dve", lambda e: e.tensor_tensor_scan(out=PSi[:, :], data0=one32[:, :], data1=cntT[:, :], initial=0.0, op0=ALU.mult, op1=ALU.add),
              r=allc + ["one32"], w=["PSi"])
        kio = self.sb(st, "kio", [32, 164], F32)
        pr.dma("sync", lambda e: e.dma_start(out=kio[:, :], in_=self.k_iota[:, :]), w=["kio"])
        UT = self.sb(st, "UT", [32, 32], F32)
        pr.dma("sync", lambda e: e.dma_start(out=UT[:, :], in_=self.k_uincl[:, :]), w=["UT"])
        pidx = self.sb(st, "pidx", [128, 1], F32)
        pr.dma("sync", lambda e: e.dma_start(out=pidx[:, :], in_=self.k_pidx[:, :]), w=["pidx"])
        cmpb = self.sb(st, "cmpb", [32, 164], F32)
        pr.op("dve", lambda e: e.tensor_scalar(out=cmpb[:, 0:132], in0=kio[:, 0:132], scalar1=PSi[:, T - 1:T], scalar2=None, op0=ALU.is_lt),
              r=["kio", "PSi"], w=["cmpb"])
        padded = self.sb(st, "padded", [32, 1], F32)
        pr.op("dve", lambda e: e.tensor_reduce(out=padded[:, :], in_=cmpb[:, 0:132], axis=AX.X, op=ALU.add), r=["cmpb"], w=["padded"])
        pr.op("dve", lambda e: e.tensor_scalar(out=padded[:, :], in0=padded[:, :], scalar1=128.0, scalar2=None, op0=ALU.mult), r=["padded"], w=["padded"])
        pend = self.sb(st, "pend", [32, 1], F32)
        pstart = self.sb(st, "pstart", [32, 1], F32)
        pr.op("pe", lambda e: e.matmul(self.psb[6][0:32, 0:1], lhsT=UT[:, :], rhs=padded[:, :], start=True, stop=True), r=["UT", "padded"], w=[("ps", 6)])
        pr.op("dve", lambda e: e.tensor_copy(out=pend[:, :], in_=self.psb[6][0:32, 0:1]), r=[("ps", 6)], w=["pend"])
        pr.op("dve", lambda e: e.tensor_tensor(out=pstart[:, :], in0=pend[:, :], in1=padded[:, :], op=ALU.subtract), r=["pend", "padded"], w=["pstart"])
        pr.op("dve", lambda e: e.tensor_scalar(out=cmpb[:, :], in0=kio[:, :], scalar1=pend[:, 0:1], scalar2=None, op0=ALU.is_ge),
              r=["kio", "pend", "padded"], w=["cmpb"])
        pr.op("pe", lambda e: e.matmul(self.psb[7][:, 0:164], lhsT=self.ones[0:32, :], rhs=cmpb[:, :], start=True, stop=True), r=["cmpb", "ones"], w=[("ps", 7)])
        EBf = self.sb(st, "EBf", [128, 164], F32)
        pr.op("dve", lambda e: e.tensor_scalar(out=EBf[:, :], in0=self.psb[7][:, 0:164], scalar1=31.0, scalar2=128.0, op0=ALU.min, op1=ALU.mult),
              r=[("ps", 7)], w=["EBf"])
        pr.op("dve", lambda e: e.tensor_scalar(out=EBf[:, :], in0=EBf[:, :], scalar1=pidx[:, 0:1], scalar2=None, op0=ALU.add), r=["EBf", "pidx"], w=["EBf"])
        pr.op("dve", lambda e: e.tensor_copy(out=self.WIDX[:, :], in_=EBf[:, :]), r=["EBf"], w=["WIDX"])
        pr.op("dve", lambda e: e.tensor_tensor(out=PSi[:, :], in0=PSi[:, :], in1=cntT[:, :], op=ALU.subtract), r=["PSi", "cmpb"] + allc, w=["PSi"])
        pr.op("dve", lambda e: e.tensor_scalar(out=PSi[:, :], in0=PSi[:, :], scalar1=pstart[:, 0:1], scalar2=None, op0=ALU.add), r=["PSi", "pstart"], w=["PSi"])
        DT = el2
        for blk in range(NB):
            bank = 4 + (blk % 2)
            pb = self.psb[bank]
            pr.op("pe", lambda e, blk=blk, pb=pb: e.matmul(pb[:, 0:32], lhsT=PSi[:, blk * 128:(blk + 1) * 128], rhs=self.ident[0:32, 0:32], start=True, stop=True),
                  r=["PSi", "ident"], w=[("ps", bank)])
            pr.op("act", lambda e, blk=blk, pb=pb: e.activation(out=DT[:, blk, :], in_=pb[:, 0:32], func=AF.Copy), r=[("ps", bank), "oh2"], w=[("DT", blk)])
        allD = [("DT", b) for b in range(NB)]
        d1 = self.sb(st, "d1f", [128, NB], F32)
        pr.op("dve", lambda e: e.tensor_tensor(out=oh1[:, :, :], in0=oh1[:, :, :], in1=DT[:, :, :], op=ALU.mult), r=allD + ["oh1", "el"], w=["oh1"])
        pr.op("dve", lambda e: e.tensor_reduce(out=d1[:, :], in_=oh1[:, :, :], axis=AX.X, op=ALU.add), r=["oh1"], w=["d1f"])
        pr.op("dve", lambda e: e.tensor_copy(out=self.D1[:, :], in_=d1[:, :]), r=["d1f"], w=["D1"])
        pr.op("dve", lambda e: e.tensor_tensor(out=oh2[:, :, :], in0=oh2[:, :, :], in1=DT[:, :, :], op=ALU.mult), r=allD + ["oh2", "el"], w=["oh2"])
        pr.op("dve", lambda e: e.tensor_reduce(out=d1[:, :], in_=oh2[:, :, :], axis=AX.X, op=ALU.add), r=["oh2", "D1"], w=["d1f"])
        pr.op("dve", lambda e: e.tensor_copy(out=self.D2[:, :], in_=d1[:, :]), r=["d1f"], w=["D2"])

    def linear(self, st, xT, Wb, K, M, evac, tm_cols=None, tm_evac=None, NB=1024, tag="lin"):
        pr = self.pr
        kt = K // 128
        xb = [self.sb(st, f"{tag}x{i}", [128, kt, NB], BF16) for i in range(2)]
        wp = [self.sb(st, f"{tag}w{i}", [128, kt, 512], BF16) for i in range(2)]
        blocks = self.tok_tiles(NB)
        wi = 0
        bi = 0
        for bidx, (t0, n) in enumerate(blocks):
            xbuf = bidx % 2
            pr.dma("sync", lambda e, xbuf=xbuf, t0=t0, n=n: e.dma_start(
                out=xb[xbuf][:, :, 0:n], in_=xT[:, t0:t0 + n].rearrange("(k p) t -> p k t", p=128)), r=[(tag + "src",)], w=[(tag + "x", xbuf)])
            for c0 in range(0, M, 512):
                cw = min(512, M - c0)
                wbuf = wi % 2
                wi += 1
                pr.dma("sync", lambda e, wbuf=wbuf, c0=c0, cw=cw: e.dma_start(
                    out=wp[wbuf][:, :, 0:cw], in_=Wb[:, c0:c0 + cw].rearrange("(k p) c -> p k c", p=128)), r=[(tag + "wsrc",)], w=[(tag + "w", wbuf)])
                if tm_cols is not None and c0 in tm_cols:
                    for s in range(n // 128):
                        bank = bi % 4
                        bi += 1
                        pb = self.psb[bank]
                        for k in range(kt):
                            pr.op("pe", lambda e, xbuf=xbuf, wbuf=wbuf, k=k, s=s, cw=cw, pb=pb: e.matmul(
                                pb[:, 0:cw], lhsT=xb[xbuf][:, k, s * 128:(s + 1) * 128], rhs=wp[wbuf][:, k, 0:cw],
                                start=(k == 0), stop=(k == kt - 1)), r=[(tag + "x", xbuf), (tag + "w", wbuf)], w=[("ps", bank)])
                        tm_evac(c0, t0 + s * 128, pb[:, 0:cw], bank)
                    continue
                for mi in range(cw // 128):
                    for s0 in range(0, n, 512):
                        sn = min(512, n - s0)
                        bank = bi % 4
                        bi += 1
                        pb = self.psb[bank]
                        for k in range(kt):
                            pr.op("pe", lambda e, xbuf=xbuf, wbuf=wbuf, k=k, mi=mi, s0=s0, sn=sn, pb=pb: e.matmul(
                                pb[:, 0:sn], lhsT=wp[wbuf][:, k, mi * 128:(mi + 1) * 128], rhs=xb[xbuf][:, k, s0:s0 + sn],
                                start=(k == 0), stop=(k == kt - 1)), r=[(tag + "x", xbuf), (tag + "w", wbuf)], w=[("ps", bank)])
                        evac(c0 // 128 + mi, t0 + s0, sn, pb[:, 0:sn], bank)

    def phase_outproj(self, l, h_in, h_out):
        pr = self.pr
        j = l // 2
        wsrc = self.att_w_out if l % 2 == 0 else self.rec_w_out
        self.cast_w(wsrc[j], self.wb_out, D, D, "wbo")
        pr.flush(barrier=True)
        with contextlib.ExitStack() as st:
            ho = [self.sb(st, f"ho{i}", [128, 512], F32) for i in range(4)]
            cnt = [0]

            def evac(mt, t0, n, pap, bank):
                b = cnt[0] % 4
                cnt[0] += 1
                col = 1 if t0 < CTX else 0
                pr.dma("sync", lambda e: e.dma_start(out=ho[b][:, 0:n], in_=h_in[mt * 128:(mt + 1) * 128, t0:t0 + n]), w=[("ho", b)])
                gate = self.modv(l, 2, mt, col)
                pr.op("dve", lambda e: e.scalar_tensor_tensor(out=ho[b][:, 0:n], in0=pap, scalar=gate, in1=ho[b][:, 0:n],
                                                              op0=ALU.mult, op1=ALU.add), r=[("ps", bank), ("ho", b), "MOD"], w=[("ho", b)])
                pr.dma("pool", lambda e: e.dma_start(out=h_out[mt * 128:(mt + 1) * 128, t0:t0 + n], in_=ho[b][:, 0:n]), r=[("ho", b)], w=[("hout", mt, t0)])

            self.linear(st, self.catT, self.wb_out, D, D, evac, tag="op")
            pr.flush(barrier=True)

    def phase_moe(self, l, h_in, h_out):
        pr = self.pr
        IOA = bass.IndirectOffsetOnAxis
        NBLK = 164
        for e_ in range(NEXP):
            pr.dma("pool", lambda e, e_=e_: e.dma_start(out=self.wb_g2[e_ * 128:(e_ + 1) * 128, :].rearrange("p (k c) -> p k c", k=16),
                                                       in_=self.moe_wg[l, e_].rearrange("(k p) c -> p k c", p=128)), w=[("wbg", e_)])
            pr.dma("pool", lambda e, e_=e_: e.dma_start(out=self.wb_u2[e_ * 128:(e_ + 1) * 128, :].rearrange("p (k c) -> p k c", k=16),
                                                       in_=self.moe_wu[l, e_].rearrange("(k p) c -> p k c", p=128)), w=[("wbu", e_)])
            pr.dma("pool", lambda e, e_=e_: e.dma_start(out=self.wb_d2[e_ * 128:(e_ + 1) * 128, :].rearrange("p (k c) -> p k c", k=6),
                                                       in_=self.moe_wd[l, e_].rearrange("(k p) c -> p k c", p=128)), w=[("wbd", e_)])
        pr.flush(barrier=True)
        with contextlib.ExitStack() as st:
            xa = [self.sb(st, f"dxa{i}", [128, 16, 128], BF16) for i in range(2)]
            xt = [self.sb(st, f"dxt{i}", [128, D], BF16) for i in range(2)]
            zt = self.sb(st, "dzt", [128, D], BF16)
            pr.op("dve", lambda e: e.memset(zt[:, :], 0.0), w=["dzt"])
            for blk in range(NBLK):
                pr.dma("sync", lambda e, blk=blk: e.dma_start(out=self.XS[blk * 128:(blk + 1) * 128, :], in_=zt[:, :]), r=["dzt"], w=[("XSz", blk)])
            pr.flush(barrier=True)
            for tt in range(66):
                b = tt % 2
                pr.dma("sync", lambda e, b=b, tt=tt: e.dma_start(out=xa[b][:, :, :], in_=self.aT[:, tt * 128:(tt + 1) * 128].rearrange("(k p) t -> p k t", p=128)),
                       w=[("dxa", b)])
                for q in range(4):
                    bank = (tt * 4 + q) % 4
                    pb = self.psb[bank]
                    for kk in range(4):
                        k = q * 4 + kk
                        pr.op("pe", lambda e, b=b, k=k, kk=kk, pb=pb: e.matmul(pb[:, kk * 128:(kk + 1) * 128], lhsT=xa[b][:, k, :], rhs=self.identb[:, :], start=True, stop=True),
                              r=[("dxa", b), "identb"], w=[("ps", bank)])
                    pr.op("act" if q % 2 == 0 else "dve",
                          (lambda e, b=b, q=q, pb=pb: e.activation(out=xt[b][:, q * 512:(q + 1) * 512], in_=pb[:, :], func=AF.Copy)) if q % 2 == 0 else
                          (lambda e, b=b, q=q, pb=pb: e.tensor_copy(out=xt[b][:, q * 512:(q + 1) * 512], in_=pb[:, :])),
                          r=[("ps", bank)], w=[("dxt", b, q)])
                xk = [("dxt", b, q) for q in range(4)]
                pr.dma("pool", lambda e, b=b, tt=tt: e.indirect_dma_start(out=self.XS[:, :], out_offset=IOA(ap=self.D1[:, tt:tt + 1], axis=0),
                                                                        in_=xt[b][:, :], in_offset=None), r=xk + ["D1"], w=[("XS", tt, 0)])
                pr.dma("pool", lambda e, b=b, tt=tt: e.indirect_dma_start(out=self.XS[:, :], out_offset=IOA(ap=self.D2[:, tt:tt + 1], axis=0),
                                                                        in_=xt[b][:, :], in_offset=None), r=xk + ["D2"], w=[("XS", tt, 1)])
            pr.flush(barrier=True)
        with contextlib.ExitStack() as st:
            wg = [self.sb(st, f"swg{i}", [128, 16 * DEXP], BF16) for i in range(2)]
            wu = [self.sb(st, f"swu{i}", [128, 16 * DEXP], BF16) for i in range(2)]
            wd = [self.sb(st, f"swd{i}", [128, 6 * D], BF16) for i in range(2)]
            xs = [self.sb(st, f"sxs{i}", [128, D], BF16) for i in range(2)]
            xT = [self.sb(st, f"sxT{i}", [128, 16, 128], BF16) for i in range(2)]
            sg = [self.sb(st, f"ssg{i}", [128, DEXP], F32) for i in range(2)]
            hT = [self.sb(st, f"shT{i}", [128, 6, 128], BF16) for i in range(2)]
            hk = [self.sb(st, f"shk{i}", [128, DEXP], BF16) for i in range(2)]
            ysb = [self.sb(st, "sys0", [128, D], F32)] * 2
            bi = 0
            for blk in range(NBLK):
                b = blk % 2
                pr.dma("pool", lambda e, b=b, blk=blk: e.indirect_dma_start(out=wg[b][:, :], out_offset=None, in_=self.wb_g2[:, :],
                                                                          in_offset=IOA(ap=self.WIDX[:, blk:blk + 1], axis=0)), r=["WIDX"], w=[("swg", b)])
                pr.dma("pool", lambda e, b=b, blk=blk: e.indirect_dma_start(out=wu[b][:, :], out_offset=None, in_=self.wb_u2[:, :],
                                                                          in_offset=IOA(ap=self.WIDX[:, blk:blk + 1], axis=0)), r=["WIDX"], w=[("swu", b)])
                pr.dma("pool", lambda e, b=b, blk=blk: e.indirect_dma_start(out=wd[b][:, :], out_offset=None, in_=self.wb_d2[:, :],
                                                                          in_offset=IOA(ap=self.WIDX[:, blk:blk + 1], axis=0)), r=["WIDX"], w=[("swd", b)])
                pr.dma("sync", lambda e, b=b, blk=blk: e.dma_start(out=xs[b][:, :], in_=self.XS[blk * 128:(blk + 1) * 128, :]), w=[("sxs", b)])
                for q in range(4):
                    bank = q
                    pb = self.psb[bank]
                    for kk in range(4):
                        k = q * 4 + kk
                        pr.op("pe", lambda e, b=b, k=k, kk=kk, pb=pb: e.matmul(pb[:, kk * 128:(kk + 1) * 128], lhsT=xs[b][:, k * 128:(k + 1) * 128], rhs=self.identb[:, :], start=True, stop=True),
                              r=[("sxs", b), "identb"], w=[("ps", bank)])
                    if q % 2 == 0:
                        pr.op("act", lambda e, b=b, q=q, pb=pb: e.activation(out=xT[b][:, q * 4:(q + 1) * 4, :], in_=pb[:, :].rearrange("p (a t) -> p a t", a=4), func=AF.Copy),
                              r=[("ps", bank)], w=[("sxT", b, q)])
                    else:
                        pr.op("dve", lambda e, b=b, q=q, pb=pb: e.tensor_copy(out=xT[b][:, q * 4:(q + 1) * 4, :], in_=pb[:, :].rearrange("p (a t) -> p a t", a=4)),
                              r=[("ps", bank)], w=[("sxT", b, q)])
                xk = [("sxT", b, q) for q in range(4)]
                for k in range(16):
                    for (wsb, wkey, b0) in ((wg, "swg", 4), (wu, "swu", 6)):
                        for half in range(2):
                            pbk = b0 + half
                            pr.op("pe", lambda e, wsb=wsb, b=b, k=k, half=half, pbk=pbk: e.matmul(
                                self.psb[pbk][:, 0:384], lhsT=xT[b][:, k, :], rhs=wsb[b][:, k * DEXP + half * 384:k * DEXP + (half + 1) * 384],
                                start=(k == 0), stop=(k == 15)), r=[(wkey, b)] + xk, w=[("ps", pbk)])
                for half in range(2):
                    pr.op("act", lambda e, b=b, half=half: e.activation(out=sg[b][:, half * 384:(half + 1) * 384], in_=self.psb[4 + half][:, 0:384], func=AF.Silu),
                          r=[("ps", 4 + half)], w=[("ssg", b, half)])
                    pr.op("dve", lambda e, b=b, half=half: e.tensor_tensor(out=hk[b][:, half * 384:(half + 1) * 384], in0=sg[b][:, half * 384:(half + 1) * 384],
                                                                         in1=self.psb[6 + half][:, 0:384], op=ALU.mult),
                          r=[("ssg", b, half), ("ps", 6 + half)], w=[("shk", b, half)])
                for jt in range(6):
                    pbk = 4 if jt < 4 else 5
                    dst = self.psb[pbk][:, (jt % 4) * 128:(jt % 4 + 1) * 128]
                    pr.op("pe", lambda e, b=b, jt=jt, dst=dst: e.matmul(dst, lhsT=hk[b][:, jt * 128:(jt + 1) * 128], rhs=self.identb[:, :], start=True, stop=True),
                          r=[("shk", b, 0), ("shk", b, 1), "identb"], w=[("ps", pbk)])
                pr.op("act", lambda e, b=b: e.activation(out=hT[b][:, 0:4, :], in_=self.psb[4][:, :].rearrange("p (a t) -> p a t", a=4), func=AF.Copy),
                      r=[("ps", 4)], w=[("shT", b, 0)])
                pr.op("dve", lambda e, b=b: e.tensor_copy(out=hT[b][:, 4:6, :], in_=self.psb[5][:, 0:256].rearrange("p (a t) -> p a t", a=2)),
                      r=[("ps", 5)], w=[("shT", b, 1)])
                for c4 in range(4):
                    bank = c4
                    pb = self.psb[bank]
                    for k in range(6):
                        pr.op("pe", lambda e, b=b, k=k, c4=c4, pb=pb: e.matmul(pb[:, :], lhsT=hT[b][:, k, :], rhs=wd[b][:, k * D + c4 * 512:k * D + (c4 + 1) * 512],
                                                                             start=(k == 0), stop=(k == 5)), r=[("shT", b, 0), ("shT", b, 1), ("swd", b)], w=[("ps", bank)])
                    if c4 % 2 == 0:
                        pr.op("act", lambda e, b=b, c4=c4, pb=pb: e.activation(out=ysb[b][:, c4 * 512:(c4 + 1) * 512], in_=pb[:, :], func=AF.Copy),
                              r=[("ps", bank)], w=[("sys", b, c4)])
                    else:
                        pr.op("dve", lambda e, b=b, c4=c4, pb=pb: e.tensor_copy(out=ysb[b][:, c4 * 512:(c4 + 1) * 512], in_=pb[:, :]),
                              r=[("ps", bank)], w=[("sys", b, c4)])
                pr.dma("sync", lambda e, b=b, blk=blk: e.dma_start(out=self.YS[blk * 128:(blk + 1) * 128, :], in_=ysb[b][:, :]),
                       r=[("sys", b, c) for c in range(4)], w=[("YS", blk)])
            pr.flush(barrier=True)
        with contextlib.ExitStack() as st:
            y1 = [self.sb(st, f"cy1{i}", [128, D], F32) for i in range(2)]
            y2 = [self.sb(st, f"cy2{i}", [128, D], F32) for i in range(2)]
            ho = [self.sb(st, f"cho{i}", [128, 16, 128], F32) for i in range(2)]
            for tt in range(66):
                b = tt % 2
                col = 1 if tt < 2 else 0
                pr.dma("pool", lambda e, b=b, tt=tt: e.indirect_dma_start(out=y1[b][:, :], out_offset=None, in_=self.YS[:, :],
                                                                        in_offset=IOA(ap=self.D1[:, tt:tt + 1], axis=0)), r=["D1"], w=[("cy1", b)])
                pr.dma("pool", lambda e, b=b, tt=tt: e.indirect_dma_start(out=y2[b][:, :], out_offset=None, in_=self.YS[:, :],
                                                                        in_offset=IOA(ap=self.D2[:, tt:tt + 1], axis=0)), r=["D2"], w=[("cy2", b)])
                pr.dma("sync", lambda e, b=b, tt=tt: e.dma_start(out=ho[b][:, :, :], in_=h_in[:, tt * 128:(tt + 1) * 128].rearrange("(k p) t -> p k t", p=128)),
                       w=[("cho", b)])
                pr.op("dve", lambda e, b=b, tt=tt: e.tensor_scalar(out=y1[b][:, :], in0=y1[b][:, :], scalar1=self.W1[:, tt:tt + 1], scalar2=None, op0=ALU.mult),
                      r=[("cy1", b), "W1"], w=[("cy1", b)])
                pr.op("dve", lambda e, b=b, tt=tt: e.scalar_tensor_tensor(out=y1[b][:, :], in0=y2[b][:, :], scalar=self.W2[:, tt:tt + 1], in1=y1[b][:, :],
                                                                         op0=ALU.mult, op1=ALU.add), r=[("cy1", b), ("cy2", b), "W2"], w=[("cy1", b)])
                for k in range(16):
                    bank = k % 8
                    pb = self.psb[bank]
                    pr.op("pe", lambda e, b=b, k=k, pb=pb: e.matmul(pb[:, 0:128], lhsT=y1[b][:, k * 128:(k + 1) * 128], rhs=self.ident[:, :], start=True, stop=True),
                          r=[("cy1", b), "ident"], w=[("ps", bank)])
                    gate = self.modv(l, 5, k, col)
                    pr.op("dve", lambda e, b=b, k=k, pb=pb, gate=gate: e.scalar_tensor_tensor(out=ho[b][:, k, :], in0=pb[:, 0:128], scalar=gate, in1=ho[b][:, k, :],
                                                                                            op0=ALU.mult, op1=ALU.add), r=[("ps", bank), ("cho", b), "MOD"], w=[("cho", b)])
                pr.dma("sync", lambda e, b=b, tt=tt: e.dma_start(out=h_out[:, tt * 128:(tt + 1) * 128].rearrange("(k p) t -> p k t", p=128), in_=ho[b][:, :, :]),
                       r=[("cho", b)], w=[("hout2", tt)])
            pr.flush(barrier=True)

    def phase_att(self, l):
        pr = self.pr
        j = l // 2
        sa = 64 ** -0.5
        sn = 128 ** -0.5
        lam_init = 0.8 - 0.6 * math.exp(-0.3 * l)
        self.cast_w(self.att_w_in[j], self.wb_in[:, 0:6144], D, 6144, "wbi")
        pr.flush(barrier=True)
        with contextlib.ExitStack() as st:
            RT = self.sb(st, "RT", [128, 128], F32)
            pr.dma("sync", lambda e: e.dma_start(out=RT[:, :], in_=self.k_RT[:, :]), w=["RT"])
            xs = [self.sb(st, f"xs{i}", [128, 512], F32) for i in range(2)]
            tmp = [self.sb(st, f"tp{i}", [128, 512], F32) for i in range(2)]
            stg = [self.sb(st, f"stg{i}", [128, 512], BF16) for i in range(2)]
            cs = [self.sb(st, f"cs{i}", [128, 2, 512], F32) for i in range(2)]
            cnt = [0]

            def evac(mt, t0, n, pap, bank):
                grp, h = mt // 8, mt % 8
                row0 = {0: 0, 1: 1024, 3: 2048, 4: 3072}[grp] + h * 128
                scale = {0: sa, 1: 1.0, 3: sn, 4: 1.0}[grp]
                b = cnt[0] % 2
                cnt[0] += 1
                if grp >= 3 or t0 < CTX:
                    pr.op("act", lambda e: e.activation(out=stg[b][:, 0:n], in_=pap, func=AF.Copy, scale=scale), r=[("ps", bank)], w=[("stg", b)])
                else:
                    rb = 4 + b
                    pr.dma("sync", lambda e: e.dma_start(out=cs[b][:, 0, 0:n], in_=self.k_cos[:, t0 - CTX:t0 - CTX + n]), w=[("cs", b, 0)])
                    pr.dma("sync", lambda e: e.dma_start(out=cs[b][:, 1, 0:n], in_=self.k_sin[:, t0 - CTX:t0 - CTX + n]), w=[("cs", b, 1)])
                    pr.op("act", lambda e: e.activation(out=xs[b][:, 0:n], in_=pap, func=AF.Copy, scale=scale), r=[("ps", bank)], w=[("xs", b)])
                    pr.op("pe", lambda e: e.matmul(self.psb[rb][:, 0:n], lhsT=RT[:, :], rhs=xs[b][:, 0:n], start=True, stop=True),
                          r=[("xs", b), "RT"], w=[("ps", rb)])
                    pr.op("dve", lambda e: e.tensor_tensor(out=tmp[b][:, 0:n], in0=self.psb[rb][:, 0:n], in1=cs[b][:, 1, 0:n], op=ALU.mult),
                          r=[("ps", rb), ("cs", b, 1)], w=[("tp", b)])
                    pr.op("dve", lambda e: e.tensor_tensor(out=xs[b][:, 0:n], in0=xs[b][:, 0:n], in1=cs[b][:, 0, 0:n], op=ALU.mult),
                          r=[("xs", b), ("cs", b, 0)], w=[("xs", b)])
                    pr.op("dve", lambda e: e.tensor_tensor(out=stg[b][:, 0:n], in0=xs[b][:, 0:n], in1=tmp[b][:, 0:n], op=ALU.add),
                          r=[("xs", b), ("tp", b)], w=[("stg", b)])
                pr.dma("pool", lambda e: e.dma_start(out=self.QK[row0:row0 + 128, t0:t0 + n], in_=stg[b][:, 0:n]), r=[("stg", b)], w=[("QK", row0, t0)])

            def tm_evac(c0, t0, pap, bank):
                colo = c0 - 2048 if c0 < 3072 else c0 - 5120 + 1024
                b = cnt[0] % 2
                cnt[0] += 1
                pr.op("act", lambda e: e.activation(out=stg[b][:, :], in_=pap, func=AF.Copy), r=[("ps", bank)], w=[("stg", b)])
                pr.dma("pool", lambda e: e.dma_start(out=self.V[t0:t0 + 128, colo:colo + 512], in_=stg[b][:, :]), r=[("stg", b)], w=[("V", t0, colo)])

            self.linear(st, self.aT, self.wb_in[:, 0:6144], D, 6144, evac, tm_cols={2048, 2560, 5120, 5632}, tm_evac=tm_evac, tag="ip")
            pr.flush(barrier=True)
        with contextlib.ExitStack() as st:
            lrow = self.sb(st, "lrow", [1, 2, 2, 64], F32)
            pr.dma("sync", lambda e: e.dma_start(out=lrow[:, :, :, :], in_=self.att_lambda[j:j + 1, :].rearrange("o (a b c) -> o a b c", a=2, b=2)), w=["lrow"])
            lp = self.sb(st, "lp", [1, 2, 64], F32)
            pr.op("dve", lambda e: e.tensor_tensor(out=lp[:, :, :], in0=lrow[:, :, 0, :], in1=lrow[:, :, 1, :], op=ALU.mult), r=["lrow"], w=["lp"])
            l2 = self.sb(st, "l2", [1, 2], F32)
            pr.op("dve", lambda e: e.tensor_reduce(out=l2[:, :], in_=lp[:, :, :], axis=AX.X, op=ALU.add), r=["lp"], w=["l2"])
            pr.op("act", lambda e: e.activation(out=l2[:, :], in_=l2[:, :], func=AF.Exp), r=["l2"], w=["l2"])
            nl = self.sb(st, "nl", [1, 2], F32)
            pr.op("dve", lambda e: e.tensor_tensor(out=nl[:, 0:1], in0=l2[:, 1:2], in1=l2[:, 0:1], op=ALU.subtract), r=["l2"], w=["nl"])
            pr.op("dve", lambda e: e.tensor_scalar(out=nl[:, 0:1], in0=nl[:, 0:1], scalar1=-lam_init, scalar2=None, op0=ALU.add), r=["nl"], w=["nl"])
            nlam = self.sb(st, "nlam", [128, 1], F32)
            pr.op("pe", lambda e: e.matmul(self.psb[0][:, 0:1], lhsT=self.ones[0:1, :], rhs=nl[0:1, 0:1], start=True, stop=True),
                  r=["nl", "ones"], w=[("ps", 0)])
            pr.op("dve", lambda e: e.tensor_copy(out=nlam[:, :], in_=self.psb[0][:, 0:1]), r=[("ps", 0)], w=["nlam"])
            scl = self.sb(st, "scl", [128, 1], F32)
            self.transpose_rows(st, self.att_subln[j:j + 1, :], 1, scl[:, 0:1], "scl", bank=1)
            pr.op("dve", lambda e: e.tensor_scalar(out=scl[:, :], in0=scl[:, :], scalar1=1.0 - lam_init, scalar2=None, op0=ALU.mult), r=["scl"], w=["scl"])
            qT = self.sb(st, "qT", [128, T], BF16)
            kT = self.sb(st, "kT", [128, T], BF16)
            Vh = self.sb(st, "Vh", [128, 66, 128], BF16)
            P = [[self.sb(st, f"P{m}{i}", [128, 512], BF16) for i in range(2)] for m in range(2)]
            rD = [self.sb(st, f"rD{i}", [128, 512], F32) for i in range(2)]
            tA = self.sb(st, "tA", [128, 512], F32)
            tB = self.sb(st, "tB", [128, 512], F32)
            ost = [self.sb(st, f"ost{i}", [128, 512], BF16) for i in range(2)]
            oi = 0
            for h in range(8):
                pr.dma("sync", lambda e, h=h: e.dma_start(out=qT[:, :], in_=self.QK[h * 128:(h + 1) * 128, :]), w=["qT"])
                pr.dma("sync", lambda e, h=h: e.dma_start(out=kT[:, :], in_=self.QK[1024 + h * 128:1024 + (h + 1) * 128, :]), w=["kT"])
                pr.dma("sync", lambda e, h=h: e.dma_start(out=Vh[:, :, :], in_=self.V[:, h * 128:(h + 1) * 128].rearrange("(j p) c -> p j c", p=128)), w=["Vh"])
                qtiles = [(0, 256, 2)] + [(CTX + 512 * i, 512, 66) for i in range(16)]
                for (q0, qn_, nk) in qtiles:
                    steps = [(kt, m) for kt in range(nk) for m in range(2)]

                    def emit_qk(si, q0=q0, qn_=qn_):
                        kt, m = steps[si]
                        sbk = m * 2 + (kt % 2)
                        pb = self.psb[sbk]
                        pr.op("pe", lambda e, m=m, kt=kt, pb=pb: e.matmul(
                            pb[:, 0:qn_], lhsT=kT[64 * m:64 * m + 64, kt * 128:(kt + 1) * 128], rhs=qT[64 * m:64 * m + 64, q0:q0 + qn_],
                            start=True, stop=True), r=["kT", "qT"], w=[("ps", sbk)])
                        Pm = P[m][kt % 2]
                        pr.op("act", lambda e, pb=pb, Pm=Pm: e.activation(out=Pm[:, 0:qn_], in_=pb[:, 0:qn_], func=AF.Exp),
                              r=[("ps", sbk)], w=[("P", m, kt % 2)])

                    def emit_av(si, qn_=qn_, nk=nk):
                        kt, m = steps[si]
                        Pm = P[m][kt % 2]
                        pr.op("pe", lambda e, m=m, kt=kt, Pm=Pm: e.matmul(
                            self.psb[4 + m][:, 0:qn_], lhsT=Vh[:, kt, :], rhs=Pm[:, 0:qn_], start=(kt == 0), stop=(kt == nk - 1)),
                            r=[("P", m, kt % 2), "Vh"], w=[("ps", 4 + m)])
                        pr.op("pe", lambda e, m=m, kt=kt, Pm=Pm: e.matmul(
                            self.psb[6 + m][:, 0:qn_], lhsT=self.onesb[:, :], rhs=Pm[:, 0:qn_], start=(kt == 0), stop=(kt == nk - 1)),
                            r=[("P", m, kt % 2), "onesb"], w=[("ps", 6 + m)])

                    LA = 2
                    for si in range(min(LA, len(steps))):
                        emit_qk(si)
                    for si in range(len(steps)):
                        if si + LA < len(steps):
                            emit_qk(si + LA)
                        emit_av(si)
                    n = qn_
                    pr.op("dve", lambda e, n=n: e.reciprocal(out=rD[0][:, 0:n], in_=self.psb[6][:, 0:n]), r=[("ps", 6)], w=[("rD", 0)])
                    pr.op("dve", lambda e, n=n: e.reciprocal(out=rD[1][:, 0:n], in_=self.psb[7][:, 0:n]), r=[("ps", 7)], w=[("rD", 1)])
                    pr.op("dve", lambda e, n=n: e.tensor_tensor(out=tA[:, 0:n], in0=self.psb[4][:, 0:n], in1=rD[0][:, 0:n], op=ALU.mult),
                          r=[("ps", 4), ("rD", 0)], w=["tA"])
                    pr.op("dve", lambda e, n=n: e.tensor_tensor(out=tB[:, 0:n], in0=self.psb[5][:, 0:n], in1=rD[1][:, 0:n], op=ALU.mult),
                          r=[("ps", 5), ("rD", 1)], w=["tB"])
                    pr.op("dve", lambda e, n=n: e.scalar_tensor_tensor(out=tA[:, 0:n], in0=tB[:, 0:n], scalar=nlam[:, 0:1], in1=tA[:, 0:n],
                                                                     op0=ALU.mult, op1=ALU.add), r=["tA", "tB", "nlam"], w=["tA"])
                    pr.op("act", lambda e, n=n: e.activation(out=tB[:, 0:n], in_=tA[:, 0:n], func=AF.Square), r=["tA"], w=["tB"])
                    pr.op("pe", lambda e, n=n: e.matmul(self.psb[0][:, 0:n], lhsT=self.ones[:, :], rhs=tB[:, 0:n], start=True, stop=True),
                          r=["tB", "ones"], w=[("ps", 0)])
                    pr.op("dve", lambda e, n=n: e.tensor_scalar(out=rD[0][:, 0:n], in0=self.psb[0][:, 0:n], scalar1=1.0 / 128, scalar2=EPS,
                                                              op0=ALU.mult, op1=ALU.add), r=[("ps", 0)], w=[("rD", 0)])
                    pr.op("act", lambda e, n=n: e.activation(out=rD[0][:, 0:n], in_=rD[0][:, 0:n], func=AF.Sqrt), r=[("rD", 0)], w=[("rD", 0)])
                    pr.op("dve", lambda e, n=n: e.reciprocal(out=rD[0][:, 0:n], in_=rD[0][:, 0:n]), r=[("rD", 0)], w=[("rD", 0)])
                    pr.op("dve", lambda e, n=n: e.tensor_tensor(out=tA[:, 0:n], in0=tA[:, 0:n], in1=rD[0][:, 0:n], op=ALU.mult),
                          r=["tA", ("rD", 0)], w=["tA"])
                    ob = oi % 2
                    oi += 1
                    pr.op("dve", lambda e, n=n, ob=ob: e.tensor_scalar(out=ost[ob][:, 0:n], in0=tA[:, 0:n], scalar1=scl[:, 0:1], scalar2=None, op0=ALU.mult),
                          r=["tA", "scl"], w=[("ost", ob)])
                    pr.dma("pool", lambda e, n=n, ob=ob, h=h, q0=q0: e.dma_start(out=self.catT[h * 128:(h + 1) * 128, q0:q0 + n], in_=ost[ob][:, 0:n]),
                           r=[("ost", ob)], w=[("catT", h, q0)])
                pr.flush()
            pr.flush(barrier=True)
        with contextlib.ExitStack() as st:
            qT = self.sb(st, "nqT", [128, T], BF16)
            kT = self.sb(st, "nkT", [128, T], BF16)
            Vh = self.sb(st, "nVh", [128, 66, 128], BF16)
            naT = self.sb(st, "naT", [128, T], BF16)
            Bt = self.sb(st, "Bt", [128, 5, 5, 128], F32)
            Ssb = [self.sb(st, f"Ssb{i}", [128, 640], F32) for i in range(2)]
            P = [self.sb(st, f"nP{i}", [128, 896], BF16) for i in range(2)]
            rD = [self.sb(st, f"nrD{i}", [128, 128], F32) for i in range(2)]
            for h in range(8):
                pr.dma("sync", lambda e, h=h: e.dma_start(out=qT[:, :], in_=self.QK[2048 + h * 128:2048 + (h + 1) * 128, :]), w=["nqT"])
                pr.dma("sync", lambda e, h=h: e.dma_start(out=kT[:, :], in_=self.QK[3072 + h * 128:3072 + (h + 1) * 128, :]), w=["nkT"])
                pr.dma("sync", lambda e, h=h: e.dma_start(out=Vh[:, :, :], in_=self.V[:, 1024 + h * 128:1024 + (h + 1) * 128].rearrange("(j p) c -> p j c", p=128)), w=["nVh"])
                pr.dma("sync", lambda e, h=h: e.dma_start(out=Bt[:, :, :, :], in_=self.na_bias[j, h].rearrange("p (a b q) -> p a b q", a=5, b=5)), w=["Bt"])
                for qi in range(66):
                    b = qi % 2
                    bA, bB = (0, 1) if b == 0 else (2, 3)
                    if qi < 2:
                        q0 = qi * 128
                        keys = [0, 128]
                        cls = None
                    else:
                        jq = qi - 2
                        q0 = CTX + jq * 128
                        kbase = int(np.clip(2 * jq - 4, 0, 118))
                        k0 = CTX + kbase * 64
                        keys = [0, 128] + [k0 + 128 * i for i in range(5)]
                        cls = _na_class(jq)
                    nkk = len(keys)
                    for i, ks in enumerate(keys):
                        dst = self.psb[bA][:, i * 128:(i + 1) * 128] if i < 4 else self.psb[bB][:, (i - 4) * 128:(i - 3) * 128]
                        pr.op("pe", lambda e, ks=ks, q0=q0, dst=dst: e.matmul(dst, lhsT=kT[:, ks:ks + 128], rhs=qT[:, q0:q0 + 128], start=True, stop=True),
                              r=["nkT", "nqT"], w=[("ps", bA if i < 4 else bB)])
                    Pb = P[b]
                    pr.op("act", lambda e, Pb=Pb, bA=bA: e.activation(out=Pb[:, 0:256], in_=self.psb[bA][:, 0:256], func=AF.Exp),
                          r=[("ps", bA)], w=[("nP", b, 0)])
                    if cls is not None:
                        pr.op("dve", lambda e, b=b, bA=bA, cls=cls: e.tensor_tensor(
                            out=Ssb[b][:, 0:256], in0=self.psb[bA][:, 256:512], in1=Bt[:, cls, 0:2, :].rearrange("p a q -> p (a q)"), op=ALU.add),
                            r=[("ps", bA), "Bt"], w=[("Ssb", b, 0)])
                        pr.op("dve", lambda e, b=b, bB=bB, cls=cls: e.tensor_tensor(
                            out=Ssb[b][:, 256:640], in0=self.psb[bB][:, 0:384], in1=Bt[:, cls, 2:5, :].rearrange("p a q -> p (a q)"), op=ALU.add),
                            r=[("ps", bB), "Bt"], w=[("Ssb", b, 1)])
                        pr.op("act", lambda e, Pb=Pb, b=b: e.activation(out=Pb[:, 256:896], in_=Ssb[b][:, 0:640], func=AF.Exp),
                              r=[("Ssb", b, 0), ("Ssb", b, 1)], w=[("nP", b, 1)])
                    for i, ks in enumerate(keys):
                        pr.op("pe", lambda e, i=i, ks=ks, Pb=Pb, b=b, nkk=nkk: e.matmul(
                            self.psb[4 + b][:, 0:128], lhsT=Vh[:, ks // 128, :], rhs=Pb[:, i * 128:(i + 1) * 128], start=(i == 0), stop=(i == nkk - 1)),
                            r=[("nP", b, 0), ("nP", b, 1), "nVh"], w=[("ps", 4 + b)])
                    for i, ks in enumerate(keys):
                        pr.op("pe", lambda e, i=i, Pb=Pb, b=b, nkk=nkk: e.matmul(
                            self.psb[6 + b][:, 0:128], lhsT=self.onesb[:, :], rhs=Pb[:, i * 128:(i + 1) * 128], start=(i == 0), stop=(i == nkk - 1)),
                            r=[("nP", b, 0), ("nP", b, 1), "onesb"], w=[("ps", 6 + b)])
                    pr.op("dve", lambda e, b=b: e.reciprocal(out=rD[b][:, :], in_=self.psb[6 + b][:, 0:128]), r=[("ps", 6 + b)], w=[("nrD", b)])
                    pr.op("dve", lambda e, b=b, q0=q0: e.tensor_tensor(out=naT[:, q0:q0 + 128], in0=self.psb[4 + b][:, 0:128], in1=rD[b][:, :], op=ALU.mult),
                          r=[("ps", 4 + b), ("nrD", b)], w=[("naT", q0)])
                pr.dma("pool", lambda e, h=h: e.dma_start(out=self.catT[1024 + h * 128:1024 + (h + 1) * 128, :], in_=naT[:, :]),
                       r=[("naT", qq) for qq in range(0, T, 128)], w=[("catTn", h)])
                pr.flush()
            pr.flush(barrier=True)

    def phase_rec(self, l):
        pr = self.pr
        j = l // 2
        if not hasattr(self, "GL"):
            self.GL = self.dint("GL", [2 * D, T], F32)
        self.cast_w(self.rec_w_in[j], self.wb_in, D, 5 * D, "wbi")
        pr.flush(barrier=True)
        SEG = 2112
        NCH = 33
        with contextlib.ExitStack() as st0:
            LBL = self.sb(st0, "LBL", [128, 128], F32)
            self.transpose_rows(st0, self.rec_lb[:, :], 128, LBL[:, :], "LBL", bank=0)
            pr.op("act", lambda e: e.activation(out=LBL[:, :], in_=LBL[:, :], func=AF.Exp), r=["LBL"], w=["LBL"])
            den = self.sb(st0, "lbden", [128, 32], F32)
            num = self.sb(st0, "lbnum", [128, 32], F32)
            pr.op("dve", lambda e: e.tensor_tensor(out=den[:, :], in0=LBL[:, 0:32], in1=LBL[:, 32:64], op=ALU.add), r=["LBL"], w=["lbden"])
            pr.op("dve", lambda e: e.tensor_tensor(out=den[:, :], in0=den[:, :], in1=LBL[:, 64:96], op=ALU.add), r=["LBL", "lbden"], w=["lbden"])
            pr.op("dve", lambda e: e.tensor_tensor(out=den[:, :], in0=den[:, :], in1=LBL[:, 96:128], op=ALU.add), r=["LBL", "lbden"], w=["lbden"])
            pr.op("dve", lambda e: e.tensor_copy(out=num[:, :], in_=LBL[:, 32:64]), r=["LBL"], w=["lbnum"])
            for lp in range(2, l + 1):
                pr.op("dve", lambda e, lp=lp: e.tensor_tensor(out=num[:, :], in0=num[:, :], in1=LBL[:, lp * 32:(lp + 1) * 32], op=ALU.add),
                      r=["LBL", "lbnum"], w=["lbnum"])
            LB = self.sb(st0, "LB", [128, 32], F32)
            OML = self.sb(st0, "OML", [128, 32], F32)
            pr.op("dve", lambda e: e.reciprocal(out=den[:, :], in_=den[:, :]), r=["lbden"], w=["lbden"])
            pr.op("dve", lambda e: e.tensor_tensor(out=LB[:, :], in0=num[:, :], in1=den[:, :], op=ALU.mult), r=["lbnum", "lbden"], w=["LB"])
            pr.op("dve", lambda e: e.tensor_scalar(out=OML[:, :], in0=LB[:, :], scalar1=-1.0, scalar2=1.0, op0=ALU.mult, op1=ALU.add), r=["LB"], w=["OML"])
            gnv = self.sb(st0, "gnv", [128, 1], F32)
            self.transpose_rows(st0, self.rec_gn[j:j + 1, :], 1, gnv[:, 0:1], "gnv", bank=1)
            with contextlib.ExitStack() as st:
                xs = [self.sb(st, f"rxs{i}", [128, 512], F32) for i in range(2)]
                tmp = [self.sb(st, f"rtp{i}", [128, 512], F32) for i in range(2)]
                stg = [self.sb(st, f"rstg{i}", [128, 512], BF16) for i in range(2)]
                cnt = [0]

                def evac(mt, t0, n, pap, bank):
                    grp, hh = mt // 16, mt % 16
                    b = cnt[0] % 2
                    cnt[0] += 1
                    if grp == 0:
                        pr.op("act", lambda e: e.activation(out=xs[b][:, 0:n], in_=pap, func=AF.Silu), r=[("ps", bank)], w=[("rxs", b)])
                        pr.op("dve", lambda e: e.tensor_scalar(out=stg[b][:, 0:n], in0=xs[b][:, 0:n], scalar1=128 ** -0.5, scalar2=None, op0=ALU.mult),
                              r=[("rxs", b)], w=[("rstg", b)])
                        row0 = hh * 128
                    elif grp == 4:
                        pr.op("act", lambda e: e.activation(out=stg[b][:, 0:n], in_=pap, func=AF.Silu), r=[("ps", bank)], w=[("rstg", b)])
                        row0 = 3 * D + hh * 128
                    else:
                        d = grp - 2
                        ci = d * 16 + hh
                        pr.op("act", lambda e: e.activation(out=xs[b][:, 0:n], in_=pap, func=AF.Sigmoid), r=[("ps", bank)], w=[("rxs", b)])
                        pr.op("dve", lambda e: e.tensor_scalar(out=xs[b][:, 0:n], in0=xs[b][:, 0:n], scalar1=OML[:, ci:ci + 1], scalar2=LB[:, ci:ci + 1],
                                                             op0=ALU.mult, op1=ALU.add), r=[("rxs", b), "OML", "LB"], w=[("rxs", b)])
                        pr.op("act", lambda e: e.activation(out=tmp[b][:, 0:n], in_=xs[b][:, 0:n], func=AF.Ln), r=[("rxs", b)], w=[("rtp", b)])
                        g0 = d * D + hh * 128
                        pr.dma("pool", lambda e: e.dma_start(out=self.GL[g0:g0 + 128, t0:t0 + n], in_=tmp[b][:, 0:n]), r=[("rtp", b)], w=[("GL", g0, t0)])
                        pr.op("dve", lambda e: e.tensor_scalar(out=stg[b][:, 0:n], in0=xs[b][:, 0:n], scalar1=-1.0, scalar2=1.0, op0=ALU.mult, op1=ALU.add),
                              r=[("rxs", b)], w=[("rstg", b)])
                        row0 = D + d * D + hh * 128
                    pr.dma("pool", lambda e: e.dma_start(out=self.QK[row0:row0 + 128, t0:t0 + n], in_=stg[b][:, 0:n]), r=[("rstg", b)], w=[("QK", row0, t0)])

                def tm_evac(c0, t0, pap, bank):
                    colo = c0 - D
                    b = cnt[0] % 2
                    cnt[0] += 1
                    pr.op("act", lambda e: e.activation(out=stg[b][:, :], in_=pap, func=AF.Copy), r=[("ps", bank)], w=[("rstg", b)])
                    pr.dma("pool", lambda e: e.dma_start(out=self.V[t0:t0 + 128, colo:colo + 512], in_=stg[b][:, :]), r=[("rstg", b)], w=[("V", t0, colo)])

                self.linear(st, self.aT, self.wb_in, D, 5 * D, evac, tm_cols={2048, 2560, 3072, 3584}, tm_evac=tm_evac, tag="rp")
                pr.flush(barrier=True)
            with contextlib.ExitStack() as st:
                mask = self.sb(st, "smask", [128, SEG], F32)
                pr.op("dve", lambda e: e.memset(mask[:, :], 1.0), w=["smask"])
                pr.op("dve", lambda e: e.memset(mask[:, :].rearrange("p (n c) -> p n c", c=64)[:, :, 0:1], 0.0), r=["smask"], w=["smask"])
                mk = [self.sb(st, "m128f", [128, 128], F32), self.sb(st, "m128b", [128, 128], F32)]
                pr.dma("sync", lambda e: e.dma_start(out=mk[0][:, :], in_=self.k_m128f[:, :]), w=["m128f"])
                pr.dma("sync", lambda e: e.dma_start(out=mk[1][:, :], in_=self.k_m128b[:, :]), w=["m128b"])
                gs = self.sb(st, "gs", [128, SEG], F32)
                Cs = self.sb(st, "Cs", [128, SEG], F32)
                As = self.sb(st, "As", [128, SEG], F32)
                Es = self.sb(st, "Es", [128, SEG], F32)
                qss = self.sb(st, "qss", [128, SEG], BF16)
                kks = self.sb(st, "kks", [128, SEG], BF16)
                qt_ = self.sb(st, "qt_", [128, T], BF16)
                kt_ = self.sb(st, "kt_", [128, T], BF16)
                kh_ = self.sb(st, "kh_", [128, T], BF16)
                qst = self.sb(st, "qst", [128, T], BF16)
                dsb = self.sb(st, "dsb", [128, 132], F32)
                Vh = self.sb(st, "rVh", [128, 66, 128], BF16)
                oT = self.sb(st, "oT", [128, T], F32)
                S = [self.sb(st, f"S{i}", [128, 128], F32) for i in range(2)]
                Sb = [self.sb(st, f"Sb{i}", [128, 128], BF16) for i in range(2)]
                attm = [self.sb(st, f"attm{i}", [128, 128], BF16) for i in range(2)]
                khT = [self.sb(st, f"khT{i}", [128, 128], BF16) for i in range(2)]
                rr = self.sb(st, "rrs", [128, 512], F32)
                gt = self.sb(st, "rgt", [128, 512], BF16)
                ost = [self.sb(st, f"rost{i}", [128, 512], BF16) for i in range(2)]
                v3 = lambda a: a[:, :].rearrange("p (n c) -> p n c", c=64)
                oi = 0
                for hh in range(16):
                    pr.dma("sync", lambda e, hh=hh: e.dma_start(out=Vh[:, :, :], in_=self.V[:, hh * 128:(hh + 1) * 128].rearrange("(j p) c -> p j c", p=128)), w=["rVh"])
                    for d in range(2):
                        mi, ei = (31, 63) if d == 0 else (32, 0)
                        for sg_ in range(4):
                            t0 = sg_ * SEG
                            g0 = d * D + hh * 128
                            k0 = D + d * D + hh * 128
                            pr.dma("sync", lambda e, g0=g0, t0=t0: e.dma_start(out=gs[:, :], in_=self.GL[g0:g0 + 128, t0:t0 + SEG]), w=["gs"])
                            pr.dma("sync", lambda e, hh=hh, t0=t0: e.dma_start(out=qss[:, :], in_=self.QK[hh * 128:(hh + 1) * 128, t0:t0 + SEG]), w=["qss"])
                            pr.dma("sync", lambda e, k0=k0, t0=t0: e.dma_start(out=kks[:, :], in_=self.QK[k0:k0 + 128, t0:t0 + SEG]), w=["kks"])
                            pr.op("dve", lambda e: e.tensor_tensor_scan(out=Cs[:, :], data0=mask[:, :], data1=gs[:, :], initial=0.0, op0=ALU.mult, op1=ALU.add),
                                  r=["gs", "smask"], w=["Cs"])
                            if d == 1:
                                pr.op("dve", lambda e: e.tensor_tensor(out=v3(As), in0=v3(Cs)[:, :, 63:64].to_broadcast([128, NCH, 64]), in1=v3(Cs), op=ALU.subtract),
                                      r=["Cs"], w=["As"])
                                pr.op("dve", lambda e: e.tensor_tensor(out=Cs[:, :], in0=As[:, :], in1=gs[:, :], op=ALU.add), r=["As", "gs"], w=["Cs"])
                            pr.op("dve", lambda e, mi=mi: e.tensor_tensor(out=v3(As), in0=v3(Cs), in1=v3(Cs)[:, :, mi:mi + 1].to_broadcast([128, NCH, 64]), op=ALU.subtract),
                                  r=["Cs"], w=["As"])
                            pr.op("act", lambda e: e.activation(out=Es[:, :], in_=As[:, :], func=AF.Exp), r=["As"], w=["Es"])
                            pr.op("dve", lambda e, t0=t0: e.tensor_tensor(out=qt_[:, t0:t0 + SEG], in0=qss[:, :], in1=Es[:, :], op=ALU.mult), r=["qss", "Es"], w=[("qt_", sg_)])
                            pr.op("act", lambda e: e.activation(out=Es[:, :], in_=As[:, :], func=AF.Exp, scale=-1.0), r=["As"], w=["Es"])
                            pr.op("dve", lambda e, t0=t0: e.tensor_tensor(out=kt_[:, t0:t0 + SEG], in0=kks[:, :], in1=Es[:, :], op=ALU.mult), r=["kks", "Es"], w=[("kt_", sg_)])
                            pr.op("dve", lambda e, ei=ei: e.tensor_tensor(out=v3(As), in0=v3(Cs)[:, :, ei:ei + 1].to_broadcast([128, NCH, 64]), in1=v3(Cs), op=ALU.subtract),
                                  r=["Cs"], w=["As"])
                            pr.op("act", lambda e: e.activation(out=Es[:, :], in_=As[:, :], func=AF.Exp), r=["As"], w=["Es"])
                            pr.op("dve", lambda e, t0=t0: e.tensor_tensor(out=kh_[:, t0:t0 + SEG], in0=kks[:, :], in1=Es[:, :], op=ALU.mult), r=["kks", "Es"], w=[("kh_", sg_)])
                            pr.op("act", lambda e: e.activation(out=Es[:, :], in_=Cs[:, :], func=AF.Exp), r=["Cs"], w=["Es"])
                            pr.op("dve", lambda e, t0=t0: e.tensor_tensor(out=qst[:, t0:t0 + SEG], in0=qss[:, :], in1=Es[:, :], op=ALU.mult), r=["qss", "Es"], w=[("qst", sg_)])
                            pr.op("dve", lambda e, sg_=sg_, ei=ei: e.tensor_copy(out=dsb[:, sg_ * NCH:(sg_ + 1) * NCH], in_=v3(Es)[:, :, ei]), r=["Es"], w=[("dsb", sg_)])
                        allk = [(nm, q) for nm in ("qt_", "kt_", "kh_", "qst", "dsb") for q in range(4)]
                        pr.op("dve", lambda e: e.memset(S[0][:, :], 0.0), w=[("S", 0)])
                        pr.op("dve", lambda e: e.memset(Sb[0][:, :], 0.0), w=[("Sb", 0)])
                        cur = 0
                        if d == 0:
                            tiles = [(tt, (0, 1)) for tt in range(66)]
                        else:
                            tiles = [(1, (1, 0)), (0, (1, 0))] + [(tt, (1, 0)) for tt in range(65, 1, -1)]
                        for ti, (tt, corder) in enumerate(tiles):
                            ab = ti % 2
                            tl = tt * 128
                            pr.op("pe", lambda e, ab=ab, tl=tl: e.matmul(self.psb[ab][:, 0:128], lhsT=kt_[:, tl:tl + 128], rhs=qt_[:, tl:tl + 128], start=True, stop=True),
                                  r=allk, w=[("ps", ab)])
                            pr.op("dve", lambda e, ab=ab, d=d: e.tensor_tensor(out=attm[ab][:, :], in0=self.psb[ab][:, 0:128], in1=mk[d][:, :], op=ALU.mult),
                                  r=[("ps", ab), "m128f", "m128b"], w=[("attm", ab)])
                            pr.op("pe", lambda e, ab=ab, tl=tl: e.matmul(self.psb[2 + ab][:, 0:128], lhsT=kh_[:, tl:tl + 128], rhs=self.identb[:, :], start=True, stop=True),
                                  r=allk + ["identb"], w=[("ps", 2 + ab)])
                            pr.op("act", lambda e, ab=ab: e.activation(out=khT[ab][:, :], in_=self.psb[2 + ab][:, 0:128], func=AF.Copy), r=[("ps", 2 + ab)], w=[("khT", ab)])
                            po = self.psb[6 + ab]
                            for c in corder:
                                n_ = tt * 2 + c
                                cl = slice(64 * c, 64 * c + 64)
                                pdb = 4 + (n_ % 2)
                                pr.op("pe", lambda e, ab=ab, tt=tt, cl=cl, po=po: e.matmul(po[:, cl], lhsT=Vh[cl, tt, :], rhs=attm[ab][cl, cl], start=True, stop=False),
                                      r=["rVh", ("attm", ab)], w=[("ps", 6 + ab)])
                                pr.op("pe", lambda e, cur=cur, tl=tl, c=c, cl=cl, po=po: e.matmul(po[:, cl], lhsT=Sb[cur][:, :], rhs=qst[:, tl + 64 * c:tl + 64 * c + 64], start=False, stop=True),
                                      r=[("Sb", cur)] + allk, w=[("ps", 6 + ab)])
                                pr.op("pe", lambda e, ab=ab, tt=tt, cl=cl, pdb=pdb: e.matmul(self.psb[pdb][:, 0:128], lhsT=khT[ab][cl, :], rhs=Vh[cl, tt, :], start=True, stop=True),
                                      r=[("khT", ab), "rVh"], w=[("ps", pdb)])
                                nx = 1 - cur
                                pr.op("dve", lambda e, cur=cur, nx=nx, n_=n_, pdb=pdb: e.scalar_tensor_tensor(
                                    out=S[nx][:, :], in0=S[cur][:, :], scalar=dsb[:, n_:n_ + 1], in1=self.psb[pdb][:, 0:128], op0=ALU.mult, op1=ALU.add),
                                    r=[("S", cur), ("ps", pdb)] + [("dsb", q) for q in range(4)], w=[("S", nx)])
                                pr.op("act", lambda e, nx=nx: e.activation(out=Sb[nx][:, :], in_=S[nx][:, :], func=AF.Copy), r=[("S", nx)], w=[("Sb", nx)])
                                cur = nx
                            if d == 0:
                                pr.op("act", lambda e, tl=tl, po=po: e.activation(out=oT[:, tl:tl + 128], in_=po[:, 0:128], func=AF.Copy), r=[("ps", 6 + ab)], w=[("oT", tt)])
                            else:
                                pr.op("dve", lambda e, tl=tl, po=po: e.tensor_tensor(out=oT[:, tl:tl + 128], in0=oT[:, tl:tl + 128], in1=po[:, 0:128], op=ALU.add),
                                      r=[("ps", 6 + ab), ("oT", tt)], w=[("oT", tt)])
                        pr.flush()
                    for (t0, n) in self.tok_tiles(512):
                        okeys = [("oT", tt) for tt in range(t0 // 128, (t0 + n) // 128)]
                        g0 = 3 * D + hh * 128
                        pr.dma("sync", lambda e, g0=g0, t0=t0, n=n: e.dma_start(out=gt[:, 0:n], in_=self.QK[g0:g0 + 128, t0:t0 + n]), w=["rgt"])
                        pr.op("act", lambda e, t0=t0, n=n: e.activation(out=rr[:, 0:n], in_=oT[:, t0:t0 + n], func=AF.Square), r=okeys, w=["rrs"])
                        pr.op("pe", lambda e, n=n: e.matmul(self.psb[0][:, 0:n], lhsT=self.ones[:, :], rhs=rr[:, 0:n], start=True, stop=True), r=["rrs", "ones"], w=[("ps", 0)])
                        pr.op("dve", lambda e, n=n: e.tensor_scalar(out=rr[:, 0:n], in0=self.psb[0][:, 0:n], scalar1=1.0 / 128, scalar2=EPS, op0=ALU.mult, op1=ALU.add),
                              r=[("ps", 0)], w=["rrs"])
                        pr.op("act", lambda e, n=n: e.activation(out=rr[:, 0:n], in_=rr[:, 0:n], func=AF.Sqrt), r=["rrs"], w=["rrs"])
                        pr.op("dve", lambda e, n=n: e.reciprocal(out=rr[:, 0:n], in_=rr[:, 0:n]), r=["rrs"], w=["rrs"])
                        pr.op("dve", lambda e, t0=t0, n=n: e.tensor_tensor(out=rr[:, 0:n], in0=oT[:, t0:t0 + n], in1=rr[:, 0:n], op=ALU.mult), r=okeys + ["rrs"], w=["rrs"])
                        ob = oi % 2
                        oi += 1
                        pr.op("dve", lambda e, n=n, ob=ob: e.scalar_tensor_tensor(out=ost[ob][:, 0:n], in0=rr[:, 0:n], scalar=gnv[:, 0:1], in1=gt[:, 0:n],
                                                                                 op0=ALU.mult, op1=ALU.mult), r=["rrs", "rgt", "gnv"], w=[("rost", ob)])
                        pr.dma("pool", lambda e, hh=hh, t0=t0, n=n, ob=ob: e.dma_start(out=self.catT[hh * 128:(hh + 1) * 128, t0:t0 + n], in_=ost[ob][:, 0:n]),
                               r=[("rost", ob)], w=[("catT", hh, t0)])
                    pr.flush()
                pr.flush(barrier=True)


IN_SHAPES = {
    "hT0": [D, T], "c2": [32, 128], "w_mod": [DEPTH, D, 6 * D], "b_mod": [384, 128], "g12": [128, 128], "gfin": [16, 128],
    "att_w_in": [2, D, 6144], "att_w_out": [2, D, D], "att_lambda": [2, 256], "att_subln": [2, 128],
    "na_bias": [2, 8, 128, 5 * 5 * 128], "rec_w_in": [2, D, 5 * D], "rec_w_out": [2, D, D], "rec_lb": [DEPTH * 2 * 16, 128],
    "rec_gn": [2, 128], "moe_wr": [DEPTH, D, 36], "moe_br": [DEPTH, 36], "moe_wg": [DEPTH, NEXP, D, DEXP],
    "moe_wu": [DEPTH, NEXP, D, DEXP], "moe_wd": [DEPTH, NEXP, DEXP, D], "k_ident": [128, 128], "k_cos": [128, NLAT],
    "k_sin": [128, NLAT], "k_RT": [128, 128], "k_m128f": [128, 128], "k_m128b": [128, 128],
    "k_iota": [32, 164], "k_uincl": [32, 32], "k_pidx": [128, 1],
}


def _mkprop(name):
    def get(self):
        if name not in self._ins:
            self._ins[name] = self.din(name, IN_SHAPES[name])
        return self._ins[name]
    return property(get)


for _n in IN_SHAPES:
    setattr(Builder, _n, _mkprop(_n))


def prep_inputs(inp):
    f = lambda a: np.ascontiguousarray(np.asarray(a, dtype=np.float32))
    k = _consts()
    out = {}
    out["hT0"] = f(np.concatenate([inp["ctx"][0], inp["x"][0]], axis=0).T)
    out["c2"] = f(np.concatenate([inp["c"].reshape(16, 128), inp["c_ctx"].reshape(16, 128)], axis=0))
    out["w_mod"] = f(inp["w_mod"])
    out["b_mod"] = f(inp["b_mod"].reshape(384, 128))
    out["g12"] = f(np.concatenate([inp["norm1_g"].reshape(64, 128), inp["norm2_g"].reshape(64, 128)], axis=0))
    out["gfin"] = f(inp["final_norm_g"].reshape(16, 128))
    out["att_w_in"] = f(inp["att_w_in"])
    out["att_w_out"] = f(inp["att_w_out"])
    out["att_lambda"] = f(inp["att_lambda"].reshape(2, 256))
    out["att_subln"] = f(inp["att_subln_g"])
    out["na_bias"] = f(np.stack([_na_bias(np.asarray(inp["att_rpb"][j])) for j in range(2)]).reshape(2, 8, 128, 5 * 5 * 128))
    out["rec_w_in"] = f(inp["rec_w_in"])
    out["rec_w_out"] = f(inp["rec_w_out"])
    out["rec_lb"] = f(inp["rec_lb_logits"].reshape(DEPTH * 2 * 16, 128))
    out["rec_gn"] = f(inp["rec_gnorm_g"])
    out["moe_wr"] = f(np.concatenate([inp["moe_w_group"], inp["moe_w_router"]], axis=2))
    out["moe_br"] = f(np.concatenate([inp["moe_b_group"], inp["moe_b_router"]], axis=1))
    out["moe_wg"] = f(inp["moe_w_gate"])
    out["moe_wu"] = f(inp["moe_w_up"])
    out["moe_wd"] = f(inp["moe_w_down"])
    out["k_ident"] = k["ident"]
    out["k_cos"] = k["cosT"]
    out["k_sin"] = k["sinT"]
    out["k_RT"] = k["RT"]
    out["k_m128f"] = k["m128f"]
    out["k_m128b"] = k["m128b"]
    out["k_iota"] = np.ascontiguousarray(np.broadcast_to(128.0 * np.arange(164, dtype=np.float32), (32, 164)))
    out["k_uincl"] = np.triu(np.ones((32, 32), np.float32))
    out["k_pidx"] = np.arange(128, dtype=np.float32).reshape(128, 1)
    return out


def kernel(**inputs):
    b = Builder()
    nc = b.build()
    allin = prep_inputs(inputs)
    in_map = {n: allin[n] for n in b._ins}
    res = run_bass_kernel_spmd(nc, [in_map], core_ids=[0])
    outT = res.results[0]["outT"]
    return np.ascontiguousarray(outT.T)[None].astype(np.float32)
```

```python
import contextlib
import math
import numpy as np
import ml_dtypes
import concourse.bass as bass
import concourse.mybir as mybir
from concourse.bass_utils import run_bass_kernel_spmd

F32 = mybir.dt.float32
BF16 = mybir.dt.bfloat16
AF = mybir.ActivationFunctionType
ALU = mybir.AluOpType
AX = mybir.AxisListType

D = 2048
KT = 16
CTX = 256
NLAT = 8192
T = CTX + NLAT
DEPTH = 4
EPS = 1e-6
NEG = -30000.0
NEXP = 32
DEXP = 768


class Tok:
    __slots__ = ("eng", "isdma", "sig", "need")

    def __init__(self, eng, isdma):
        self.eng = eng
        self.isdma = isdma
        self.sig = None
        self.need = False


class Prog:
    NSLOT = 8

    def __init__(self, nc, es):
        self.nc = nc
        self.es = es
        self.E = {"pe": nc.tensor, "act": nc.scalar, "dve": nc.vector, "pool": nc.gpsimd, "sync": nc.sync}
        self.csem = {e: es.enter_context(nc.semaphore("cs_" + e)) for e in ("pe", "act", "dve", "pool")}
        self.ccnt = {e: 0 for e in self.csem}
        self.nslot = {"sync": 8, "pool": 3}
        self.dsem = {q: [es.enter_context(nc.semaphore(f"ds_{q}{i}")) for i in range(self.nslot[q])] for q in ("sync", "pool")}
        self.dcnt = {q: 0 for q in self.dsem}
        self.waited = {e: {} for e in self.E}
        self.last_w = {}
        self.readers = {}
        self.ops = []
        self.semobj = {}
        self.pending_barrier = []
        self.n_inst = 0

    def op(self, eng, fn, r=(), w=()):
        self.ops.append((eng, fn, tuple(r), tuple(w), False))

    def dma(self, q, fn, r=(), w=()):
        self.ops.append((q, fn, tuple(r), tuple(w), True))

    def _wait(self, eng, sem, val):
        k = id(sem)
        if self.waited[eng].get(k, 0) < val:
            self.E[eng].wait_ge(sem, val)
            self.waited[eng][k] = val

    def flush(self, barrier=False):
        ops = self.ops
        self.ops = []
        n = len(ops)
        toks = [Tok(o[0], o[4]) for o in ops]
        deps = [None] * n
        last_idx = {}
        for i, (eng, fn, r, w, isdma) in enumerate(ops):
            d = set()
            for k in r:
                t = self.last_w.get(k)
                if t is not None:
                    d.add(t)
            for k in w:
                t = self.last_w.get(k)
                if t is not None:
                    d.add(t)
                for t2 in self.readers.get(k, ()):
                    d.add(t2)
            me = toks[i]
            for k in w:
                self.last_w[k] = me
                self.readers[k] = []
            for k in r:
                self.readers.setdefault(k, []).append(me)
            dd = []
            for t in d:
                if t is me:
                    continue
                if t.eng == eng and eng == "pe" and not t.isdma and not isdma:
                    continue
                dd.append(t)
                t.need = True
            deps[i] = dd
            if not isdma:
                last_idx[eng] = i
        for e, i in last_idx.items():
            toks[i].need = True
        unsig = {e: [] for e in self.csem}
        first_on = set()
        for i, (eng, fn, r, w, isdma) in enumerate(ops):
            me = toks[i]
            if self.pending_barrier and eng not in first_on:
                first_on.add(eng)
                for (sem, val) in self.pending_barrier:
                    self._wait(eng, sem, val)
            if isdma:
                j = self.dcnt[eng]
                ns = self.nslot[eng]
                slot = j % ns
                sem = self.dsem[eng][slot]
                if j >= ns:
                    self._wait(eng, sem, 16 * (j // ns))
                self.dcnt[eng] = j + 1
                me.sig = (sem, 16 * (j // ns + 1))
            need = {}
            for t in deps[i]:
                sem, val = t.sig
                k = id(sem)
                if k not in need or need[k][1] < val:
                    need[k] = (sem, val)
            for sem, val in need.values():
                self._wait(eng, sem, val)
            inst = fn(self.E[eng])
            self.n_inst += 1
            if isdma:
                inst.then_inc(me.sig[0], 16)
            elif me.need:
                self.ccnt[eng] += 1
                inst.then_inc(self.csem[eng], 1)
                me.sig = (self.csem[eng], self.ccnt[eng])
                for t in unsig[eng]:
                    t.sig = me.sig
                unsig[eng] = []
            else:
                unsig[eng].append(me)
        for e in unsig:
            assert not unsig[e]
        if barrier:
            self.pending_barrier = self.all_sigs()

    def all_sigs(self):
        sigs = [(self.csem[e], self.ccnt[e]) for e in self.csem if self.ccnt[e] > 0]
        for q in self.dsem:
            j = self.dcnt[q]
            ns = self.nslot[q]
            for s in range(ns):
                if j > s:
                    cnt = (j - 1 - s) // ns + 1
                    sigs.append((self.dsem[q][s], 16 * cnt))
        return sigs

    def finish(self):
        self.flush()
        for sem, val in self.all_sigs():
            self._wait("sync", sem, val)


def _consts():
    ident = np.eye(128, dtype=np.float32)
    ones = np.ones((128, 128), np.float32)
    tpos = np.arange(NLAT)
    rows = (tpos // 64).astype(np.float32)
    cols = (tpos % 64).astype(np.float32)
    inv = (10000.0 ** (-np.arange(16, dtype=np.float32) / 16)).astype(np.float32)
    cosT = np.zeros((128, NLAT), np.float32)
    sinT = np.zeros((128, NLAT), np.float32)
    for d in range(128):
        dd = d % 64
        axis = dd // 32
        fr = dd % 16
        ang = (rows if axis == 0 else cols) * inv[fr]
        cosT[d] = np.cos(ang)
        sinT[d] = np.sin(ang)
    RT = np.zeros((128, 128), np.float32)
    for m in range(128):
        if (m % 32) < 16:
            RT[m + 16, m] = -1.0
        else:
            RT[m - 16, m] = 1.0
    s_ = np.arange(64)[:, None]
    t_ = np.arange(64)[None, :]
    mfwd = (s_ <= t_).astype(np.float32)
    mbwd = (s_ >= t_).astype(np.float32)
    m128f = np.zeros((128, 128), np.float32)
    m128b = np.zeros((128, 128), np.float32)
    for c in range(2):
        m128f[c * 64:(c + 1) * 64, c * 64:(c + 1) * 64] = mfwd
        m128b[c * 64:(c + 1) * 64, c * 64:(c + 1) * 64] = mbwd
    return dict(ident=ident, ones=ones, cosT=cosT, sinT=sinT, RT=RT, m128f=m128f, m128b=m128b)


NA_CLASS_J = [0, 1, 2, 62, 63]


def _na_class(j):
    if j <= 1:
        return j
    if j >= 62:
        return j - 59
    return 2


def _na_geom():
    idx = np.zeros((5, 640, 128, 2), np.int64)
    valid = np.zeros((5, 640, 128), bool)
    for c, j in enumerate(NA_CLASS_J):
        kbase = int(np.clip(2 * j - 4, 0, 118))
        ql = np.arange(128)
        r = 2 * j + ql // 64
        cq = ql % 64
        rs = np.clip(r - 4, 0, 120)
        cs = np.clip(cq - 8, 0, 48)
        kl = np.arange(640)
        kr = kbase + kl // 64
        kc = kl % 64
        v = (kr[:, None] >= rs[None, :]) & (kr[:, None] < rs[None, :] + 8) & (kc[:, None] >= cs[None, :]) & (kc[:, None] < cs[None, :] + 16)
        ro = kr[:, None] - r[None, :] + 7
        co = kc[:, None] - cq[None, :] + 15
        valid[c] = v
        idx[c, :, :, 0] = np.clip(ro, 0, 14)
        idx[c, :, :, 1] = np.clip(co, 0, 30)
    return idx, valid


def _na_bias(rpb):
    idx, valid = _na_geom()
    out = np.empty((8, 5, 640, 128), np.float32)
    for h in range(8):
        g = rpb[h][idx[..., 0], idx[..., 1]]
        out[h] = np.where(valid, g, np.float32(NEG))
    out = out.reshape(8, 5, 5, 128, 128).transpose(0, 3, 1, 2, 4)
    return np.ascontiguousarray(out)


class Builder:
    def __init__(self, n_layers=DEPTH, debug_out=None):
        self.n_layers = n_layers
        self.debug_out = debug_out
        self.nc = bass.Bass("TRN2", target_bir_lowering=False)
        self.es = contextlib.ExitStack()
        self.pr = Prog(self.nc, self.es)
        self.uid = 0
        self._ins = {}

    def din(self, name, shape, dt=F32):
        return self.nc.dram_tensor(name, list(shape), dt, kind="ExternalInput").ap()

    def dint(self, name, shape, dt):
        return self.nc.dram_tensor(name, list(shape), dt, kind="Internal").ap()

    def sb(self, stack, name, shape, dt):
        self.uid += 1
        return stack.enter_context(self.nc.sbuf_tensor(f"{name}_{self.uid}", list(shape), dt))

    def ps(self, stack, name, shape, dt=F32):
        self.uid += 1
        return stack.enter_context(self.nc.psum_tensor(f"{name}_{self.uid}", list(shape), dt))

    def build(self):
        nc, pr = self.nc, self.pr
        L = self.n_layers
        self.outT = self.nc.dram_tensor("outT", [D, NLAT], F32, kind="ExternalOutput").ap()
        self.hA = self.dint("hA", [D, T], F32)
        self.hB = self.dint("hB", [D, T], F32)
        self.aT = self.dint("aT", [D, T], BF16)
        self.QK = self.dint("QK", [5 * D, T], BF16)
        self.V = self.dint("V", [T, D], BF16)
        self.catT = self.dint("catT", [D, T], BF16)
        self.wb_in = self.dint("wb_in", [D, 5 * D], BF16)
        self.wb_out = self.dint("wb_out", [D, D], BF16)
        self.alloc_moe()

        es = self.es
        self.ident = self.sb(es, "ident", [128, 128], F32)
        self.ones = self.sb(es, "ones", [128, 128], F32)
        self.onesb = self.sb(es, "onesb", [128, 128], BF16)
        self.identb = self.sb(es, "identb", [128, 128], BF16)
        self.MOD = self.sb(es, "MOD", [128, DEPTH * 6 * 16 * 2], F32)
        self.AB = self.sb(es, "AB", [128, DEPTH * 2 * 2 * 16 * 2], F32)
        self.GF = self.sb(es, "GF", [128, 16], F32)
        self.G12 = self.sb(es, "G12", [128, 128], F32)
        pr.dma("sync", lambda e: e.dma_start(out=self.ident[:], in_=self.k_ident[:, :]), w=["ident"])
        pr.op("dve", lambda e: e.memset(self.ones[:], 1.0), w=["ones"])
        pr.op("dve", lambda e: e.memset(self.onesb[:], 1.0), w=["onesb"])
        pr.op("dve", lambda e: e.tensor_copy(out=self.identb[:], in_=self.ident[:]), r=["ident"], w=["identb"])
        self.psb = [self.ps(es, f"bank{i}", [128, 512], F32) for i in range(8)]

        self.phase_mod()
        h_in = self.hT0
        for l in range(L):
            h_mid = self.hA
            h_out = self.hB
            self.phase_norm(l, 1, h_in, router=False)
            if l % 2 == 0:
                self.phase_att(l)
            else:
                self.phase_rec(l)
            self.phase_outproj(l, h_in, h_mid)
            self.phase_norm(l, 2, h_mid, router=True)
            self.phase_moe(l, h_mid, h_out)
            h_in = h_out
        self.phase_final(h_in)
        pr.finish()
        return nc

    def alloc_moe(self):
        self.wb_g2 = self.dint("wb_g2", [NEXP * 128, 16 * DEXP], BF16)
        self.wb_u2 = self.dint("wb_u2", [NEXP * 128, 16 * DEXP], BF16)
        self.wb_d2 = self.dint("wb_d2", [NEXP * 128, 6 * D], BF16)
        self.XS = self.dint("XS", [164 * 128, D], BF16)
        self.YS = self.dint("YS", [164 * 128, D], F32)
        es = self.es
        self.Lsb = self.sb(es, "Lsb", [128, 66, 36], F32)
        self.W1 = self.sb(es, "W1", [128, 66], F32)
        self.W2 = self.sb(es, "W2", [128, 66], F32)
        self.D1 = self.sb(es, "D1", [128, 66], mybir.dt.uint32)
        self.D2 = self.sb(es, "D2", [128, 66], mybir.dt.uint32)
        self.WIDX = self.sb(es, "WIDX", [128, 164], mybir.dt.uint32)

    def modv(self, l, j, f, col):
        o = ((l * 6 + j) * 16 + f) * 2 + col
        return self.MOD[:, o:o + 1]

    def abv(self, l, which, ab, f, col):
        o = ((((l * 2 + (which - 1)) * 2 + ab) * 16) + f) * 2 + col
        return self.AB[:, o:o + 1]

    def cast_w(self, src2d, dst2d, rows, cols, tag):
        pr = self.pr
        for r0 in range(0, rows, 1024):
            r1 = min(rows, r0 + 1024)
            for c0 in range(0, cols, 2048):
                c1 = min(cols, c0 + 2048)
                pr.dma("pool", lambda e, r0=r0, r1=r1, c0=c0, c1=c1: e.dma_start(out=dst2d[r0:r1, c0:c1], in_=src2d[r0:r1, c0:c1]),
                       r=[], w=[(tag, r0, c0)])

    def transpose_rows(self, stack, src_rows_ap, R, dst_ap, tag, bank=0):
        pr = self.pr
        tmp = self.sb(stack, "trtmp", [128, 128], F32)
        kt = ("trtmp", self.uid)
        pr.dma("sync", lambda e: e.dma_start(out=tmp[0:R, :], in_=src_rows_ap), w=[kt])
        pb = self.psb[bank]
        pr.op("pe", lambda e: e.matmul(pb[:, 0:R], lhsT=tmp[0:R, :], rhs=self.ident[0:R, 0:R], start=True, stop=True),
              r=[kt, "ident"], w=[("ps", bank)])
        pr.op("dve", lambda e: e.tensor_copy(out=dst_ap, in_=pb[:, 0:R]), r=[("ps", bank)], w=[tag])

    def phase_mod(self):
        pr = self.pr
        with contextlib.ExitStack() as st:
            cc = self.sb(st, "cc", [128, 32], F32)
            BM = self.sb(st, "BM", [128, 384], F32)
            self.transpose_rows(st, self.c2[:, :], 32, cc[:, :], "cc", bank=0)
            pr.op("act", lambda e: e.activation(out=cc[:, :], in_=cc[:, :], func=AF.Silu), r=["cc"], w=["cc"])
            for a in range(3):
                self.transpose_rows(st, self.b_mod[a * 128:(a + 1) * 128, :], 128, BM[:, a * 128:(a + 1) * 128], ("BM", a), bank=1 + a)
            self.transpose_rows(st, self.g12[:, :], 128, self.G12[:, :], "G12", bank=4)
            self.transpose_rows(st, self.gfin[:, :], 16, self.GF[:, :], "GF", bank=5)
            cc2 = self.sb(st, "cc2", [128, 16, 2], F32)
            pr.op("dve", lambda e: e.tensor_copy(out=cc2[:, :, 0], in_=cc[:, 0:16]), r=["cc"], w=["cc2a"])
            pr.op("dve", lambda e: e.tensor_copy(out=cc2[:, :, 1], in_=cc[:, 16:32]), r=["cc"], w=["cc2b"])
            wm = [self.sb(st, f"wm{i}", [128, 16, 1024], F32) for i in range(2)]
            it = 0
            for l in range(self.n_layers):
                for j in range(6):
                    for half in range(2):
                        b = it % 2
                        bank = 6 + (it % 2)
                        c0 = j * D + half * 1024
                        src = self.w_mod[l, :, c0:c0 + 1024].rearrange("(k p) c -> p k c", p=128)
                        pr.dma("sync", lambda e, b=b, src=src: e.dma_start(out=wm[b][:, :, :], in_=src), w=[("wm", b)])
                        pb = self.psb[bank]
                        for f8 in range(8):
                            for k in range(16):
                                pr.op("pe", lambda e, b=b, f8=f8, k=k, pb=pb: e.matmul(
                                    pb[:, f8 * 2:f8 * 2 + 2], lhsT=wm[b][:, k, f8 * 128:(f8 + 1) * 128], rhs=cc2[:, k, :],
                                    start=(k == 0), stop=(k == 15)), r=[("wm", b), "cc2a", "cc2b"], w=[("ps", bank)])
                        o = ((l * 6 + j) * 16 + half * 8) * 2
                        bo = l * 96 + j * 16 + half * 8
                        for col in range(2):
                            pr.op("dve", lambda e, o=o, bo=bo, col=col, pb=pb: e.tensor_tensor(
                                out=self.MOD[:, o + col:o + 16:2], in0=pb[:, col:16:2], in1=BM[:, bo:bo + 8], op=ALU.add),
                                r=[("ps", bank), ("BM", 0), ("BM", 1), ("BM", 2)], w=["MOD"])
                        it += 1
            for l in range(self.n_layers):
                for which in (1, 2):
                    jsh, jsc = (0, 1) if which == 1 else (3, 4)
                    g = self.G12[:, (which - 1) * 64 + l * 16:(which - 1) * 64 + l * 16 + 16]
                    for col in range(2):
                        osc = ((l * 6 + jsc) * 16) * 2 + col
                        osh = ((l * 6 + jsh) * 16) * 2 + col
                        oa = ((((l * 2 + (which - 1)) * 2 + 0) * 16)) * 2 + col
                        ob = ((((l * 2 + (which - 1)) * 2 + 1) * 16)) * 2 + col
                        pr.op("dve", lambda e, osc=osc, oa=oa, g=g: e.scalar_tensor_tensor(
                            out=self.AB[:, oa:oa + 31:2], in0=self.MOD[:, osc:osc + 31:2], scalar=1.0, in1=g, op0=ALU.add, op1=ALU.mult),
                            r=["MOD", "G12"], w=["AB"])
                        pr.op("dve", lambda e, osh=osh, ob=ob: e.tensor_copy(out=self.AB[:, ob:ob + 31:2], in_=self.MOD[:, osh:osh + 31:2]),
                              r=["MOD"], w=["AB"])
            pr.flush(barrier=True)

    def tok_tiles(self, n):
        out = []
        t0 = 0
        while t0 < T:
            lim = CTX if t0 < CTX else T
            m = min(n, lim - t0)
            out.append((t0, m))
            t0 += m
        return out

    def phase_norm(self, l, which, h_src, router, final=False):
        pr = self.pr
        NT = 256
        with contextlib.ExitStack() as st:
            hb = [self.sb(st, f"nh{i}", [128, 16, NT], F32) for i in range(2)]
            sq = [self.sb(st, f"nsq{i}", [128, 16, NT], F32) for i in range(2)]
            ab = [self.sb(st, f"nab{i}", [128, 16, NT], BF16) for i in range(2)]
            rs = [self.sb(st, f"nrs{i}", [128, NT], F32) for i in range(2)]
            if router:
                WR = self.sb(st, "WR", [128, 16, 36], F32)
                Lsb = self.Lsb
                pr.dma("sync", lambda e: e.dma_start(out=WR[:, :, :], in_=self.moe_wr[l].rearrange("(k p) c -> p k c", p=128)), w=["WR"])
            for i, (t0, n) in enumerate(self.tok_tiles(NT)):
                b = i % 2
                col = 1 if t0 < CTX else 0
                bank = i % 2
                pr.dma("sync", lambda e, b=b, t0=t0, n=n: e.dma_start(
                    out=hb[b][:, :, 0:n], in_=h_src[:, t0:t0 + n].rearrange("(k p) t -> p k t", p=128)), w=[("nh", b)])
                pr.op("act", lambda e, b=b, n=n: e.activation(out=sq[b][:, :, 0:n], in_=hb[b][:, :, 0:n], func=AF.Square),
                      r=[("nh", b)], w=[("nsq", b)])
                pb = self.psb[bank]
                for k in range(16):
                    pr.op("pe", lambda e, b=b, k=k, n=n, pb=pb: e.matmul(pb[:, 0:n], lhsT=self.ones[:, :], rhs=sq[b][:, k, 0:n],
                                                                       start=(k == 0), stop=(k == 15)),
                          r=[("nsq", b), "ones"], w=[("ps", bank)])
                pr.op("dve", lambda e, b=b, n=n, pb=pb: e.tensor_scalar(out=rs[b][:, 0:n], in0=pb[:, 0:n], scalar1=1.0 / D, scalar2=EPS,
                                                                      op0=ALU.mult, op1=ALU.add), r=[("ps", bank)], w=[("nrs", b)])
                pr.op("act", lambda e, b=b, n=n: e.activation(out=rs[b][:, 0:n], in_=rs[b][:, 0:n], func=AF.Sqrt), r=[("nrs", b)], w=[("nrs", b)])
                pr.op("dve", lambda e, b=b, n=n: e.reciprocal(out=rs[b][:, 0:n], in_=rs[b][:, 0:n]), r=[("nrs", b)], w=[("nrs", b)])
                for k in range(16):
                    pr.op("dve", lambda e, b=b, k=k, n=n: e.tensor_tensor(out=sq[b][:, k, 0:n], in0=hb[b][:, k, 0:n], in1=rs[b][:, 0:n],
                                                                        op=ALU.mult), r=[("nh", b), ("nrs", b)], w=[("nsq", b)])
                for k in range(16):
                    if final:
                        sc1, sc2, o1 = self.GF[:, k:k + 1], None, ALU.bypass
                        pr.op("dve", lambda e, b=b, k=k, n=n, sc1=sc1: e.tensor_scalar(
                            out=hb[b][:, k, 0:n], in0=sq[b][:, k, 0:n], scalar1=sc1, scalar2=None, op0=ALU.mult),
                            r=[("nsq", b), "GF"], w=[("nh", b)])
                    else:
                        A = self.abv(l, which, 0, k, col)
                        Bv = self.abv(l, which, 1, k, col)
                        dst = sq[b] if router else ab[b]
                        pr.op("dve", lambda e, b=b, k=k, n=n, A=A, Bv=Bv, dst=dst: e.tensor_scalar(
                            out=dst[:, k, 0:n], in0=sq[b][:, k, 0:n], scalar1=A, scalar2=Bv, op0=ALU.mult, op1=ALU.add),
                            r=[("nsq", b), "AB"], w=[("nsq", b) if router else ("nab", b)])
                if final:
                    if t0 >= CTX:
                        pr.dma("pool", lambda e, b=b, t0=t0, n=n: e.dma_start(
                            out=self.outT[:, t0 - CTX:t0 - CTX + n].rearrange("(k p) t -> p k t", p=128), in_=hb[b][:, :, 0:n]),
                            r=[("nh", b)], w=[("outT", t0)])
                    continue
                if router:
                    pr.op("act", lambda e, b=b, n=n: e.activation(out=ab[b][:, :, 0:n], in_=sq[b][:, :, 0:n], func=AF.Copy),
                          r=[("nsq", b)], w=[("nab", b)])
                    for s in range(n // 128):
                        rb = 2 + (s % 2)
                        pbl = self.psb[rb]
                        for k in range(16):
                            pr.op("pe", lambda e, b=b, k=k, s=s, pbl=pbl: e.matmul(
                                pbl[:, 0:36], lhsT=sq[b][:, k, s * 128:(s + 1) * 128], rhs=WR[:, k, :], start=(k == 0), stop=(k == 15)),
                                r=[("nsq", b), "WR"], w=[("ps", rb)])
                        blk = t0 // 128 + s
                        pr.op("act", lambda e, blk=blk, pbl=pbl: e.activation(out=Lsb[:, blk, :], in_=pbl[:, 0:36], func=AF.Copy),
                              r=[("ps", rb)], w=[("Lsb", blk)])
                pr.dma("pool", lambda e, b=b, t0=t0, n=n: e.dma_start(
                    out=self.aT[:, t0:t0 + n].rearrange("(k p) t -> p k t", p=128), in_=ab[b][:, :, 0:n]),
                    r=[("nab", b)], w=[("aT", t0)])
            pr.flush(barrier=True)
        if router:
            with contextlib.ExitStack() as st2:
                self.route(st2, l, self.Lsb)
                pr.flush(barrier=True)

    def phase_final(self, h_src):
        self.phase_norm(0, 1, h_src, router=False, final=True)

    def route(self, st, l, Lsb):
        pr = self.pr
        NB = 66
        br = self.sb(st, "br", [128, 36], F32)
        pr.dma("sync", lambda e: e.dma_start(out=br[:, :], in_=self.moe_br[l:l + 1, :].partition_broadcast(128)), w=["br"])
        allL = [("Lsb", b) for b in range(NB)]
        Lb = self.sb(st, "Lb", [128, NB, 36], F32)
        pr.op("dve", lambda e: e.tensor_tensor(out=Lb[:, :, :], in0=Lsb[:, :, :], in1=br[:, :].unsqueeze(1).to_broadcast([128, NB, 36]),
                                               op=ALU.add), r=allL + ["br"], w=["Lb"])
        gmax = self.sb(st, "gmax", [128, NB], F32)
        pr.op("dve", lambda e: e.tensor_reduce(out=gmax[:, :], in_=Lb[:, :, 0:4], axis=AX.X, op=ALU.max), r=["Lb"], w=["gmax"])
        gd = self.sb(st, "gd", [128, NB, 4], F32)
        pr.op("dve", lambda e: e.tensor_tensor(out=gd[:, :, :], in0=Lb[:, :, 0:4], in1=gmax[:, :].unsqueeze(2).to_broadcast([128, NB, 4]),
                                               op=ALU.subtract), r=["Lb", "gmax"], w=["gd"])
        ohg = self.sb(st, "ohg", [128, NB, 4], F32)
        pr.op("dve", lambda e: e.tensor_scalar(out=ohg[:, :, :], in0=gd[:, :, :], scalar1=0.0, scalar2=None, op0=ALU.is_ge), r=["gd"], w=["ohg"])
        ge = self.sb(st, "ge", [128, NB, 4], F32)
        pr.op("act", lambda e: e.activation(out=ge[:, :, :], in_=gd[:, :, :], func=AF.Exp), r=["gd"], w=["ge"])
        gs = self.sb(st, "gs", [128, NB], F32)
        pr.op("dve", lambda e: e.tensor_reduce(out=gs[:, :], in_=ge[:, :, :], axis=AX.X, op=ALU.add), r=["ge"], w=["gs"])
        gw = self.sb(st, "gw", [128, NB], F32)
        pr.op("dve", lambda e: e.reciprocal(out=gw[:, :], in_=gs[:, :]), r=["gs"], w=["gw"])
        pen = self.sb(st, "pen", [128, NB, 4], F32)
        pr.op("dve", lambda e: e.tensor_scalar(out=pen[:, :, :], in0=ohg[:, :, :], scalar1=-1.0, scalar2=1.0e4, op0=ALU.add, op1=ALU.mult),
              r=["ohg"], w=["pen"])
        el = self.sb(st, "el", [128, NB, 32], F32)
        for g in range(4):
            pr.op("dve", lambda e, g=g: e.tensor_tensor(out=el[:, :, g * 8:(g + 1) * 8], in0=Lb[:, :, 4 + g * 8:4 + (g + 1) * 8],
                                                        in1=pen[:, :, g:g + 1].to_broadcast([128, NB, 8]), op=ALU.add),
                  r=["Lb", "pen"], w=["el"])
        v1 = self.sb(st, "v1", [128, NB], F32)
        pr.op("dve", lambda e: e.tensor_reduce(out=v1[:, :], in_=el[:, :, :], axis=AX.X, op=ALU.max), r=["el"], w=["v1"])
        oh1 = self.sb(st, "oh1", [128, NB, 32], F32)
        pr.op("dve", lambda e: e.tensor_tensor(out=oh1[:, :, :], in0=el[:, :, :], in1=v1[:, :].unsqueeze(2).to_broadcast([128, NB, 32]),
                                               op=ALU.is_ge), r=["el", "v1"], w=["oh1"])
        el2 = self.sb(st, "el2", [128, NB, 32], F32)
        pr.op("dve", lambda e: e.scalar_tensor_tensor(out=el2[:, :, :], in0=oh1[:, :, :], scalar=-1.0e4, in1=el[:, :, :],
                                                      op0=ALU.mult, op1=ALU.add), r=["oh1", "el"], w=["el2"])
        v2 = self.sb(st, "v2", [128, NB], F32)
        pr.op("dve", lambda e: e.tensor_reduce(out=v2[:, :], in_=el2[:, :, :], axis=AX.X, op=ALU.max), r=["el2"], w=["v2"])
        oh2 = self.sb(st, "oh2", [128, NB, 32], F32)
        pr.op("dve", lambda e: e.tensor_tensor(out=oh2[:, :, :], in0=el2[:, :, :], in1=v2[:, :].unsqueeze(2).to_broadcast([128, NB, 32]),
                                               op=ALU.is_ge), r=["el2", "v2"], w=["oh2"])
        rr = self.sb(st, "rr", [128, NB], F32)
        pr.op("dve", lambda e: e.tensor_tensor(out=rr[:, :], in0=v2[:, :], in1=v1[:, :], op=ALU.subtract), r=["v1", "v2"], w=["rr"])
        pr.op("act", lambda e: e.activation(out=rr[:, :], in_=rr[:, :], func=AF.Exp), r=["rr"], w=["rr"])
        W1, W2 = self.W1, self.W2
        pr.op("dve", lambda e: e.tensor_scalar(out=W1[:, :], in0=rr[:, :], scalar1=1.0, scalar2=None, op0=ALU.add), r=["rr"], w=["W1"])
        pr.op("dve", lambda e: e.reciprocal(out=W1[:, :], in_=W1[:, :]), r=["W1"], w=["W1"])
        pr.op("dve", lambda e: e.tensor_tensor(out=W1[:, :], in0=W1[:, :], in1=gw[:, :], op=ALU.mult), r=["W1", "gw"], w=["W1"])
        pr.op("dve", lambda e: e.tensor_tensor(out=W2[:, :], in0=W1[:, :], in1=rr[:, :], op=ALU.mult), r=["W1", "rr"], w=["W2"])
        cnt = el
        pr.op("dve", lambda e: e.tensor_tensor(out=cnt[:, :, :], in0=oh1[:, :, :], in1=oh2[:, :, :], op=ALU.add), r=["oh1", "oh2", "el2"], w=["el"])
        cntT = self.sb(st, "cntT", [32, T], F32)
        PSi = self.sb(st, "PSi", [32, T], F32)
        one32 = self.sb(st, "one32", [32, T], F32)
        pr.op("dve", lambda e: e.memset(one32[:, :], 1.0), w=["one32"])
        for blk in range(NB):
            bank = 4 + (blk % 2)
            pb = self.psb[bank]
            pr.op("pe", lambda e, blk=blk, pb=pb: e.matmul(pb[0:32, 0:128], lhsT=cnt[:, blk, :], rhs=self.ident[:, :], start=True, stop=True),
                  r=["el", "ident"], w=[("ps", bank)])
            pr.op("act", lambda e, blk=blk, pb=pb: e.activation(out=cntT[:, blk * 128:(blk + 1) * 128], in_=pb[0:32, 0:128], func=AF.Copy),
                  r=[("ps", bank)], w=[("cntT", blk)])
        allc = [("cntT", b) for b in range(NB)]
        pr.op("dve", lambda e: e.tensor_tensor_scan(out=PSi[:, :], data0=one32[:, :], data1=cntT[:, :], initial=0.0, op0=ALU.mult, op1=ALU.add),
              r=allc + ["one32"], w=["PSi"])
        kio = self.sb(st, "kio", [32, 164], F32)
        pr.dma("sync", lambda e: e.dma_start(out=kio[:, :], in_=self.k_iota[:, :]), w=["kio"])
        UT = self.sb(st, "UT", [32, 32], F32)
        pr.dma("sync", lambda e: e.dma_start(out=UT[:, :], in_=self.k_uincl[:, :]), w=["UT"])
        pidx = self.sb(st, "pidx", [128, 1], F32)
        pr.dma("sync", lambda e: e.dma_start(out=pidx[:, :], in_=self.k_pidx[:, :]), w=["pidx"])
        cmpb = self.sb(st, "cmpb", [32, 164], F32)
        pr.op("dve", lambda e: e.tensor_scalar(out=cmpb[:, 0:132], in0=kio[:, 0:132], scalar1=PSi[:, T - 1:T], scalar2=None, op0=ALU.is_lt),
              r=["kio", "PSi"], w=["cmpb"])
        padded = self.sb(st, "padded", [32, 1], F32)
        pr.op("dve", lambda e: e.tensor_reduce(out=padded[:, :], in_=cmpb[:, 0:132], axis=AX.X, op=ALU.add), r=["cmpb"], w=["padded"])
        pr.op("dve", lambda e: e.tensor_scalar(out=padded[:, :], in0=padded[:, :], scalar1=128.0, scalar2=None, op0=ALU.mult), r=["padded"], w=["padded"])
        pend = self.sb(st, "pend", [32, 1], F32)
        pstart = self.sb(st, "pstart", [32, 1], F32)
        pr.op("pe", lambda e: e.matmul(self.psb[6][0:32, 0:1], lhsT=UT[:, :], rhs=padded[:, :], start=True, stop=True), r=["UT", "padded"], w=[("ps", 6)])
        pr.op("dve", lambda e: e.tensor_copy(out=pend[:, :], in_=self.psb[6][0:32, 0:1]), r=[("ps", 6)], w=["pend"])
        pr.op("dve", lambda e: e.tensor_tensor(out=pstart[:, :], in0=pend[:, :], in1=padded[:, :], op=ALU.subtract), r=["pend", "padded"], w=["pstart"])
        pr.op("dve", lambda e: e.tensor_scalar(out=cmpb[:, :], in0=kio[:, :], scalar1=pend[:, 0:1], scalar2=None, op0=ALU.is_ge),
              r=["kio", "pend", "padded"], w=["cmpb"])
        pr.op("pe", lambda e: e.matmul(self.psb[7][:, 0:164], lhsT=self.ones[0:32, :], rhs=cmpb[:, :], start=True, stop=True), r=["cmpb", "ones"], w=[("ps", 7)])
        EBf = self.sb(st, "EBf", [128, 164], F32)
        pr.op("dve", lambda e: e.tensor_scalar(out=EBf[:, :], in0=self.psb[7][:, 0:164], scalar1=31.0, scalar2=128.0, op0=ALU.min, op1=ALU.mult),
              r=[("ps", 7)], w=["EBf"])
        pr.op("dve", lambda e: e.tensor_scalar(out=EBf[:, :], in0=EBf[:, :], scalar1=pidx[:, 0:1], scalar2=None, op0=ALU.add), r=["EBf", "pidx"], w=["EBf"])
        pr.op("dve", lambda e: e.tensor_copy(out=self.WIDX[:, :], in_=EBf[:, :]), r=["EBf"], w=["WIDX"])
        pr.op("dve", lambda e: e.tensor_tensor(out=PSi[:, :], in0=PSi[:, :], in1=cntT[:, :], op=ALU.subtract), r=["PSi", "cmpb"] + allc, w=["PSi"])
        pr.op("dve", lambda e: e.tensor_scalar(out=PSi[:, :], in0=PSi[:, :], scalar1=pstart[:, 0:1], scalar2=None, op0=ALU.add), r=["PSi", "pstart"], w=["PSi"])
        DT = el2
        for blk in range(NB):
            bank = 4 + (blk % 2)
            pb = self.psb[bank]
            pr.op("pe", lambda e, blk=blk, pb=pb: e.matmul(pb[:, 0:32], lhsT=PSi[:, blk * 128:(blk + 1) * 128], rhs=self.ident[0:32, 0:32], start=True, stop=True),
                  r=["PSi", "ident"], w=[("ps", bank)])
            pr.op("act", lambda e, blk=blk, pb=pb: e.activation(out=DT[:, blk, :], in_=pb[:, 0:32], func=AF.Copy), r=[("ps", bank), "oh2"], w=[("DT", blk)])
        allD = [("DT", b) for b in range(NB)]
        d1 = self.sb(st, "d1f", [128, NB], F32)
        pr.op("dve", lambda e: e.tensor_tensor(out=oh1[:, :, :], in0=oh1[:, :, :], in1=DT[:, :, :], op=ALU.mult), r=allD + ["oh1", "el"], w=["oh1"])
        pr.op("dve", lambda e: e.tensor_reduce(out=d1[:, :], in_=oh1[:, :, :], axis=AX.X, op=ALU.add), r=["oh1"], w=["d1f"])
        pr.op("dve", lambda e: e.tensor_copy(out=self.D1[:, :], in_=d1[:, :]), r=["d1f"], w=["D1"])
        pr.op("dve", lambda e: e.tensor_tensor(out=oh2[:, :, :], in0=oh2[:, :, :], in1=DT[:, :, :], op=ALU.mult), r=allD + ["oh2", "el"], w=["oh2"])
        pr.op("dve", lambda e: e.tensor_reduce(out=d1[:, :], in_=oh2[:, :, :], axis=AX.X, op=ALU.add), r=["oh2", "D1"], w=["d1f"])
        pr.op("dve", lambda e: e.tensor_copy(out=self.D2[:, :], in_=d1[:, :]), r=["d1f"], w=["D2"])

    def linear(self, st, xT, Wb, K, M, evac, tm_cols=None, tm_evac=None, NB=1024, tag="lin"):
        pr = self.pr
        kt = K // 128
        xb = [self.sb(st, f"{tag}x{i}", [128, kt, NB], BF16) for i in range(2)]
        wp = [self.sb(st, f"{tag}w{i}", [128, kt, 512], BF16) for i in range(2)]
        blocks = self.tok_tiles(NB)
        wi = 0
        bi = 0
        for bidx, (t0, n) in enumerate(blocks):
            xbuf = bidx % 2
            pr.dma("sync", lambda e, xbuf=xbuf, t0=t0, n=n: e.dma_start(
                out=xb[xbuf][:, :, 0:n], in_=xT[:, t0:t0 + n].rearrange("(k p) t -> p k t", p=128)), r=[(tag + "src",)], w=[(tag + "x", xbuf)])
            for c0 in range(0, M, 512):
                cw = min(512, M - c0)
                wbuf = wi % 2
                wi += 1
                pr.dma("sync", lambda e, wbuf=wbuf, c0=c0, cw=cw: e.dma_start(
                    out=wp[wbuf][:, :, 0:cw], in_=Wb[:, c0:c0 + cw].rearrange("(k p) c -> p k c", p=128)), r=[(tag + "wsrc",)], w=[(tag + "w", wbuf)])
                if tm_cols is not None and c0 in tm_cols:
                    for s in range(n // 128):
                        bank = bi % 4
                        bi += 1
                        pb = self.psb[bank]
                        for k in range(kt):
                            pr.op("pe", lambda e, xbuf=xbuf, wbuf=wbuf, k=k, s=s, cw=cw, pb=pb: e.matmul(
                                pb[:, 0:cw], lhsT=xb[xbuf][:, k, s * 128:(s + 1) * 128], rhs=wp[wbuf][:, k, 0:cw],
                                start=(k == 0), stop=(k == kt - 1)), r=[(tag + "x", xbuf), (tag + "w", wbuf)], w=[("ps", bank)])
                        tm_evac(c0, t0 + s * 128, pb[:, 0:cw], bank)
                    continue
                for mi in range(cw // 128):
                    for s0 in range(0, n, 512):
                        sn = min(512, n - s0)
                        bank = bi % 4
                        bi += 1
                        pb = self.psb[bank]
                        for k in range(kt):
                            pr.op("pe", lambda e, xbuf=xbuf, wbuf=wbuf, k=k, mi=mi, s0=s0, sn=sn, pb=pb: e.matmul(
                                pb[:, 0:sn], lhsT=wp[wbuf][:, k, mi * 128:(mi + 1) * 128], rhs=xb[xbuf][:, k, s0:s0 + sn],
                                start=(k == 0), stop=(k == kt - 1)), r=[(tag + "x", xbuf), (tag + "w", wbuf)], w=[("ps", bank)])
                        evac(c0 // 128 + mi, t0 + s0, sn, pb[:, 0:sn], bank)

    def phase_outproj(self, l, h_in, h_out):
        pr = self.pr
        j = l // 2
        wsrc = self.att_w_out if l % 2 == 0 else self.rec_w_out
        self.cast_w(wsrc[j], self.wb_out, D, D, "wbo")
        pr.flush(barrier=True)
        with contextlib.ExitStack() as st:
            ho = [self.sb(st, f"ho{i}", [128, 512], F32) for i in range(4)]
            cnt = [0]

            def evac(mt, t0, n, pap, bank):
                b = cnt[0] % 4
                cnt[0] += 1
                col = 1 if t0 < CTX else 0
                pr.dma("sync", lambda e: e.dma_start(out=ho[b][:, 0:n], in_=h_in[mt * 128:(mt + 1) * 128, t0:t0 + n]), w=[("ho", b)])
                gate = self.modv(l, 2, mt, col)
                pr.op("dve", lambda e: e.scalar_tensor_tensor(out=ho[b][:, 0:n], in0=pap, scalar=gate, in1=ho[b][:, 0:n],
                                                              op0=ALU.mult, op1=ALU.add), r=[("ps", bank), ("ho", b), "MOD"], w=[("ho", b)])
                pr.dma("pool", lambda e: e.dma_start(out=h_out[mt * 128:(mt + 1) * 128, t0:t0 + n], in_=ho[b][:, 0:n]), r=[("ho", b)], w=[("hout", mt, t0)])

            self.linear(st, self.catT, self.wb_out, D, D, evac, tag="op")
            pr.flush(barrier=True)

    def phase_moe(self, l, h_in, h_out):
        pr = self.pr
        IOA = bass.IndirectOffsetOnAxis
        NBLK = 164
        for e_ in range(NEXP):
            pr.dma("pool", lambda e, e_=e_: e.dma_start(out=self.wb_g2[e_ * 128:(e_ + 1) * 128, :].rearrange("p (k c) -> p k c", k=16),
                                                       in_=self.moe_wg[l, e_].rearrange("(k p) c -> p k c", p=128)), w=[("wbg", e_)])
            pr.dma("pool", lambda e, e_=e_: e.dma_start(out=self.wb_u2[e_ * 128:(e_ + 1) * 128, :].rearrange("p (k c) -> p k c", k=16),
                                                       in_=self.moe_wu[l, e_].rearrange("(k p) c -> p k c", p=128)), w=[("wbu", e_)])
            pr.dma("pool", lambda e, e_=e_: e.dma_start(out=self.wb_d2[e_ * 128:(e_ + 1) * 128, :].rearrange("p (k c) -> p k c", k=6),
                                                       in_=self.moe_wd[l, e_].rearrange("(k p) c -> p k c", p=128)), w=[("wbd", e_)])
        pr.flush(barrier=True)
        with contextlib.ExitStack() as st:
            xa = [self.sb(st, f"dxa{i}", [128, 16, 128], BF16) for i in range(2)]
            xt = [self.sb(st, f"dxt{i}", [128, D], BF16) for i in range(2)]
            zt = self.sb(st, "dzt", [128, D], BF16)
            pr.op("dve", lambda e: e.memset(zt[:, :], 0.0), w=["dzt"])
            for blk in range(NBLK):
                pr.dma("sync", lambda e, blk=blk: e.dma_start(out=self.XS[blk * 128:(blk + 1) * 128, :], in_=zt[:, :]), r=["dzt"], w=[("XSz", blk)])
            pr.flush(barrier=True)
            for tt in range(66):
                b = tt % 2
                pr.dma("sync", lambda e, b=b, tt=tt: e.dma_start(out=xa[b][:, :, :], in_=self.aT[:, tt * 128:(tt + 1) * 128].rearrange("(k p) t -> p k t", p=128)),
                       w=[("dxa", b)])
                for q in range(4):
                    bank = (tt * 4 + q) % 4
                    pb = self.psb[bank]
                    for kk in range(4):
                        k = q * 4 + kk
                        pr.op("pe", lambda e, b=b, k=k, kk=kk, pb=pb: e.matmul(pb[:, kk * 128:(kk + 1) * 128], lhsT=xa[b][:, k, :], rhs=self.identb[:, :], start=True, stop=True),
                              r=[("dxa", b), "identb"], w=[("ps", bank)])
                    pr.op("act" if q % 2 == 0 else "dve",
                          (lambda e, b=b, q=q, pb=pb: e.activation(out=xt[b][:, q * 512:(q + 1) * 512], in_=pb[:, :], func=AF.Copy)) if q % 2 == 0 else
                          (lambda e, b=b, q=q, pb=pb: e.tensor_copy(out=xt[b][:, q * 512:(q + 1) * 512], in_=pb[:, :])),
                          r=[("ps", bank)], w=[("dxt", b, q)])
                xk = [("dxt", b, q) for q in range(4)]
                pr.dma("pool", lambda e, b=b, tt=tt: e.indirect_dma_start(out=self.XS[:, :], out_offset=IOA(ap=self.D1[:, tt:tt + 1], axis=0),
                                                                        in_=xt[b][:, :], in_offset=None), r=xk + ["D1"], w=[("XS", tt, 0)])
                pr.dma("pool", lambda e, b=b, tt=tt: e.indirect_dma_start(out=self.XS[:, :], out_offset=IOA(ap=self.D2[:, tt:tt + 1], axis=0),
                                                                        in_=xt[b][:, :], in_offset=None), r=xk + ["D2"], w=[("XS", tt, 1)])
            pr.flush(barrier=True)
        with contextlib.ExitStack() as st:
            wg = [self.sb(st, f"swg{i}", [128, 16 * DEXP], BF16) for i in range(2)]
            wu = [self.sb(st, f"swu{i}", [128, 16 * DEXP], BF16) for i in range(2)]
            wd = [self.sb(st, f"swd{i}", [128, 6 * D], BF16) for i in range(2)]
            xs = [self.sb(st, f"sxs{i}", [128, D], BF16) for i in range(2)]
            xT = [self.sb(st, f"sxT{i}", [128, 16, 128], BF16) for i in range(2)]
            sg = [self.sb(st, f"ssg{i}", [128, DEXP], F32) for i in range(2)]
            hT = [self.sb(st, f"shT{i}", [128, 6, 128], BF16) for i in range(2)]
            hk = [self.sb(st, f"shk{i}", [128, DEXP], BF16) for i in range(2)]
            ysb = [self.sb(st, "sys0", [128, D], F32)] * 2
            bi = 0
            for blk in range(NBLK):
                b = blk % 2
                pr.dma("pool", lambda e, b=b, blk=blk: e.indirect_dma_start(out=wg[b][:, :], out_offset=None, in_=self.wb_g2[:, :],
                                                                          in_offset=IOA(ap=self.WIDX[:, blk:blk + 1], axis=0)), r=["WIDX"], w=[("swg", b)])
                pr.dma("pool", lambda e, b=b, blk=blk: e.indirect_dma_start(out=wu[b][:, :], out_offset=None, in_=self.wb_u2[:, :],
                                                                          in_offset=IOA(ap=self.WIDX[:, blk:blk + 1], axis=0)), r=["WIDX"], w=[("swu", b)])
                pr.dma("pool", lambda e, b=b, blk=blk: e.indirect_dma_start(out=wd[b][:, :], out_offset=None, in_=self.wb_d2[:, :],
                                                                          in_offset=IOA(ap=self.WIDX[:, blk:blk + 1], axis=0)), r=["WIDX"], w=[("swd", b)])
                pr.dma("sync", lambda e, b=b, blk=blk: e.dma_start(out=xs[b][:, :], in_=self.XS[blk * 128:(blk + 1) * 128, :]), w=[("sxs", b)])
                for q in range(4):
                    bank = q
                    pb = self.psb[bank]
                    for kk in range(4):
                        k = q * 4 + kk
                        pr.op("pe", lambda e, b=b, k=k, kk=kk, pb=pb: e.matmul(pb[:, kk * 128:(kk + 1) * 128], lhsT=xs[b][:, k * 128:(k + 1) * 128], rhs=self.identb[:, :], start=True, stop=True),
                              r=[("sxs", b), "identb"], w=[("ps", bank)])
                    if q % 2 == 0:
                        pr.op("act", lambda e, b=b, q=q, pb=pb: e.activation(out=xT[b][:, q * 4:(q + 1) * 4, :], in_=pb[:, :].rearrange("p (a t) -> p a t", a=4), func=AF.Copy),
                              r=[("ps", bank)], w=[("sxT", b, q)])
                    else:
                        pr.op("dve", lambda e, b=b, q=q, pb=pb: e.tensor_copy(out=xT[b][:, q * 4:(q + 1) * 4, :], in_=pb[:, :].rearrange("p (a t) -> p a t", a=4)),
                              r=[("ps", bank)], w=[("sxT", b, q)])
                xk = [("sxT", b, q) for q in range(4)]
                for k in range(16):
                    for (wsb, wkey, b0) in ((wg, "swg", 4), (wu, "swu", 6)):
                        for half in range(2):
                            pbk = b0 + half
                            pr.op("pe", lambda e, wsb=wsb, b=b, k=k, half=half, pbk=pbk: e.matmul(
                                self.psb[pbk][:, 0:384], lhsT=xT[b][:, k, :], rhs=wsb[b][:, k * DEXP + half * 384:k * DEXP + (half + 1) * 384],
                                start=(k == 0), stop=(k == 15)), r=[(wkey, b)] + xk, w=[("ps", pbk)])
                for half in range(2):
                    pr.op("act", lambda e, b=b, half=half: e.activation(out=sg[b][:, half * 384:(half + 1) * 384], in_=self.psb[4 + half][:, 0:384], func=AF.Silu),
                          r=[("ps", 4 + half)], w=[("ssg", b, half)])
                    pr.op("dve", lambda e, b=b, half=half: e.tensor_tensor(out=hk[b][:, half * 384:(half + 1) * 384], in0=sg[b][:, half * 384:(half + 1) * 384],
                                                                         in1=self.psb[6 + half][:, 0:384], op=ALU.mult),
                          r=[("ssg", b, half), ("ps", 6 + half)], w=[("shk", b, half)])
                for jt in range(6):
                    pbk = 4 if jt < 4 else 5
                    dst = self.psb[pbk][:, (jt % 4) * 128:(jt % 4 + 1) * 128]
                    pr.op("pe", lambda e, b=b, jt=jt, dst=dst: e.matmul(dst, lhsT=hk[b][:, jt * 128:(jt + 1) * 128], rhs=self.identb[:, :], start=True, stop=True),
                          r=[("shk", b, 0), ("shk", b, 1), "identb"], w=[("ps", pbk)])
                pr.op("act", lambda e, b=b: e.activation(out=hT[b][:, 0:4, :], in_=self.psb[4][:, :].rearrange("p (a t) -> p a t", a=4), func=AF.Copy),
                      r=[("ps", 4)], w=[("shT", b, 0)])
                pr.op("dve", lambda e, b=b: e.tensor_copy(out=hT[b][:, 4:6, :], in_=self.psb[5][:, 0:256].rearrange("p (a t) -> p a t", a=2)),
                      r=[("ps", 5)], w=[("shT", b, 1)])
                for c4 in range(4):
                    bank = c4
                    pb = self.psb[bank]
                    for k in range(6):
                        pr.op("pe", lambda e, b=b, k=k, c4=c4, pb=pb: e.matmul(pb[:, :], lhsT=hT[b][:, k, :], rhs=wd[b][:, k * D + c4 * 512:k * D + (c4 + 1) * 512],
                                                                             start=(k == 0), stop=(k == 5)), r=[("shT", b, 0), ("shT", b, 1), ("swd", b)], w=[("ps", bank)])
                    if c4 % 2 == 0:
                        pr.op("act", lambda e, b=b, c4=c4, pb=pb: e.activation(out=ysb[b][:, c4 * 512:(c4 + 1) * 512], in_=pb[:, :], func=AF.Copy),
                              r=[("ps", bank)], w=[("sys", b, c4)])
                    else:
                        pr.op("dve", lambda e, b=b, c4=c4, pb=pb: e.tensor_copy(out=ysb[b][:, c4 * 512:(c4 + 1) * 512], in_=pb[:, :]),
                              r=[("ps", bank)], w=[("sys", b, c4)])
                pr.dma("sync", lambda e, b=b, blk=blk: e.dma_start(out=self.YS[blk * 128:(blk + 1) * 128, :], in_=ysb[b][:, :]),
                       r=[("sys", b, c) for c in range(4)], w=[("YS", blk)])
            pr.flush(barrier=True)
        with contextlib.ExitStack() as st:
            y1 = [self.sb(st, f"cy1{i}", [128, D], F32) for i in range(2)]
            y2 = [self.sb(st, f"cy2{i}", [128, D], F32) for i in range(2)]
            ho = [self.sb(st, f"cho{i}", [128, 16, 128], F32) for i in range(2)]
            for tt in range(66):
                b = tt % 2
                col = 1 if tt < 2 else 0
                pr.dma("pool", lambda e, b=b, tt=tt: e.indirect_dma_start(out=y1[b][:, :], out_offset=None, in_=self.YS[:, :],
                                                                        in_offset=IOA(ap=self.D1[:, tt:tt + 1], axis=0)), r=["D1"], w=[("cy1", b)])
                pr.dma("pool", lambda e, b=b, tt=tt: e.indirect_dma_start(out=y2[b][:, :], out_offset=None, in_=self.YS[:, :],
                                                                        in_offset=IOA(ap=self.D2[:, tt:tt + 1], axis=0)), r=["D2"], w=[("cy2", b)])
                pr.dma("sync", lambda e, b=b, tt=tt: e.dma_start(out=ho[b][:, :, :], in_=h_in[:, tt * 128:(tt + 1) * 128].rearrange("(k p) t -> p k t", p=128)),
                       w=[("cho", b)])
                pr.op("dve", lambda e, b=b, tt=tt: e.tensor_scalar(out=y1[b][:, :], in0=y1[b][:, :], scalar1=self.W1[:, tt:tt + 1], scalar2=None, op0=ALU.mult),
                      r=[("cy1", b), "W1"], w=[("cy1", b)])
                pr.op("dve", lambda e, b=b, tt=tt: e.scalar_tensor_tensor(out=y1[b][:, :], in0=y2[b][:, :], scalar=self.W2[:, tt:tt + 1], in1=y1[b][:, :],
                                                                         op0=ALU.mult, op1=ALU.add), r=[("cy1", b), ("cy2", b), "W2"], w=[("cy1", b)])
                for k in range(16):
                    bank = k % 8
                    pb = self.psb[bank]
                    pr.op("pe", lambda e, b=b, k=k, pb=pb: e.matmul(pb[:, 0:128], lhsT=y1[b][:, k * 128:(k + 1) * 128], rhs=self.ident[:, :], start=True, stop=True),
                          r=[("cy1", b), "ident"], w=[("ps", bank)])
                    gate = self.modv(l, 5, k, col)
                    pr.op("dve", lambda e, b=b, k=k, pb=pb, gate=gate: e.scalar_tensor_tensor(out=ho[b][:, k, :], in0=pb[:, 0:128], scalar=gate, in1=ho[b][:, k, :],
                                                                                            op0=ALU.mult, op1=ALU.add), r=[("ps", bank), ("cho", b), "MOD"], w=[("cho", b)])
                pr.dma("sync", lambda e, b=b, tt=tt: e.dma_start(out=h_out[:, tt * 128:(tt + 1) * 128].rearrange("(k p) t -> p k t", p=128), in_=ho[b][:, :, :]),
                       r=[("cho", b)], w=[("hout2", tt)])
            pr.flush(barrier=True)

    def phase_att(self, l):
        pr = self.pr
        j = l // 2
        sa = 64 ** -0.5
        sn = 128 ** -0.5
        lam_init = 0.8 - 0.6 * math.exp(-0.3 * l)
        self.cast_w(self.att_w_in[j], self.wb_in[:, 0:6144], D, 6144, "wbi")
        pr.flush(barrier=True)
        with contextlib.ExitStack() as st:
            RT = self.sb(st, "RT", [128, 128], F32)
            pr.dma("sync", lambda e: e.dma_start(out=RT[:, :], in_=self.k_RT[:, :]), w=["RT"])
            xs = [self.sb(st, f"xs{i}", [128, 512], F32) for i in range(2)]
            tmp = [self.sb(st, f"tp{i}", [128, 512], F32) for i in range(2)]
            stg = [self.sb(st, f"stg{i}", [128, 512], BF16) for i in range(2)]
            cs = [self.sb(st, f"cs{i}", [128, 2, 512], F32) for i in range(2)]
            cnt = [0]

            def evac(mt, t0, n, pap, bank):
                grp, h = mt // 8, mt % 8
                row0 = {0: 0, 1: 1024, 3: 2048, 4: 3072}[grp] + h * 128
                scale = {0: sa, 1: 1.0, 3: sn, 4: 1.0}[grp]
                b = cnt[0] % 2
                cnt[0] += 1
                if grp >= 3 or t0 < CTX:
                    pr.op("act", lambda e: e.activation(out=stg[b][:, 0:n], in_=pap, func=AF.Copy, scale=scale), r=[("ps", bank)], w=[("stg", b)])
                else:
                    rb = 4 + b
                    pr.dma("sync", lambda e: e.dma_start(out=cs[b][:, 0, 0:n], in_=self.k_cos[:, t0 - CTX:t0 - CTX + n]), w=[("cs", b, 0)])
                    pr.dma("sync", lambda e: e.dma_start(out=cs[b][:, 1, 0:n], in_=self.k_sin[:, t0 - CTX:t0 - CTX + n]), w=[("cs", b, 1)])
                    pr.op("act", lambda e: e.activation(out=xs[b][:, 0:n], in_=pap, func=AF.Copy, scale=scale), r=[("ps", bank)], w=[("xs", b)])
                    pr.op("pe", lambda e: e.matmul(self.psb[rb][:, 0:n], lhsT=RT[:, :], rhs=xs[b][:, 0:n], start=True, stop=True),
                          r=[("xs", b), "RT"], w=[("ps", rb)])
                    pr.op("dve", lambda e: e.tensor_tensor(out=tmp[b][:, 0:n], in0=self.psb[rb][:, 0:n], in1=cs[b][:, 1, 0:n], op=ALU.mult),
                          r=[("ps", rb), ("cs", b, 1)], w=[("tp", b)])
                    pr.op("dve", lambda e: e.tensor_tensor(out=xs[b][:, 0:n], in0=xs[b][:, 0:n], in1=cs[b][:, 0, 0:n], op=ALU.mult),
                          r=[("xs", b), ("cs", b, 0)], w=[("xs", b)])
                    pr.op("dve", lambda e: e.tensor_tensor(out=stg[b][:, 0:n], in0=xs[b][:, 0:n], in1=tmp[b][:, 0:n], op=ALU.add),
                          r=[("xs", b), ("tp", b)], w=[("stg", b)])
                pr.dma("pool", lambda e: e.dma_start(out=self.QK[row0:row0 + 128, t0:t0 + n], in_=stg[b][:, 0:n]), r=[("stg", b)], w=[("QK", row0, t0)])

            def tm_evac(c0, t0, pap, bank):
                colo = c0 - 2048 if c0 < 3072 else c0 - 5120 + 1024
                b = cnt[0] % 2
                cnt[0] += 1
                pr.op("act", lambda e: e.activation(out=stg[b][:, :], in_=pap, func=AF.Copy), r=[("ps", bank)], w=[("stg", b)])
                pr.dma("pool", lambda e: e.dma_start(out=self.V[t0:t0 + 128, colo:colo + 512], in_=stg[b][:, :]), r=[("stg", b)], w=[("V", t0, colo)])

            self.linear(st, self.aT, self.wb_in[:, 0:6144], D, 6144, evac, tm_cols={2048, 2560, 5120, 5632}, tm_evac=tm_evac, tag="ip")
            pr.flush(barrier=True)
        with contextlib.ExitStack() as st:
            lrow = self.sb(st, "lrow", [1, 2, 2, 64], F32)
            pr.dma("sync", lambda e: e.dma_start(out=lrow[:, :, :, :], in_=self.att_lambda[j:j + 1, :].rearrange("o (a b c) -> o a b c", a=2, b=2)), w=["lrow"])
            lp = self.sb(st, "lp", [1, 2, 64], F32)
            pr.op("dve", lambda e: e.tensor_tensor(out=lp[:, :, :], in0=lrow[:, :, 0, :], in1=lrow[:, :, 1, :], op=ALU.mult), r=["lrow"], w=["lp"])
            l2 = self.sb(st, "l2", [1, 2], F32)
            pr.op("dve", lambda e: e.tensor_reduce(out=l2[:, :], in_=lp[:, :, :], axis=AX.X, op=ALU.add), r=["lp"], w=["l2"])
            pr.op("act", lambda e: e.activation(out=l2[:, :], in_=l2[:, :], func=AF.Exp), r=["l2"], w=["l2"])
            nl = self.sb(st, "nl", [1, 2], F32)
            pr.op("dve", lambda e: e.tensor_tensor(out=nl[:, 0:1], in0=l2[:, 1:2], in1=l2[:, 0:1], op=ALU.subtract), r=["l2"], w=["nl"])
            pr.op("dve", lambda e: e.tensor_scalar(out=nl[:, 0:1], in0=nl[:, 0:1], scalar1=-lam_init, scalar2=None, op0=ALU.add), r=["nl"], w=["nl"])
            nlam = self.sb(st, "nlam", [128, 1], F32)
            pr.op("pe", lambda e: e.matmul(self.psb[0][:, 0:1], lhsT=self.ones[0:1, :], rhs=nl[0:1, 0:1], start=True, stop=True),
                  r=["nl", "ones"], w=[("ps", 0)])
            pr.op("dve", lambda e: e.tensor_copy(out=nlam[:, :], in_=self.psb[0][:, 0:1]), r=[("ps", 0)], w=["nlam"])
            scl = self.sb(st, "scl", [128, 1], F32)
            self.transpose_rows(st, self.att_subln[j:j + 1, :], 1, scl[:, 0:1], "scl", bank=1)
            pr.op("dve", lambda e: e.tensor_scalar(out=scl[:, :], in0=scl[:, :], scalar1=1.0 - lam_init, scalar2=None, op0=ALU.mult), r=["scl"], w=["scl"])
            qT = self.sb(st, "qT", [128, T], BF16)
            kT = self.sb(st, "kT", [128, T], BF16)
            Vh = self.sb(st, "Vh", [128, 66, 128], BF16)
            P = [[self.sb(st, f"P{m}{i}", [128, 512], BF16) for i in range(2)] for m in range(2)]
            rD = [self.sb(st, f"rD{i}", [128, 512], F32) for i in range(2)]
            tA = self.sb(st, "tA", [128, 512], F32)
            tB = self.sb(st, "tB", [128, 512], F32)
            Dacc = [self.sb(st, f"Dacc{i}", [128, 512], F32) for i in range(2)]
            ost = [self.sb(st, f"ost{i}", [128, 512], BF16) for i in range(2)]
            oi = 0
            for h in range(8):
                pr.dma("sync", lambda e, h=h: e.dma_start(out=qT[:, :], in_=self.QK[h * 128:(h + 1) * 128, :]), w=["qT"])
                pr.dma("sync", lambda e, h=h: e.dma_start(out=kT[:, :], in_=self.QK[1024 + h * 128:1024 + (h + 1) * 128, :]), w=["kT"])
                pr.dma("sync", lambda e, h=h: e.dma_start(out=Vh[:, :, :], in_=self.V[:, h * 128:(h + 1) * 128].rearrange("(j p) c -> p j c", p=128)), w=["Vh"])
                qtiles = [(0, 256, 2)] + [(CTX + 512 * i, 512, 66) for i in range(16)]
                for (q0, qn_, nk) in qtiles:
                    def emit_qk_pair(kt, q0=q0, qn_=qn_):
                        for m in range(2):
                            sbk = m * 2 + (kt % 2)
                            pb = self.psb[sbk]
                            pr.op("pe", lambda e, m=m, kt=kt, pb=pb: e.matmul(
                                pb[:, 0:qn_], lhsT=kT[64 * m:64 * m + 64, kt * 128:(kt + 1) * 128], rhs=qT[64 * m:64 * m + 64, q0:q0 + qn_],
                                start=True, stop=True), r=["kT", "qT"], w=[("ps", sbk)])
                        for m in range(2):
                            sbk = m * 2 + (kt % 2)
                            pb = self.psb[sbk]
                            Pm = P[m][kt % 2]
                            pr.op("act", lambda e, pb=pb, Pm=Pm: e.activation(out=Pm[:, 0:qn_], in_=pb[:, 0:qn_], func=AF.Exp),
                                  r=[("ps", sbk)], w=[("P", m, kt % 2)])

                    def emit_av(kt, m, qn_=qn_, nk=nk):
                        Pm = P[m][kt % 2]
                        pr.op("pe", lambda e, m=m, kt=kt, Pm=Pm: e.matmul(
                            self.psb[4 + m][:, 0:qn_], lhsT=Vh[:, kt, :], rhs=Pm[:, 0:qn_], start=(kt == 0), stop=(kt == nk - 1)),
                            r=[("P", m, kt % 2), "Vh"], w=[("ps", 4 + m)])
                        aeng = "dve" if m == 0 else "pool"
                        if kt == 0:
                            pr.op(aeng, lambda e, m=m, Pm=Pm: e.tensor_copy(out=Dacc[m][:, 0:qn_], in_=Pm[:, 0:qn_]),
                                  r=[("P", m, kt % 2)], w=[("Dacc", m)])
                        else:
                            pr.op(aeng, lambda e, m=m, Pm=Pm: e.tensor_tensor(out=Dacc[m][:, 0:qn_], in0=Dacc[m][:, 0:qn_], in1=Pm[:, 0:qn_], op=ALU.add),
                                  r=[("P", m, kt % 2), ("Dacc", m)], w=[("Dacc", m)])

                    emit_qk_pair(0)
                    for kt in range(nk):
                        if kt + 1 < nk:
                            emit_qk_pair(kt + 1)
                        emit_av(kt, 0)
                        emit_av(kt, 1)
                    for m in range(2):
                        pr.op("pe", lambda e, m=m, qn_=qn_: e.matmul(self.psb[6 + m][:, 0:qn_], lhsT=self.ones[:, :], rhs=Dacc[m][:, 0:qn_], start=True, stop=True),
                              r=[("Dacc", m), "ones"], w=[("ps", 6 + m)])
                    n = qn_
                    pr.op("dve", lambda e, n=n: e.reciprocal(out=rD[0][:, 0:n], in_=self.psb[6][:, 0:n]), r=[("ps", 6)], w=[("rD", 0)])
                    pr.op("dve", lambda e, n=n: e.reciprocal(out=rD[1][:, 0:n], in_=self.psb[7][:, 0:n]), r=[("ps", 7)], w=[("rD", 1)])
                    pr.op("dve", lambda e, n=n: e.tensor_tensor(out=tA[:, 0:n], in0=self.psb[4][:, 0:n], in1=rD[0][:, 0:n], op=ALU.mult),
                          r=[("ps", 4), ("rD", 0)], w=["tA"])
                    pr.op("dve", lambda e, n=n: e.tensor_tensor(out=tB[:, 0:n], in0=self.psb[5][:, 0:n], in1=rD[1][:, 0:n], op=ALU.mult),
                          r=[("ps", 5), ("rD", 1)], w=["tB"])
                    pr.op("dve", lambda e, n=n: e.scalar_tensor_tensor(out=tA[:, 0:n], in0=tB[:, 0:n], scalar=nlam[:, 0:1], in1=tA[:, 0:n],
                                                                     op0=ALU.mult, op1=ALU.add), r=["tA", "tB", "nlam"], w=["tA"])
                    pr.op("act", lambda e, n=n: e.activation(out=tB[:, 0:n], in_=tA[:, 0:n], func=AF.Square), r=["tA"], w=["tB"])
                    pr.op("pe", lambda e, n=n: e.matmul(self.psb[0][:, 0:n], lhsT=self.ones[:, :], rhs=tB[:, 0:n], start=True, stop=True),
                          r=["tB", "ones"], w=[("ps", 0)])
                    pr.op("dve", lambda e, n=n: e.tensor_scalar(out=rD[0][:, 0:n], in0=self.psb[0][:, 0:n], scalar1=1.0 / 128, scalar2=EPS,
                                                              op0=ALU.mult, op1=ALU.add), r=[("ps", 0)], w=[("rD", 0)])
                    pr.op("act", lambda e, n=n: e.activation(out=rD[0][:, 0:n], in_=rD[0][:, 0:n], func=AF.Sqrt), r=[("rD", 0)], w=[("rD", 0)])
                    pr.op("dve", lambda e, n=n: e.reciprocal(out=rD[0][:, 0:n], in_=rD[0][:, 0:n]), r=[("rD", 0)], w=[("rD", 0)])
                    pr.op("dve", lambda e, n=n: e.tensor_tensor(out=tA[:, 0:n], in0=tA[:, 0:n], in1=rD[0][:, 0:n], op=ALU.mult),
                          r=["tA", ("rD", 0)], w=["tA"])
                    ob = oi % 2
                    oi += 1
                    pr.op("dve", lambda e, n=n, ob=ob: e.tensor_scalar(out=ost[ob][:, 0:n], in0=tA[:, 0:n], scalar1=scl[:, 0:1], scalar2=None, op0=ALU.mult),
                          r=["tA", "scl"], w=[("ost", ob)])
                    pr.dma("pool", lambda e, n=n, ob=ob, h=h, q0=q0: e.dma_start(out=self.catT[h * 128:(h + 1) * 128, q0:q0 + n], in_=ost[ob][:, 0:n]),
                           r=[("ost", ob)], w=[("catT", h, q0)])
                pr.flush()
            pr.flush(barrier=True)
        with contextlib.ExitStack() as st:
            qT = self.sb(st, "nqT", [128, T], BF16)
            kT = self.sb(st, "nkT", [128, T], BF16)
            Vh = self.sb(st, "nVh", [128, 66, 128], BF16)
            naT = self.sb(st, "naT", [128, T], BF16)
            Bt = self.sb(st, "Bt", [128, 5, 5, 128], F32)
            Ssb = [self.sb(st, f"Ssb{i}", [128, 640], F32) for i in range(2)]
            P = [self.sb(st, f"nP{i}", [128, 896], BF16) for i in range(2)]
            rD = [self.sb(st, f"nrD{i}", [128, 128], F32) for i in range(2)]
            for h in range(8):
                pr.dma("sync", lambda e, h=h: e.dma_start(out=qT[:, :], in_=self.QK[2048 + h * 128:2048 + (h + 1) * 128, :]), w=["nqT"])
                pr.dma("sync", lambda e, h=h: e.dma_start(out=kT[:, :], in_=self.QK[3072 + h * 128:3072 + (h + 1) * 128, :]), w=["nkT"])
                pr.dma("sync", lambda e, h=h: e.dma_start(out=Vh[:, :, :], in_=self.V[:, 1024 + h * 128:1024 + (h + 1) * 128].rearrange("(j p) c -> p j c", p=128)), w=["nVh"])
                pr.dma("sync", lambda e, h=h: e.dma_start(out=Bt[:, :, :, :], in_=self.na_bias[j, h].rearrange("p (a b q) -> p a b q", a=5, b=5)), w=["Bt"])
                for qi in range(66):
                    b = qi % 2
                    bA, bB = (0, 1) if b == 0 else (2, 3)
                    if qi < 2:
                        q0 = qi * 128
                        keys = [0, 128]
                        cls = None
                    else:
                        jq = qi - 2
                        q0 = CTX + jq * 128
                        kbase = int(np.clip(2 * jq - 4, 0, 118))
                        k0 = CTX + kbase * 64
                        keys = [0, 128] + [k0 + 128 * i for i in range(5)]
                        cls = _na_class(jq)
                    nkk = len(keys)
                    for i, ks in enumerate(keys):
                        dst = self.psb[bA][:, i * 128:(i + 1) * 128] if i < 4 else self.psb[bB][:, (i - 4) * 128:(i - 3) * 128]
                        pr.op("pe", lambda e, ks=ks, q0=q0, dst=dst: e.matmul(dst, lhsT=kT[:, ks:ks + 128], rhs=qT[:, q0:q0 + 128], start=True, stop=True),
                              r=["nkT", "nqT"], w=[("ps", bA if i < 4 else bB)])
                    Pb = P[b]
                    pr.op("act", lambda e, Pb=Pb, bA=bA: e.activation(out=Pb[:, 0:256], in_=self.psb[bA][:, 0:256], func=AF.Exp),
                          r=[("ps", bA)], w=[("nP", b, 0)])
                    if cls is not None:
                        pr.op("dve", lambda e, b=b, bA=bA, cls=cls: e.tensor_tensor(
                            out=Ssb[b][:, 0:256], in0=self.psb[bA][:, 256:512], in1=Bt[:, cls, 0:2, :].rearrange("p a q -> p (a q)"), op=ALU.add),
                            r=[("ps", bA), "Bt"], w=[("Ssb", b, 0)])
                        pr.op("dve", lambda e, b=b, bB=bB, cls=cls: e.tensor_tensor(
                            out=Ssb[b][:, 256:640], in0=self.psb[bB][:, 0:384], in1=Bt[:, cls, 2:5, :].rearrange("p a q -> p (a q)"), op=ALU.add),
                            r=[("ps", bB), "Bt"], w=[("Ssb", b, 1)])
                        pr.op("act", lambda e, Pb=Pb, b=b: e.activation(out=Pb[:, 256:896], in_=Ssb[b][:, 0:640], func=AF.Exp),
                              r=[("Ssb", b, 0), ("Ssb", b, 1)], w=[("nP", b, 1)])
                    for i, ks in enumerate(keys):
                        pr.op("pe", lambda e, i=i, ks=ks, Pb=Pb, b=b, nkk=nkk: e.matmul(
                            self.psb[4 + b][:, 0:128], lhsT=Vh[:, ks // 128, :], rhs=Pb[:, i * 128:(i + 1) * 128], start=(i == 0), stop=(i == nkk - 1)),
                            r=[("nP", b, 0), ("nP", b, 1), "nVh"], w=[("ps", 4 + b)])
                    for i, ks in enumerate(keys):
                        pr.op("pe", lambda e, i=i, Pb=Pb, b=b, nkk=nkk: e.matmul(
                            self.psb[6 + b][:, 0:128], lhsT=self.onesb[:, :], rhs=Pb[:, i * 128:(i + 1) * 128], start=(i == 0), stop=(i == nkk - 1)),
                            r=[("nP", b, 0), ("nP", b, 1), "onesb"], w=[("ps", 6 + b)])
                    pr.op("dve", lambda e, b=b: e.reciprocal(out=rD[b][:, :], in_=self.psb[6 + b][:, 0:128]), r=[("ps", 6 + b)], w=[("nrD", b)])
                    pr.op("dve", lambda e, b=b, q0=q0: e.tensor_tensor(out=naT[:, q0:q0 + 128], in0=self.psb[4 + b][:, 0:128], in1=rD[b][:, :], op=ALU.mult),
                          r=[("ps", 4 + b), ("nrD", b)], w=[("naT", q0)])
                pr.dma("pool", lambda e, h=h: e.dma_start(out=self.catT[1024 + h * 128:1024 + (h + 1) * 128, :], in_=naT[:, :]),
                       r=[("naT", qq) for qq in range(0, T, 128)], w=[("catTn", h)])
                pr.flush()
            pr.flush(barrier=True)

    def phase_rec(self, l):
        pr = self.pr
        j = l // 2
        if not hasattr(self, "GL"):
            self.GL = self.dint("GL", [2 * D, T], F32)
        self.cast_w(self.rec_w_in[j], self.wb_in, D, 5 * D, "wbi")
        pr.flush(barrier=True)
        SEG = 2112
        NCH = 33
        with contextlib.ExitStack() as st0:
            LBL = self.sb(st0, "LBL", [128, 128], F32)
            self.transpose_rows(st0, self.rec_lb[:, :], 128, LBL[:, :], "LBL", bank=0)
            pr.op("act", lambda e: e.activation(out=LBL[:, :], in_=LBL[:, :], func=AF.Exp), r=["LBL"], w=["LBL"])
            den = self.sb(st0, "lbden", [128, 32], F32)
            num = self.sb(st0, "lbnum", [128, 32], F32)
            pr.op("dve", lambda e: e.tensor_tensor(out=den[:, :], in0=LBL[:, 0:32], in1=LBL[:, 32:64], op=ALU.add), r=["LBL"], w=["lbden"])
            pr.op("dve", lambda e: e.tensor_tensor(out=den[:, :], in0=den[:, :], in1=LBL[:, 64:96], op=ALU.add), r=["LBL", "lbden"], w=["lbden"])
            pr.op("dve", lambda e: e.tensor_tensor(out=den[:, :], in0=den[:, :], in1=LBL[:, 96:128], op=ALU.add), r=["LBL", "lbden"], w=["lbden"])
            pr.op("dve", lambda e: e.tensor_copy(out=num[:, :], in_=LBL[:, 32:64]), r=["LBL"], w=["lbnum"])
            for lp in range(2, l + 1):
                pr.op("dve", lambda e, lp=lp: e.tensor_tensor(out=num[:, :], in0=num[:, :], in1=LBL[:, lp * 32:(lp + 1) * 32], op=ALU.add),
                      r=["LBL", "lbnum"], w=["lbnum"])
            LB = self.sb(st0, "LB", [128, 32], F32)
            OML = self.sb(st0, "OML", [128, 32], F32)
            pr.op("dve", lambda e: e.reciprocal(out=den[:, :], in_=den[:, :]), r=["lbden"], w=["lbden"])
            pr.op("dve", lambda e: e.tensor_tensor(out=LB[:, :], in0=num[:, :], in1=den[:, :], op=ALU.mult), r=["lbnum", "lbden"], w=["LB"])
            pr.op("dve", lambda e: e.tensor_scalar(out=OML[:, :], in0=LB[:, :], scalar1=-1.0, scalar2=1.0, op0=ALU.mult, op1=ALU.add), r=["LB"], w=["OML"])
            gnv = self.sb(st0, "gnv", [128, 1], F32)
            self.transpose_rows(st0, self.rec_gn[j:j + 1, :], 1, gnv[:, 0:1], "gnv", bank=1)
            with contextlib.ExitStack() as st:
                xs = [self.sb(st, f"rxs{i}", [128, 512], F32) for i in range(2)]
                tmp = [self.sb(st, f"rtp{i}", [128, 512], F32) for i in range(2)]
                stg = [self.sb(st, f"rstg{i}", [128, 512], BF16) for i in range(2)]
                cnt = [0]

                def evac(mt, t0, n, pap, bank):
                    grp, hh = mt // 16, mt % 16
                    b = cnt[0] % 2
                    cnt[0] += 1
                    if grp == 0:
                        pr.op("act", lambda e: e.activation(out=xs[b][:, 0:n], in_=pap, func=AF.Silu), r=[("ps", bank)], w=[("rxs", b)])
                        pr.op("dve", lambda e: e.tensor_scalar(out=stg[b][:, 0:n], in0=xs[b][:, 0:n], scalar1=128 ** -0.5, scalar2=None, op0=ALU.mult),
                              r=[("rxs", b)], w=[("rstg", b)])
                        row0 = hh * 128
                    elif grp == 4:
                        pr.op("act", lambda e: e.activation(out=stg[b][:, 0:n], in_=pap, func=AF.Silu), r=[("ps", bank)], w=[("rstg", b)])
                        row0 = 3 * D + hh * 128
                    else:
                        d = grp - 2
                        ci = d * 16 + hh
                        pr.op("act", lambda e: e.activation(out=xs[b][:, 0:n], in_=pap, func=AF.Sigmoid), r=[("ps", bank)], w=[("rxs", b)])
                        pr.op("dve", lambda e: e.tensor_scalar(out=xs[b][:, 0:n], in0=xs[b][:, 0:n], scalar1=OML[:, ci:ci + 1], scalar2=LB[:, ci:ci + 1],
                                                             op0=ALU.mult, op1=ALU.add), r=[("rxs", b), "OML", "LB"], w=[("rxs", b)])
                        pr.op("act", lambda e: e.activation(out=tmp[b][:, 0:n], in_=xs[b][:, 0:n], func=AF.Ln), r=[("rxs", b)], w=[("rtp", b)])
                        g0 = d * D + hh * 128
                        pr.dma("pool", lambda e: e.dma_start(out=self.GL[g0:g0 + 128, t0:t0 + n], in_=tmp[b][:, 0:n]), r=[("rtp", b)], w=[("GL", g0, t0)])
                        pr.op("dve", lambda e: e.tensor_scalar(out=stg[b][:, 0:n], in0=xs[b][:, 0:n], scalar1=-1.0, scalar2=1.0, op0=ALU.mult, op1=ALU.add),
                              r=[("rxs", b)], w=[("rstg", b)])
                        row0 = D + d * D + hh * 128
                    pr.dma("pool", lambda e: e.dma_start(out=self.QK[row0:row0 + 128, t0:t0 + n], in_=stg[b][:, 0:n]), r=[("rstg", b)], w=[("QK", row0, t0)])

                def tm_evac(c0, t0, pap, bank):
                    colo = c0 - D
                    b = cnt[0] % 2
                    cnt[0] += 1
                    pr.op("act", lambda e: e.activation(out=stg[b][:, :], in_=pap, func=AF.Copy), r=[("ps", bank)], w=[("rstg", b)])
                    pr.dma("pool", lambda e: e.dma_start(out=self.V[t0:t0 + 128, colo:colo + 512], in_=stg[b][:, :]), r=[("rstg", b)], w=[("V", t0, colo)])

                self.linear(st, self.aT, self.wb_in, D, 5 * D, evac, tm_cols={2048, 2560, 3072, 3584}, tm_evac=tm_evac, tag="rp")
                pr.flush(barrier=True)
            with contextlib.ExitStack() as st:
                mask = self.sb(st, "smask", [128, SEG], F32)
                pr.op("dve", lambda e: e.memset(mask[:, :], 1.0), w=["smask"])
                pr.op("dve", lambda e: e.memset(mask[:, :].rearrange("p (n c) -> p n c", c=64)[:, :, 0:1], 0.0), r=["smask"], w=["smask"])
                mk = [self.sb(st, "m128f", [128, 128], F32), self.sb(st, "m128b", [128, 128], F32)]
                pr.dma("sync", lambda e: e.dma_start(out=mk[0][:, :], in_=self.k_m128f[:, :]), w=["m128f"])
                pr.dma("sync", lambda e: e.dma_start(out=mk[1][:, :], in_=self.k_m128b[:, :]), w=["m128b"])
                gs = self.sb(st, "gs", [128, SEG], F32)
                Cs = self.sb(st, "Cs", [128, SEG], F32)
                As = self.sb(st, "As", [128, SEG], F32)
                Es = self.sb(st, "Es", [128, SEG], F32)
                qss = self.sb(st, "qss", [128, SEG], BF16)
                kks = self.sb(st, "kks", [128, SEG], BF16)
                qt_ = self.sb(st, "qt_", [128, T], BF16)
                kt_ = self.sb(st, "kt_", [128, T], BF16)
                kh_ = self.sb(st, "kh_", [128, T], BF16)
                qst = self.sb(st, "qst", [128, T], BF16)
                dsb = self.sb(st, "dsb", [128, 132], F32)
                Vh = self.sb(st, "rVh", [128, 66, 128], BF16)
                oT = self.sb(st, "oT", [128, T], F32)
                S = [self.sb(st, f"S{i}", [128, 128], F32) for i in range(2)]
                Sb = [self.sb(st, f"Sb{i}", [128, 128], BF16) for i in range(2)]
                attm = [self.sb(st, f"attm{i}", [128, 128], BF16) for i in range(2)]
                khT = [self.sb(st, f"khT{i}", [128, 128], BF16) for i in range(2)]
                rr = self.sb(st, "rrs", [128, 512], F32)
                gt = self.sb(st, "rgt", [128, 512], BF16)
                ost = [self.sb(st, f"rost{i}", [128, 512], BF16) for i in range(2)]
                v3 = lambda a: a[:, :].rearrange("p (n c) -> p n c", c=64)
                oi = 0
                for hh in range(16):
                    pr.dma("sync", lambda e, hh=hh: e.dma_start(out=Vh[:, :, :], in_=self.V[:, hh * 128:(hh + 1) * 128].rearrange("(j p) c -> p j c", p=128)), w=["rVh"])
                    for d in range(2):
                        mi, ei = (31, 63) if d == 0 else (32, 0)
                        for sg_ in range(4):
                            t0 = sg_ * SEG
                            g0 = d * D + hh * 128
                            k0 = D + d * D + hh * 128
                            pr.dma("sync", lambda e, g0=g0, t0=t0: e.dma_start(out=gs[:, :], in_=self.GL[g0:g0 + 128, t0:t0 + SEG]), w=["gs"])
                            pr.dma("sync", lambda e, hh=hh, t0=t0: e.dma_start(out=qss[:, :], in_=self.QK[hh * 128:(hh + 1) * 128, t0:t0 + SEG]), w=["qss"])
                            pr.dma("sync", lambda e, k0=k0, t0=t0: e.dma_start(out=kks[:, :], in_=self.QK[k0:k0 + 128, t0:t0 + SEG]), w=["kks"])
                            pr.op("dve", lambda e: e.tensor_tensor_scan(out=Cs[:, :], data0=mask[:, :], data1=gs[:, :], initial=0.0, op0=ALU.mult, op1=ALU.add),
                                  r=["gs", "smask"], w=["Cs"])
                            if d == 1:
                                pr.op("dve", lambda e: e.tensor_tensor(out=v3(As), in0=v3(Cs)[:, :, 63:64].to_broadcast([128, NCH, 64]), in1=v3(Cs), op=ALU.subtract),
                                      r=["Cs"], w=["As"])
                                pr.op("dve", lambda e: e.tensor_tensor(out=Cs[:, :], in0=As[:, :], in1=gs[:, :], op=ALU.add), r=["As", "gs"], w=["Cs"])
                            pr.op("dve", lambda e, mi=mi: e.tensor_tensor(out=v3(As), in0=v3(Cs), in1=v3(Cs)[:, :, mi:mi + 1].to_broadcast([128, NCH, 64]), op=ALU.subtract),
                                  r=["Cs"], w=["As"])
                            pr.op("act", lambda e: e.activation(out=Es[:, :], in_=As[:, :], func=AF.Exp), r=["As"], w=["Es"])
                            pr.op("dve", lambda e, t0=t0: e.tensor_tensor(out=qt_[:, t0:t0 + SEG], in0=qss[:, :], in1=Es[:, :], op=ALU.mult), r=["qss", "Es"], w=[("qt_", sg_)])
                            pr.op("act", lambda e: e.activation(out=Es[:, :], in_=As[:, :], func=AF.Exp, scale=-1.0), r=["As"], w=["Es"])
                            pr.op("dve", lambda e, t0=t0: e.tensor_tensor(out=kt_[:, t0:t0 + SEG], in0=kks[:, :], in1=Es[:, :], op=ALU.mult), r=["kks", "Es"], w=[("kt_", sg_)])
                            pr.op("dve", lambda e, ei=ei: e.tensor_tensor(out=v3(As), in0=v3(Cs)[:, :, ei:ei + 1].to_broadcast([128, NCH, 64]), in1=v3(Cs), op=ALU.subtract),
                                  r=["Cs"], w=["As"])
                            pr.op("act", lambda e: e.activation(out=Es[:, :], in_=As[:, :], func=AF.Exp), r=["As"], w=["Es"])
                            pr.op("dve", lambda e, t0=t0: e.tensor_tensor(out=kh_[:, t0:t0 + SEG], in0=kks[:, :], in1=Es[:, :], op=ALU.mult), r=["kks", "Es"], w=[("kh_", sg_)])
                            pr.op("act", lambda e: e.activation(out=Es[:, :], in_=Cs[:, :], func=AF.Exp), r=["Cs"], w=["Es"])
                            pr.op("dve", lambda e, t0=t0: e.tensor_tensor(out=qst[:, t0:t0 + SEG], in0=qss[:, :], in1=Es[:, :], op=ALU.mult), r=["qss", "Es"], w=[("qst", sg_)])
                            pr.op("dve", lambda e, sg_=sg_, ei=ei: e.tensor_copy(out=dsb[:, sg_ * NCH:(sg_ + 1) * NCH], in_=v3(Es)[:, :, ei]), r=["Es"], w=[("dsb", sg_)])
                        allk = [(nm, q) for nm in ("qt_", "kt_", "kh_", "qst", "dsb") for q in range(4)]
                        pr.op("dve", lambda e: e.memset(S[0][:, :], 0.0), w=[("S", 0)])
                        pr.op("dve", lambda e: e.memset(Sb[0][:, :], 0.0), w=[("Sb", 0)])
                        cur = 0
                        if d == 0:
                            tiles = [(tt, (0, 1)) for tt in range(66)]
                        else:
                            tiles = [(1, (1, 0)), (0, (1, 0))] + [(tt, (1, 0)) for tt in range(65, 1, -1)]
                        for ti, (tt, corder) in enumerate(tiles):
                            ab = ti % 2
                            tl = tt * 128
                            pr.op("pe", lambda e, ab=ab, tl=tl: e.matmul(self.psb[ab][:, 0:128], lhsT=kt_[:, tl:tl + 128], rhs=qt_[:, tl:tl + 128], start=True, stop=True),
                                  r=allk, w=[("ps", ab)])
                            pr.op("dve", lambda e, ab=ab, d=d: e.tensor_tensor(out=attm[ab][:, :], in0=self.psb[ab][:, 0:128], in1=mk[d][:, :], op=ALU.mult),
                                  r=[("ps", ab), "m128f", "m128b"], w=[("attm", ab)])
                            pr.op("pe", lambda e, ab=ab, tl=tl: e.matmul(self.psb[2 + ab][:, 0:128], lhsT=kh_[:, tl:tl + 128], rhs=self.identb[:, :], start=True, stop=True),
                                  r=allk + ["identb"], w=[("ps", 2 + ab)])
                            pr.op("act", lambda e, ab=ab: e.activation(out=khT[ab][:, :], in_=self.psb[2 + ab][:, 0:128], func=AF.Copy), r=[("ps", 2 + ab)], w=[("khT", ab)])
                            po = self.psb[6 + ab]
                            for c in corder:
                                n_ = tt * 2 + c
                                cl = slice(64 * c, 64 * c + 64)
                                pdb = 4 + (n_ % 2)
                                pr.op("pe", lambda e, ab=ab, tt=tt, cl=cl, po=po: e.matmul(po[:, cl], lhsT=Vh[cl, tt, :], rhs=attm[ab][cl, cl], start=True, stop=False),
                                      r=["rVh", ("attm", ab)], w=[("ps", 6 + ab)])
                                pr.op("pe", lambda e, cur=cur, tl=tl, c=c, cl=cl, po=po: e.matmul(po[:, cl], lhsT=Sb[cur][:, :], rhs=qst[:, tl + 64 * c:tl + 64 * c + 64], start=False, stop=True),
                                      r=[("Sb", cur)] + allk, w=[("ps", 6 + ab)])
                                pr.op("pe", lambda e, ab=ab, tt=tt, cl=cl, pdb=pdb: e.matmul(self.psb[pdb][:, 0:128], lhsT=khT[ab][cl, :], rhs=Vh[cl, tt, :], start=True, stop=True),
                                      r=[("khT", ab), "rVh"], w=[("ps", pdb)])
                                nx = 1 - cur
                                pr.op("dve", lambda e, cur=cur, nx=nx, n_=n_, pdb=pdb: e.scalar_tensor_tensor(
                                    out=S[nx][:, :], in0=S[cur][:, :], scalar=dsb[:, n_:n_ + 1], in1=self.psb[pdb][:, 0:128], op0=ALU.mult, op1=ALU.add),
                                    r=[("S", cur), ("ps", pdb)] + [("dsb", q) for q in range(4)], w=[("S", nx)])
                                pr.op("act", lambda e, nx=nx: e.activation(out=Sb[nx][:, :], in_=S[nx][:, :], func=AF.Copy), r=[("S", nx)], w=[("Sb", nx)])
                                cur = nx
                            if d == 0:
                                pr.op("act", lambda e, tl=tl, po=po: e.activation(out=oT[:, tl:tl + 128], in_=po[:, 0:128], func=AF.Copy), r=[("ps", 6 + ab)], w=[("oT", tt)])
                            else:
                                pr.op("dve", lambda e, tl=tl, po=po: e.tensor_tensor(out=oT[:, tl:tl + 128], in0=oT[:, tl:tl + 128], in1=po[:, 0:128], op=ALU.add),
                                      r=[("ps", 6 + ab), ("oT", tt)], w=[("oT", tt)])
                        pr.flush()
                    for (t0, n) in self.tok_tiles(512):
                        okeys = [("oT", tt) for tt in range(t0 // 128, (t0 + n) // 128)]
                        g0 = 3 * D + hh * 128
                        pr.dma("sync", lambda e, g0=g0, t0=t0, n=n: e.dma_start(out=gt[:, 0:n], in_=self.QK[g0:g0 + 128, t0:t0 + n]), w=["rgt"])
                        pr.op("act", lambda e, t0=t0, n=n: e.activation(out=rr[:, 0:n], in_=oT[:, t0:t0 + n], func=AF.Square), r=okeys, w=["rrs"])
                        pr.op("pe", lambda e, n=n: e.matmul(self.psb[0][:, 0:n], lhsT=self.ones[:, :], rhs=rr[:, 0:n], start=True, stop=True), r=["rrs", "ones"], w=[("ps", 0)])
                        pr.op("dve", lambda e, n=n: e.tensor_scalar(out=rr[:, 0:n], in0=self.psb[0][:, 0:n], scalar1=1.0 / 128, scalar2=EPS, op0=ALU.mult, op1=ALU.add),
                              r=[("ps", 0)], w=["rrs"])
                        pr.op("act", lambda e, n=n: e.activation(out=rr[:, 0:n], in_=rr[:, 0:n], func=AF.Sqrt), r=["rrs"], w=["rrs"])
                        pr.op("dve", lambda e, n=n: e.reciprocal(out=rr[:, 0:n], in_=rr[:, 0:n]), r=["rrs"], w=["rrs"])
                        pr.op("dve", lambda e, t0=t0, n=n: e.tensor_tensor(out=rr[:, 0:n], in0=oT[:, t0:t0 + n], in1=rr[:, 0:n], op=ALU.mult), r=okeys + ["rrs"], w=["rrs"])
                        ob = oi % 2
                        oi += 1
                        pr.op("dve", lambda e, n=n, ob=ob: e.scalar_tensor_tensor(out=ost[ob][:, 0:n], in0=rr[:, 0:n], scalar=gnv[:, 0:1], in1=gt[:, 0:n],
                                                                                 op0=ALU.mult, op1=ALU.mult), r=["rrs", "rgt", "gnv"], w=[("rost", ob)])
                        pr.dma("pool", lambda e, hh=hh, t0=t0, n=n, ob=ob: e.dma_start(out=self.catT[hh * 128:(hh + 1) * 128, t0:t0 + n], in_=ost[ob][:, 0:n]),
                               r=[("rost", ob)], w=[("catT", hh, t0)])
                    pr.flush()
                pr.flush(barrier=True)


IN_SHAPES = {
    "hT0": [D, T], "c2": [32, 128], "w_mod": [DEPTH, D, 6 * D], "b_mod": [384, 128], "g12": [128, 128], "gfin": [16, 128],
    "att_w_in": [2, D, 6144], "att_w_out": [2, D, D], "att_lambda": [2, 256], "att_subln": [2, 128],
    "na_bias": [2, 8, 128, 5 * 5 * 128], "rec_w_in": [2, D, 5 * D], "rec_w_out": [2, D, D], "rec_lb": [DEPTH * 2 * 16, 128],
    "rec_gn": [2, 128], "moe_wr": [DEPTH, D, 36], "moe_br": [DEPTH, 36], "moe_wg": [DEPTH, NEXP, D, DEXP],
    "moe_wu": [DEPTH, NEXP, D, DEXP], "moe_wd": [DEPTH, NEXP, DEXP, D], "k_ident": [128, 128], "k_cos": [128, NLAT],
    "k_sin": [128, NLAT], "k_RT": [128, 128], "k_m128f": [128, 128], "k_m128b": [128, 128],
    "k_iota": [32, 164], "k_uincl": [32, 32], "k_pidx": [128, 1],
}


def _mkprop(name):
    def get(self):
        if name not in self._ins:
            self._ins[name] = self.din(name, IN_SHAPES[name])
        return self._ins[name]
    return property(get)


for _n in IN_SHAPES:
    setattr(Builder, _n, _mkprop(_n))


def prep_inputs(inp):
    f = lambda a: np.ascontiguousarray(np.asarray(a, dtype=np.float32))
    k = _consts()
    out = {}
    out["hT0"] = f(np.concatenate([inp["ctx"][0], inp["x"][0]], axis=0).T)
    out["c2"] = f(np.concatenate([inp["c"].reshape(16, 128), inp["c_ctx"].reshape(16, 128)], axis=0))
    out["w_mod"] = f(inp["w_mod"])
    out["b_mod"] = f(inp["b_mod"].reshape(384, 128))
    out["g12"] = f(np.concatenate([inp["norm1_g"].reshape(64, 128), inp["norm2_g"].reshape(64, 128)], axis=0))
    out["gfin"] = f(inp["final_norm_g"].reshape(16, 128))
    out["att_w_in"] = f(inp["att_w_in"])
    out["att_w_out"] = f(inp["att_w_out"])
    out["att_lambda"] = f(inp["att_lambda"].reshape(2, 256))
    out["att_subln"] = f(inp["att_subln_g"])
    out["na_bias"] = f(np.stack([_na_bias(np.asarray(inp["att_rpb"][j])) for j in range(2)]).reshape(2, 8, 128, 5 * 5 * 128))
    out["rec_w_in"] = f(inp["rec_w_in"])
    out["rec_w_out"] = f(inp["rec_w_out"])
    out["rec_lb"] = f(inp["rec_lb_logits"].reshape(DEPTH * 2 * 16, 128))
    out["rec_gn"] = f(inp["rec_gnorm_g"])
    out["moe_wr"] = f(np.concatenate([inp["moe_w_group"], inp["moe_w_router"]], axis=2))
    out["moe_br"] = f(np.concatenate([inp["moe_b_group"], inp["moe_b_router"]], axis=1))
    out["moe_wg"] = f(inp["moe_w_gate"])
    out["moe_wu"] = f(inp["moe_w_up"])
    out["moe_wd"] = f(inp["moe_w_down"])
    out["k_ident"] = k["ident"]
    out["k_cos"] = k["cosT"]
    out["k_sin"] = k["sinT"]
    out["k_RT"] = k["RT"]
    out["k_m128f"] = k["m128f"]
    out["k_m128b"] = k["m128b"]
    out["k_iota"] = np.ascontiguousarray(np.broadcast_to(128.0 * np.arange(164, dtype=np.float32), (32, 164)))
    out["k_uincl"] = np.triu(np.ones((32, 32), np.float32))
    out["k_pidx"] = np.arange(128, dtype=np.float32).reshape(128, 1)
    return out


def kernel(**inputs):
    b = Builder()
    nc = b.build()
    allin = prep_inputs(inputs)
    in_map = {n: allin[n] for n in b._ins}
    res = run_bass_kernel_spmd(nc, [in_map], core_ids=[0])
    outT = res.results[0]["outT"]
    return np.ascontiguousarray(outT.T)[None].astype(np.float32)
```

```python
import contextlib
import math
import numpy as np
import ml_dtypes
import concourse.bass as bass
import concourse.mybir as mybir
from concourse.bass_utils import run_bass_kernel_spmd

F32 = mybir.dt.float32
BF16 = mybir.dt.bfloat16
AF = mybir.ActivationFunctionType
ALU = mybir.AluOpType
AX = mybir.AxisListType

D = 2048
KT = 16
CTX = 256
NLAT = 8192
T = CTX + NLAT
DEPTH = 4
EPS = 1e-6
NEG = -30000.0
NEXP = 32
DEXP = 768


class Tok:
    __slots__ = ("eng", "isdma", "sig", "need")

    def __init__(self, eng, isdma):
        self.eng = eng
        self.isdma = isdma
        self.sig = None
        self.need = False


class Prog:
    NSLOT = 8

    def __init__(self, nc, es):
        self.nc = nc
        self.es = es
        self.E = {"pe": nc.tensor, "act": nc.scalar, "dve": nc.vector, "pool": nc.gpsimd, "sync": nc.sync}
        self.csem = {e: es.enter_context(nc.semaphore("cs_" + e)) for e in ("pe", "act", "dve", "pool")}
        self.ccnt = {e: 0 for e in self.csem}
        self.nslot = {"sync": 8, "pool": 3}
        self.dsem = {q: [es.enter_context(nc.semaphore(f"ds_{q}{i}")) for i in range(self.nslot[q])] for q in ("sync", "pool")}
        self.dcnt = {q: 0 for q in self.dsem}
        self.waited = {e: {} for e in self.E}
        self.last_w = {}
        self.readers = {}
        self.ops = []
        self.semobj = {}
        self.pending_barrier = []
        self.n_inst = 0

    def op(self, eng, fn, r=(), w=()):
        self.ops.append((eng, fn, tuple(r), tuple(w), False))

    def dma(self, q, fn, r=(), w=()):
        self.ops.append((q, fn, tuple(r), tuple(w), True))

    def _wait(self, eng, sem, val):
        k = id(sem)
        if self.waited[eng].get(k, 0) < val:
            self.E[eng].wait_ge(sem, val)
            self.waited[eng][k] = val

    def flush(self, barrier=False):
        ops = self.ops
        self.ops = []
        n = len(ops)
        toks = [Tok(o[0], o[4]) for o in ops]
        deps = [None] * n
        last_idx = {}
        for i, (eng, fn, r, w, isdma) in enumerate(ops):
            d = set()
            for k in r:
                t = self.last_w.get(k)
                if t is not None:
                    d.add(t)
            for k in w:
                t = self.last_w.get(k)
                if t is not None:
                    d.add(t)
                for t2 in self.readers.get(k, ()):
                    d.add(t2)
            me = toks[i]
            for k in w:
                self.last_w[k] = me
                self.readers[k] = []
            for k in r:
                self.readers.setdefault(k, []).append(me)
            dd = []
            for t in d:
                if t is me:
                    continue
                if t.eng == eng and eng == "pe" and not t.isdma and not isdma:
                    continue
                dd.append(t)
                t.need = True
            deps[i] = dd
            if not isdma:
                last_idx[eng] = i
        for e, i in last_idx.items():
            toks[i].need = True
        unsig = {e: [] for e in self.csem}
        first_on = set()
        for i, (eng, fn, r, w, isdma) in enumerate(ops):
            me = toks[i]
            if self.pending_barrier and eng not in first_on:
                first_on.add(eng)
                for (sem, val) in self.pending_barrier:
                    self._wait(eng, sem, val)
            if isdma:
                j = self.dcnt[eng]
                ns = self.nslot[eng]
                slot = j % ns
                sem = self.dsem[eng][slot]
                if j >= ns:
                    self._wait(eng, sem, 16 * (j // ns))
                self.dcnt[eng] = j + 1
                me.sig = (sem, 16 * (j // ns + 1))
            need = {}
            for t in deps[i]:
                sem, val = t.sig
                k = id(sem)
                if k not in need or need[k][1] < val:
                    need[k] = (sem, val)
            for sem, val in need.values():
                self._wait(eng, sem, val)
            inst = fn(self.E[eng])
            self.n_inst += 1
            if isdma:
                inst.then_inc(me.sig[0], 16)
            elif me.need:
                self.ccnt[eng] += 1
                inst.then_inc(self.csem[eng], 1)
                me.sig = (self.csem[eng], self.ccnt[eng])
                for t in unsig[eng]:
                    t.sig = me.sig
                unsig[eng] = []
            else:
                unsig[eng].append(me)
        for e in unsig:
            assert not unsig[e]
        if barrier:
            self.pending_barrier = self.all_sigs()

    def all_sigs(self):
        sigs = [(self.csem[e], self.ccnt[e]) for e in self.csem if self.ccnt[e] > 0]
        for q in self.dsem:
            j = self.dcnt[q]
            ns = self.nslot[q]
            for s in range(ns):
                if j > s:
                    cnt = (j - 1 - s) // ns + 1
                    sigs.append((self.dsem[q][s], 16 * cnt))
        return sigs

    def finish(self):
        self.flush()
        for sem, val in self.all_sigs():
            self._wait("sync", sem, val)


def _consts():
    ident = np.eye(128, dtype=np.float32)
    ones = np.ones((128, 128), np.float32)
    tpos = np.arange(NLAT)
    rows = (tpos // 64).astype(np.float32)
    cols = (tpos % 64).astype(np.float32)
    inv = (10000.0 ** (-np.arange(16, dtype=np.float32) / 16)).astype(np.float32)
    cosT = np.zeros((128, NLAT), np.float32)
    sinT = np.zeros((128, NLAT), np.float32)
    for d in range(128):
        dd = d % 64
        axis = dd // 32
        fr = dd % 16
        ang = (rows if axis == 0 else cols) * inv[fr]
        cosT[d] = np.cos(ang)
        sinT[d] = np.sin(ang)
    RT = np.zeros((128, 128), np.float32)
    for m in range(128):
        if (m % 32) < 16:
            RT[m + 16, m] = -1.0
        else:
            RT[m - 16, m] = 1.0
    s_ = np.arange(64)[:, None]
    t_ = np.arange(64)[None, :]
    mfwd = (s_ <= t_).astype(np.float32)
    mbwd = (s_ >= t_).astype(np.float32)
    m128f = np.zeros((128, 128), np.float32)
    m128b = np.zeros((128, 128), np.float32)
    for c in range(2):
        m128f[c * 64:(c + 1) * 64, c * 64:(c + 1) * 64] = mfwd
        m128b[c * 64:(c + 1) * 64, c * 64:(c + 1) * 64] = mbwd
    return dict(ident=ident, ones=ones, cosT=cosT, sinT=sinT, RT=RT, m128f=m128f, m128b=m128b)


NA_CLASS_J = [0, 1, 2, 62, 63]


def _na_class(j):
    if j <= 1:
        return j
    if j >= 62:
        return j - 59
    return 2


def _na_geom():
    idx = np.zeros((5, 640, 128, 2), np.int64)
    valid = np.zeros((5, 640, 128), bool)
    for c, j in enumerate(NA_CLASS_J):
        kbase = int(np.clip(2 * j - 4, 0, 118))
        ql = np.arange(128)
        r = 2 * j + ql // 64
        cq = ql % 64
        rs = np.clip(r - 4, 0, 120)
        cs = np.clip(cq - 8, 0, 48)
        kl = np.arange(640)
        kr = kbase + kl // 64
        kc = kl % 64
        v = (kr[:, None] >= rs[None, :]) & (kr[:, None] < rs[None, :] + 8) & (kc[:, None] >= cs[None, :]) & (kc[:, None] < cs[None, :] + 16)
        ro = kr[:, None] - r[None, :] + 7
        co = kc[:, None] - cq[None, :] + 15
        valid[c] = v
        idx[c, :, :, 0] = np.clip(ro, 0, 14)
        idx[c, :, :, 1] = np.clip(co, 0, 30)
    return idx, valid


def _na_bias(rpb):
    idx, valid = _na_geom()
    out = np.empty((8, 5, 640, 128), np.float32)
    for h in range(8):
        g = rpb[h][idx[..., 0], idx[..., 1]]
        out[h] = np.where(valid, g, np.float32(NEG))
    out = out.reshape(8, 5, 5, 128, 128).transpose(0, 3, 1, 2, 4)
    return np.ascontiguousarray(out)


class Builder:
    def __init__(self, n_layers=DEPTH, debug_out=None):
        self.n_layers = n_layers
        self.debug_out = debug_out
        self.nc = bass.Bass("TRN2", target_bir_lowering=False)
        self.es = contextlib.ExitStack()
        self.pr = Prog(self.nc, self.es)
        self.uid = 0
        self._ins = {}

    def din(self, name, shape, dt=F32):
        return self.nc.dram_tensor(name, list(shape), dt, kind="ExternalInput").ap()

    def dint(self, name, shape, dt):
        return self.nc.dram_tensor(name, list(shape), dt, kind="Internal").ap()

    def sb(self, stack, name, shape, dt):
        self.uid += 1
        return stack.enter_context(self.nc.sbuf_tensor(f"{name}_{self.uid}", list(shape), dt))

    def ps(self, stack, name, shape, dt=F32):
        self.uid += 1
        return stack.enter_context(self.nc.psum_tensor(f"{name}_{self.uid}", list(shape), dt))

    def build(self):
        nc, pr = self.nc, self.pr
        L = self.n_layers
        self.outT = self.nc.dram_tensor("outT", [D, NLAT], F32, kind="ExternalOutput").ap()
        self.hA = self.dint("hA", [D, T], F32)
        self.hB = self.dint("hB", [D, T], F32)
        self.aT = self.dint("aT", [D, T], BF16)
        self.QK = self.dint("QK", [5 * D, T], BF16)
        self.V = self.dint("V", [T, D], BF16)
        self.catT = self.dint("catT", [D, T], BF16)
        self.wb_in = self.dint("wb_in", [D, 5 * D], BF16)
        self.wb_out = self.dint("wb_out", [D, D], BF16)
        self.alloc_moe()

        es = self.es
        self.ident = self.sb(es, "ident", [128, 128], F32)
        self.ones = self.sb(es, "ones", [128, 128], F32)
        self.onesb = self.sb(es, "onesb", [128, 128], BF16)
        self.identb = self.sb(es, "identb", [128, 128], BF16)
        self.MOD = self.sb(es, "MOD", [128, DEPTH * 6 * 16 * 2], F32)
        self.AB = self.sb(es, "AB", [128, DEPTH * 2 * 2 * 16 * 2], F32)
        self.GF = self.sb(es, "GF", [128, 16], F32)
        self.G12 = self.sb(es, "G12", [128, 128], F32)
        pr.dma("sync", lambda e: e.dma_start(out=self.ident[:], in_=self.k_ident[:, :]), w=["ident"])
        pr.op("dve", lambda e: e.memset(self.ones[:], 1.0), w=["ones"])
        pr.op("dve", lambda e: e.memset(self.onesb[:], 1.0), w=["onesb"])
        pr.op("dve", lambda e: e.tensor_copy(out=self.identb[:], in_=self.ident[:]), r=["ident"], w=["identb"])
        self.psb = [self.ps(es, f"bank{i}", [128, 512], F32) for i in range(8)]

        self.phase_mod()
        h_in = self.hT0
        for l in range(L):
            h_mid = self.hA
            h_out = self.hB
            self.phase_norm(l, 1, h_in, router=False)
            if l % 2 == 0:
                self.phase_att(l)
            else:
                self.phase_rec(l)
            self.phase_outproj(l, h_in, h_mid)
            self.phase_norm(l, 2, h_mid, router=True)
            self.phase_moe(l, h_mid, h_out)
            h_in = h_out
        self.phase_final(h_in)
        pr.finish()
        return nc

    def alloc_moe(self):
        self.wb_g2 = self.dint("wb_g2", [NEXP * 128, 16 * DEXP], BF16)
        self.wb_u2 = self.dint("wb_u2", [NEXP * 128, 16 * DEXP], BF16)
        self.wb_d2 = self.dint("wb_d2", [NEXP * 128, 6 * D], BF16)
        self.XS = self.dint("XS", [164 * 128, D], BF16)
        self.YS = self.dint("YS", [164 * 128, D], F32)
        es = self.es
        self.Lsb = self.sb(es, "Lsb", [128, 66, 36], F32)
        self.W1 = self.sb(es, "W1", [128, 66], F32)
        self.W2 = self.sb(es, "W2", [128, 66], F32)
        self.D1 = self.sb(es, "D1", [128, 66], mybir.dt.uint32)
        self.D2 = self.sb(es, "D2", [128, 66], mybir.dt.uint32)
        self.WIDX = self.sb(es, "WIDX", [128, 164], mybir.dt.uint32)

    def modv(self, l, j, f, col):
        o = ((l * 6 + j) * 16 + f) * 2 + col
        return self.MOD[:, o:o + 1]

    def abv(self, l, which, ab, f, col):
        o = ((((l * 2 + (which - 1)) * 2 + ab) * 16) + f) * 2 + col
        return self.AB[:, o:o + 1]

    def cast_w(self, src2d, dst2d, rows, cols, tag):
        pr = self.pr
        for r0 in range(0, rows, 1024):
            r1 = min(rows, r0 + 1024)
            for c0 in range(0, cols, 2048):
                c1 = min(cols, c0 + 2048)
                pr.dma("pool", lambda e, r0=r0, r1=r1, c0=c0, c1=c1: e.dma_start(out=dst2d[r0:r1, c0:c1], in_=src2d[r0:r1, c0:c1]),
                       r=[], w=[(tag, r0, c0)])

    def transpose_rows(self, stack, src_rows_ap, R, dst_ap, tag, bank=0):
        pr = self.pr
        tmp = self.sb(stack, "trtmp", [128, 128], F32)
        kt = ("trtmp", self.uid)
        pr.dma("sync", lambda e: e.dma_start(out=tmp[0:R, :], in_=src_rows_ap), w=[kt])
        pb = self.psb[bank]
        pr.op("pe", lambda e: e.matmul(pb[:, 0:R], lhsT=tmp[0:R, :], rhs=self.ident[0:R, 0:R], start=True, stop=True),
              r=[kt, "ident"], w=[("ps", bank)])
        pr.op("dve", lambda e: e.tensor_copy(out=dst_ap, in_=pb[:, 0:R]), r=[("ps", bank)], w=[tag])

    def phase_mod(self):
        pr = self.pr
        with contextlib.ExitStack() as st:
            cc = self.sb(st, "cc", [128, 32], F32)
            BM = self.sb(st, "BM", [128, 384], F32)
            self.transpose_rows(st, self.c2[:, :], 32, cc[:, :], "cc", bank=0)
            pr.op("act", lambda e: e.activation(out=cc[:, :], in_=cc[:, :], func=AF.Silu), r=["cc"], w=["cc"])
            for a in range(3):
                self.transpose_rows(st, self.b_mod[a * 128:(a + 1) * 128, :], 128, BM[:, a * 128:(a + 1) * 128], ("BM", a), bank=1 + a)
            self.transpose_rows(st, self.g12[:, :], 128, self.G12[:, :], "G12", bank=4)
            self.transpose_rows(st, self.gfin[:, :], 16, self.GF[:, :], "GF", bank=5)
            cc2 = self.sb(st, "cc2", [128, 16, 2], F32)
            pr.op("dve", lambda e: e.tensor_copy(out=cc2[:, :, 0], in_=cc[:, 0:16]), r=["cc"], w=["cc2a"])
            pr.op("dve", lambda e: e.tensor_copy(out=cc2[:, :, 1], in_=cc[:, 16:32]), r=["cc"], w=["cc2b"])
            wm = [self.sb(st, f"wm{i}", [128, 16, 1024], F32) for i in range(2)]
            it = 0
            for l in range(self.n_layers):
                for j in range(6):
                    for half in range(2):
                        b = it % 2
                        bank = 6 + (it % 2)
                        c0 = j * D + half * 1024
                        src = self.w_mod[l, :, c0:c0 + 1024].rearrange("(k p) c -> p k c", p=128)
                        pr.dma("sync", lambda e, b=b, src=src: e.dma_start(out=wm[b][:, :, :], in_=src), w=[("wm", b)])
                        pb = self.psb[bank]
                        for f8 in range(8):
                            for k in range(16):
                                pr.op("pe", lambda e, b=b, f8=f8, k=k, pb=pb: e.matmul(
                                    pb[:, f8 * 2:f8 * 2 + 2], lhsT=wm[b][:, k, f8 * 128:(f8 + 1) * 128], rhs=cc2[:, k, :],
                                    start=(k == 0), stop=(k == 15)), r=[("wm", b), "cc2a", "cc2b"], w=[("ps", bank)])
                        o = ((l * 6 + j) * 16 + half * 8) * 2
                        bo = l * 96 + j * 16 + half * 8
                        for col in range(2):
                            pr.op("dve", lambda e, o=o, bo=bo, col=col, pb=pb: e.tensor_tensor(
                                out=self.MOD[:, o + col:o + 16:2], in0=pb[:, col:16:2], in1=BM[:, bo:bo + 8], op=ALU.add),
                                r=[("ps", bank), ("BM", 0), ("BM", 1), ("BM", 2)], w=["MOD"])
                        it += 1
            for l in range(self.n_layers):
                for which in (1, 2):
                    jsh, jsc = (0, 1) if which == 1 else (3, 4)
                    g = self.G12[:, (which - 1) * 64 + l * 16:(which - 1) * 64 + l * 16 + 16]
                    for col in range(2):
                        osc = ((l * 6 + jsc) * 16) * 2 + col
                        osh = ((l * 6 + jsh) * 16) * 2 + col
                        oa = ((((l * 2 + (which - 1)) * 2 + 0) * 16)) * 2 + col
                        ob = ((((l * 2 + (which - 1)) * 2 + 1) * 16)) * 2 + col
                        pr.op("dve", lambda e, osc=osc, oa=oa, g=g: e.scalar_tensor_tensor(
                            out=self.AB[:, oa:oa + 31:2], in0=self.MOD[:, osc:osc + 31:2], scalar=1.0, in1=g, op0=ALU.add, op1=ALU.mult),
                            r=["MOD", "G12"], w=["AB"])
                        pr.op("dve", lambda e, osh=osh, ob=ob: e.tensor_copy(out=self.AB[:, ob:ob + 31:2], in_=self.MOD[:, osh:osh + 31:2]),
                              r=["MOD"], w=["AB"])
            pr.flush(barrier=True)

    def tok_tiles(self, n):
        out = []
        t0 = 0
        while t0 < T:
            lim = CTX if t0 < CTX else T
            m = min(n, lim - t0)
            out.append((t0, m))
            t0 += m
        return out

    def phase_norm(self, l, which, h_src, router, final=False):
        pr = self.pr
        NT = 256
        with contextlib.ExitStack() as st:
            hb = [self.sb(st, f"nh{i}", [128, 16, NT], F32) for i in range(2)]
            sq = [self.sb(st, f"nsq{i}", [128, 16, NT], F32) for i in range(2)]
            ab = [self.sb(st, f"nab{i}", [128, 16, NT], BF16) for i in range(2)]
            rs = [self.sb(st, f"nrs{i}", [128, NT], F32) for i in range(2)]
            if router:
                WR = self.sb(st, "WR", [128, 16, 36], F32)
                Lsb = self.Lsb
                pr.dma("sync", lambda e: e.dma_start(out=WR[:, :, :], in_=self.moe_wr[l].rearrange("(k p) c -> p k c", p=128)), w=["WR"])
            for i, (t0, n) in enumerate(self.tok_tiles(NT)):
                b = i % 2
                col = 1 if t0 < CTX else 0
                bank = i % 2
                pr.dma("sync", lambda e, b=b, t0=t0, n=n: e.dma_start(
                    out=hb[b][:, :, 0:n], in_=h_src[:, t0:t0 + n].rearrange("(k p) t -> p k t", p=128)), w=[("nh", b)])
                pr.op("act", lambda e, b=b, n=n: e.activation(out=sq[b][:, :, 0:n], in_=hb[b][:, :, 0:n], func=AF.Square),
                      r=[("nh", b)], w=[("nsq", b)])
                pb = self.psb[bank]
                for k in range(16):
                    pr.op("pe", lambda e, b=b, k=k, n=n, pb=pb: e.matmul(pb[:, 0:n], lhsT=self.ones[:, :], rhs=sq[b][:, k, 0:n],
                                                                       start=(k == 0), stop=(k == 15)),
                          r=[("nsq", b), "ones"], w=[("ps", bank)])
                pr.op("dve", lambda e, b=b, n=n, pb=pb: e.tensor_scalar(out=rs[b][:, 0:n], in0=pb[:, 0:n], scalar1=1.0 / D, scalar2=EPS,
                                                                      op0=ALU.mult, op1=ALU.add), r=[("ps", bank)], w=[("nrs", b)])
                pr.op("act", lambda e, b=b, n=n: e.activation(out=rs[b][:, 0:n], in_=rs[b][:, 0:n], func=AF.Sqrt), r=[("nrs", b)], w=[("nrs", b)])
                pr.op("dve", lambda e, b=b, n=n: e.reciprocal(out=rs[b][:, 0:n], in_=rs[b][:, 0:n]), r=[("nrs", b)], w=[("nrs", b)])
                for k in range(16):
                    pr.op("dve", lambda e, b=b, k=k, n=n: e.tensor_tensor(out=sq[b][:, k, 0:n], in0=hb[b][:, k, 0:n], in1=rs[b][:, 0:n],
                                                                        op=ALU.mult), r=[("nh", b), ("nrs", b)], w=[("nsq", b)])
                for k in range(16):
                    if final:
                        sc1, sc2, o1 = self.GF[:, k:k + 1], None, ALU.bypass
                        pr.op("dve", lambda e, b=b, k=k, n=n, sc1=sc1: e.tensor_scalar(
                            out=hb[b][:, k, 0:n], in0=sq[b][:, k, 0:n], scalar1=sc1, scalar2=None, op0=ALU.mult),
                            r=[("nsq", b), "GF"], w=[("nh", b)])
                    else:
                        A = self.abv(l, which, 0, k, col)
                        Bv = self.abv(l, which, 1, k, col)
                        dst = sq[b] if router else ab[b]
                        pr.op("dve", lambda e, b=b, k=k, n=n, A=A, Bv=Bv, dst=dst: e.tensor_scalar(
                            out=dst[:, k, 0:n], in0=sq[b][:, k, 0:n], scalar1=A, scalar2=Bv, op0=ALU.mult, op1=ALU.add),
                            r=[("nsq", b), "AB"], w=[("nsq", b) if router else ("nab", b)])
                if final:
                    if t0 >= CTX:
                        pr.dma("pool", lambda e, b=b, t0=t0, n=n: e.dma_start(
                            out=self.outT[:, t0 - CTX:t0 - CTX + n].rearrange("(k p) t -> p k t", p=128), in_=hb[b][:, :, 0:n]),
                            r=[("nh", b)], w=[("outT", t0)])
                    continue
                if router:
                    pr.op("act", lambda e, b=b, n=n: e.activation(out=ab[b][:, :, 0:n], in_=sq[b][:, :, 0:n], func=AF.Copy),
                          r=[("nsq", b)], w=[("nab", b)])
                    for s in range(n // 128):
                        rb = 2 + (s % 2)
                        pbl = self.psb[rb]
                        for k in range(16):
                            pr.op("pe", lambda e, b=b, k=k, s=s, pbl=pbl: e.matmul(
                                pbl[:, 0:36], lhsT=sq[b][:, k, s * 128:(s + 1) * 128], rhs=WR[:, k, :], start=(k == 0), stop=(k == 15)),
                                r=[("nsq", b), "WR"], w=[("ps", rb)])
                        blk = t0 // 128 + s
                        pr.op("act", lambda e, blk=blk, pbl=pbl: e.activation(out=Lsb[:, blk, :], in_=pbl[:, 0:36], func=AF.Copy),
                              r=[("ps", rb)], w=[("Lsb", blk)])
                pr.dma("pool", lambda e, b=b, t0=t0, n=n: e.dma_start(
                    out=self.aT[:, t0:t0 + n].rearrange("(k p) t -> p k t", p=128), in_=ab[b][:, :, 0:n]),
                    r=[("nab", b)], w=[("aT", t0)])
            pr.flush(barrier=True)
        if router:
            with contextlib.ExitStack() as st2:
                self.route(st2, l, self.Lsb)
                pr.flush(barrier=True)

    def phase_final(self, h_src):
        self.phase_norm(0, 1, h_src, router=False, final=True)

    def route(self, st, l, Lsb):
        pr = self.pr
        NB = 66
        br = self.sb(st, "br", [128, 36], F32)
        pr.dma("sync", lambda e: e.dma_start(out=br[:, :], in_=self.moe_br[l:l + 1, :].partition_broadcast(128)), w=["br"])
        allL = [("Lsb", b) for b in range(NB)]
        Lb = self.sb(st, "Lb", [128, NB, 36], F32)
        pr.op("dve", lambda e: e.tensor_tensor(out=Lb[:, :, :], in0=Lsb[:, :, :], in1=br[:, :].unsqueeze(1).to_broadcast([128, NB, 36]),
                                               op=ALU.add), r=allL + ["br"], w=["Lb"])
        gmax = self.sb(st, "gmax", [128, NB], F32)
        pr.op("dve", lambda e: e.tensor_reduce(out=gmax[:, :], in_=Lb[:, :, 0:4], axis=AX.X, op=ALU.max), r=["Lb"], w=["gmax"])
        gd = self.sb(st, "gd", [128, NB, 4], F32)
        pr.op("dve", lambda e: e.tensor_tensor(out=gd[:, :, :], in0=Lb[:, :, 0:4], in1=gmax[:, :].unsqueeze(2).to_broadcast([128, NB, 4]),
                                               op=ALU.subtract), r=["Lb", "gmax"], w=["gd"])
        ohg = self.sb(st, "ohg", [128, NB, 4], F32)
        pr.op("dve", lambda e: e.tensor_scalar(out=ohg[:, :, :], in0=gd[:, :, :], scalar1=0.0, scalar2=None, op0=ALU.is_ge), r=["gd"], w=["ohg"])
        ge = self.sb(st, "ge", [128, NB, 4], F32)
        pr.op("act", lambda e: e.activation(out=ge[:, :, :], in_=gd[:, :, :], func=AF.Exp), r=["gd"], w=["ge"])
        gs = self.sb(st, "gs", [128, NB], F32)
        pr.op("dve", lambda e: e.tensor_reduce(out=gs[:, :], in_=ge[:, :, :], axis=AX.X, op=ALU.add), r=["ge"], w=["gs"])
        gw = self.sb(st, "gw", [128, NB], F32)
        pr.op("dve", lambda e: e.reciprocal(out=gw[:, :], in_=gs[:, :]), r=["gs"], w=["gw"])
        pen = self.sb(st, "pen", [128, NB, 4], F32)
        pr.op("dve", lambda e: e.tensor_scalar(out=pen[:, :, :], in0=ohg[:, :, :], scalar1=-1.0, scalar2=1.0e4, op0=ALU.add, op1=ALU.mult),
              r=["ohg"], w=["pen"])
        el = self.sb(st, "el", [128, NB, 32], F32)
        for g in range(4):
            pr.op("dve", lambda e, g=g: e.tensor_tensor(out=el[:, :, g * 8:(g + 1) * 8], in0=Lb[:, :, 4 + g * 8:4 + (g + 1) * 8],
                                                        in1=pen[:, :, g:g + 1].to_broadcast([128, NB, 8]), op=ALU.add),
                  r=["Lb", "pen"], w=["el"])
        v1 = self.sb(st, "v1", [128, NB], F32)
        pr.op("dve", lambda e: e.tensor_reduce(out=v1[:, :], in_=el[:, :, :], axis=AX.X, op=ALU.max), r=["el"], w=["v1"])
        oh1 = self.sb(st, "oh1", [128, NB, 32], F32)
        pr.op("dve", lambda e: e.tensor_tensor(out=oh1[:, :, :], in0=el[:, :, :], in1=v1[:, :].unsqueeze(2).to_broadcast([128, NB, 32]),
                                               op=ALU.is_ge), r=["el", "v1"], w=["oh1"])
        el2 = self.sb(st, "el2", [128, NB, 32], F32)
        pr.op("dve", lambda e: e.scalar_tensor_tensor(out=el2[:, :, :], in0=oh1[:, :, :], scalar=-1.0e4, in1=el[:, :, :],
                                                      op0=ALU.mult, op1=ALU.add), r=["oh1", "el"], w=["el2"])
        v2 = self.sb(st, "v2", [128, NB], F32)
        pr.op("dve", lambda e: e.tensor_reduce(out=v2[:, :], in_=el2[:, :, :], axis=AX.X, op=ALU.max), r=["el2"], w=["v2"])
        oh2 = self.sb(st, "oh2", [128, NB, 32], F32)
        pr.op("dve", lambda e: e.tensor_tensor(out=oh2[:, :, :], in0=el2[:, :, :], in1=v2[:, :].unsqueeze(2).to_broadcast([128, NB, 32]),
                                               op=ALU.is_ge), r=["el2", "v2"], w=["oh2"])
        rr = self.sb(st, "rr", [128, NB], F32)
        pr.op("dve", lambda e: e.tensor_tensor(out=rr[:, :], in0=v2[:, :], in1=v1[:, :], op=ALU.subtract), r=["v1", "v2"], w=["rr"])
        pr.op("act", lambda e: e.activation(out=rr[:, :], in_=rr[:, :], func=AF.Exp), r=["rr"], w=["rr"])
        W1, W2 = self.W1, self.W2
        pr.op("dve", lambda e: e.tensor_scalar(out=W1[:, :], in0=rr[:, :], scalar1=1.0, scalar2=None, op0=ALU.add), r=["rr"], w=["W1"])
        pr.op("dve", lambda e: e.reciprocal(out=W1[:, :], in_=W1[:, :]), r=["W1"], w=["W1"])
        pr.op("dve", lambda e: e.tensor_tensor(out=W1[:, :], in0=W1[:, :], in1=gw[:, :], op=ALU.mult), r=["W1", "gw"], w=["W1"])
        pr.op("dve", lambda e: e.tensor_tensor(out=W2[:, :], in0=W1[:, :], in1=rr[:, :], op=ALU.mult), r=["W1", "rr"], w=["W2"])
        cnt = el
        pr.op("dve", lambda e: e.tensor_tensor(out=cnt[:, :, :], in0=oh1[:, :, :], in1=oh2[:, :, :], op=ALU.add), r=["oh1", "oh2", "el2"], w=["el"])
        cntT = self.sb(st, "cntT", [32, T], F32)
        PSi = self.sb(st, "PSi", [32, T], F32)
        one32 = self.sb(st, "one32", [32, T], F32)
        pr.op("dve", lambda e: e.memset(one32[:, :], 1.0), w=["one32"])
        for blk in range(NB):
            bank = 4 + (blk % 2)
            pb = self.psb[bank]
            pr.op("pe", lambda e, blk=blk, pb=pb: e.matmul(pb[0:32, 0:128], lhsT=cnt[:, blk, :], rhs=self.ident[:, :], start=True, stop=True),
                  r=["el", "ident"], w=[("ps", bank)])
            pr.op("act", lambda e, blk=blk, pb=pb: e.activation(out=cntT[:, blk * 128:(blk + 1) * 128], in_=pb[0:32, 0:128], func=AF.Copy),
                  r=[("ps", bank)], w=[("cntT", blk)])
        allc = [("cntT", b) for b in range(NB)]
        pr.op("dve", lambda e: e.tensor_tensor_scan(out=PSi[:, :], data0=one32[:, :], data1=cntT[:, :], initial=0.0, op0=ALU.mult, op1=ALU.add),
              r=allc + ["one32"], w=["PSi"])
        kio = self.sb(st, "kio", [32, 164], F32)
        pr.dma("sync", lambda e: e.dma_start(out=kio[:, :], in_=self.k_iota[:, :]), w=["kio"])
        UT = self.sb(st, "UT", [32, 32], F32)
        pr.dma("sync", lambda e: e.dma_start(out=UT[:, :], in_=self.k_uincl[:, :]), w=["UT"])
        pidx = self.sb(st, "pidx", [128, 1], F32)
        pr.dma("sync", lambda e: e.dma_start(out=pidx[:, :], in_=self.k_pidx[:, :]), w=["pidx"])
        cmpb = self.sb(st, "cmpb", [32, 164], F32)
        pr.op("dve", lambda e: e.tensor_scalar(out=cmpb[:, 0:132], in0=kio[:, 0:132], scalar1=PSi[:, T - 1:T], scalar2=None, op0=ALU.is_lt),
              r=["kio", "PSi"], w=["cmpb"])
        padded = self.sb(st, "padded", [32, 1], F32)
        pr.op("dve", lambda e: e.tensor_reduce(out=padded[:, :], in_=cmpb[:, 0:132], axis=AX.X, op=ALU.add), r=["cmpb"], w=["padded"])
        pr.op("dve", lambda e: e.tensor_scalar(out=padded[:, :], in0=padded[:, :], scalar1=128.0, scalar2=None, op0=ALU.mult), r=["padded"], w=["padded"])
        pend = self.sb(st, "pend", [32, 1], F32)
        pstart = self.sb(st, "pstart", [32, 1], F32)
        pr.op("pe", lambda e: e.matmul(self.psb[6][0:32, 0:1], lhsT=UT[:, :], rhs=padded[:, :], start=True, stop=True), r=["UT", "padded"], w=[("ps", 6)])
        pr.op("dve", lambda e: e.tensor_copy(out=pend[:, :], in_=self.psb[6][0:32, 0:1]), r=[("ps", 6)], w=["pend"])
        pr.op("dve", lambda e: e.tensor_tensor(out=pstart[:, :], in0=pend[:, :], in1=padded[:, :], op=ALU.subtract), r=["pend", "padded"], w=["pstart"])
        pr.op("dve", lambda e: e.tensor_scalar(out=cmpb[:, :], in0=kio[:, :], scalar1=pend[:, 0:1], scalar2=None, op0=ALU.is_ge),
              r=["kio", "pend", "padded"], w=["cmpb"])
        pr.op("pe", lambda e: e.matmul(self.psb[7][:, 0:164], lhsT=self.ones[0:32, :], rhs=cmpb[:, :], start=True, stop=True), r=["cmpb", "ones"], w=[("ps", 7)])
        EBf = self.sb(st, "EBf", [128, 164], F32)
        pr.op("dve", lambda e: e.tensor_scalar(out=EBf[:, :], in0=self.psb[7][:, 0:164], scalar1=31.0, scalar2=128.0, op0=ALU.min, op1=ALU.mult),
              r=[("ps", 7)], w=["EBf"])
        pr.op("dve", lambda e: e.tensor_scalar(out=EBf[:, :], in0=EBf[:, :], scalar1=pidx[:, 0:1], scalar2=None, op0=ALU.add), r=["EBf", "pidx"], w=["EBf"])
        pr.op("dve", lambda e: e.tensor_copy(out=self.WIDX[:, :], in_=EBf[:, :]), r=["EBf"], w=["WIDX"])
        pr.op("dve", lambda e: e.tensor_tensor(out=PSi[:, :], in0=PSi[:, :], in1=cntT[:, :], op=ALU.subtract), r=["PSi", "cmpb"] + allc, w=["PSi"])
        pr.op("dve", lambda e: e.tensor_scalar(out=PSi[:, :], in0=PSi[:, :], scalar1=pstart[:, 0:1], scalar2=None, op0=ALU.add), r=["PSi", "pstart"], w=["PSi"])
        DT = el2
        for blk in range(NB):
            bank = 4 + (blk % 2)
            pb = self.psb[bank]
            pr.op("pe", lambda e, blk=blk, pb=pb: e.matmul(pb[:, 0:32], lhsT=PSi[:, blk * 128:(blk + 1) * 128], rhs=self.ident[0:32, 0:32], start=True, stop=True),
                  r=["PSi", "ident"], w=[("ps", bank)])
            pr.op("act", lambda e, blk=blk, pb=pb: e.activation(out=DT[:, blk, :], in_=pb[:, 0:32], func=AF.Copy), r=[("ps", bank), "oh2"], w=[("DT", blk)])
        allD = [("DT", b) for b in range(NB)]
        d1 = self.sb(st, "d1f", [128, NB], F32)
        pr.op("dve", lambda e: e.tensor_tensor(out=oh1[:, :, :], in0=oh1[:, :, :], in1=DT[:, :, :], op=ALU.mult), r=allD + ["oh1", "el"], w=["oh1"])
        pr.op("dve", lambda e: e.tensor_reduce(out=d1[:, :], in_=oh1[:, :, :], axis=AX.X, op=ALU.add), r=["oh1"], w=["d1f"])
        pr.op("dve", lambda e: e.tensor_copy(out=self.D1[:, :], in_=d1[:, :]), r=["d1f"], w=["D1"])
        pr.op("dve", lambda e: e.tensor_tensor(out=oh2[:, :, :], in0=oh2[:, :, :], in1=DT[:, :, :], op=ALU.mult), r=allD + ["oh2", "el"], w=["oh2"])
        pr.op("dve", lambda e: e.tensor_reduce(out=d1[:, :], in_=oh2[:, :, :], axis=AX.X, op=ALU.add), r=["oh2", "D1"], w=["d1f"])
        pr.op("dve", lambda e: e.tensor_copy(out=self.D2[:, :], in_=d1[:, :]), r=["d1f"], w=["D2"])

    def linear(self, st, xT, Wb, K, M, evac, tm_cols=None, tm_evac=None, NB=1024, tag="lin"):
        pr = self.pr
        kt = K // 128
        xb = [self.sb(st, f"{tag}x{i}", [128, kt, NB], BF16) for i in range(2)]
        wp = [self.sb(st, f"{tag}w{i}", [128, kt, 512], BF16) for i in range(2)]
        blocks = self.tok_tiles(NB)
        wi = 0
        bi = 0
        for bidx, (t0, n) in enumerate(blocks):
            xbuf = bidx % 2
            pr.dma("sync", lambda e, xbuf=xbuf, t0=t0, n=n: e.dma_start(
                out=xb[xbuf][:, :, 0:n], in_=xT[:, t0:t0 + n].rearrange("(k p) t -> p k t", p=128)), r=[(tag + "src",)], w=[(tag + "x", xbuf)])
            for c0 in range(0, M, 512):
                cw = min(512, M - c0)
                wbuf = wi % 2
                wi += 1
                pr.dma("sync", lambda e, wbuf=wbuf, c0=c0, cw=cw: e.dma_start(
                    out=wp[wbuf][:, :, 0:cw], in_=Wb[:, c0:c0 + cw].rearrange("(k p) c -> p k c", p=128)), r=[(tag + "wsrc",)], w=[(tag + "w", wbuf)])
                if tm_cols is not None and c0 in tm_cols:
                    for s in range(n // 128):
                        bank = bi % 4
                        bi += 1
                        pb = self.psb[bank]
                        for k in range(kt):
                            pr.op("pe", lambda e, xbuf=xbuf, wbuf=wbuf, k=k, s=s, cw=cw, pb=pb: e.matmul(
                                pb[:, 0:cw], lhsT=xb[xbuf][:, k, s * 128:(s + 1) * 128], rhs=wp[wbuf][:, k, 0:cw],
                                start=(k == 0), stop=(k == kt - 1)), r=[(tag + "x", xbuf), (tag + "w", wbuf)], w=[("ps", bank)])
                        tm_evac(c0, t0 + s * 128, pb[:, 0:cw], bank)
                    continue
                for mi in range(cw // 128):
                    for s0 in range(0, n, 512):
                        sn = min(512, n - s0)
                        bank = bi % 4
                        bi += 1
                        pb = self.psb[bank]
                        for k in range(kt):
                            pr.op("pe", lambda e, xbuf=xbuf, wbuf=wbuf, k=k, mi=mi, s0=s0, sn=sn, pb=pb: e.matmul(
                                pb[:, 0:sn], lhsT=wp[wbuf][:, k, mi * 128:(mi + 1) * 128], rhs=xb[xbuf][:, k, s0:s0 + sn],
                                start=(k == 0), stop=(k == kt - 1)), r=[(tag + "x", xbuf), (tag + "w", wbuf)], w=[("ps", bank)])
                        evac(c0 // 128 + mi, t0 + s0, sn, pb[:, 0:sn], bank)

    def phase_outproj(self, l, h_in, h_out):
        pr = self.pr
        j = l // 2
        wsrc = self.att_w_out if l % 2 == 0 else self.rec_w_out
        self.cast_w(wsrc[j], self.wb_out, D, D, "wbo")
        pr.flush(barrier=True)
        with contextlib.ExitStack() as st:
            ho = [self.sb(st, f"ho{i}", [128, 512], F32) for i in range(4)]
            cnt = [0]

            def evac(mt, t0, n, pap, bank):
                b = cnt[0] % 4
                cnt[0] += 1
                col = 1 if t0 < CTX else 0
                pr.dma("sync", lambda e: e.dma_start(out=ho[b][:, 0:n], in_=h_in[mt * 128:(mt + 1) * 128, t0:t0 + n]), w=[("ho", b)])
                gate = self.modv(l, 2, mt, col)
                pr.op("dve", lambda e: e.scalar_tensor_tensor(out=ho[b][:, 0:n], in0=pap, scalar=gate, in1=ho[b][:, 0:n],
                                                              op0=ALU.mult, op1=ALU.add), r=[("ps", bank), ("ho", b), "MOD"], w=[("ho", b)])
                pr.dma("pool", lambda e: e.dma_start(out=h_out[mt * 128:(mt + 1) * 128, t0:t0 + n], in_=ho[b][:, 0:n]), r=[("ho", b)], w=[("hout", mt, t0)])

            self.linear(st, self.catT, self.wb_out, D, D, evac, tag="op")
            pr.flush(barrier=True)

    def phase_moe(self, l, h_in, h_out):
        pr = self.pr
        IOA = bass.IndirectOffsetOnAxis
        NBLK = 164
        for e_ in range(NEXP):
            pr.dma("pool", lambda e, e_=e_: e.dma_start(out=self.wb_g2[e_ * 128:(e_ + 1) * 128, :].rearrange("p (k c) -> p k c", k=16),
                                                       in_=self.moe_wg[l, e_].rearrange("(k p) c -> p k c", p=128)), w=[("wbg", e_)])
            pr.dma("pool", lambda e, e_=e_: e.dma_start(out=self.wb_u2[e_ * 128:(e_ + 1) * 128, :].rearrange("p (k c) -> p k c", k=16),
                                                       in_=self.moe_wu[l, e_].rearrange("(k p) c -> p k c", p=128)), w=[("wbu", e_)])
            pr.dma("pool", lambda e, e_=e_: e.dma_start(out=self.wb_d2[e_ * 128:(e_ + 1) * 128, :].rearrange("p (k c) -> p k c", k=6),
                                                       in_=self.moe_wd[l, e_].rearrange("(k p) c -> p k c", p=128)), w=[("wbd", e_)])
        pr.flush(barrier=True)
        with contextlib.ExitStack() as st:
            xa = [self.sb(st, f"dxa{i}", [128, 16, 128], BF16) for i in range(2)]
            xt = [self.sb(st, f"dxt{i}", [128, D], BF16) for i in range(2)]
            zt = self.sb(st, "dzt", [128, D], BF16)
            pr.op("dve", lambda e: e.memset(zt[:, :], 0.0), w=["dzt"])
            for blk in range(NBLK):
                pr.dma("sync", lambda e, blk=blk: e.dma_start(out=self.XS[blk * 128:(blk + 1) * 128, :], in_=zt[:, :]), r=["dzt"], w=[("XSz", blk)])
            pr.flush(barrier=True)
            for tt in range(66):
                b = tt % 2
                pr.dma("sync", lambda e, b=b, tt=tt: e.dma_start(out=xa[b][:, :, :], in_=self.aT[:, tt * 128:(tt + 1) * 128].rearrange("(k p) t -> p k t", p=128)),
                       w=[("dxa", b)])
                for q in range(4):
                    bank = (tt * 4 + q) % 4
                    pb = self.psb[bank]
                    for kk in range(4):
                        k = q * 4 + kk
                        pr.op("pe", lambda e, b=b, k=k, kk=kk, pb=pb: e.matmul(pb[:, kk * 128:(kk + 1) * 128], lhsT=xa[b][:, k, :], rhs=self.identb[:, :], start=True, stop=True),
                              r=[("dxa", b), "identb"], w=[("ps", bank)])
                    pr.op("act" if q % 2 == 0 else "dve",
                          (lambda e, b=b, q=q, pb=pb: e.activation(out=xt[b][:, q * 512:(q + 1) * 512], in_=pb[:, :], func=AF.Copy)) if q % 2 == 0 else
                          (lambda e, b=b, q=q, pb=pb: e.tensor_copy(out=xt[b][:, q * 512:(q + 1) * 512], in_=pb[:, :])),
                          r=[("ps", bank)], w=[("dxt", b, q)])
                xk = [("dxt", b, q) for q in range(4)]
                pr.dma("pool", lambda e, b=b, tt=tt: e.indirect_dma_start(out=self.XS[:, :], out_offset=IOA(ap=self.D1[:, tt:tt + 1], axis=0),
                                                                        in_=xt[b][:, :], in_offset=None), r=xk + ["D1"], w=[("XS", tt, 0)])
                pr.dma("pool", lambda e, b=b, tt=tt: e.indirect_dma_start(out=self.XS[:, :], out_offset=IOA(ap=self.D2[:, tt:tt + 1], axis=0),
                                                                        in_=xt[b][:, :], in_offset=None), r=xk + ["D2"], w=[("XS", tt, 1)])
            pr.flush(barrier=True)
        with contextlib.ExitStack() as st:
            wg = [self.sb(st, f"swg{i}", [128, 16 * DEXP], BF16) for i in range(2)]
            wu = [self.sb(st, f"swu{i}", [128, 16 * DEXP], BF16) for i in range(2)]
            wd = [self.sb(st, f"swd{i}", [128, 6 * D], BF16) for i in range(2)]
            xs = [self.sb(st, f"sxs{i}", [128, D], BF16) for i in range(2)]
            xT = [self.sb(st, f"sxT{i}", [128, 16, 128], BF16) for i in range(2)]
            sg = [self.sb(st, f"ssg{i}", [128, DEXP], F32) for i in range(2)]
            hT = [self.sb(st, f"shT{i}", [128, 6, 128], BF16) for i in range(2)]
            hk = [self.sb(st, f"shk{i}", [128, DEXP], BF16) for i in range(2)]
            ysb = [self.sb(st, "sys0", [128, D], F32)] * 2
            bi = 0
            for blk in range(NBLK):
                b = blk % 2
                pr.dma("pool", lambda e, b=b, blk=blk: e.indirect_dma_start(out=wg[b][:, :], out_offset=None, in_=self.wb_g2[:, :],
                                                                          in_offset=IOA(ap=self.WIDX[:, blk:blk + 1], axis=0)), r=["WIDX"], w=[("swg", b)])
                pr.dma("pool", lambda e, b=b, blk=blk: e.indirect_dma_start(out=wu[b][:, :], out_offset=None, in_=self.wb_u2[:, :],
                                                                          in_offset=IOA(ap=self.WIDX[:, blk:blk + 1], axis=0)), r=["WIDX"], w=[("swu", b)])
                pr.dma("pool", lambda e, b=b, blk=blk: e.indirect_dma_start(out=wd[b][:, :], out_offset=None, in_=self.wb_d2[:, :],
                                                                          in_offset=IOA(ap=self.WIDX[:, blk:blk + 1], axis=0)), r=["WIDX"], w=[("swd", b)])
                pr.dma("sync", lambda e, b=b, blk=blk: e.dma_start(out=xs[b][:, :], in_=self.XS[blk * 128:(blk + 1) * 128, :]), w=[("sxs", b)])
                for q in range(4):
                    bank = q
                    pb = self.psb[bank]
                    for kk in range(4):
                        k = q * 4 + kk
                        pr.op("pe", lambda e, b=b, k=k, kk=kk, pb=pb: e.matmul(pb[:, kk * 128:(kk + 1) * 128], lhsT=xs[b][:, k * 128:(k + 1) * 128], rhs=self.identb[:, :], start=True, stop=True),
                              r=[("sxs", b), "identb"], w=[("ps", bank)])
                    if q % 2 == 0:
                        pr.op("act", lambda e, b=b, q=q, pb=pb: e.activation(out=xT[b][:, q * 4:(q + 1) * 4, :], in_=pb[:, :].rearrange("p (a t) -> p a t", a=4), func=AF.Copy),
                              r=[("ps", bank)], w=[("sxT", b, q)])
                    else:
                        pr.op("dve", lambda e, b=b, q=q, pb=pb: e.tensor_copy(out=xT[b][:, q * 4:(q + 1) * 4, :], in_=pb[:, :].rearrange("p (a t) -> p a t", a=4)),
                              r=[("ps", bank)], w=[("sxT", b, q)])
                xk = [("sxT", b, q) for q in range(4)]
                for k in range(16):
                    for (wsb, wkey, b0) in ((wg, "swg", 4), (wu, "swu", 6)):
                        for half in range(2):
                            pbk = b0 + half
                            pr.op("pe", lambda e, wsb=wsb, b=b, k=k, half=half, pbk=pbk: e.matmul(
                                self.psb[pbk][:, 0:384], lhsT=xT[b][:, k, :], rhs=wsb[b][:, k * DEXP + half * 384:k * DEXP + (half + 1) * 384],
                                start=(k == 0), stop=(k == 15)), r=[(wkey, b)] + xk, w=[("ps", pbk)])
                for half in range(2):
                    pr.op("act", lambda e, b=b, half=half: e.activation(out=sg[b][:, half * 384:(half + 1) * 384], in_=self.psb[4 + half][:, 0:384], func=AF.Silu),
                          r=[("ps", 4 + half)], w=[("ssg", b, half)])
                    pr.op("dve", lambda e, b=b, half=half: e.tensor_tensor(out=hk[b][:, half * 384:(half + 1) * 384], in0=sg[b][:, half * 384:(half + 1) * 384],
                                                                         in1=self.psb[6 + half][:, 0:384], op=ALU.mult),
                          r=[("ssg", b, half), ("ps", 6 + half)], w=[("shk", b, half)])
                for jt in range(6):
                    pbk = 4 if jt < 4 else 5
                    dst = self.psb[pbk][:, (jt % 4) * 128:(jt % 4 + 1) * 128]
                    pr.op("pe", lambda e, b=b, jt=jt, dst=dst: e.matmul(dst, lhsT=hk[b][:, jt * 128:(jt + 1) * 128], rhs=self.identb[:, :], start=True, stop=True),
                          r=[("shk", b, 0), ("shk", b, 1), "identb"], w=[("ps", pbk)])
                pr.op("act", lambda e, b=b: e.activation(out=hT[b][:, 0:4, :], in_=self.psb[4][:, :].rearrange("p (a t) -> p a t", a=4), func=AF.Copy),
                      r=[("ps", 4)], w=[("shT", b, 0)])
                pr.op("dve", lambda e, b=b: e.tensor_copy(out=hT[b][:, 4:6, :], in_=self.psb[5][:, 0:256].rearrange("p (a t) -> p a t", a=2)),
                      r=[("ps", 5)], w=[("shT", b, 1)])
                for c4 in range(4):
                    bank = c4
                    pb = self.psb[bank]
                    for k in range(6):
                        pr.op("pe", lambda e, b=b, k=k, c4=c4, pb=pb: e.matmul(pb[:, :], lhsT=hT[b][:, k, :], rhs=wd[b][:, k * D + c4 * 512:k * D + (c4 + 1) * 512],
                                                                             start=(k == 0), stop=(k == 5)), r=[("shT", b, 0), ("shT", b, 1), ("swd", b)], w=[("ps", bank)])
                    if c4 % 2 == 0:
                        pr.op("act", lambda e, b=b, c4=c4, pb=pb: e.activation(out=ysb[b][:, c4 * 512:(c4 + 1) * 512], in_=pb[:, :], func=AF.Copy),
                              r=[("ps", bank)], w=[("sys", b, c4)])
                    else:
                        pr.op("dve", lambda e, b=b, c4=c4, pb=pb: e.tensor_copy(out=ysb[b][:, c4 * 512:(c4 + 1) * 512], in_=pb[:, :]),
                              r=[("ps", bank)], w=[("sys", b, c4)])
                pr.dma("sync", lambda e, b=b, blk=blk: e.dma_start(out=self.YS[blk * 128:(blk + 1) * 128, :], in_=ysb[b][:, :]),
                       r=[("sys", b, c) for c in range(4)], w=[("YS", blk)])
            pr.flush(barrier=True)
        with contextlib.ExitStack() as st:
            y1 = [self.sb(st, f"cy1{i}", [128, D], F32) for i in range(2)]
            y2 = [self.sb(st, f"cy2{i}", [128, D], F32) for i in range(2)]
            ho = [self.sb(st, f"cho{i}", [128, 16, 128], F32) for i in range(2)]
            for tt in range(66):
                b = tt % 2
                col = 1 if tt < 2 else 0
                pr.dma("pool", lambda e, b=b, tt=tt: e.indirect_dma_start(out=y1[b][:, :], out_offset=None, in_=self.YS[:, :],
                                                                        in_offset=IOA(ap=self.D1[:, tt:tt + 1], axis=0)), r=["D1"], w=[("cy1", b)])
                pr.dma("pool", lambda e, b=b, tt=tt: e.indirect_dma_start(out=y2[b][:, :], out_offset=None, in_=self.YS[:, :],
                                                                        in_offset=IOA(ap=self.D2[:, tt:tt + 1], axis=0)), r=["D2"], w=[("cy2", b)])
                pr.dma("sync", lambda e, b=b, tt=tt: e.dma_start(out=ho[b][:, :, :], in_=h_in[:, tt * 128:(tt + 1) * 128].rearrange("(k p) t -> p k t", p=128)),
                       w=[("cho", b)])
                pr.op("dve", lambda e, b=b, tt=tt: e.tensor_scalar(out=y1[b][:, :], in0=y1[b][:, :], scalar1=self.W1[:, tt:tt + 1], scalar2=None, op0=ALU.mult),
                      r=[("cy1", b), "W1"], w=[("cy1", b)])
                pr.op("dve", lambda e, b=b, tt=tt: e.scalar_tensor_tensor(out=y1[b][:, :], in0=y2[b][:, :], scalar=self.W2[:, tt:tt + 1], in1=y1[b][:, :],
                                                                         op0=ALU.mult, op1=ALU.add), r=[("cy1", b), ("cy2", b), "W2"], w=[("cy1", b)])
                for k in range(16):
                    bank = k % 8
                    pb = self.psb[bank]
                    pr.op("pe", lambda e, b=b, k=k, pb=pb: e.matmul(pb[:, 0:128], lhsT=y1[b][:, k * 128:(k + 1) * 128], rhs=self.ident[:, :], start=True, stop=True),
                          r=[("cy1", b), "ident"], w=[("ps", bank)])
                    gate = self.modv(l, 5, k, col)
                    pr.op("dve", lambda e, b=b, k=k, pb=pb, gate=gate: e.scalar_tensor_tensor(out=ho[b][:, k, :], in0=pb[:, 0:128], scalar=gate, in1=ho[b][:, k, :],
                                                                                            op0=ALU.mult, op1=ALU.add), r=[("ps", bank), ("cho", b), "MOD"], w=[("cho", b)])
                pr.dma("sync", lambda e, b=b, tt=tt: e.dma_start(out=h_out[:, tt * 128:(tt + 1) * 128].rearrange("(k p) t -> p k t", p=128), in_=ho[b][:, :, :]),
                       r=[("cho", b)], w=[("hout2", tt)])
            pr.flush(barrier=True)

    def phase_att(self, l):
        pr = self.pr
        j = l // 2
        sa = 64 ** -0.5
        sn = 128 ** -0.5
        lam_init = 0.8 - 0.6 * math.exp(-0.3 * l)
        self.cast_w(self.att_w_in[j], self.wb_in[:, 0:6144], D, 6144, "wbi")
        pr.flush(barrier=True)
        with contextlib.ExitStack() as st:
            RT = self.sb(st, "RT", [128, 128], F32)
            pr.dma("sync", lambda e: e.dma_start(out=RT[:, :], in_=self.k_RT[:, :]), w=["RT"])
            xs = [self.sb(st, f"xs{i}", [128, 512], F32) for i in range(2)]
            tmp = [self.sb(st, f"tp{i}", [128, 512], F32) for i in range(2)]
            stg = [self.sb(st, f"stg{i}", [128, 512], BF16) for i in range(2)]
            cs = [self.sb(st, f"cs{i}", [128, 2, 512], F32) for i in range(2)]
            cnt = [0]

            def evac(mt, t0, n, pap, bank):
                grp, h = mt // 8, mt % 8
                row0 = {0: 0, 1: 1024, 3: 2048, 4: 3072}[grp] + h * 128
                scale = {0: sa, 1: 1.0, 3: sn, 4: 1.0}[grp]
                b = cnt[0] % 2
                cnt[0] += 1
                if grp >= 3 or t0 < CTX:
                    pr.op("act", lambda e: e.activation(out=stg[b][:, 0:n], in_=pap, func=AF.Copy, scale=scale), r=[("ps", bank)], w=[("stg", b)])
                else:
                    rb = 4 + b
                    pr.dma("sync", lambda e: e.dma_start(out=cs[b][:, 0, 0:n], in_=self.k_cos[:, t0 - CTX:t0 - CTX + n]), w=[("cs", b, 0)])
                    pr.dma("sync", lambda e: e.dma_start(out=cs[b][:, 1, 0:n], in_=self.k_sin[:, t0 - CTX:t0 - CTX + n]), w=[("cs", b, 1)])
                    pr.op("act", lambda e: e.activation(out=xs[b][:, 0:n], in_=pap, func=AF.Copy, scale=scale), r=[("ps", bank)], w=[("xs", b)])
                    pr.op("pe", lambda e: e.matmul(self.psb[rb][:, 0:n], lhsT=RT[:, :], rhs=xs[b][:, 0:n], start=True, stop=True),
                          r=[("xs", b), "RT"], w=[("ps", rb)])
                    pr.op("dve", lambda e: e.tensor_tensor(out=tmp[b][:, 0:n], in0=self.psb[rb][:, 0:n], in1=cs[b][:, 1, 0:n], op=ALU.mult),
                          r=[("ps", rb), ("cs", b, 1)], w=[("tp", b)])
                    pr.op("dve", lambda e: e.tensor_tensor(out=xs[b][:, 0:n], in0=xs[b][:, 0:n], in1=cs[b][:, 0, 0:n], op=ALU.mult),
                          r=[("xs", b), ("cs", b, 0)], w=[("xs", b)])
                    pr.op("dve", lambda e: e.tensor_tensor(out=stg[b][:, 0:n], in0=xs[b][:, 0:n], in1=tmp[b][:, 0:n], op=ALU.add),
                          r=[("xs", b), ("tp", b)], w=[("stg", b)])
                pr.dma("pool", lambda e: e.dma_start(out=self.QK[row0:row0 + 128, t0:t0 + n], in_=stg[b][:, 0:n]), r=[("stg", b)], w=[("QK", row0, t0)])

            def tm_evac(c0, t0, pap, bank):
                colo = c0 - 2048 if c0 < 3072 else c0 - 5120 + 1024
                b = cnt[0] % 2
                cnt[0] += 1
                pr.op("act", lambda e: e.activation(out=stg[b][:, :], in_=pap, func=AF.Copy), r=[("ps", bank)], w=[("stg", b)])
                pr.dma("pool", lambda e: e.dma_start(out=self.V[t0:t0 + 128, colo:colo + 512], in_=stg[b][:, :]), r=[("stg", b)], w=[("V", t0, colo)])

            self.linear(st, self.aT, self.wb_in[:, 0:6144], D, 6144, evac, tm_cols={2048, 2560, 5120, 5632}, tm_evac=tm_evac, tag="ip")
            pr.flush(barrier=True)
        with contextlib.ExitStack() as st:
            lrow = self.sb(st, "lrow", [1, 2, 2, 64], F32)
            pr.dma("sync", lambda e: e.dma_start(out=lrow[:, :, :, :], in_=self.att_lambda[j:j + 1, :].rearrange("o (a b c) -> o a b c", a=2, b=2)), w=["lrow"])
            lp = self.sb(st, "lp", [1, 2, 64], F32)
            pr.op("dve", lambda e: e.tensor_tensor(out=lp[:, :, :], in0=lrow[:, :, 0, :], in1=lrow[:, :, 1, :], op=ALU.mult), r=["lrow"], w=["lp"])
            l2 = self.sb(st, "l2", [1, 2], F32)
            pr.op("dve", lambda e: e.tensor_reduce(out=l2[:, :], in_=lp[:, :, :], axis=AX.X, op=ALU.add), r=["lp"], w=["l2"])
            pr.op("act", lambda e: e.activation(out=l2[:, :], in_=l2[:, :], func=AF.Exp), r=["l2"], w=["l2"])
            nl = self.sb(st, "nl", [1, 2], F32)
            pr.op("dve", lambda e: e.tensor_tensor(out=nl[:, 0:1], in0=l2[:, 1:2], in1=l2[:, 0:1], op=ALU.subtract), r=["l2"], w=["nl"])
            pr.op("dve", lambda e: e.tensor_scalar(out=nl[:, 0:1], in0=nl[:, 0:1], scalar1=-lam_init, scalar2=None, op0=ALU.add), r=["nl"], w=["nl"])
            nlam = self.sb(st, "nlam", [128, 1], F32)
            pr.op("pe", lambda e: e.matmul(self.psb[0][:, 0:1], lhsT=self.ones[0:1, :], rhs=nl[0:1, 0:1], start=True, stop=True),
                  r=["nl", "ones"], w=[("ps", 0)])
            pr.op("dve", lambda e: e.tensor_copy(out=nlam[:, :], in_=self.psb[0][:, 0:1]), r=[("ps", 0)], w=["nlam"])
            scl = self.sb(st, "scl", [128, 1], F32)
            self.transpose_rows(st, self.att_subln[j:j + 1, :], 1, scl[:, 0:1], "scl", bank=1)
            pr.op("dve", lambda e: e.tensor_scalar(out=scl[:, :], in0=scl[:, :], scalar1=1.0 - lam_init, scalar2=None, op0=ALU.mult), r=["scl"], w=["scl"])
            qT = self.sb(st, "qT", [128, T], BF16)
            kT = self.sb(st, "kT", [128, T], BF16)
            Vh = self.sb(st, "Vh", [128, 66, 128], BF16)
            P = [[self.sb(st, f"P{m}{i}", [128, 512], BF16) for i in range(2)] for m in range(2)]
            rD = [self.sb(st, f"rD{i}", [128, 512], F32) for i in range(2)]
            tA = self.sb(st, "tA", [128, 512], F32)
            tB = self.sb(st, "tB", [128, 512], F32)
            Dacc = [self.sb(st, f"Dacc{i}", [128, 512], F32) for i in range(2)]
            ost = [self.sb(st, f"ost{i}", [128, 512], BF16) for i in range(2)]
            oi = 0
            for h in range(8):
                pr.dma("sync", lambda e, h=h: e.dma_start(out=qT[:, :], in_=self.QK[h * 128:(h + 1) * 128, :]), w=["qT"])
                pr.dma("sync", lambda e, h=h: e.dma_start(out=kT[:, :], in_=self.QK[1024 + h * 128:1024 + (h + 1) * 128, :]), w=["kT"])
                pr.dma("sync", lambda e, h=h: e.dma_start(out=Vh[:, :, :], in_=self.V[:, h * 128:(h + 1) * 128].rearrange("(j p) c -> p j c", p=128)), w=["Vh"])
                qtiles = [(0, 256, 2)] + [(CTX + 512 * i, 512, 66) for i in range(16)]
                for (q0, qn_, nk) in qtiles:
                    def emit_qk_pair(kt, q0=q0, qn_=qn_):
                        for m in range(2):
                            sbk = m * 2 + (kt % 2)
                            pb = self.psb[sbk]
                            pr.op("pe", lambda e, m=m, kt=kt, pb=pb: e.matmul(
                                pb[:, 0:qn_], lhsT=kT[64 * m:64 * m + 64, kt * 128:(kt + 1) * 128], rhs=qT[64 * m:64 * m + 64, q0:q0 + qn_],
                                start=True, stop=True), r=["kT", "qT"], w=[("ps", sbk)])
                        for m in range(2):
                            sbk = m * 2 + (kt % 2)
                            pb = self.psb[sbk]
                            Pm = P[m][kt % 2]
                            pr.op("act", lambda e, pb=pb, Pm=Pm: e.activation(out=Pm[:, 0:qn_], in_=pb[:, 0:qn_], func=AF.Exp),
                                  r=[("ps", sbk)], w=[("P", m, kt % 2)])

                    def emit_av(kt, m, qn_=qn_, nk=nk):
                        Pm = P[m][kt % 2]
                        pr.op("pe", lambda e, m=m, kt=kt, Pm=Pm: e.matmul(
                            self.psb[4 + m][:, 0:qn_], lhsT=Vh[:, kt, :], rhs=Pm[:, 0:qn_], start=(kt == 0), stop=(kt == nk - 1)),
                            r=[("P", m, kt % 2), "Vh"], w=[("ps", 4 + m)])
                        aeng = "dve" if m == 0 else "pool"
                        if kt == 0:
                            pr.op(aeng, lambda e, m=m, Pm=Pm: e.tensor_copy(out=Dacc[m][:, 0:qn_], in_=Pm[:, 0:qn_]),
                                  r=[("P", m, kt % 2)], w=[("Dacc", m)])
                        else:
                            pr.op(aeng, lambda e, m=m, Pm=Pm: e.tensor_tensor(out=Dacc[m][:, 0:qn_], in0=Dacc[m][:, 0:qn_], in1=Pm[:, 0:qn_], op=ALU.add),
                                  r=[("P", m, kt % 2), ("Dacc", m)], w=[("Dacc", m)])

                    emit_qk_pair(0)
                    for kt in range(nk):
                        if kt + 1 < nk:
                            emit_qk_pair(kt + 1)
                        emit_av(kt, 0)
                        emit_av(kt, 1)
                    for m in range(2):
                        pr.op("pe", lambda e, m=m, qn_=qn_: e.matmul(self.psb[6 + m][:, 0:qn_], lhsT=self.ones[:, :], rhs=Dacc[m][:, 0:qn_], start=True, stop=True),
                              r=[("Dacc", m), "ones"], w=[("ps", 6 + m)])
                    n = qn_
                    pr.op("dve", lambda e, n=n: e.reciprocal(out=rD[0][:, 0:n], in_=self.psb[6][:, 0:n]), r=[("ps", 6)], w=[("rD", 0)])
                    pr.op("dve", lambda e, n=n: e.reciprocal(out=rD[1][:, 0:n], in_=self.psb[7][:, 0:n]), r=[("ps", 7)], w=[("rD", 1)])
                    pr.op("dve", lambda e, n=n: e.tensor_tensor(out=tA[:, 0:n], in0=self.psb[4][:, 0:n], in1=rD[0][:, 0:n], op=ALU.mult),
                          r=[("ps", 4), ("rD", 0)], w=["tA"])
                    pr.op("dve", lambda e, n=n: e.tensor_tensor(out=tB[:, 0:n], in0=self.psb[5][:, 0:n], in1=rD[1][:, 0:n], op=ALU.mult),
                          r=[("ps", 5), ("rD", 1)], w=["tB"])
                    pr.op("dve", lambda e, n=n: e.scalar_tensor_tensor(out=tA[:, 0:n], in0=tB[:, 0:n], scalar=nlam[:, 0:1], in1=tA[:, 0:n],
                                                                     op0=ALU.mult, op1=ALU.add), r=["tA", "tB", "nlam"], w=["tA"])
                    pr.op("act", lambda e, n=n: e.activation(out=tB[:, 0:n], in_=tA[:, 0:n], func=AF.Square), r=["tA"], w=["tB"])
                    pr.op("pe", lambda e, n=n: e.matmul(self.psb[0][:, 0:n], lhsT=self.ones[:, :], rhs=tB[:, 0:n], start=True, stop=True),
                          r=["tB", "ones"], w=[("ps", 0)])
                    pr.op("dve", lambda e, n=n: e.tensor_scalar(out=rD[0][:, 0:n], in0=self.psb[0][:, 0:n], scalar1=1.0 / 128, scalar2=EPS,
                                                              op0=ALU.mult, op1=ALU.add), r=[("ps", 0)], w=[("rD", 0)])
                    pr.op("act", lambda e, n=n: e.activation(out=rD[0][:, 0:n], in_=rD[0][:, 0:n], func=AF.Sqrt), r=[("rD", 0)], w=[("rD", 0)])
                    pr.op("dve", lambda e, n=n: e.reciprocal(out=rD[0][:, 0:n], in_=rD[0][:, 0:n]), r=[("rD", 0)], w=[("rD", 0)])
                    pr.op("dve", lambda e, n=n: e.tensor_tensor(out=tA[:, 0:n], in0=tA[:, 0:n], in1=rD[0][:, 0:n], op=ALU.mult),
                          r=["tA", ("rD", 0)], w=["tA"])
                    ob = oi % 2
                    oi += 1
                    pr.op("dve", lambda e, n=n, ob=ob: e.tensor_scalar(out=ost[ob][:, 0:n], in0=tA[:, 0:n], scalar1=scl[:, 0:1], scalar2=None, op0=ALU.mult),
                          r=["tA", "scl"], w=[("ost", ob)])
                    pr.dma("pool", lambda e, n=n, ob=ob, h=h, q0=q0: e.dma_start(out=self.catT[h * 128:(h + 1) * 128, q0:q0 + n], in_=ost[ob][:, 0:n]),
                           r=[("ost", ob)], w=[("catT", h, q0)])
                pr.flush()
            pr.flush(barrier=True)
        with contextlib.ExitStack() as st:
            qT = self.sb(st, "nqT", [128, T], BF16)
            kT = self.sb(st, "nkT", [128, T], BF16)
            Vh = self.sb(st, "nVh", [128, 66, 128], BF16)
            naT = self.sb(st, "naT", [128, T], BF16)
            Bt = self.sb(st, "Bt", [128, 5, 5, 128], F32)
            Ssb = [self.sb(st, f"Ssb{i}", [128, 640], F32) for i in range(2)]
            P = [self.sb(st, f"nP{i}", [128, 896], BF16) for i in range(2)]
            rD = [self.sb(st, f"nrD{i}", [128, 128], F32) for i in range(2)]
            for h in range(8):
                pr.dma("sync", lambda e, h=h: e.dma_start(out=qT[:, :], in_=self.QK[2048 + h * 128:2048 + (h + 1) * 128, :]), w=["nqT"])
                pr.dma("sync", lambda e, h=h: e.dma_start(out=kT[:, :], in_=self.QK[3072 + h * 128:3072 + (h + 1) * 128, :]), w=["nkT"])
                pr.dma("sync", lambda e, h=h: e.dma_start(out=Vh[:, :, :], in_=self.V[:, 1024 + h * 128:1024 + (h + 1) * 128].rearrange("(j p) c -> p j c", p=128)), w=["nVh"])
                pr.dma("sync", lambda e, h=h: e.dma_start(out=Bt[:, :, :, :], in_=self.na_bias[j, h].rearrange("p (a b q) -> p a b q", a=5, b=5)), w=["Bt"])
                for qi in range(66):
                    b = qi % 2
                    bA, bB = (0, 1) if b == 0 else (2, 3)
                    if qi < 2:
                        q0 = qi * 128
                        keys = [0, 128]
                        cls = None
                    else:
                        jq = qi - 2
                        q0 = CTX + jq * 128
                        kbase = int(np.clip(2 * jq - 4, 0, 118))
                        k0 = CTX + kbase * 64
                        keys = [0, 128] + [k0 + 128 * i for i in range(5)]
                        cls = _na_class(jq)
                    nkk = len(keys)
                    for i, ks in enumerate(keys):
                        dst = self.psb[bA][:, i * 128:(i + 1) * 128] if i < 4 else self.psb[bB][:, (i - 4) * 128:(i - 3) * 128]
                        pr.op("pe", lambda e, ks=ks, q0=q0, dst=dst: e.matmul(dst, lhsT=kT[:, ks:ks + 128], rhs=qT[:, q0:q0 + 128], start=True, stop=True),
                              r=["nkT", "nqT"], w=[("ps", bA if i < 4 else bB)])
                    Pb = P[b]
                    pr.op("act", lambda e, Pb=Pb, bA=bA: e.activation(out=Pb[:, 0:256], in_=self.psb[bA][:, 0:256], func=AF.Exp),
                          r=[("ps", bA)], w=[("nP", b, 0)])
                    if cls is not None:
                        pr.op("dve", lambda e, b=b, bA=bA, cls=cls: e.tensor_tensor(
                            out=Ssb[b][:, 0:256], in0=self.psb[bA][:, 256:512], in1=Bt[:, cls, 0:2, :].rearrange("p a q -> p (a q)"), op=ALU.add),
                            r=[("ps", bA), "Bt"], w=[("Ssb", b, 0)])
                        pr.op("dve", lambda e, b=b, bB=bB, cls=cls: e.tensor_tensor(
                            out=Ssb[b][:, 256:640], in0=self.psb[bB][:, 0:384], in1=Bt[:, cls, 2:5, :].rearrange("p a q -> p (a q)"), op=ALU.add),
                            r=[("ps", bB), "Bt"], w=[("Ssb", b, 1)])
                        pr.op("act", lambda e, Pb=Pb, b=b: e.activation(out=Pb[:, 256:896], in_=Ssb[b][:, 0:640], func=AF.Exp),
                              r=[("Ssb", b, 0), ("Ssb", b, 1)], w=[("nP", b, 1)])
                    for i, ks in enumerate(keys):
                        pr.op("pe", lambda e, i=i, ks=ks, Pb=Pb, b=b, nkk=nkk: e.matmul(
                            self.psb[4 + b][:, 0:128], lhsT=Vh[:, ks // 128, :], rhs=Pb[:, i * 128:(i + 1) * 128], start=(i == 0), stop=(i == nkk - 1)),
                            r=[("nP", b, 0), ("nP", b, 1), "nVh"], w=[("ps", 4 + b)])
                    for i, ks in enumerate(keys):
                        pr.op("pe", lambda e, i=i, Pb=Pb, b=b, nkk=nkk: e.matmul(
                            self.psb[6 + b][:, 0:128], lhsT=self.onesb[:, :], rhs=Pb[:, i * 128:(i + 1) * 128], start=(i == 0), stop=(i == nkk - 1)),
                            r=[("nP", b, 0), ("nP", b, 1), "onesb"], w=[("ps", 6 + b)])
                    pr.op("dve", lambda e, b=b: e.reciprocal(out=rD[b][:, :], in_=self.psb[6 + b][:, 0:128]), r=[("ps", 6 + b)], w=[("nrD", b)])
                    pr.op("dve", lambda e, b=b, q0=q0: e.tensor_tensor(out=naT[:, q0:q0 + 128], in0=self.psb[4 + b][:, 0:128], in1=rD[b][:, :], op=ALU.mult),
                          r=[("ps", 4 + b), ("nrD", b)], w=[("naT", q0)])
                pr.dma("pool", lambda e, h=h: e.dma_start(out=self.catT[1024 + h * 128:1024 + (h + 1) * 128, :], in_=naT[:, :]),
                       r=[("naT", qq) for qq in range(0, T, 128)], w=[("catTn", h)])
                pr.flush()
            pr.flush(barrier=True)

    def phase_rec(self, l):
        pr = self.pr
        j = l // 2
        if not hasattr(self, "GL"):
            self.GL = self.dint("GL", [2 * D, T], F32)
        self.cast_w(self.rec_w_in[j], self.wb_in, D, 5 * D, "wbi")
        pr.flush(barrier=True)
        SEG = 2112
        NCH = 33
        with contextlib.ExitStack() as st0:
            LBL = self.sb(st0, "LBL", [128, 128], F32)
            self.transpose_rows(st0, self.rec_lb[:, :], 128, LBL[:, :], "LBL", bank=0)
            pr.op("act", lambda e: e.activation(out=LBL[:, :], in_=LBL[:, :], func=AF.Exp), r=["LBL"], w=["LBL"])
            den = self.sb(st0, "lbden", [128, 32], F32)
            num = self.sb(st0, "lbnum", [128, 32], F32)
            pr.op("dve", lambda e: e.tensor_tensor(out=den[:, :], in0=LBL[:, 0:32], in1=LBL[:, 32:64], op=ALU.add), r=["LBL"], w=["lbden"])
            pr.op("dve", lambda e: e.tensor_tensor(out=den[:, :], in0=den[:, :], in1=LBL[:, 64:96], op=ALU.add), r=["LBL", "lbden"], w=["lbden"])
            pr.op("dve", lambda e: e.tensor_tensor(out=den[:, :], in0=den[:, :], in1=LBL[:, 96:128], op=ALU.add), r=["LBL", "lbden"], w=["lbden"])
            pr.op("dve", lambda e: e.tensor_copy(out=num[:, :], in_=LBL[:, 32:64]), r=["LBL"], w=["lbnum"])
            for lp in range(2, l + 1):
                pr.op("dve", lambda e, lp=lp: e.tensor_tensor(out=num[:, :], in0=num[:, :], in1=LBL[:, lp * 32:(lp + 1) * 32], op=ALU.add),
                      r=["LBL", "lbnum"], w=["lbnum"])
            LB = self.sb(st0, "LB", [128, 32], F32)
            OML = self.sb(st0, "OML", [128, 32], F32)
            pr.op("dve", lambda e: e.reciprocal(out=den[:, :], in_=den[:, :]), r=["lbden"], w=["lbden"])
            pr.op("dve", lambda e: e.tensor_tensor(out=LB[:, :], in0=num[:, :], in1=den[:, :], op=ALU.mult), r=["lbnum", "lbden"], w=["LB"])
            pr.op("dve", lambda e: e.tensor_scalar(out=OML[:, :], in0=LB[:, :], scalar1=-1.0, scalar2=1.0, op0=ALU.mult, op1=ALU.add), r=["LB"], w=["OML"])
            gnv = self.sb(st0, "gnv", [128, 1], F32)
            self.transpose_rows(st0, self.rec_gn[j:j + 1, :], 1, gnv[:, 0:1], "gnv", bank=1)
            with contextlib.ExitStack() as st:
                xs = [self.sb(st, f"rxs{i}", [128, 512], F32) for i in range(2)]
                tmp = [self.sb(st, f"rtp{i}", [128, 512], F32) for i in range(2)]
                stg = [self.sb(st, f"rstg{i}", [128, 512], BF16) for i in range(2)]
                cnt = [0]

                def evac(mt, t0, n, pap, bank):
                    grp, hh = mt // 16, mt % 16
                    b = cnt[0] % 2
                    cnt[0] += 1
                    if grp == 0:
                        pr.op("act", lambda e: e.activation(out=xs[b][:, 0:n], in_=pap, func=AF.Silu), r=[("ps", bank)], w=[("rxs", b)])
                        pr.op("dve", lambda e: e.tensor_scalar(out=stg[b][:, 0:n], in0=xs[b][:, 0:n], scalar1=128 ** -0.5, scalar2=None, op0=ALU.mult),
                              r=[("rxs", b)], w=[("rstg", b)])
                        row0 = hh * 128
                    elif grp == 4:
                        pr.op("act", lambda e: e.activation(out=stg[b][:, 0:n], in_=pap, func=AF.Silu), r=[("ps", bank)], w=[("rstg", b)])
                        row0 = 3 * D + hh * 128
                    else:
                        d = grp - 2
                        ci = d * 16 + hh
                        pr.op("act", lambda e: e.activation(out=xs[b][:, 0:n], in_=pap, func=AF.Sigmoid), r=[("ps", bank)], w=[("rxs", b)])
                        pr.op("dve", lambda e: e.tensor_scalar(out=xs[b][:, 0:n], in0=xs[b][:, 0:n], scalar1=OML[:, ci:ci + 1], scalar2=LB[:, ci:ci + 1],
                                                             op0=ALU.mult, op1=ALU.add), r=[("rxs", b), "OML", "LB"], w=[("rxs", b)])
                        pr.op("act", lambda e: e.activation(out=tmp[b][:, 0:n], in_=xs[b][:, 0:n], func=AF.Ln), r=[("rxs", b)], w=[("rtp", b)])
                        g0 = d * D + hh * 128
                        pr.dma("pool", lambda e: e.dma_start(out=self.GL[g0:g0 + 128, t0:t0 + n], in_=tmp[b][:, 0:n]), r=[("rtp", b)], w=[("GL", g0, t0)])
                        pr.op("dve", lambda e: e.tensor_scalar(out=stg[b][:, 0:n], in0=xs[b][:, 0:n], scalar1=-1.0, scalar2=1.0, op0=ALU.mult, op1=ALU.add),
                              r=[("rxs", b)], w=[("rstg", b)])
                        row0 = D + d * D + hh * 128
                    pr.dma("pool", lambda e: e.dma_start(out=self.QK[row0:row0 + 128, t0:t0 + n], in_=stg[b][:, 0:n]), r=[("rstg", b)], w=[("QK", row0, t0)])

                def tm_evac(c0, t0, pap, bank):
                    colo = c0 - D
                    b = cnt[0] % 2
                    cnt[0] += 1
                    pr.op("act", lambda e: e.activation(out=stg[b][:, :], in_=pap, func=AF.Copy), r=[("ps", bank)], w=[("rstg", b)])
                    pr.dma("pool", lambda e: e.dma_start(out=self.V[t0:t0 + 128, colo:colo + 512], in_=stg[b][:, :]), r=[("rstg", b)], w=[("V", t0, colo)])

                self.linear(st, self.aT, self.wb_in, D, 5 * D, evac, tm_cols={2048, 2560, 3072, 3584}, tm_evac=tm_evac, tag="rp")
                pr.flush(barrier=True)
            with contextlib.ExitStack() as st:
                mask = self.sb(st, "smask", [128, SEG], F32)
                pr.op("dve", lambda e: e.memset(mask[:, :], 1.0), w=["smask"])
                pr.op("dve", lambda e: e.memset(mask[:, :].rearrange("p (n c) -> p n c", c=64)[:, :, 0:1], 0.0), r=["smask"], w=["smask"])
                mk = [self.sb(st, "m128f", [128, 128], F32), self.sb(st, "m128b", [128, 128], F32)]
                pr.dma("sync", lambda e: e.dma_start(out=mk[0][:, :], in_=self.k_m128f[:, :]), w=["m128f"])
                pr.dma("sync", lambda e: e.dma_start(out=mk[1][:, :], in_=self.k_m128b[:, :]), w=["m128b"])
                gs = self.sb(st, "gs", [128, SEG], F32)
                Cs = self.sb(st, "Cs", [128, SEG], F32)
                As = self.sb(st, "As", [128, SEG], F32)
                Es = self.sb(st, "Es", [128, SEG], F32)
                qss = self.sb(st, "qss", [128, SEG], BF16)
                kks = self.sb(st, "kks", [128, SEG], BF16)
                qt_ = self.sb(st, "qt_", [128, T], BF16)
                kt_ = self.sb(st, "kt_", [128, T], BF16)
                kh_ = self.sb(st, "kh_", [128, T], BF16)
                qst = self.sb(st, "qst", [128, T], BF16)
                dsb = self.sb(st, "dsb", [128, 132], F32)
                Vh = self.sb(st, "rVh", [128, 66, 128], BF16)
                oT = self.sb(st, "oT", [128, T], F32)
                S = [self.sb(st, f"S{i}", [128, 128], F32) for i in range(2)]
                Sb = [self.sb(st, f"Sb{i}", [128, 128], BF16) for i in range(2)]
                attm = [self.sb(st, f"attm{i}", [128, 128], BF16) for i in range(2)]
                khT = [self.sb(st, f"khT{i}", [128, 128], BF16) for i in range(2)]
                rr = self.sb(st, "rrs", [128, 512], F32)
                gt = self.sb(st, "rgt", [128, 512], BF16)
                ost = [self.sb(st, f"rost{i}", [128, 512], BF16) for i in range(2)]
                v3 = lambda a: a[:, :].rearrange("p (n c) -> p n c", c=64)
                oi = 0
                for hh in range(16):
                    pr.dma("sync", lambda e, hh=hh: e.dma_start(out=Vh[:, :, :], in_=self.V[:, hh * 128:(hh + 1) * 128].rearrange("(j p) c -> p j c", p=128)), w=["rVh"])
                    for d in range(2):
                        mi, ei = (31, 63) if d == 0 else (32, 0)
                        for sg_ in range(4):
                            t0 = sg_ * SEG
                            g0 = d * D + hh * 128
                            k0 = D + d * D + hh * 128
                            pr.dma("sync", lambda e, g0=g0, t0=t0: e.dma_start(out=gs[:, :], in_=self.GL[g0:g0 + 128, t0:t0 + SEG]), w=["gs"])
                            pr.dma("sync", lambda e, hh=hh, t0=t0: e.dma_start(out=qss[:, :], in_=self.QK[hh * 128:(hh + 1) * 128, t0:t0 + SEG]), w=["qss"])
                            pr.dma("sync", lambda e, k0=k0, t0=t0: e.dma_start(out=kks[:, :], in_=self.QK[k0:k0 + 128, t0:t0 + SEG]), w=["kks"])
                            pr.op("dve", lambda e: e.tensor_tensor_scan(out=Cs[:, :], data0=mask[:, :], data1=gs[:, :], initial=0.0, op0=ALU.mult, op1=ALU.add),
                                  r=["gs", "smask"], w=["Cs"])
                            if d == 1:
                                pr.op("dve", lambda e: e.tensor_tensor(out=v3(As), in0=v3(Cs)[:, :, 63:64].to_broadcast([128, NCH, 64]), in1=v3(Cs), op=ALU.subtract),
                                      r=["Cs"], w=["As"])
                                pr.op("dve", lambda e: e.tensor_tensor(out=Cs[:, :], in0=As[:, :], in1=gs[:, :], op=ALU.add), r=["As", "gs"], w=["Cs"])
                            pr.op("dve", lambda e, mi=mi: e.tensor_tensor(out=v3(As), in0=v3(Cs), in1=v3(Cs)[:, :, mi:mi + 1].to_broadcast([128, NCH, 64]), op=ALU.subtract),
                                  r=["Cs"], w=["As"])
                            pr.op("act", lambda e: e.activation(out=Es[:, :], in_=As[:, :], func=AF.Exp), r=["As"], w=["Es"])
                            pr.op("dve", lambda e, t0=t0: e.tensor_tensor(out=qt_[:, t0:t0 + SEG], in0=qss[:, :], in1=Es[:, :], op=ALU.mult), r=["qss", "Es"], w=[("qt_", sg_)])
                            pr.op("act", lambda e: e.activation(out=Es[:, :], in_=As[:, :], func=AF.Exp, scale=-1.0), r=["As"], w=["Es"])
                            pr.op("dve", lambda e, t0=t0: e.tensor_tensor(out=kt_[:, t0:t0 + SEG], in0=kks[:, :], in1=Es[:, :], op=ALU.mult), r=["kks", "Es"], w=[("kt_", sg_)])
                            pr.op("dve", lambda e, ei=ei: e.tensor_tensor(out=v3(As), in0=v3(Cs)[:, :, ei:ei + 1].to_broadcast([128, NCH, 64]), in1=v3(Cs), op=ALU.subtract),
                                  r=["Cs"], w=["As"])
                            pr.op("act", lambda e: e.activation(out=Es[:, :], in_=As[:, :], func=AF.Exp), r=["As"], w=["Es"])
                            pr.op("dve", lambda e, t0=t0: e.tensor_tensor(out=kh_[:, t0:t0 + SEG], in0=kks[:, :], in1=Es[:, :], op=ALU.mult), r=["kks", "Es"], w=[("kh_", sg_)])
                            pr.op("act", lambda e: e.activation(out=Es[:, :], in_=Cs[:, :], func=AF.Exp), r=["Cs"], w=["Es"])
                            pr.op("dve", lambda e, t0=t0: e.tensor_tensor(out=qst[:, t0:t0 + SEG], in0=qss[:, :], in1=Es[:, :], op=ALU.mult), r=["qss", "Es"], w=[("qst", sg_)])
                            pr.op("dve", lambda e, sg_=sg_, ei=ei: e.tensor_copy(out=dsb[:, sg_ * NCH:(sg_ + 1) * NCH], in_=v3(Es)[:, :, ei]), r=["Es"], w=[("dsb", sg_)])
                        allk = [(nm, q) for nm in ("qt_", "kt_", "kh_", "qst", "dsb") for q in range(4)]
                        pr.op("dve", lambda e: e.memset(S[0][:, :], 0.0), w=[("S", 0)])
                        pr.op("dve", lambda e: e.memset(Sb[0][:, :], 0.0), w=[("Sb", 0)])
                        cur = 0
                        if d == 0:
                            tiles = [(tt, (0, 1)) for tt in range(66)]
                        else:
                            tiles = [(1, (1, 0)), (0, (1, 0))] + [(tt, (1, 0)) for tt in range(65, 1, -1)]
                        for ti, (tt, corder) in enumerate(tiles):
                            ab = ti % 2
                            tl = tt * 128
                            pr.op("pe", lambda e, ab=ab, tl=tl: e.matmul(self.psb[ab][:, 0:128], lhsT=kt_[:, tl:tl + 128], rhs=qt_[:, tl:tl + 128], start=True, stop=True),
                                  r=allk, w=[("ps", ab)])
                            pr.op("dve", lambda e, ab=ab, d=d: e.tensor_tensor(out=attm[ab][:, :], in0=self.psb[ab][:, 0:128], in1=mk[d][:, :], op=ALU.mult),
                                  r=[("ps", ab), "m128f", "m128b"], w=[("attm", ab)])
                            pr.op("pe", lambda e, ab=ab, tl=tl: e.matmul(self.psb[2 + ab][:, 0:128], lhsT=kh_[:, tl:tl + 128], rhs=self.identb[:, :], start=True, stop=True),
                                  r=allk + ["identb"], w=[("ps", 2 + ab)])
                            pr.op("act", lambda e, ab=ab: e.activation(out=khT[ab][:, :], in_=self.psb[2 + ab][:, 0:128], func=AF.Copy), r=[("ps", 2 + ab)], w=[("khT", ab)])
                            po = self.psb[6 + ab]
                            for c in corder:
                                n_ = tt * 2 + c
                                cl = slice(64 * c, 64 * c + 64)
                                pdb = 4 + (n_ % 2)
                                pr.op("pe", lambda e, ab=ab, tt=tt, cl=cl, po=po: e.matmul(po[:, cl], lhsT=Vh[cl, tt, :], rhs=attm[ab][cl, cl], start=True, stop=False),
                                      r=["rVh", ("attm", ab)], w=[("ps", 6 + ab)])
                                pr.op("pe", lambda e, cur=cur, tl=tl, c=c, cl=cl, po=po: e.matmul(po[:, cl], lhsT=Sb[cur][:, :], rhs=qst[:, tl + 64 * c:tl + 64 * c + 64], start=False, stop=True),
                                      r=[("Sb", cur)] + allk, w=[("ps", 6 + ab)])
                                pr.op("pe", lambda e, ab=ab, tt=tt, cl=cl, pdb=pdb: e.matmul(self.psb[pdb][:, 0:128], lhsT=khT[ab][cl, :], rhs=Vh[cl, tt, :], start=True, stop=True),
                                      r=[("khT", ab), "rVh"], w=[("ps", pdb)])
                                nx = 1 - cur
                                pr.op("dve", lambda e, cur=cur, nx=nx, n_=n_, pdb=pdb: e.scalar_tensor_tensor(
                                    out=Sb[nx][:, :], in0=S[cur][:, :], scalar=dsb[:, n_:n_ + 1], in1=self.psb[pdb][:, 0:128], op0=ALU.mult, op1=ALU.add),
                                    r=[("S", cur), ("ps", pdb)] + [("dsb", q) for q in range(4)], w=[("Sb", nx)])
                                pr.op("dve", lambda e, cur=cur, nx=nx, n_=n_, pdb=pdb: e.scalar_tensor_tensor(
                                    out=S[nx][:, :], in0=S[cur][:, :], scalar=dsb[:, n_:n_ + 1], in1=self.psb[pdb][:, 0:128], op0=ALU.mult, op1=ALU.add),
                                    r=[("S", cur), ("ps", pdb)] + [("dsb", q) for q in range(4)], w=[("S", nx)])
                                cur = nx
                            if d == 0:
                                pr.op("act", lambda e, tl=tl, po=po: e.activation(out=oT[:, tl:tl + 128], in_=po[:, 0:128], func=AF.Copy), r=[("ps", 6 + ab)], w=[("oT", tt)])
                            else:
                                pr.op("dve", lambda e, tl=tl, po=po: e.tensor_tensor(out=oT[:, tl:tl + 128], in0=oT[:, tl:tl + 128], in1=po[:, 0:128], op=ALU.add),
                                      r=[("ps", 6 + ab), ("oT", tt)], w=[("oT", tt)])
                        pr.flush()
                    for (t0, n) in self.tok_tiles(512):
                        okeys = [("oT", tt) for tt in range(t0 // 128, (t0 + n) // 128)]
                        g0 = 3 * D + hh * 128
                        pr.dma("sync", lambda e, g0=g0, t0=t0, n=n: e.dma_start(out=gt[:, 0:n], in_=self.QK[g0:g0 + 128, t0:t0 + n]), w=["rgt"])
                        pr.op("act", lambda e, t0=t0, n=n: e.activation(out=rr[:, 0:n], in_=oT[:, t0:t0 + n], func=AF.Square), r=okeys, w=["rrs"])
                        pr.op("pe", lambda e, n=n: e.matmul(self.psb[0][:, 0:n], lhsT=self.ones[:, :], rhs=rr[:, 0:n], start=True, stop=True), r=["rrs", "ones"], w=[("ps", 0)])
                        pr.op("dve", lambda e, n=n: e.tensor_scalar(out=rr[:, 0:n], in0=self.psb[0][:, 0:n], scalar1=1.0 / 128, scalar2=EPS, op0=ALU.mult, op1=ALU.add),
                              r=[("ps", 0)], w=["rrs"])
                        pr.op("act", lambda e, n=n: e.activation(out=rr[:, 0:n], in_=rr[:, 0:n], func=AF.Sqrt), r=["rrs"], w=["rrs"])
                        pr.op("dve", lambda e, n=n: e.reciprocal(out=rr[:, 0:n], in_=rr[:, 0:n]), r=["rrs"], w=["rrs"])
                        pr.op("dve", lambda e, t0=t0, n=n: e.tensor_tensor(out=rr[:, 0:n], in0=oT[:, t0:t0 + n], in1=rr[:, 0:n], op=ALU.mult), r=okeys + ["rrs"], w=["rrs"])
                        ob = oi % 2
                        oi += 1
                        pr.op("dve", lambda e, n=n, ob=ob: e.scalar_tensor_tensor(out=ost[ob][:, 0:n], in0=rr[:, 0:n], scalar=gnv[:, 0:1], in1=gt[:, 0:n],
                                                                                 op0=ALU.mult, op1=ALU.mult), r=["rrs", "rgt", "gnv"], w=[("rost", ob)])
                        pr.dma("pool", lambda e, hh=hh, t0=t0, n=n, ob=ob: e.dma_start(out=self.catT[hh * 128:(hh + 1) * 128, t0:t0 + n], in_=ost[ob][:, 0:n]),
                               r=[("rost", ob)], w=[("catT", hh, t0)])
                    pr.flush()
                pr.flush(barrier=True)


IN_SHAPES = {
    "hT0": [D, T], "c2": [32, 128], "w_mod": [DEPTH, D, 6 * D], "b_mod": [384, 128], "g12": [128, 128], "gfin": [16, 128],
    "att_w_in": [2, D, 6144], "att_w_out": [2, D, D], "att_lambda": [2, 256], "att_subln": [2, 128],
    "na_bias": [2, 8, 128, 5 * 5 * 128], "rec_w_in": [2, D, 5 * D], "rec_w_out": [2, D, D], "rec_lb": [DEPTH * 2 * 16, 128],
    "rec_gn": [2, 128], "moe_wr": [DEPTH, D, 36], "moe_br": [DEPTH, 36], "moe_wg": [DEPTH, NEXP, D, DEXP],
    "moe_wu": [DEPTH, NEXP, D, DEXP], "moe_wd": [DEPTH, NEXP, DEXP, D], "k_ident": [128, 128], "k_cos": [128, NLAT],
    "k_sin": [128, NLAT], "k_RT": [128, 128], "k_m128f": [128, 128], "k_m128b": [128, 128],
    "k_iota": [32, 164], "k_uincl": [32, 32], "k_pidx": [128, 1],
}


def _mkprop(name):
    def get(self):
        if name not in self._ins:
            self._ins[name] = self.din(name, IN_SHAPES[name])
        return self._ins[name]
    return property(get)


for _n in IN_SHAPES:
    setattr(Builder, _n, _mkprop(_n))


def prep_inputs(inp):
    f = lambda a: np.ascontiguousarray(np.asarray(a, dtype=np.float32))
    k = _consts()
    out = {}
    out["hT0"] = f(np.concatenate([inp["ctx"][0], inp["x"][0]], axis=0).T)
    out["c2"] = f(np.concatenate([inp["c"].reshape(16, 128), inp["c_ctx"].reshape(16, 128)], axis=0))
    out["w_mod"] = f(inp["w_mod"])
    out["b_mod"] = f(inp["b_mod"].reshape(384, 128))
    out["g12"] = f(np.concatenate([inp["norm1_g"].reshape(64, 128), inp["norm2_g"].reshape(64, 128)], axis=0))
    out["gfin"] = f(inp["final_norm_g"].reshape(16, 128))
    out["att_w_in"] = f(inp["att_w_in"])
    out["att_w_out"] = f(inp["att_w_out"])
    out["att_lambda"] = f(inp["att_lambda"].reshape(2, 256))
    out["att_subln"] = f(inp["att_subln_g"])
    out["na_bias"] = f(np.stack([_na_bias(np.asarray(inp["att_rpb"][j])) for j in range(2)]).reshape(2, 8, 128, 5 * 5 * 128))
    out["rec_w_in"] = f(inp["rec_w_in"])
    out["rec_w_out"] = f(inp["rec_w_out"])
    out["rec_lb"] = f(inp["rec_lb_logits"].reshape(DEPTH * 2 * 16, 128))
    out["rec_gn"] = f(inp["rec_gnorm_g"])
    out["moe_wr"] = f(np.concatenate([inp["moe_w_group"], inp["moe_w_router"]], axis=2))
    out["moe_br"] = f(np.concatenate([inp["moe_b_group"], inp["moe_b_router"]], axis=1))
    out["moe_wg"] = f(inp["moe_w_gate"])
    out["moe_wu"] = f(inp["moe_w_up"])
    out["moe_wd"] = f(inp["moe_w_down"])
    out["k_ident"] = k["ident"]
    out["k_cos"] = k["cosT"]
    out["k_sin"] = k["sinT"]
    out["k_RT"] = k["RT"]
    out["k_m128f"] = k["m128f"]
    out["k_m128b"] = k["m128b"]
    out["k_iota"] = np.ascontiguousarray(np.broadcast_to(128.0 * np.arange(164, dtype=np.float32), (32, 164)))
    out["k_uincl"] = np.triu(np.ones((32, 32), np.float32))
    out["k_pidx"] = np.arange(128, dtype=np.float32).reshape(128, 1)
    return out


def kernel(**inputs):
    b = Builder()
    nc = b.build()
    allin = prep_inputs(inputs)
    in_map = {n: allin[n] for n in b._ins}
    res = run_bass_kernel_spmd(nc, [in_map], core_ids=[0])
    outT = res.results[0]["outT"]
    return np.ascontiguousarray(outT.T)[None].astype(np.float32)
```
